# Optimizing a Trainium2 kernel written in Bass

```python
import jax, jax.numpy as jnp
from jax import lax
import numpy as np

D_MODEL = 1024
BATCH = 2
SEQ = 16384
DEPTH = 1

HEAD_DIM = 64
ATTN_HEADS = 8
ATTN_WIDTH = ATTN_HEADS * HEAD_DIM
MOBA_BLOCK = 256
MOBA_TOPK = 3
Q_CHUNK = 32
ROPE_THETA = 10000.0

RWKV_HEADS = 8
RWKV_WIDTH = RWKV_HEADS * HEAD_DIM
DECAY_LORA = 64
AAA_LORA = 64
GATE_LORA = 160
GN_EPS = 64e-5

N_EXPERTS = 32
TOP_K = 4
D_EXPERT = 1024
SWIGLU_ALPHA = 1.702
SWIGLU_LIMIT = 7.0
EXPERT_BLOCK = 128

LN_EPS = 1e-5
DEEPNORM_ALPHA = (2 * DEPTH) ** 0.25
DEEPNORM_BETA = (8 * DEPTH) ** -0.25

OFF_R = 0
OFF_K = OFF_R + RWKV_WIDTH
OFF_V = OFF_K + RWKV_WIDTH
OFF_WD = OFF_V + RWKV_WIDTH
OFF_AD = OFF_WD + DECAY_LORA
OFF_GD = OFF_AD + AAA_LORA
SHIFT_COLS = OFF_GD + GATE_LORA
OFF_Q = SHIFT_COLS
OFF_AK = OFF_Q + ATTN_WIDTH
OFF_AV = OFF_AK + ATTN_WIDTH
OFF_GATE_A = OFF_AV + ATTN_WIDTH
OFF_GATE_R = OFF_GATE_A + D_MODEL
IN_COLS = OFF_GATE_R + D_MODEL

kernel_name = 'hybrid_moba_rwkv7_moe_deepnorm'


def layer_norm(x, g, b):
    xf = x.astype(jnp.float32)
    mu = xf.mean(-1, keepdims=True)
    var = jnp.square(xf - mu).mean(-1, keepdims=True)
    return ((xf - mu) * lax.rsqrt(var + LN_EPS) * g + b).astype(x.dtype)


def token_shift(t, mu):
    prev = jnp.pad(t, ((0, 0), (1, 0), (0, 0)))[:, :-1]
    return t + (prev - t) * mu


def rope(t):
    s = t.shape[1]
    half = HEAD_DIM // 2
    inv_freq = ROPE_THETA ** (-jnp.arange(half, dtype=jnp.float32) / half)
    ang = jnp.arange(s, dtype=jnp.float32)[:, None] * inv_freq[None, :]
    cos = jnp.cos(ang)[None, :, None, :]
    sin = jnp.sin(ang)[None, :, None, :]
    t1 = t[..., :half].astype(jnp.float32)
    t2 = t[..., half:].astype(jnp.float32)
    return jnp.concatenate([t1 * cos - t2 * sin, t2 * cos + t1 * sin], -1).astype(t.dtype)


def moba_attention(q, k, v):
    b, s, h, dh = q.shape
    nb = -(-s // MOBA_BLOCK)
    s_pad = nb * MOBA_BLOCK
    q, k = rope(q), rope(k)
    pad = ((0, 0), (0, s_pad - s), (0, 0), (0, 0))
    q, k, v = [jnp.pad(t, pad).transpose(0, 2, 1, 3) for t in (q, k, v)]
    kb = k.reshape(b, h, nb, MOBA_BLOCK, dh)
    vb = v.reshape(b, h, nb, MOBA_BLOCK, dh)
    k_mean = kb.astype(jnp.float32).mean(axis=3)
    topk = min(MOBA_TOPK, nb)
    n_chunks = s_pad // Q_CHUNK
    q_chunks = q.reshape(b, h, n_chunks, Q_CHUNK, dh).transpose(2, 0, 1, 3, 4)
    scale = dh ** -0.5
    gather = jax.vmap(jax.vmap(lambda blocks, idx: blocks[idx]))

    def one_chunk(args):
        c, qc = args
        blk = (c * Q_CHUNK) // MOBA_BLOCK
        q_pos = c * Q_CHUNK + jnp.arange(Q_CHUNK)
        gate = jnp.einsum('bhqd,bhnd->bhqn', qc.astype(jnp.float32), k_mean)
        gate = jnp.where(jnp.arange(nb) < blk, gate, -jnp.inf)
        _, sel = lax.top_k(gate, topk)
        sel_ok = sel < blk
        k_sel = gather(kb, sel)
        v_sel = gather(vb, sel)
        s_sel = jnp.einsum('bhqd,bhqjkd->bhqjk', qc, k_sel).astype(jnp.float32) * scale
        s_sel = jnp.where(sel_ok[..., None], s_sel, -jnp.inf).reshape(b, h, Q_CHUNK, topk * MOBA_BLOCK)
        k_own = lax.dynamic_index_in_dim(kb, blk, axis=2, keepdims=False)
        v_own = lax.dynamic_index_in_dim(vb, blk, axis=2, keepdims=False)
        s_own = jnp.einsum('bhqd,bhkd->bhqk', qc, k_own).astype(jnp.float32) * scale
        key_pos = blk * MOBA_BLOCK + jnp.arange(MOBA_BLOCK)
        s_own = jnp.where(key_pos[None, :] <= q_pos[:, None], s_own, -jnp.inf)
        p = jax.nn.softmax(jnp.concatenate([s_sel, s_own], -1), axis=-1).astype(v.dtype)
        p_sel = p[..., :topk * MOBA_BLOCK].reshape(b, h, Q_CHUNK, topk, MOBA_BLOCK)
        p_own = p[..., topk * MOBA_BLOCK:]
        return (jnp.einsum('bhqjk,bhqjkd->bhqd', p_sel, v_sel)
                + jnp.einsum('bhqk,bhkd->bhqd', p_own, v_own))

    out = lax.map(one_chunk, (jnp.arange(n_chunks), q_chunks))
    return out.transpose(1, 0, 3, 2, 4).reshape(b, s_pad, h * dh)[:, :s]


def rwkv7_time_mix(r, k, v, h_w, h_a, h_g, w0, w_decay_up, a0, w_aaa_up, w_gate_up,
                   k_k, k_a, r_k, lnx_g, lnx_b):
    b, s, _ = r.shape
    nh, n = RWKV_HEADS, HEAD_DIM
    f32 = jnp.float32
    w_log = -jax.nn.softplus(-(w0 + jnp.tanh(h_w) @ w_decay_up).astype(f32)) - 0.5
    decay = jnp.exp(-jnp.exp(w_log))
    a = jax.nn.sigmoid((a0 + h_a @ w_aaa_up).astype(f32))
    g = jax.nn.sigmoid(h_g) @ w_gate_up
    kk = (k * k_k).astype(f32).reshape(b, s, nh, n)
    kk = kk / jnp.maximum(jnp.sqrt(jnp.sum(kk * kk, -1, keepdims=True)), 1e-12)
    k = k * (1.0 + (a - 1.0) * k_a)
    to_heads = lambda t: t.astype(f32).reshape(b, s, nh, n)
    r_h, k_h, v_h, a_h, w_h = to_heads(r), to_heads(k), to_heads(v), to_heads(a), to_heads(decay)
    seq_first = lambda t: jnp.swapaxes(t, 0, 1)

    def step(state, inp):
        r_t, w_t, k_t, v_t, kk_t, a_t = inp
        s_kk = jnp.einsum('bhvk,bhk->bhv', state, kk_t)
        state = (state * w_t[:, :, None, :]
                 - s_kk[..., None] * (kk_t * a_t)[:, :, None, :]
                 + v_t[..., None] * k_t[:, :, None, :])
        return state, jnp.einsum('bhvk,bhk->bhv', state, r_t)

    state0 = jnp.zeros((b, nh, n, n), f32)
    _, y = lax.scan(step, state0, [seq_first(t) for t in (r_h, w_h, k_h, v_h, kk, a_h)])
    y = seq_first(y)
    mu = y.mean(-1, keepdims=True)
    var = jnp.square(y - mu).mean(-1, keepdims=True)
    y = ((y - mu) * lax.rsqrt(var + GN_EPS)).reshape(b, s, nh * n) * lnx_g + lnx_b
    bonus = (jnp.sum(r_h * k_h * r_k, -1, keepdims=True) * v_h).reshape(b, s, nh * n)
    return ((y + bonus) * g).astype(r.dtype)


def clamped_swiglu(h):
    h = h.astype(jnp.float32)
    x_glu = jnp.minimum(h[..., ::2], SWIGLU_LIMIT)
    x_lin = jnp.clip(h[..., 1::2], -SWIGLU_LIMIT, SWIGLU_LIMIT)
    return x_glu * jax.nn.sigmoid(SWIGLU_ALPHA * x_glu) * (x_lin + 1.0)


def moe_ffn(x, w_router, b_router, w1, b1, w2, b2):
    b, s, d = x.shape
    t = b * s
    n_slots = t * TOP_K
    xt = x.reshape(t, d)
    logits = (xt @ w_router + b_router).astype(jnp.float32)
    top_vals, top_idx = lax.top_k(logits, TOP_K)
    gates = jax.nn.softmax(top_vals, axis=-1)
    flat_e = top_idx.reshape(-1)
    flat_tok = jnp.arange(n_slots, dtype=jnp.int32) // TOP_K
    flat_g = gates.reshape(-1)
    order = jnp.argsort(flat_e)
    e_sorted = flat_e[order]
    counts = jnp.bincount(flat_e, length=N_EXPERTS)
    starts = jnp.cumsum(counts) - counts
    padded = (counts + EXPERT_BLOCK - 1) // EXPERT_BLOCK * EXPERT_BLOCK
    pad_ends = jnp.cumsum(padded)
    pad_starts = pad_ends - padded
    dest = pad_starts[e_sorted] + (jnp.arange(n_slots) - starts[e_sorted])
    n_blocks = -(-n_slots // EXPERT_BLOCK) + N_EXPERTS
    cap = n_blocks * EXPERT_BLOCK
    slot_tok = jnp.full((cap,), t, jnp.int32).at[dest].set(flat_tok[order])
    slot_gate = jnp.zeros((cap,), jnp.float32).at[dest].set(flat_g[order])
    block_start = jnp.arange(n_blocks) * EXPERT_BLOCK
    block_expert = jnp.minimum(jnp.searchsorted(pad_ends, block_start, side='right'), N_EXPERTS - 1)
    x_pad = jnp.concatenate([xt, jnp.zeros((1, d), xt.dtype)], axis=0)

    def block_step(acc, inp):
        tok, gate, e = inp
        hid = x_pad[tok] @ w1[e] + b1[e]
        y = clamped_swiglu(hid).astype(w2.dtype) @ w2[e] + b2[e]
        return acc.at[tok].add(y.astype(jnp.float32) * gate[:, None]), None

    acc0 = jnp.zeros((t + 1, d), jnp.float32)
    acc, _ = lax.scan(block_step, acc0, (slot_tok.reshape(n_blocks, EXPERT_BLOCK),
                                          slot_gate.reshape(n_blocks, EXPERT_BLOCK), block_expert))
    return acc[:t].reshape(b, s, d).astype(x.dtype)


def hybrid_layer(x, w_in, mu_shift, w0, w_decay_up, a0, w_aaa_up, w_gate_up, k_k, k_a, r_k,
                 lnx_g, lnx_b, w_attn_br, w_rwkv_br, w_out, ln1_g, ln1_b,
                 w_router, b_router, w1, b1, w2, b2, ln2_g, ln2_b):
    b, s, _ = x.shape
    proj = x @ w_in
    sh = token_shift(proj[..., :SHIFT_COLS], mu_shift)
    y_rwkv = rwkv7_time_mix(sh[..., OFF_R:OFF_K], sh[..., OFF_K:OFF_V], sh[..., OFF_V:OFF_WD],
                            sh[..., OFF_WD:OFF_AD], sh[..., OFF_AD:OFF_GD], sh[..., OFF_GD:SHIFT_COLS],
                            w0, w_decay_up, a0, w_aaa_up, w_gate_up, k_k, k_a, r_k, lnx_g, lnx_b)
    heads = lambda t: t.reshape(b, s, ATTN_HEADS, HEAD_DIM)
    y_attn = moba_attention(heads(proj[..., OFF_Q:OFF_AK]), heads(proj[..., OFF_AK:OFF_AV]),
                            heads(proj[..., OFF_AV:OFF_GATE_A]))
    mixed = (jax.nn.sigmoid(proj[..., OFF_GATE_A:OFF_GATE_R]) * (y_attn @ w_attn_br)
             + jax.nn.sigmoid(proj[..., OFF_GATE_R:]) * (y_rwkv @ w_rwkv_br))
    h = layer_norm(DEEPNORM_ALPHA * x + mixed @ w_out, ln1_g, ln1_b)
    return layer_norm(DEEPNORM_ALPHA * h + moe_ffn(h, w_router, b_router, w1, b1, w2, b2), ln2_g, ln2_b)


def setup_inputs(seed: int = 0) -> dict:
    key = jax.random.key(seed)
    ks = iter(jax.random.split(key, 40))
    nrm = lambda shape, scale: jax.random.normal(next(ks), shape, jnp.float32) * scale
    L, D, E, F = DEPTH, D_MODEL, N_EXPERTS, D_EXPERT
    return {
        'x': nrm((BATCH, SEQ, D), 1.0),
        'w_in': nrm((L, D, IN_COLS), D ** -0.5),
        'mu_shift': jax.random.uniform(next(ks), (L, SHIFT_COLS), jnp.float32),
        'w0': nrm((L, RWKV_WIDTH), 0.5),
        'w_decay_up': nrm((L, DECAY_LORA, RWKV_WIDTH), DECAY_LORA ** -0.5),
        'a0': nrm((L, RWKV_WIDTH), 0.1),
        'w_aaa_up': nrm((L, AAA_LORA, RWKV_WIDTH), AAA_LORA ** -0.5),
        'w_gate_up': nrm((L, GATE_LORA, RWKV_WIDTH), GATE_LORA ** -0.5),
        'k_k': 0.85 + nrm((L, RWKV_WIDTH), 0.05),
        'k_a': 1.0 + nrm((L, RWKV_WIDTH), 0.05),
        'r_k': nrm((L, RWKV_HEADS, HEAD_DIM), 0.1),
        'lnx_g': 1.0 + nrm((L, RWKV_WIDTH), 0.02),
        'lnx_b': nrm((L, RWKV_WIDTH), 0.02),
        'w_attn_br': nrm((L, ATTN_WIDTH, D), ATTN_WIDTH ** -0.5),
        'w_rwkv_br': nrm((L, RWKV_WIDTH, D), RWKV_WIDTH ** -0.5),
        'w_out': nrm((L, D, D), D ** -0.5 * DEEPNORM_BETA),
        'ln1_g': 1.0 + nrm((L, D), 0.02),
        'ln1_b': nrm((L, D), 0.02),
        'w_router': nrm((L, D, E), D ** -0.5),
        'b_router': nrm((L, E), 0.01),
        'w1': nrm((L, E, D, 2 * F), D ** -0.5),
        'b1': nrm((L, E, 2 * F), 0.01),
        'w2': nrm((L, E, F, D), F ** -0.5 * DEEPNORM_BETA),
        'b2': nrm((L, E, D), 0.01),
        'ln2_g': 1.0 + nrm((L, D), 0.02),
        'ln2_b': nrm((L, D), 0.02),
    }


def reference(x, w_in, mu_shift, w0, w_decay_up, a0, w_aaa_up, w_gate_up, k_k, k_a, r_k,
              lnx_g, lnx_b, w_attn_br, w_rwkv_br, w_out, ln1_g, ln1_b,
              w_router, b_router, w1, b1, w2, b2, ln2_g, ln2_b):
    for l in range(DEPTH):
        x = hybrid_layer(x, w_in[l], mu_shift[l], w0[l], w_decay_up[l], a0[l], w_aaa_up[l],
                         w_gate_up[l], k_k[l], k_a[l], r_k[l], lnx_g[l], lnx_b[l],
                         w_attn_br[l], w_rwkv_br[l], w_out[l], ln1_g[l], ln1_b[l],
                         w_router[l], b_router[l], w1[l], b1[l], w2[l], b2[l], ln2_g[l], ln2_b[l])
    return x
```

```python
import contextlib
import numpy as np
import ml_dtypes
import concourse.bass as bass
import concourse.mybir as mybir
from concourse.bass_utils import run_bass_kernel_spmd

F32 = mybir.dt.float32
BF16 = mybir.dt.bfloat16
AF = mybir.ActivationFunctionType
ALU = mybir.AluOpType
AX = mybir.AxisListType


ENGS = ("sync", "tensor", "vector", "scalar", "gpsimd")


class Op:
    __slots__ = ("eng", "fn", "deps", "marked", "dma", "sem", "val", "idx", "inc")

    def __init__(self, eng, fn, dma):
        self.eng = eng
        self.fn = fn
        self.deps = []
        self.marked = False
        self.dma = dma
        self.sem = None
        self.val = 0
        self.inc = 16


class Sched:
    def __init__(self, nc, es, n_dma_sems=12, same_engine_sync=True):
        self.nc = nc
        self.es = es
        self.q = {e: [] for e in ENGS}
        self.last_w = {}
        self.readers = {}
        self.same_engine_sync = same_engine_sync
        self.fence = []
        self.esem = {e: es.enter_context(nc.semaphore("s_" + e)) for e in ENGS if e != "sync"}
        self.dsem = {}
        self.dcnt = {}
        self.dlast = {}
        self.dnext = {}
        for e in ("sync", "scalar", "gpsimd"):
            self.dsem[e] = [es.enter_context(nc.semaphore("d_%s_%d" % (e, i))) for i in range(n_dma_sems)]
            self.dcnt[e] = [0] * n_dma_sems
            self.dlast[e] = [None] * n_dma_sems
            self.dnext[e] = 0

    def _add(self, op, reads, writes):
        deps = []
        for k in reads:
            w = self.last_w.get(k)
            if w is not None:
                deps.append(w)
        for k in writes:
            w = self.last_w.get(k)
            if w is not None:
                deps.append(w)
            deps.extend(self.readers.get(k, ()))
        for k in writes:
            self.last_w[k] = op
            self.readers[k] = []
        for k in reads:
            self.readers.setdefault(k, []).append(op)
        for d in deps:
            if d is op:
                continue
            if (not d.dma) and d.eng == op.eng and (not op.dma):
                if d.eng == "tensor" or not self.same_engine_sync:
                    continue
            op.deps.append(d)
        op.deps.extend(self.fence)
        self.q[op.eng].append(op)
        return op

    def barrier(self):
        f = []
        for e in ENGS:
            if self.q[e]:
                f.append(self.q[e][-1])
        for e in ("sync", "scalar", "gpsimd"):
            for o in self.dlast[e]:
                if o is not None:
                    f.append(o)
        f.extend(getattr(self, "async_ops", []))
        for o in f:
            o.marked = True
        self.fence = f
        self.last_w = {}
        self.readers = {}

    def async_op(self, eng, fn, sem, reads=(), writes=()):
        op = Op(eng, fn, True)
        if not hasattr(self, "async_ops"):
            self.async_ops = []
        op.sem = sem
        op.val = len(self.async_ops) + 1
        op.inc = 1
        self.async_ops.append(op)
        return self._add(op, reads, writes)

    def op(self, eng, fn, reads=(), writes=()):
        return self._add(Op(eng, fn, False), reads, writes)

    def dma(self, eng, out, in_, reads=(), writes=(), **kw):
        op = Op(eng, None, True)
        i = self.dnext[eng]
        self.dnext[eng] = (i + 1) % len(self.dsem[eng])
        self.dcnt[eng][i] += 1
        op.sem = self.dsem[eng][i]
        op.val = 16 * self.dcnt[eng][i]
        prev = self.dlast[eng][i]
        if prev is not None:
            op.deps.append(prev)
        self.dlast[eng][i] = op
        op.fn = lambda e, out=out, in_=in_, kw=kw: e.dma_start(out=out, in_=in_, **kw)
        return self._add(op, reads, writes)

    def flush(self):
        nc = self.nc
        if not hasattr(self, "_pos"):
            self._pos = {e: 0 for e in ENGS}
            self._cnt = {e: 0 for e in ENGS}
            self._waited = {e: {} for e in ENGS}
        pend = {e: self.q[e][self._pos[e]:] for e in ENGS}
        for e in ENGS:
            for op in pend[e]:
                for d in op.deps:
                    d.marked = True
        for e in ENGS:
            c = self._cnt[e]
            for op in pend[e]:
                if (not op.dma) and op.marked and op.sem is None:
                    c += 1
                    op.sem = self.esem.get(e)
                    op.val = c
            self._cnt[e] = c
            self._pos[e] = len(self.q[e])

        def run(eng, e):
            waited = self._waited[e]
            for op in pend[e]:
                need = {}
                for d in op.deps:
                    if d.sem is None:
                        continue
                    k = id(d.sem)
                    if waited.get(k, 0) >= d.val:
                        continue
                    if k not in need or need[k][1] < d.val:
                        need[k] = (d.sem, d.val)
                for k, (s, v) in need.items():
                    eng.wait_ge(s, v)
                    waited[k] = v
                if op.fn is None:
                    continue
                ins = op.fn(eng)
                if op.dma:
                    ins.then_inc(op.sem, op.inc)
                elif op.marked:
                    ins.then_inc(op.sem, 1)

        with nc.Block() as block:
            @block.sync
            def _(eng):
                run(eng, "sync")

            @block.tensor
            def _(eng):
                run(eng, "tensor")

            @block.vector
            def _(eng):
                run(eng, "vector")

            @block.scalar
            def _(eng):
                run(eng, "scalar")

            @block.gpsimd
            def _(eng):
                run(eng, "gpsimd")

    def emit(self, final_wait_engine="sync"):
        self.barrier()
        op = Op(final_wait_engine, None, False)
        op.deps = list(self.fence)
        self.q[final_wait_engine].append(op)
        self.flush()


S_TOK = 16384
XB = 512
C = 128
GN_EPS = 64e-5
C0 = -float(np.exp(-0.5))


def rwkv_consts():
    i = np.arange(128)
    m1 = (i[:, None] <= i[None, :]).astype(np.float32)
    m2 = (i[:, None] > i[None, :]).astype(np.float32)
    mus = (i[:, None] < i[None, :]).astype(np.float32)
    mls = mus.T.copy()
    ident = np.eye(128, dtype=np.float32)
    mask4 = np.stack([mus, m1, mus, m1], axis=1)
    return {"c_m1": m1, "c_m2": m2, "c_mus": mus, "c_mls": mls, "c_ident": ident,
            "c_mask4": np.ascontiguousarray(mask4)}


def build_rwkv(nc, S, es, dram, n_chunks=S_TOK // C):
    sbn = [0]

    def sb(shape, dt=F32, name=None):
        sbn[0] += 1
        return es.enter_context(nc.sbuf_tensor(name or ("rw%d" % sbn[0]), shape, dt))

    def ps(shape, dt=F32, name=None):
        sbn[0] += 1
        return es.enter_context(nc.psum_tensor(name or ("rp%d" % sbn[0]), shape, dt))

    BANKS = {"P_RKV": 0, "P_Z": 2, "P_T": 4, "P_L1a": 5, "P_L1b": 1, "P_LDa": 6, "P_LDb": 3,
             "P_G": 7, "P_U": 7, "P_H": 7, "P_Y": 7, "P_OT": 7}

    def _bk(r, w):
        b = set()
        for k in list(r) + list(w):
            if k.startswith("P_"):
                for pfx, bn in BANKS.items():
                    if k.startswith(pfx):
                        b.add(bn)
        return list(w) + ["BANK%d" % x for x in b]

    V = lambda fn, r=(), w=(): S.op("vector", fn, r, _bk(r, w))
    A = lambda fn, r=(), w=(): S.op("scalar", fn, r, _bk(r, w))
    G = lambda fn, r=(), w=(): S.op("gpsimd", fn, r, w)
    T = lambda fn, r=(), w=(): S.op("tensor", fn, r, _bk(r, w))

    cst = {}
    for nm in ("c_m1", "c_m2", "c_mus", "c_mls", "c_ident"):
        t = sb([128, 128]); cst[nm] = t
        S.dma("sync", t[:], dram[nm], writes=[nm])
    mask4 = sb([128, 4, 128])
    S.dma("sync", mask4[:], dram["c_mask4"], writes=["mask4"])
    ones_col = sb([128, 1])
    V(lambda e: e.memset(ones_col[:], 1.0), w=["ones_col"])
    bc = {}
    for nm in ("w0", "a0", "kk", "ka", "rk", "lng", "lnb"):
        t = sb([128, 128]); bc[nm] = t
        S.dma("sync", t[:], dram[nm].partition_broadcast(128), writes=["bc_" + nm])
    NW = 384 + 288
    wst = sb([128, 8, NW])
    S.dma("sync", wst[:, :, 0:384], dram["w_rkv"].rearrange("(dc p) n -> p dc n", p=128), writes=["wst"])
    S.dma("sync", wst[:, :, 384:NW], dram["w_lo"].rearrange("(dc p) n -> p dc n", p=128), writes=["wst"])
    mub = sb([128, NW])
    S.dma("sync", mub[:, 0:384], dram["mu_rkv"].partition_broadcast(128), writes=["mub"])
    S.dma("sync", mub[:, 384:NW], dram["mu_lo"].partition_broadcast(128), writes=["mub"])
    omu = sb([128, NW])
    V(lambda e: e.tensor_scalar(out=omu[:], in0=mub[:], scalar1=-1.0, scalar2=1.0, op0=ALU.mult, op1=ALU.add), r=["mub"], w=["omu"])
    Wa = sb([128, 8, NW], BF16); Wb = sb([128, 8, NW], BF16)
    for dc in range(8):
        V(lambda e, dc=dc: e.tensor_tensor(out=Wa[:, dc, :], in0=wst[:, dc, :], in1=omu[:], op=ALU.mult), r=["wst", "omu"], w=["Wa"])
        G(lambda e, dc=dc: e.tensor_tensor(out=Wb[:, dc, :], in0=wst[:, dc, :], in1=mub[:], op=ALU.mult), r=["wst", "mub"], w=["Wb"])
    wdu_f = sb([64, 128]); wau_f = sb([64, 128]); wgu0_f = sb([128, 128]); wgu1_f = sb([32, 128])
    S.dma("sync", wdu_f[:], dram["wdu"], writes=["wdu_f"])
    S.dma("sync", wau_f[:], dram["wau"], writes=["wau_f"])
    S.dma("sync", wgu0_f[:], dram["wgu"][0:128, :], writes=["wgu0_f"])
    S.dma("sync", wgu1_f[:], dram["wgu"][128:160, :], writes=["wgu1_f"])
    wdu = sb([64, 128], BF16); wau = sb([64, 128], BF16); wgu0 = sb([128, 128], BF16); wgu1 = sb([32, 128], BF16)
    V(lambda e: e.tensor_copy(out=wdu[:], in_=wdu_f[:]), r=["wdu_f"], w=["wdu"])
    V(lambda e: e.tensor_copy(out=wau[:], in_=wau_f[:]), r=["wau_f"], w=["wau"])
    V(lambda e: e.tensor_copy(out=wgu0[:], in_=wgu0_f[:]), r=["wgu0_f"], w=["wgu0"])
    V(lambda e: e.tensor_copy(out=wgu1[:], in_=wgu1_f[:]), r=["wgu1_f"], w=["wgu1"])

    xs = [sb([128, 8, XB + 1]) for _ in range(2)]
    xb = [sb([128, 8, XB + 1], BF16) for _ in range(2)]
    hwT = sb([64, XB], BF16); haT = sb([64, XB], BF16); hg0T = sb([128, XB], BF16); hg1T = sb([32, XB], BF16)
    Hst = sb([128, 64])
    V(lambda e: e.memset(Hst[:], 0.0), w=["Hst"])
    Bpad = sb([128, 2, 128]); Kpad = sb([128, 2, 128])
    V(lambda e: e.memset(Bpad[:], 0.0), w=["Bpad"])
    V(lambda e: e.memset(Kpad[:], 0.0), w=["Kpad"])

    P_RKV = ps([128, 512]); P_Z = ps([128, 4, 128])
    P_T = ps([128, 4, 128]); P_L1 = [ps([128, 4, 128]) for _ in range(2)]; P_LD = [ps([128, 4, 128]) for _ in range(2)]; P_M = ps([128, 512])
    P_LH = P_T[:].rearrange("p a n -> p (a n)")

    def t(shape=(128, 128), dt=F32):
        return sb(list(shape), dt)

    zw = t(); logw = t(); av = t(); gv = t(); kkr = t(); sq = t(); ss = t((128, 2)); rn = t((128, 2))
    kk = t(); tmp1 = t(); kmod = t(); kka = t(); rkr = t(); sbon = t((128, 2)); vsb = t()
    eW = t(); eWi = t(); eWp = t(); eR = t(); wc = t((128, 1))
    TM = sb([128, 4, 128])
    FT = sb([128, 4, 128])
    LM1 = [sb([128, 4, 128]) for _ in range(2)]
    PN = [[sb([128, 2, 128]) for _ in range(2)] for _ in range(2)]
    Lk = [[t() for _ in range(2)] for _ in range(2)]
    Gs = [t((128, 64)) for _ in range(2)]; Us = [t((128, 64)) for _ in range(2)]
    s1 = t((128, 2)); nmean = t((128, 2)); Yc = t(); ss2 = t((128, 2)); rstd = t((128, 2)); Yn = t(); Yo = t(); YoT = t()

    n_blocks = (n_chunks * C) // XB
    xT = dram["xT"].rearrange("(dc p) n -> p dc n", p=128)
    for blk in range(n_blocks):
        xi = blk % 2
        kx = "xs%d" % xi; kb = "xb%d" % xi
        c0 = blk * XB
        for dc in range(8):
            S.dma("sync", xs[xi][:, dc, :], xT[:, dc, c0:c0 + XB + 1], writes=[kx])
        for dc in range(8):
            eng = G if dc % 2 == 0 else A
            if dc % 2 == 0:
                G(lambda e, dc=dc, xi=xi: e.tensor_copy(out=xb[xi][:, dc, :], in_=xs[xi][:, dc, :]), r=[kx], w=[kb])
            else:
                A(lambda e, dc=dc, xi=xi: e.copy(out=xb[xi][:, dc, :], in_=xs[xi][:, dc, :]), r=[kx], w=[kb])
        for (lo, n, dst, fn, key) in ((384, 64, hwT, AF.Tanh, "hwT"), (448, 64, haT, AF.Copy, "haT"),
                                      (512, 128, hg0T, AF.Sigmoid, "hg0T"), (640, 32, hg1T, AF.Sigmoid, "hg1T")):
            for dc in range(8):
                T(lambda e, dc=dc, lo=lo, n=n, xi=xi: e.matmul(P_LH[0:n, :], lhsT=Wa[:, dc, lo:lo + n], rhs=xb[xi][:, dc, 1:XB + 1],
                                                              start=(dc == 0), stop=False), r=["Wa", kb], w=["P_T"])
            for dc in range(8):
                T(lambda e, dc=dc, lo=lo, n=n, xi=xi: e.matmul(P_LH[0:n, :], lhsT=Wb[:, dc, lo:lo + n], rhs=xb[xi][:, dc, 0:XB],
                                                              start=False, stop=(dc == 7)), r=["Wb", kb], w=["P_T"])
            A(lambda e, n=n, dst=dst, fn=fn: e.activation(out=dst[:], in_=P_LH[0:n, :], func=fn), r=["P_T"], w=[key])
        for ci in range(XB // C):
            ch = blk * (XB // C) + ci
            o = ci * C
            for dc in range(8):
                T(lambda e, dc=dc, xi=xi, o=o: e.matmul(P_RKV[:, 0:384], lhsT=xb[xi][:, dc, 1 + o:1 + o + C], rhs=Wa[:, dc, 0:384],
                                                        start=(dc == 0), stop=False), r=["Wa", kb], w=["P_RKV"])
            for dc in range(8):
                T(lambda e, dc=dc, xi=xi, o=o: e.matmul(P_RKV[:, 0:384], lhsT=xb[xi][:, dc, o:o + C], rhs=Wb[:, dc, 0:384],
                                                        start=False, stop=(dc == 7)), r=["Wb", kb], w=["P_RKV"])
            T(lambda e, o=o: e.matmul(P_Z[:, 0, :], lhsT=hwT[:, o:o + C], rhs=wdu[:], start=True, stop=True), r=["hwT", "wdu"], w=["P_Z0"])
            T(lambda e, o=o: e.matmul(P_Z[:, 1, :], lhsT=haT[:, o:o + C], rhs=wau[:], start=True, stop=True), r=["haT", "wau"], w=["P_Z1"])
            T(lambda e, o=o: e.matmul(P_RKV[:, 384:512], lhsT=hg0T[:, o:o + C], rhs=wgu0[:], start=True, stop=False), r=["hg0T", "wgu0"], w=["P_RKVg"])
            T(lambda e, o=o: e.matmul(P_RKV[:, 384:512], lhsT=hg1T[:, o:o + C], rhs=wgu1[:], start=False, stop=True), r=["hg1T", "wgu1"], w=["P_RKVg"])
            V(lambda e: e.tensor_tensor(out=zw[:], in0=P_Z[:, 0, :], in1=bc["w0"][:], op=ALU.add), r=["P_Z0", "bc_w0"], w=["zw"])
            A(lambda e: e.activation(out=zw[:], in_=zw[:], func=AF.Sigmoid), r=["zw"], w=["zw"])
            G(lambda e: e.tensor_scalar(out=logw[:], in0=zw[:], scalar1=C0, scalar2=None, op0=ALU.mult), r=["zw"], w=["logw"])
            V(lambda e: e.tensor_tensor(out=av[:], in0=P_Z[:, 1, :], in1=bc["a0"][:], op=ALU.add), r=["P_Z1", "bc_a0"], w=["av"])
            A(lambda e: e.activation(out=av[:], in_=av[:], func=AF.Sigmoid), r=["av"], w=["av"])
            A(lambda e: e.copy(out=gv[:], in_=P_RKV[:, 384:512]), r=["P_RKVg"], w=["gv"])
            A(lambda e: e.copy(out=vsb[:], in_=P_RKV[:, 256:384]), r=["P_RKV"], w=["vsb"])
            V(lambda e: e.tensor_tensor(out=kkr[:], in0=P_RKV[:, 128:256], in1=bc["kk"][:], op=ALU.mult), r=["P_RKV", "bc_kk"], w=["kkr"])
            G(lambda e: e.tensor_tensor(out=sq[:], in0=kkr[:], in1=kkr[:], op=ALU.mult), r=["kkr"], w=["sq"])
            V(lambda e: e.tensor_reduce(out=ss[:], in_=sq[:].rearrange("p (h c) -> p h c", h=2), axis=AX.X, op=ALU.add), r=["sq"], w=["ss"])
            A(lambda e: e.activation(out=ss[:], in_=ss[:], func=AF.Sqrt), r=["ss"], w=["ss"])
            V(lambda e: e.tensor_scalar(out=ss[:], in0=ss[:], scalar1=1e-12, scalar2=None, op0=ALU.max), r=["ss"], w=["ss"])
            V(lambda e: e.reciprocal(out=rn[:], in_=ss[:]), r=["ss"], w=["rn"])
            for h in range(2):
                V(lambda e, h=h: e.tensor_scalar(out=kk[:, h * 64:(h + 1) * 64], in0=kkr[:, h * 64:(h + 1) * 64], scalar1=rn[:, h:h + 1],
                                                 scalar2=None, op0=ALU.mult), r=["kkr", "rn"], w=["kk"])
            V(lambda e: e.scalar_tensor_tensor(out=tmp1[:], in0=av[:], scalar=-1.0, in1=bc["ka"][:], op0=ALU.add, op1=ALU.mult), r=["av", "bc_ka"], w=["tmp1"])
            V(lambda e: e.scalar_tensor_tensor(out=kmod[:], in0=tmp1[:], scalar=1.0, in1=P_RKV[:, 128:256], op0=ALU.add, op1=ALU.mult), r=["tmp1", "P_RKV"], w=["kmod"])
            G(lambda e: e.tensor_tensor(out=kka[:], in0=kk[:], in1=av[:], op=ALU.mult), r=["kk", "av"], w=["kka"])
            V(lambda e: e.tensor_tensor(out=rkr[:], in0=P_RKV[:, 0:128], in1=kmod[:], op=ALU.mult), r=["P_RKV", "kmod"], w=["rkr"])
            G(lambda e: e.tensor_tensor(out=rkr[:], in0=rkr[:], in1=bc["rk"][:], op=ALU.mult), r=["rkr", "bc_rk"], w=["rkr"])
            V(lambda e: e.tensor_reduce(out=sbon[:], in_=rkr[:].rearrange("p (h c) -> p h c", h=2), axis=AX.X, op=ALU.add), r=["rkr"], w=["sbon"])
            T(lambda e: e.matmul(P_Z[:, 2, :], lhsT=cst["c_m1"][:], rhs=logw[:], start=True, stop=True), r=["c_m1", "logw"], w=["P_Z2"])
            T(lambda e: e.matmul(P_Z[:, 3, :], lhsT=cst["c_m2"][:], rhs=logw[:], start=True, stop=True), r=["c_m2", "logw"], w=["P_Z3"])
            T(lambda e: e.matmul(P_M[:, 448:449], lhsT=logw[:], rhs=ones_col[:], start=True, stop=True), r=["ones_col", "logw"], w=["P_Hwc"])
            A(lambda e: e.activation(out=eW[:], in_=P_Z[:, 2, :], func=AF.Exp), r=["P_Z2"], w=["eW"])
            A(lambda e: e.activation(out=eWi[:], in_=P_Z[:, 2, :], func=AF.Exp, scale=-1.0), r=["P_Z2"], w=["eWi"])
            A(lambda e: e.activation(out=eWp[:], in_=logw[:], func=AF.Exp, scale=-1.0), r=["logw"], w=["eWp"])
            G(lambda e: e.tensor_tensor(out=eWp[:], in0=eWp[:], in1=eW[:], op=ALU.mult), r=["eWp", "eW"], w=["eWp"])
            A(lambda e: e.activation(out=eR[:], in_=P_Z[:, 3, :], func=AF.Exp), r=["P_Z3"], w=["eR"])
            A(lambda e: e.activation(out=wc[:], in_=P_M[:, 448:449], func=AF.Exp), r=["P_Hwc"], w=["wc"])
            V(lambda e: e.scalar_tensor_tensor(out=TM[:, 0, :], in0=kk[:], scalar=-1.0, in1=eWp[:], op0=ALU.mult, op1=ALU.mult), r=["kk", "eWp"], w=["TM0"])
            V(lambda e: e.tensor_tensor(out=TM[:, 1, :], in0=P_RKV[:, 0:128], in1=eW[:], op=ALU.mult), r=["P_RKV", "eW"], w=["TM1"])
            G(lambda e: e.tensor_tensor(out=TM[:, 2, :], in0=kka[:], in1=eWi[:], op=ALU.mult), r=["kka", "eWi"], w=["TM2"])
            V(lambda e: e.tensor_tensor(out=TM[:, 3, :], in0=kmod[:], in1=eWi[:], op=ALU.mult), r=["kmod", "eWi"], w=["TM3"])
            for h in range(2):
                hs = slice(h * 64, (h + 1) * 64)
                G(lambda e, h=h, hs=hs: e.tensor_tensor(out=Bpad[:, h, hs], in0=kka[:, hs], in1=eR[:, hs], op=ALU.mult), r=["kka", "eR"], w=["Bpad"])
                V(lambda e, h=h, hs=hs: e.tensor_tensor(out=Kpad[:, h, hs], in0=kmod[:, hs], in1=eR[:, hs], op=ALU.mult), r=["kmod", "eR"], w=["Kpad"])
            for j in range(4):
                T(lambda e, j=j: e.transpose(P_T[:, j, :], TM[:, j, :], cst["c_ident"][:]), r=["TM%d" % j, "c_ident"], w=["P_T"])
            A(lambda e: e.copy(out=FT[:], in_=P_T[:]), r=["P_T"], w=["FT"])
            HSL = [slice(0, 64), slice(64, 128)]
            BK = ["a", "b"]
            for h in range(2):
                hs = HSL[h]
                T(lambda e, hs=hs, h=h: e.matmul(P_L1[h][:, 0:2, :], lhsT=FT[hs, 2, :], rhs=FT[hs, 0:2, :], start=True, stop=True), r=["FT"], w=["P_L1%s" % BK[h]])
                T(lambda e, hs=hs, h=h: e.matmul(P_L1[h][:, 2:4, :], lhsT=FT[hs, 3, :], rhs=FT[hs, 0:2, :], start=True, stop=True), r=["FT"], w=["P_L1%s" % BK[h]])
                T(lambda e, hs=hs, h=h: e.matmul(P_LD[h][:, 0, :], lhsT=FT[hs, 0, :], rhs=FT[hs, 2, :], start=True, stop=True), r=["FT"], w=["P_LD%s0" % BK[h]])
            for h in range(2):
                V(lambda e, h=h: e.tensor_tensor(out=LM1[h][:], in0=P_L1[h][:], in1=mask4[:], op=ALU.mult), r=["P_L1%s" % BK[h], "mask4"], w=["LM1%d" % h])
                V(lambda e, h=h: e.tensor_tensor(out=Lk[h][0][:], in0=P_LD[h][:, 0, :], in1=cst["c_mls"][:], op=ALU.mult), r=["P_LD%s0" % BK[h], "c_mls"], w=["Lk%d.0" % h])
            for h in range(2):
                G(lambda e, h=h: e.tensor_tensor(out=PN[h][0][:, 0, :], in0=LM1[h][:, 0, :], in1=cst["c_ident"][:], op=ALU.add), r=["LM1%d" % h, "c_ident"], w=["PN%d.0.P" % h])
                T(lambda e, h=h: e.matmul(P_LD[h][:, 2, :], lhsT=Lk[h][0][:], rhs=LM1[h][:, 0, :], start=True, stop=True), r=["Lk%d.0" % h, "LM1%d" % h], w=["P_LD%s12" % BK[h]])
                T(lambda e, h=h: e.matmul(P_LD[h][:, 3, :], lhsT=LM1[h][:, 0, :], rhs=Lk[h][0][:], start=True, stop=True), r=["Lk%d.0" % h, "LM1%d" % h], w=["P_LD%s3" % BK[h]])
            for h in range(2):
                A(lambda e, h=h: e.copy(out=PN[h][0][:, 1, :], in_=P_LD[h][:, 2, :]), r=["P_LD%s12" % BK[h]], w=["PN%d.0.N" % h])
                A(lambda e, h=h: e.copy(out=Lk[h][1][:], in_=P_LD[h][:, 3, :]), r=["P_LD%s3" % BK[h]], w=["Lk%d.1" % h])
            cur = 0
            lcur = 1
            for lev in range(1, 7):
                nxt = 1 - cur
                lnx = 1 - lcur
                last = (lev == 6)
                for h in range(2):
                    pk = "PN%d.%d" % (h, cur); lk = "Lk%d.%d" % (h, lcur)
                    if not last:
                        T(lambda e, cur=cur, lcur=lcur, h=h: e.matmul(P_LD[h][:, 1:3, :], lhsT=Lk[h][lcur][:], rhs=PN[h][cur][:], start=True, stop=True),
                          r=[lk, pk + ".P", pk + ".N"], w=["P_LD%s12" % BK[h]])
                        T(lambda e, cur=cur, lcur=lcur, h=h: e.matmul(P_LD[h][:, 3, :], lhsT=PN[h][cur][:, 1, :], rhs=Lk[h][lcur][:], start=True, stop=True),
                          r=[lk, pk + ".N"], w=["P_LD%s3" % BK[h]])
                    else:
                        T(lambda e, cur=cur, lcur=lcur, h=h: e.matmul(P_LD[h][:, 1, :], lhsT=Lk[h][lcur][:], rhs=PN[h][cur][:, 0, :], start=True, stop=True),
                          r=[lk, pk + ".P"], w=["P_LD%s12" % BK[h]])
                for h in range(2):
                    pk = "PN%d.%d" % (h, cur); pn = "PN%d.%d" % (h, nxt); ln_ = "Lk%d.%d" % (h, lnx)
                    V(lambda e, cur=cur, nxt=nxt, h=h: e.tensor_tensor(out=PN[h][nxt][:, 0, :], in0=P_LD[h][:, 1, :], in1=PN[h][cur][:, 0, :], op=ALU.add),
                      r=["P_LD%s12" % BK[h], pk + ".P"], w=[pn + ".P"])
                    if not last:
                        A(lambda e, nxt=nxt, h=h: e.copy(out=PN[h][nxt][:, 1, :], in_=P_LD[h][:, 2, :]), r=["P_LD%s12" % BK[h]], w=[pn + ".N"])
                        A(lambda e, lnx=lnx, h=h: e.copy(out=Lk[h][lnx][:], in_=P_LD[h][:, 3, :]), r=["P_LD%s3" % BK[h]], w=[ln_])
                cur = nxt
                lcur = lnx
            for h in range(2):
                hs = HSL[h]
                T(lambda e, hs=hs, h=h: e.matmul(P_M[:, 64 * h:64 * h + 64], lhsT=FT[hs, 0, :], rhs=Hst[hs, :], start=True, stop=False), r=["FT", "Hst"], w=["P_G%d" % h])
                T(lambda e, hs=hs, h=h: e.matmul(P_M[:, 64 * h:64 * h + 64], lhsT=LM1[h][:, 2, :], rhs=vsb[:, hs], start=False, stop=True), r=["LM1%d" % h, "vsb"], w=["P_G%d" % h])
            A(lambda e: e.copy(out=Gs[0][:], in_=P_M[:, 0:64]), r=["P_G0"], w=["Gs0"])
            V(lambda e: e.tensor_copy(out=Gs[1][:], in_=P_M[:, 64:128]), r=["P_G1"], w=["Gs1"])
            for h in range(2):
                T(lambda e, h=h, cur=cur: e.matmul(P_M[:, 320 + 64 * h:384 + 64 * h], lhsT=PN[h][cur][:, 0, :], rhs=Gs[h][:], start=True, stop=True),
                  r=["PN%d.%d.P" % (h, cur), "Gs%d" % h], w=["P_U%d" % h])
            A(lambda e: e.copy(out=Us[0][:], in_=P_M[:, 320:384]), r=["P_U0"], w=["Us0"])
            V(lambda e: e.tensor_copy(out=Us[1][:], in_=P_M[:, 384:448]), r=["P_U1"], w=["Us1"])
            for h in range(2):
                hs = HSL[h]
                yk = "P_Y%d" % h
                uk = "Us%d" % h
                T(lambda e, hs=hs, h=h: e.matmul(P_M[:, 192 + 64 * h:256 + 64 * h], lhsT=FT[hs, 1, :], rhs=Hst[hs, :], start=True, stop=False), r=["FT", "Hst"], w=[yk])
                T(lambda e, h=h: e.matmul(P_M[:, 192 + 64 * h:256 + 64 * h], lhsT=LM1[h][:, 1, :], rhs=Us[h][:], start=False, stop=False), r=["LM1%d" % h, uk], w=[yk])
                T(lambda e, hs=hs, h=h: e.matmul(P_M[:, 192 + 64 * h:256 + 64 * h], lhsT=LM1[h][:, 3, :], rhs=vsb[:, hs], start=False, stop=True), r=["LM1%d" % h, "vsb"], w=[yk])
            T(lambda e: e.matmul(P_M[:, 128:192], lhsT=Bpad[:, 0, :], rhs=Us[0][:], start=True, stop=False), r=["Bpad", "Us0"], w=["P_H"])
            T(lambda e: e.matmul(P_M[:, 128:192], lhsT=Kpad[:, 0, :], rhs=vsb[:, 0:64], start=False, stop=False), r=["Kpad", "vsb"], w=["P_H"])
            T(lambda e: e.matmul(P_M[:, 128:192], lhsT=Bpad[:, 1, :], rhs=Us[1][:], start=False, stop=False), r=["Bpad", "Us1"], w=["P_H"])
            T(lambda e: e.matmul(P_M[:, 128:192], lhsT=Kpad[:, 1, :], rhs=vsb[:, 64:128], start=False, stop=True), r=["Kpad", "vsb"], w=["P_H"])
            V(lambda e: e.scalar_tensor_tensor(out=Hst[:], in0=Hst[:], scalar=wc[:, 0:1], in1=P_M[:, 128:192], op0=ALU.mult, op1=ALU.add),
              r=["Hst", "wc", "P_H"], w=["Hst"])
            Yv = P_M[:, 192:320]
            V(lambda e: e.tensor_reduce(out=s1[:], in_=Yv.rearrange("p (h c) -> p h c", h=2), axis=AX.X, op=ALU.add), r=["P_Y0", "P_Y1"], w=["s1"])
            V(lambda e: e.tensor_scalar(out=nmean[:], in0=s1[:], scalar1=-1.0 / 64, scalar2=None, op0=ALU.mult), r=["s1"], w=["nmean"])
            for h in range(2):
                hs = slice(h * 64, (h + 1) * 64)
                V(lambda e, h=h, hs=hs: e.tensor_scalar(out=Yc[:, hs], in0=P_M[:, 192 + 64 * h:256 + 64 * h], scalar1=nmean[:, h:h + 1], scalar2=None, op0=ALU.add),
                  r=["P_Y%d" % h, "nmean"], w=["Yc"])
            G(lambda e: e.tensor_tensor(out=sq[:], in0=Yc[:], in1=Yc[:], op=ALU.mult), r=["Yc"], w=["sq"])
            V(lambda e: e.tensor_reduce(out=ss2[:], in_=sq[:].rearrange("p (h c) -> p h c", h=2), axis=AX.X, op=ALU.add), r=["sq"], w=["ss2"])
            V(lambda e: e.tensor_scalar(out=ss2[:], in0=ss2[:], scalar1=1.0 / 64, scalar2=GN_EPS, op0=ALU.mult, op1=ALU.add), r=["ss2"], w=["ss2"])
            A(lambda e: e.activation(out=ss2[:], in_=ss2[:], func=AF.Sqrt), r=["ss2"], w=["ss2"])
            V(lambda e: e.reciprocal(out=rstd[:], in_=ss2[:]), r=["ss2"], w=["rstd"])
            for h in range(2):
                hs = slice(h * 64, (h + 1) * 64)
                V(lambda e, h=h, hs=hs: e.tensor_scalar(out=Yn[:, hs], in0=Yc[:, hs], scalar1=rstd[:, h:h + 1], scalar2=None, op0=ALU.mult), r=["Yc", "rstd"], w=["Yn"])
            G(lambda e: e.tensor_tensor(out=Yn[:], in0=Yn[:], in1=bc["lng"][:], op=ALU.mult), r=["Yn", "bc_lng"], w=["Yn"])
            G(lambda e: e.tensor_tensor(out=Yn[:], in0=Yn[:], in1=bc["lnb"][:], op=ALU.add), r=["Yn", "bc_lnb"], w=["Yn"])
            for h in range(2):
                hs = slice(h * 64, (h + 1) * 64)
                V(lambda e, h=h, hs=hs: e.scalar_tensor_tensor(out=Yo[:, hs], in0=vsb[:, hs], scalar=sbon[:, h:h + 1], in1=Yn[:, hs], op0=ALU.mult, op1=ALU.add),
                  r=["vsb", "sbon", "Yn"], w=["Yo"])
            G(lambda e: e.tensor_tensor(out=Yo[:], in0=Yo[:], in1=gv[:], op=ALU.mult), r=["Yo", "gv"], w=["Yo"])
            T(lambda e: e.transpose(P_T[:, 0, :], Yo[:], cst["c_ident"][:]), r=["Yo", "c_ident"], w=["P_T"])
            A(lambda e: e.copy(out=YoT[:], in_=P_T[:, 0, :]), r=["P_T"], w=["YoT"])
            S.dma("sync", dram["cin"][(ch * C) // 1024][0:128, (ch * C) % 1024:(ch * C) % 1024 + C], YoT[:], reads=["YoT"])


S_TOK = 16384
AXB = 256
KB_BOUND = 16.0
NEG = -30000.0


def attn_consts():
    s = S_TOK
    half = 32
    inv_freq = (10000.0 ** (-np.arange(half, dtype=np.float32) / half)).astype(np.float32)
    ang = (np.arange(s, dtype=np.float32)[:, None] * inv_freq[None, :]).astype(np.float32)
    cos = np.cos(ang).astype(np.float32)
    sin = np.sin(ang).astype(np.float32)
    ropeA = np.tile(cos, (1, 8)).astype(np.float32)
    ropeB = np.tile(np.concatenate([-sin, sin], 1), (1, 4)).astype(np.float32)
    kind = np.zeros((64, s), np.float32)
    for n in range(64):
        kind[n, n * 256:(n + 1) * 256] = 1.0
    i = np.arange(128)
    tri = (i[None, :] >= i[:, None]).astype(np.float32)
    return {"a_ropeA": ropeA, "a_ropeB": ropeB, "a_kind": kind.astype(ml_dtypes.bfloat16),
            "a_tri": tri.astype(ml_dtypes.bfloat16), "a_ident": np.eye(128, dtype=np.float32),
            "a_identb": np.eye(128, dtype=np.float32).astype(ml_dtypes.bfloat16)}


def build_attn(nc, S, es, dram, n_blocks=S_TOK // 256):
    sbn = [0]

    def sb(shape, dt=F32):
        sbn[0] += 1
        return es.enter_context(nc.sbuf_tensor("at%d" % sbn[0], shape, dt))

    def ps(shape, dt=F32):
        sbn[0] += 1
        return es.enter_context(nc.psum_tensor("ap%d" % sbn[0], shape, dt))

    def _bk(r, w):
        b = set(k.split(".")[0] for k in list(r) + list(w) if k.startswith("Q_"))
        return list(w) + ["BANK_" + x for x in b]

    defer = [None]

    def _rec(eng, fn, r, w):
        if defer[0] is None:
            S.op(eng, fn, r, w)
        else:
            defer[0].append((eng, fn, r, w))

    def DMA(out, in_, reads=(), writes=()):
        if defer[0] is None:
            S.dma("sync", out, in_, reads=reads, writes=writes)
        else:
            defer[0].append(("dma", (out, in_), reads, writes))

    def drain(lst, n):
        for _ in range(min(n, len(lst))):
            eng, fn, r, w = lst.pop(0)
            if eng == "dma":
                S.dma("sync", fn[0], fn[1], reads=r, writes=w)
            else:
                S.op(eng, fn, r, w)

    V = lambda fn, r=(), w=(): _rec("vector", fn, r, _bk(r, w))
    A = lambda fn, r=(), w=(): _rec("scalar", fn, r, _bk(r, w))
    G = lambda fn, r=(), w=(): _rec("gpsimd", fn, r, list(w))
    T = lambda fn, r=(), w=(): _rec("tensor", fn, r, _bk(r, w))

    ident = sb([128, 128]); identb = sb([128, 128], BF16); tri = sb([128, 128], BF16)
    S.dma("sync", ident[:], dram["a_ident"], writes=["ident"])
    S.dma("sync", identb[:], dram["a_identb"], writes=["identb"])
    S.dma("sync", tri[:], dram["a_tri"], writes=["tri"])
    wst = sb([128, 8, 384])
    S.dma("sync", wst[:], dram["w_qkv"].rearrange("(dc p) n -> p dc n", p=128), writes=["wst"])
    Wq = sb([128, 8, 384], BF16)
    for dc in range(8):
        V(lambda e, dc=dc: e.tensor_copy(out=Wq[:, dc, :], in_=wst[:, dc, :]), r=["wst"], w=["Wq"])
    inv256 = sb([128, 1]); ones_r = sb([128, 64])
    V(lambda e: e.memset(inv256[:], 1.0 / 256), w=["inv256"])
    V(lambda e: e.memset(ones_r[:], 1.0), w=["ones_r"])
    KT = [sb([128, S_TOK], BF16) for _ in range(2)]
    VA = [sb([128, 128, 65], BF16) for _ in range(2)]
    kmT = [sb([64, 64]) for _ in range(2)]
    Gt = [sb([128, 64]) for _ in range(2)]
    for h in range(2):
        S.dma("sync", KT[h][64:128, :], dram["a_kind"], writes=["KT%d.%d" % (h, b_) for b_ in range(64)])
        G(lambda e, h=h: e.memset(VA[h][:], 1.0), w=["VA%d.%d" % (h, b_) for b_ in range(64)])
        V(lambda e, h=h: e.memset(kmT[h][:], 0.0), w=["kmT%d" % h])
        V(lambda e, h=h: e.memset(Gt[h][:], -1e30), w=["Gt%d" % h])
    xs = [sb([128, 8, AXB + 1]) for _ in range(2)]
    xb = [sb([128, 8, AXB + 1], BF16) for _ in range(2)]
    rA = [sb([128, 256]) for _ in range(2)]; rB = [sb([128, 256]) for _ in range(2)]
    tmpA = sb([128, 256]); tmpB = sb([128, 256]); QK = sb([128, 4, 64])
    qT32 = sb([64, 128]); mx = sb([128, 8]); negm = sb([128, 64]); ssq = sb([128, 1]); cpos = sb([128, 1]); qjunk = sb([128, 64])
    Qaug = [sb([128, 128], BF16) for _ in range(2)]
    QT = [[sb([128, 256], BF16) for _ in range(2)] for _ in range(2)]
    PT = [sb([128, 256], BF16) for _ in range(2)]
    rec = sb([128, 256]); ysb = sb([64, 256]); yo = sb([64, 256])

    Q_A = ps([128, 512]); Q_B = ps([128, 512]); Q_C = ps([128, 512]); Q_D = ps([128, 256], BF16)
    Q_S = [ps([128, 512]) for _ in range(2)]; Q_O = [ps([128, 512]) for _ in range(2)]

    xT = dram["xT"].rearrange("(dc p) n -> p dc n", p=128)
    sidx = 0

    def gen_proj(blk):
        xi = blk % 2
        kx = "xs%d" % xi; kb = "xb%d" % xi
        c0 = blk * AXB
        for dc in range(8):
            DMA(xs[xi][:, dc, :], xT[:, dc, c0:c0 + AXB + 1], writes=[kx])
        for dc in range(8):
            if dc % 2 == 0:
                G(lambda e, dc=dc, xi=xi: e.tensor_copy(out=xb[xi][:, dc, :], in_=xs[xi][:, dc, :]), r=[kx], w=[kb])
            else:
                V(lambda e, dc=dc, xi=xi: e.tensor_copy(out=xb[xi][:, dc, :], in_=xs[xi][:, dc, :]), r=[kx], w=[kb])
        for half in range(2):
            tt = blk * 2 + half
            o = half * 128
            ri = tt % 2
            DMA(rA[ri][:], dram["a_ropeA"][tt * 128:(tt + 1) * 128, :], writes=["rA%d" % ri])
            DMA(rB[ri][:], dram["a_ropeB"][tt * 128:(tt + 1) * 128, :], writes=["rB%d" % ri])
            for dc in range(8):
                T(lambda e, dc=dc, xi=xi, o=o: e.matmul(Q_A[:, 0:384], lhsT=xb[xi][:, dc, 1 + o:1 + o + 128], rhs=Wq[:, dc, :],
                                                        start=(dc == 0), stop=(dc == 7)), r=["Wq", kb], w=["Q_A"])
            X4 = Q_A[:, 0:256].rearrange("p (g h d) -> p g h d", g=4, h=2)
            B4 = lambda t_: t_[:].rearrange("p (g h d) -> p g h d", g=4, h=2)
            V(lambda e, ri=ri: e.tensor_tensor(out=tmpA[:], in0=Q_A[:, 0:256], in1=rA[ri][:], op=ALU.mult), r=["Q_A", "rA%d" % ri], w=["tmpA"])
            V(lambda e, ri=ri: e.tensor_tensor(out=B4(tmpB)[:, :, 0, :], in0=X4[:, :, 1, :], in1=B4(rB[ri])[:, :, 0, :], op=ALU.mult), r=["Q_A", "rB%d" % ri], w=["tmpB"])
            V(lambda e, ri=ri: e.tensor_tensor(out=B4(tmpB)[:, :, 1, :], in0=X4[:, :, 0, :], in1=B4(rB[ri])[:, :, 1, :], op=ALU.mult), r=["Q_A", "rB%d" % ri], w=["tmpB"])
            G(lambda e: e.tensor_tensor(out=QK[:].rearrange("p g d -> p (g d)"), in0=tmpA[:], in1=tmpB[:], op=ALU.add), r=["tmpA", "tmpB"], w=["QK"])
            for h in range(2):
                A(lambda e, h=h, tt=tt: e.copy(out=VA[h][:, tt, 0:64], in_=Q_A[:, 256 + 64 * h:320 + 64 * h]), r=["Q_A"], w=["VA%d.%d" % (h, blk)])
                T(lambda e, h=h: e.transpose(Q_B[0:64, 0:128], QK[:, 2 + h, :], ident[:]), r=["QK", "ident"], w=["Q_B.k"])
                A(lambda e, h=h, tt=tt: e.copy(out=KT[h][0:64, tt * 128:(tt + 1) * 128], in_=Q_B[0:64, 0:128]), r=["Q_B.k"], w=["KT%d.%d" % (h, blk)])
                T(lambda e, h=h: e.transpose(Q_B[0:64, 128:256], QK[:, h, :], ident[:]), r=["QK", "ident"], w=["Q_B.q"])
                V(lambda e: e.tensor_copy(out=qT32[:], in_=Q_B[0:64, 128:256]), r=["Q_B.q"], w=["qT32"])
                if blk > 0:
                    T(lambda e, h=h: e.matmul(Q_C[:, 0:64], lhsT=qT32[:], rhs=kmT[h][:], start=True, stop=True), r=["qT32", "kmT%d" % h], w=["Q_C.g"])
                    V(lambda e, h=h, blk=blk: e.tensor_copy(out=Gt[h][:, 0:blk], in_=Q_C[:, 0:blk]), r=["Q_C.g"], w=["Gt%d" % h])
                V(lambda e, h=h: e.max(out=mx[:], in_=Gt[h][:]), r=["Gt%d" % h], w=["mx"])
                V(lambda e, h=h: e.tensor_scalar(out=negm[:], in0=Gt[h][:], scalar1=mx[:, 2:3], scalar2=NEG, op0=ALU.is_lt, op1=ALU.mult), r=["Gt%d" % h, "mx"], w=["negm"])
                V(lambda e, blk=blk: e.memset(negm[:, blk:blk + 1], 0.0), w=["negm"])
                V(lambda e: e.memset(ssq[:], 0.0), w=["ssq"])
                A(lambda e, h=h: e.activation(out=qjunk[:], in_=QK[:, h, :], func=AF.Square, accum_out=ssq[:]), r=["QK", "ssq"], w=["qjunk", "ssq"])
                A(lambda e: e.activation(out=cpos[:], in_=ssq[:], func=AF.Sqrt, scale=KB_BOUND * KB_BOUND), r=["ssq"], w=["cpos"])
                G(lambda e, h=h: e.tensor_copy(out=Qaug[h][:, 0:64], in_=QK[:, h, :]), r=["QK"], w=["Qaug%d" % h])
                V(lambda e, h=h: e.tensor_scalar(out=Qaug[h][:, 64:128], in0=negm[:], scalar1=cpos[:, 0:1], scalar2=None, op0=ALU.subtract), r=["negm", "cpos"], w=["Qaug%d" % h])
                T(lambda e, h=h: e.transpose(Q_D[:, 0:128], Qaug[h][:], identb[:]), r=["Qaug%d" % h, "identb"], w=["Q_D"])
                A(lambda e, h=h, o=o: e.copy(out=QT[h][blk % 2][:, o:o + 128], in_=Q_D[:, 0:128]), r=["Q_D"], w=["QT%d.%d" % (h, blk % 2)])
                T(lambda e, h=h: e.matmul(Q_C[0:64, 64:65], lhsT=QK[:, 2 + h, :], rhs=inv256[:], start=True, stop=True), r=["QK", "inv256"], w=["Q_C.m"])
                if half == 0:
                    V(lambda e, h=h, blk=blk: e.tensor_copy(out=kmT[h][:, blk:blk + 1], in_=Q_C[0:64, 64:65]), r=["Q_C.m"], w=["kmT%d" % h])
                else:
                    V(lambda e, h=h, blk=blk: e.tensor_tensor(out=kmT[h][:, blk:blk + 1], in0=Q_C[0:64, 64:65], in1=kmT[h][:, blk:blk + 1], op=ALU.add),
                      r=["Q_C.m", "kmT%d" % h], w=["kmT%d" % h])

    gen_proj(0)
    for blk in range(n_blocks):
        pend = []
        if blk + 1 < n_blocks:
            defer[0] = pend
            gen_proj(blk + 1)
            defer[0] = None
        steps = []
        for h in range(2):
            nkt = 2 * blk + 2
            for kt in range(nkt):
                steps.append((h, kt, nkt))

        def issue_qk(i):
            h, kt, nkt = steps[i]
            j = kt - 2 * blk
            q0 = 128 if j == 1 else 0
            si = (sbase + i) % 2
            T(lambda e, h=h, kt=kt, q0=q0, si=si, bp=blk % 2: e.matmul(Q_S[si][:, q0:256], lhsT=KT[h][:, kt * 128:(kt + 1) * 128], rhs=QT[h][bp][:, q0:256],
                                                           start=True, stop=True), r=["KT%d.%d" % (h, kt // 2), "QT%d.%d" % (h, blk % 2)], w=["Q_S%d" % si])

        sbase = sidx
        issue_qk(0)
        for i in range(len(steps)):
            h, kt, nkt = steps[i]
            j = kt - 2 * blk
            q0 = 128 if j == 1 else 0
            si = (sbase + i) % 2
            sk = "Q_S%d" % si
            pk = "PT%d" % si
            ok = "Q_O%d" % h
            if i + 1 < len(steps):
                issue_qk(i + 1)
            A(lambda e, q0=q0, si=si: e.activation(out=PT[si][:, q0:256], in_=Q_S[si][:, q0:256], func=AF.Exp, scale=0.125), r=[sk], w=[pk])
            if j >= 0:
                G(lambda e, q0=q0, si=si, j=j: e.tensor_tensor(out=PT[si][:, j * 128:(j + 1) * 128], in0=PT[si][:, j * 128:(j + 1) * 128], in1=tri[:], op=ALU.mult),
                  r=[pk, "tri"], w=[pk])
            T(lambda e, h=h, kt=kt, q0=q0, si=si, nkt=nkt: e.matmul(Q_O[h][0:65, q0:256], lhsT=VA[h][:, kt, :], rhs=PT[si][:, q0:256],
                                                                    start=(kt == 0), stop=(kt == nkt - 1)), r=["VA%d.%d" % (h, kt // 2), pk], w=[ok])
            if kt == nkt - 1:
                V(lambda e, h=h: e.reciprocal(out=rec[64:65, :], in_=Q_O[h][64:65, 0:256]), r=[ok], w=["rec"])
                T(lambda e: e.matmul(Q_C[0:64, 256:512], lhsT=ones_r[64:65, :], rhs=rec[64:65, :], start=True, stop=True), r=["ones_r", "rec"], w=["Q_C.bc"])
                A(lambda e, h=h: e.copy(out=ysb[:], in_=Q_O[h][0:64, 0:256]), r=[ok], w=["ysb"])
                V(lambda e: e.tensor_tensor(out=yo[:], in0=ysb[:], in1=Q_C[0:64, 256:512], op=ALU.mult), r=["ysb", "Q_C.bc"], w=["yo"])
                S.dma("sync", dram["cin"][(blk * 256) // 1024][128 + h * 64:128 + (h + 1) * 64, (blk * 256) % 1024:(blk * 256) % 1024 + 256], yo[:], reads=["yo"])
            if pend:
                drain(pend, -(-len(pend) // max(1, len(steps) - i)))
        drain(pend, len(pend))
        sidx += len(steps)


NTOK = 4096
ALPHA = float(2.0 ** 0.25)
LN_EPS = 1e-5
MOE_NSTG = 3


def _mk(nc, S, es, pfx):
    sbn = [0]

    def sb(shape, dt=F32):
        sbn[0] += 1
        return es.enter_context(nc.sbuf_tensor("%s%d" % (pfx, sbn[0]), shape, dt))

    def ps(shape, dt=F32):
        sbn[0] += 1
        return es.enter_context(nc.psum_tensor("%sp%d" % (pfx, sbn[0]), shape, dt))

    def _bk(r, w):
        b = set(k.split(".")[0] for k in list(r) + list(w) if k.startswith("Q_"))
        return list(w) + ["BANK_" + x for x in b]

    V = lambda fn, r=(), w=(): S.op("vector", fn, r, _bk(r, w))
    A = lambda fn, r=(), w=(): S.op("scalar", fn, r, _bk(r, w))
    G = lambda fn, r=(), w=(): S.op("gpsimd", fn, r, w)
    T = lambda fn, r=(), w=(): S.op("tensor", fn, r, _bk(r, w))
    return sb, ps, V, A, G, T


def layer_norm_tile(V, A, G, t, tk, junk, st, gbc, bbc, gk, bk):
    V(lambda e: e.tensor_reduce(out=st[:, 0:1], in_=t[:], axis=AX.X, op=ALU.add), r=[tk], w=["st0"])
    V(lambda e: e.tensor_scalar(out=st[:, 0:1], in0=st[:, 0:1], scalar1=-1.0 / 1024, scalar2=None, op0=ALU.mult), r=["st0"], w=["st0"])
    V(lambda e: e.tensor_scalar(out=t[:], in0=t[:], scalar1=st[:, 0:1], scalar2=None, op0=ALU.add), r=[tk, "st0"], w=[tk])
    V(lambda e: e.memset(st[:, 1:2], 0.0), w=["st1"])
    A(lambda e: e.activation(out=junk[:], in_=t[:], func=AF.Square, accum_out=st[:, 1:2]), r=[tk, "st1"], w=["junk", "st1"])
    V(lambda e: e.tensor_scalar(out=st[:, 1:2], in0=st[:, 1:2], scalar1=1.0 / 1024, scalar2=LN_EPS, op0=ALU.mult, op1=ALU.add), r=["st1"], w=["st1"])
    A(lambda e: e.activation(out=st[:, 1:2], in_=st[:, 1:2], func=AF.Sqrt), r=["st1"], w=["st1"])
    V(lambda e: e.reciprocal(out=st[:, 2:3], in_=st[:, 1:2]), r=["st1"], w=["st2"])
    V(lambda e: e.scalar_tensor_tensor(out=t[:], in0=t[:], scalar=st[:, 2:3], in1=gbc[:], op0=ALU.mult, op1=ALU.mult), r=[tk, "st2", gk], w=[tk])
    G(lambda e: e.tensor_tensor(out=t[:], in0=t[:], in1=bbc[:], op=ALU.add), r=[tk, bk], w=[tk])


def build_stage_a(nc, S, es, dram, n_groups=NTOK // 512):
    sb, ps, V, A, G, T = _mk(nc, S, es, "sa")
    stg = [sb([128, 2048]) for _ in range(2)]
    Wg = sb([128, 8, 2048], BF16); Wab = sb([128, 4, 1024], BF16); Wrb = sb([128, 4, 1024], BF16); Wo = sb([128, 8, 1024], BF16)
    si = 0
    wg_v = dram["w_gate"].rearrange("(dc p) n -> p dc n", p=128)
    for dc in range(8):
        k = "stg%d" % (si % 2); st_ = stg[si % 2]; si += 1
        S.dma("sync", st_[:], wg_v[:, dc, :], writes=[k])
        V(lambda e, dc=dc, st_=st_: e.tensor_copy(out=Wg[:, dc, :], in_=st_[:]), r=[k], w=["Wg"])
    for (src, dst, key, ndc) in (("w_ab", Wab, "Wab", 4), ("w_rb", Wrb, "Wrb", 4), ("w_out", Wo, "Wo", 8)):
        v = dram[src].rearrange("(dc p) n -> p dc n", p=128)
        for dc in range(ndc):
            k = "stg%d" % (si % 2); st_ = stg[si % 2]; si += 1
            S.dma("sync", st_[:, 0:1024], v[:, dc, :], writes=[k])
            G(lambda e, dc=dc, st_=st_, dst=dst: e.tensor_copy(out=dst[:, dc, :], in_=st_[:, 0:1024]), r=[k], w=[key])
    gbc = sb([128, 1024]); bbc = sb([128, 1024])
    S.dma("sync", gbc[:], dram["ln1g"].partition_broadcast(128), writes=["gbc"])
    S.dma("sync", bbc[:], dram["ln1b"].partition_broadcast(128), writes=["bbc"])
    xs = sb([128, 8, 512]); xb = sb([128, 8, 512], BF16)
    ys = [sb([128, 4, 512]) for _ in range(2)]; yb = sb([128, 8, 512], BF16)
    ytmp = sb([128, 512])
    mq = sb([128, 4])
    S.dma("sync", mq[:], dram["maskq"], writes=["mq"])
    ident = sb([128, 128])
    S.dma("sync", ident[:], dram["c_ident"], writes=["ident"])
    hTs = sb([128, 8, 128])
    Q_t = [ps([128, 512]) for _ in range(2)]
    mixT = sb([128, 8, 512], BF16)
    sa = sb([128, 512]); sr = sb([128, 512]); m1 = sb([128, 512]); m2 = sb([128, 512])
    xt = sb([128, 1024]); ht = sb([128, 1024]); junk = sb([128, 1024]); st = sb([128, 4])
    Q_ga = ps([128, 512]); Q_gr = ps([128, 512]); Q_ba = ps([128, 512]); Q_br = ps([128, 512])
    Q_o = [ps([128, 512]) for _ in range(2)]
    xT = dram["xT2"].rearrange("(dc p) n -> p dc n", p=128)
    cout = dram["cout"]
    for g in range(n_groups):
        c0 = g * 512
        for dc in range(8):
            S.dma("sync", xs[:, dc, :], xT[:, dc, c0:c0 + 512], writes=["xs"])
        for dc in range(8):
            G(lambda e, dc=dc: e.tensor_copy(out=xb[:, dc, :], in_=xs[:, dc, :]), r=["xs"], w=["xb"])
        for dc in range(8):
            r0 = (dc % 4) * 256 + (128 if dc < 4 else 0)
            yi = dc % 2
            yk = "ys%d" % yi
            for q in range(4):
                kch = q * 4 + c0 // 1024
                S.dma("sync", ys[yi][:, q, :], cout[kch][r0:r0 + 128, c0 % 1024:c0 % 1024 + 512], writes=[yk])
            V(lambda e, yi=yi: e.tensor_scalar(out=ytmp[:], in0=ys[yi][:, 0, :], scalar1=mq[:, 0:1], scalar2=None, op0=ALU.mult), r=[yk, "mq"], w=["ytmp"])
            for q in (1, 2):
                V(lambda e, yi=yi, q=q: e.scalar_tensor_tensor(out=ytmp[:], in0=ys[yi][:, q, :], scalar=mq[:, q:q + 1], in1=ytmp[:], op0=ALU.mult, op1=ALU.add),
                  r=[yk, "mq", "ytmp"], w=["ytmp"])
            V(lambda e, yi=yi, dc=dc: e.scalar_tensor_tensor(out=yb[:, dc, :], in0=ys[yi][:, 3, :], scalar=mq[:, 3:4], in1=ytmp[:], op0=ALU.mult, op1=ALU.add),
              r=[yk, "mq", "ytmp"], w=["yb"])
        for j in range(8):
            js = slice(j * 128, (j + 1) * 128)
            for dc in range(8):
                T(lambda e, dc=dc, js=js: e.matmul(Q_ga[:], lhsT=Wg[:, dc, js], rhs=xb[:, dc, :], start=(dc == 0), stop=(dc == 7)), r=["Wg", "xb"], w=["Q_ga"])
            for dc in range(8):
                T(lambda e, dc=dc, j=j: e.matmul(Q_gr[:], lhsT=Wg[:, dc, 1024 + j * 128:1024 + (j + 1) * 128], rhs=xb[:, dc, :], start=(dc == 0), stop=(dc == 7)),
                  r=["Wg", "xb"], w=["Q_gr"])
            for dc in range(4):
                T(lambda e, dc=dc, js=js: e.matmul(Q_ba[:], lhsT=Wab[:, dc, js], rhs=yb[:, dc, :], start=(dc == 0), stop=(dc == 3)), r=["Wab", "yb"], w=["Q_ba"])
            for dc in range(4):
                T(lambda e, dc=dc, js=js: e.matmul(Q_br[:], lhsT=Wrb[:, dc, js], rhs=yb[:, 4 + dc, :], start=(dc == 0), stop=(dc == 3)), r=["Wrb", "yb"], w=["Q_br"])
            A(lambda e: e.activation(out=sa[:], in_=Q_ga[:], func=AF.Sigmoid), r=["Q_ga"], w=["sa"])
            A(lambda e: e.activation(out=sr[:], in_=Q_gr[:], func=AF.Sigmoid), r=["Q_gr"], w=["sr"])
            V(lambda e: e.tensor_tensor(out=m1[:], in0=sa[:], in1=Q_ba[:], op=ALU.mult), r=["sa", "Q_ba"], w=["m1"])
            V(lambda e: e.tensor_tensor(out=m2[:], in0=sr[:], in1=Q_br[:], op=ALU.mult), r=["sr", "Q_br"], w=["m2"])
            G(lambda e, j=j: e.tensor_tensor(out=mixT[:, j, :], in0=m1[:], in1=m2[:], op=ALU.add), r=["m1", "m2"], w=["mixT"])
        for tt in range(4):
            tok0 = c0 + tt * 128
            S.dma("sync", xt[:], dram["x2"][tok0:tok0 + 128, :], writes=["xt"])
            for hf in range(2):
                for j in range(8):
                    T(lambda e, j=j, hf=hf, tt=tt: e.matmul(Q_o[hf][:], lhsT=mixT[:, j, tt * 128:(tt + 1) * 128], rhs=Wo[:, j, hf * 512:(hf + 1) * 512],
                                                            start=(j == 0), stop=(j == 7)), r=["mixT", "Wo"], w=["Q_o%d" % hf])
                V(lambda e, hf=hf: e.scalar_tensor_tensor(out=ht[:, hf * 512:(hf + 1) * 512], in0=xt[:, hf * 512:(hf + 1) * 512], scalar=ALPHA, in1=Q_o[hf][:],
                                                          op0=ALU.mult, op1=ALU.add), r=["xt", "Q_o%d" % hf], w=["ht"])
            layer_norm_tile(V, A, G, ht, "ht", junk, st, gbc, bbc, "gbc", "bbc")
            S.dma("sync", dram["hS"][tok0:tok0 + 128, :], ht[:], reads=["ht"])
            for hf in range(2):
                for j4 in range(4):
                    j = hf * 4 + j4
                    T(lambda e, j=j, j4=j4, hf=hf: e.transpose(Q_t[hf][:, j4 * 128:(j4 + 1) * 128], ht[:, j * 128:(j + 1) * 128], ident[:]), r=["ht", "ident"], w=["Q_t%d" % hf])
                A(lambda e, hf=hf: e.copy(out=hTs[:, hf * 4:(hf + 1) * 4, :], in_=Q_t[hf][:].rearrange("p (a n) -> p a n", a=4)), r=["Q_t%d" % hf], w=["hTs"])
            S.dma("sync", dram["hTS"].rearrange("(dc p) n -> p dc n", p=128)[:, :, tok0:tok0 + 128], hTs[:], reads=["hTs"])


def build_moe(nc, S, es, dram, n_quarters=4, n_experts=32):
    sb, ps, V, A, G, T = _mk(nc, S, es, "mo")
    QT_ = 1024
    NSTG = MOE_NSTG
    ident = sb([128, 128])
    S.dma("sync", ident[:], dram["c_ident"], writes=["ident"])
    Wr = sb([128, 8, 32])
    S.dma("sync", Wr[:], dram["w_r"].rearrange("(dc p) n -> p dc n", p=128), writes=["Wr"])
    brb = sb([128, 32])
    S.dma("sync", brb[:], dram["b_r"].partition_broadcast(128), writes=["brb"])
    b1T = sb([128, 32 * 16])
    S.dma("sync", b1T[:], dram["b1T"], writes=["b1T"])
    b1Tp = sb([128, 32 * 16])
    V(lambda e: e.tensor_scalar(out=b1Tp[:], in0=b1T[:], scalar1=1.0, scalar2=None, op0=ALU.add), r=["b1T"], w=["b1Tp"])
    b2s = sb([32, 1024])
    S.dma("sync", b2s[:], dram["b2"], writes=["b2s"])
    gbc = sb([128, 1024]); bbc = sb([128, 1024])
    S.dma("sync", gbc[:], dram["ln2g"].partition_broadcast(128), writes=["gbc"])
    S.dma("sync", bbc[:], dram["ln2b"].partition_broadcast(128), writes=["bbc"])
    hb = sb([128, 8, QT_], BF16)
    acc = sb([128, 8, 1024])
    GW = sb([128, 8, 32])
    lg = sb([128, 32]); mx = sb([128, 8]); nm0 = sb([128, 1]); ex = sb([128, 32]); msk = sb([128, 32]); den = sb([128, 1]); gT = sb([32, 128])
    htk = sb([128, 1024]); junk = sb([128, 1024]); st = sb([128, 4])
    stg = [sb([128, 2048]) for _ in range(NSTG)]
    w1b = sb([128, 8, 2048], BF16); w2b = sb([128, 8, 1024], BF16)
    actT = sb([128, 8, QT_], BF16)
    gg = [sb([128, 512]) for _ in range(2)]; sg = [sb([128, 512]) for _ in range(2)]; ll = [sb([128, 512]) for _ in range(2)]
    Q_r = ps([128, 512]); Q_g = [ps([128, 512]) for _ in range(2)]; Q_l = [ps([128, 512]) for _ in range(2)]; Q_y = [ps([128, 512]) for _ in range(2)]
    hT = dram["hTS"].rearrange("(dc p) n -> p dc n", p=128)
    si = 0
    fci = 0
    for qt in range(n_quarters):
        t0 = qt * QT_
        for g2 in range(QT_ // 512):
            hsv = [stg[0][:].rearrange("p (a n) -> p a n", a=4), stg[1][:].rearrange("p (a n) -> p a n", a=4)]
            hs_dc = lambda dc: hsv[dc // 4][:, dc % 4, :]
            for dc in range(8):
                S.dma("sync", hs_dc(dc), hT[:, dc, t0 + g2 * 512:t0 + (g2 + 1) * 512], writes=["stg%d" % (dc // 4)])
            for t4 in range(4):
                tt = g2 * 4 + t4
                ts_ = slice(t4 * 128, (t4 + 1) * 128)
                for dc in range(8):
                    T(lambda e, dc=dc, ts_=ts_: e.matmul(Q_r[:, 0:32], lhsT=hs_dc(dc)[:, ts_], rhs=Wr[:, dc, :], start=(dc == 0), stop=(dc == 7)),
                      r=["stg0", "stg1", "Wr"], w=["Q_r.l"])
                V(lambda e: e.tensor_tensor(out=lg[:], in0=Q_r[:, 0:32], in1=brb[:], op=ALU.add), r=["Q_r.l", "brb"], w=["lg"])
                V(lambda e: e.max(out=mx[:], in_=lg[:]), r=["lg"], w=["mx"])
                V(lambda e: e.tensor_scalar(out=nm0[:], in0=mx[:, 0:1], scalar1=-1.0, scalar2=None, op0=ALU.mult), r=["mx"], w=["nm0"])
                A(lambda e: e.activation(out=ex[:], in_=lg[:], func=AF.Exp, bias=nm0[:, 0:1], scale=1.0), r=["lg", "nm0"], w=["ex"])
                V(lambda e: e.tensor_scalar(out=msk[:], in0=lg[:], scalar1=mx[:, 3:4], scalar2=None, op0=ALU.is_ge), r=["lg", "mx"], w=["msk"])
                V(lambda e: e.tensor_tensor(out=ex[:], in0=ex[:], in1=msk[:], op=ALU.mult), r=["ex", "msk"], w=["ex"])
                V(lambda e: e.tensor_reduce(out=den[:], in_=ex[:], axis=AX.X, op=ALU.add), r=["ex"], w=["den"])
                V(lambda e: e.reciprocal(out=den[:], in_=den[:]), r=["den"], w=["den"])
                V(lambda e, tt=tt: e.tensor_scalar(out=GW[:, tt, :], in0=ex[:], scalar1=den[:, 0:1], scalar2=None, op0=ALU.mult), r=["ex", "den"], w=["GW"])
                T(lambda e, tt=tt: e.transpose(Q_r[0:32, 128:256], GW[:, tt, :], ident[:]), r=["GW", "ident"], w=["Q_r.t"])
                V(lambda e: e.tensor_copy(out=gT[:], in_=Q_r[0:32, 128:256]), r=["Q_r.t"], w=["gT"])
                S.dma("sync", htk[:], dram["hS"][t0 + tt * 128:t0 + (tt + 1) * 128, :], writes=["htk"])
                for hf in range(2):
                    T(lambda e, hf=hf: e.matmul(Q_y[hf][:], lhsT=gT[:], rhs=b2s[:, hf * 512:(hf + 1) * 512], start=True, stop=True), r=["gT", "b2s"], w=["Q_y%d" % hf])
                    V(lambda e, hf=hf, tt=tt: e.scalar_tensor_tensor(out=acc[:, tt, hf * 512:(hf + 1) * 512], in0=htk[:, hf * 512:(hf + 1) * 512], scalar=ALPHA, in1=Q_y[hf][:],
                                                                     op0=ALU.mult, op1=ALU.add), r=["htk", "Q_y%d" % hf], w=["acc%d" % tt])
            for dc in range(8):
                if dc % 2 == 0:
                    V(lambda e, dc=dc, g2=g2: e.tensor_copy(out=hb[:, dc, g2 * 512:(g2 + 1) * 512], in_=hs_dc(dc)), r=["stg%d" % (dc // 4)], w=["hb"])
                else:
                    A(lambda e, dc=dc, g2=g2: e.copy(out=hb[:, dc, g2 * 512:(g2 + 1) * 512], in_=hs_dc(dc)), r=["stg%d" % (dc // 4)], w=["hb"])
        for ex_i in range(n_experts):
            w1v = dram["w1"][ex_i].rearrange("(dc p) n -> p dc n", p=128)
            w2v = dram["w2"][ex_i].rearrange("(dc p) n -> p dc n", p=128)
            for dc in range(8):
                k = "stg%d" % (si % NSTG); st_ = stg[si % NSTG]; si += 1
                S.dma("sync", st_[:], w1v[:, dc, :], writes=[k])
                A(lambda e, dc=dc, st_=st_: e.copy(out=w1b[:, dc, :], in_=st_[:]), r=[k], w=["w1b"])
            for d2 in range(4):
                k = "stg%d" % (si % NSTG); st_ = stg[si % NSTG]; si += 1
                S.dma("sync", st_[:].rearrange("p (a n) -> p a n", a=2), w2v[:, 2 * d2:2 * d2 + 2, :], writes=[k])
                if d2 % 2 == 0:
                    V(lambda e, d2=d2, st_=st_: e.tensor_copy(out=w2b[:, 2 * d2:2 * d2 + 2, :], in_=st_[:].rearrange("p (a n) -> p a n", a=2)), r=[k], w=["w2b"])
                else:
                    A(lambda e, d2=d2, st_=st_: e.copy(out=w2b[:, 2 * d2:2 * d2 + 2, :], in_=st_[:].rearrange("p (a n) -> p a n", a=2)), r=[k], w=["w2b"])
            for tg in range(QT_ // 512):
                gs = slice(tg * 512, (tg + 1) * 512)
                for fc in range(8):
                    bi = fci % 2
                    fci += 1
                    kg = "Q_g%d" % bi; kl = "Q_l%d" % bi
                    for dc in range(8):
                        T(lambda e, dc=dc, fc=fc, gs=gs, bi=bi: e.matmul(Q_g[bi][:], lhsT=w1b[:, dc, fc * 128:(fc + 1) * 128], rhs=hb[:, dc, gs], start=(dc == 0), stop=(dc == 7)),
                          r=["w1b", "hb"], w=[kg])
                    for dc in range(8):
                        T(lambda e, dc=dc, fc=fc, gs=gs, bi=bi: e.matmul(Q_l[bi][:], lhsT=w1b[:, dc, 1024 + fc * 128:1024 + (fc + 1) * 128], rhs=hb[:, dc, gs], start=(dc == 0), stop=(dc == 7)),
                          r=["w1b", "hb"], w=[kl])
                    bg = b1T[:, ex_i * 16 + fc:ex_i * 16 + fc + 1]
                    bl = b1Tp[:, ex_i * 16 + 8 + fc:ex_i * 16 + 8 + fc + 1]
                    V(lambda e, bg=bg, bi=bi: e.tensor_scalar(out=gg[bi][:], in0=Q_g[bi][:], scalar1=bg, scalar2=7.0, op0=ALU.add, op1=ALU.min), r=[kg, "b1T"], w=["gg%d" % bi])
                    A(lambda e, bi=bi: e.activation(out=sg[bi][:], in_=gg[bi][:], func=AF.Sigmoid, scale=1.702), r=["gg%d" % bi], w=["sg%d" % bi])
                    V(lambda e, bl=bl, bi=bi: e.tensor_scalar(out=ll[bi][:], in0=Q_l[bi][:], scalar1=bl, scalar2=8.0, op0=ALU.add, op1=ALU.min), r=[kl, "b1Tp"], w=["ll%d" % bi])
                    G(lambda e, bi=bi: e.tensor_tensor(out=gg[bi][:], in0=gg[bi][:], in1=sg[bi][:], op=ALU.mult), r=["gg%d" % bi, "sg%d" % bi], w=["gg%d" % bi])
                    V(lambda e, fc=fc, gs=gs, bi=bi: e.scalar_tensor_tensor(out=actT[:, fc, gs], in0=ll[bi][:], scalar=-6.0, in1=gg[bi][:], op0=ALU.max, op1=ALU.mult),
                      r=["gg%d" % bi, "ll%d" % bi], w=["actT%d" % tg])
            for tile_i in range(QT_ // 128):
                tg = tile_i // 4
                for hf in range(2):
                    for fc in range(8):
                        T(lambda e, fc=fc, tile_i=tile_i, hf=hf: e.matmul(Q_y[hf][:], lhsT=actT[:, fc, tile_i * 128:(tile_i + 1) * 128], rhs=w2b[:, fc, hf * 512:(hf + 1) * 512],
                                                                          start=(fc == 0), stop=(fc == 7)), r=["actT%d" % tg, "w2b"], w=["Q_y%d" % hf])
                    V(lambda e, hf=hf, tile_i=tile_i, ex_i=ex_i: e.scalar_tensor_tensor(
                        out=acc[:, tile_i, hf * 512:(hf + 1) * 512], in0=Q_y[hf][:], scalar=GW[:, tile_i, ex_i:ex_i + 1],
                        in1=acc[:, tile_i, hf * 512:(hf + 1) * 512], op0=ALU.mult, op1=ALU.add), r=["Q_y%d" % hf, "GW", "acc%d" % tile_i], w=["acc%d" % tile_i])
        for tt in range(8):
            V(lambda e, tt=tt: e.tensor_copy(out=htk[:], in_=acc[:, tt, :]), r=["acc%d" % tt], w=["htk"])
            layer_norm_tile(V, A, G, htk, "htk", junk, st, gbc, bbc, "gbc", "bbc")
            S.dma("sync", dram["out"][t0 + tt * 128:t0 + (tt + 1) * 128, :], htk[:], reads=["htk"])


def _dt(v):
    return F32 if v.dtype == np.float32 else BF16


_SZ = {}


def build_all(nc, S, dram, cc_sem):
    with contextlib.ExitStack() as e1:
        build_rwkv(nc, S, e1, dram, **_SZ.get('rwkv', {}))
        S.barrier()
        S.flush()
    with contextlib.ExitStack() as e2:
        build_attn(nc, S, e2, dram, **_SZ.get('attn', {}))
        S.barrier()
        S.flush()
    if not _SZ.get("nocc"):
        for k in range(16):
            S.async_op("gpsimd", lambda e, k=k: e.collective_compute("AllGather", ALU.bypass, replica_groups=[[0, 1, 2, 3], [4, 5, 6, 7]],
                                                                 ins=[dram["cin"][k]], outs=[dram["cout"][k]]), cc_sem)
    S.barrier()
    S.flush()
    with contextlib.ExitStack() as e3:
        build_stage_a(nc, S, e3, dram, **_SZ.get('sa', {}))
        S.barrier()
        S.flush()
    with contextlib.ExitStack() as e4:
        build_moe(nc, S, e4, dram, **_SZ.get('moe', {}))
        S.emit()


def kernel(x, w_in, mu_shift, w0, w_decay_up, a0, w_aaa_up, w_gate_up, k_k, k_a, r_k,
           lnx_g, lnx_b, w_attn_br, w_rwkv_br, w_out, ln1_g, ln1_b,
           w_router, b_router, w1, b1, w2, b2, ln2_g, ln2_b):
    f = lambda a: np.ascontiguousarray(np.asarray(a, dtype=np.float32))
    x = f(x); w_in = f(w_in)[0]; mu = f(mu_shift)[0]
    B, S_, D = x.shape
    NC = 8
    TQ = S_ // 4
    xTp = []
    for b in range(B):
        t = np.zeros((D, S_ + 1), np.float32)
        t[:, 1:] = x[b].T
        xTp.append(t)
    rc = rwkv_consts()
    ac = attn_consts()
    OQ = 1824
    row = lambda v: np.ascontiguousarray(v[None, :])
    w_gate = np.ascontiguousarray(w_in[:, OQ + 1536:OQ + 1536 + 2048])
    w1p = f(w1)[0][:_SZ.get("ne", 32)]
    w1p = np.ascontiguousarray(np.concatenate([w1p[:, :, 0::2], w1p[:, :, 1::2]], 2))
    b1p = f(b1)[0]
    b1p = np.concatenate([b1p[:, 0::2], b1p[:, 1::2]], 1)
    b1T = np.ascontiguousarray(b1p.reshape(32, 16, 128).transpose(2, 0, 1).reshape(128, 512))
    shared = {
        "w_lo": np.ascontiguousarray(w_in[:, 1536:1824]), "mu_lo": row(mu[1536:1824]),
        "w_gate": w_gate, "w_ab": f(w_attn_br)[0], "w_rb": f(w_rwkv_br)[0], "w_out": f(w_out)[0],
        "ln1g": row(f(ln1_g)[0]), "ln1b": row(f(ln1_b)[0]),
        "w_r": f(w_router)[0], "b_r": row(f(b_router)[0]), "w1": w1p, "b1T": b1T, "w2": f(w2)[0][:_SZ.get("ne", 32)], "b2": f(b2)[0],
        "ln2g": row(f(ln2_g)[0]), "ln2b": row(f(ln2_b)[0]),
    }
    shared.update(rc)
    shared.update(ac)
    in_maps = []
    for c in range(NC):
        b, hp = c // 4, c % 4
        tq = hp
        hc = slice(128 * hp, 128 * hp + 128)
        ts = slice(tq * TQ, (tq + 1) * TQ)
        mq = np.zeros((128, 4), np.float32); mq[:, tq] = 1.0
        m = {
            "xT": xTp[b],
            "w_rkv": np.ascontiguousarray(np.concatenate([w_in[:, 0:512][:, hc], w_in[:, 512:1024][:, hc], w_in[:, 1024:1536][:, hc]], 1)),
            "mu_rkv": row(np.concatenate([mu[0:512][hc], mu[512:1024][hc], mu[1024:1536][hc]])),
            "wdu": np.ascontiguousarray(f(w_decay_up)[0][:, hc]), "wau": np.ascontiguousarray(f(w_aaa_up)[0][:, hc]),
            "wgu": np.ascontiguousarray(f(w_gate_up)[0][:, hc]),
            "w0": row(f(w0)[0][hc]), "a0": row(f(a0)[0][hc]), "kk": row(f(k_k)[0][hc]), "ka": row(f(k_a)[0][hc]),
            "rk": row(f(r_k)[0].reshape(-1)[hc]), "lng": row(f(lnx_g)[0][hc]), "lnb": row(f(lnx_b)[0][hc]),
            "w_qkv": np.ascontiguousarray(np.concatenate([w_in[:, OQ:OQ + 512][:, hc], w_in[:, OQ + 512:OQ + 1024][:, hc],
                                                          w_in[:, OQ + 1024:OQ + 1536][:, hc]], 1)),
            "xT2": np.ascontiguousarray(x[b, ts].T), "x2": np.ascontiguousarray(x[b, ts]),
            "maskq": mq,
        }
        m.update(shared)
        in_maps.append(m)
    nc = bass.Bass("TRN2", target_bir_lowering=False)
    dram = {}
    for k, v in in_maps[0].items():
        dram[k] = nc.dram_tensor(k, list(v.shape), _dt(v), kind="ExternalInput").ap()
    dram["out"] = nc.dram_tensor("out", [TQ, D], F32, kind="ExternalOutput").ap()
    dram["cin"] = [nc.dram_tensor("cin%d" % k, [256, 1024], F32, kind="Internal").ap() for k in range(16)]
    dram["cout"] = [nc.dram_tensor("cout%d" % k, [1024, 1024], F32, kind="Internal").ap() for k in range(16)]
    dram["hS"] = nc.dram_tensor("hS", [TQ, D], F32, kind="Internal").ap()
    dram["hTS"] = nc.dram_tensor("hTS", [D, TQ], F32, kind="Internal").ap()
    with contextlib.ExitStack() as es:
        S = Sched(nc, es)
        cc_sem = es.enter_context(nc.semaphore("cc_sem"))
        build_all(nc, S, dram, cc_sem)
    res = run_bass_kernel_spmd(nc, in_maps, core_ids=list(range(NC)))
    out = np.zeros((B, S_, D), np.float32)
    for c in range(NC):
        b, tq = c // 4, c % 4
        out[b, tq * TQ:(tq + 1) * TQ] = res.results[c]["out"]
    return out
```

```python
import contextlib
import numpy as np
import ml_dtypes
import concourse.bass as bass
import concourse.mybir as mybir
from concourse.bass_utils import run_bass_kernel_spmd

F32 = mybir.dt.float32
BF16 = mybir.dt.bfloat16
AF = mybir.ActivationFunctionType
ALU = mybir.AluOpType
AX = mybir.AxisListType


ENGS = ("sync", "tensor", "vector", "scalar", "gpsimd")


class Op:
    __slots__ = ("eng", "fn", "deps", "marked", "dma", "sem", "val", "idx", "inc")

    def __init__(self, eng, fn, dma):
        self.eng = eng
        self.fn = fn
        self.deps = []
        self.marked = False
        self.dma = dma
        self.sem = None
        self.val = 0
        self.inc = 16


class Sched:
    def __init__(self, nc, es, n_dma_sems=12, same_engine_sync=True):
        self.nc = nc
        self.es = es
        self.q = {e: [] for e in ENGS}
        self.last_w = {}
        self.readers = {}
        self.same_engine_sync = same_engine_sync
        self.fence = []
        self.esem = {e: es.enter_context(nc.semaphore("s_" + e)) for e in ENGS if e != "sync"}
        self.dsem = {}
        self.dcnt = {}
        self.dlast = {}
        self.dnext = {}
        for e in ("sync", "scalar", "gpsimd"):
            self.dsem[e] = [es.enter_context(nc.semaphore("d_%s_%d" % (e, i))) for i in range(n_dma_sems)]
            self.dcnt[e] = [0] * n_dma_sems
            self.dlast[e] = [None] * n_dma_sems
            self.dnext[e] = 0

    def _add(self, op, reads, writes):
        deps = []
        for k in reads:
            w = self.last_w.get(k)
            if w is not None:
                deps.append(w)
        for k in writes:
            w = self.last_w.get(k)
            if w is not None:
                deps.append(w)
            deps.extend(self.readers.get(k, ()))
        for k in writes:
            self.last_w[k] = op
            self.readers[k] = []
        for k in reads:
            self.readers.setdefault(k, []).append(op)
        for d in deps:
            if d is op:
                continue
            if (not d.dma) and d.eng == op.eng and (not op.dma):
                if d.eng == "tensor" or not self.same_engine_sync:
                    continue
            op.deps.append(d)
        op.deps.extend(self.fence)
        self.q[op.eng].append(op)
        return op

    def barrier(self):
        f = []
        for e in ENGS:
            if self.q[e]:
                f.append(self.q[e][-1])
        for e in ("sync", "scalar", "gpsimd"):
            for o in self.dlast[e]:
                if o is not None:
                    f.append(o)
        f.extend(getattr(self, "async_ops", []))
        for o in f:
            o.marked = True
        self.fence = f
        self.last_w = {}
        self.readers = {}

    def async_op(self, eng, fn, sem, reads=(), writes=()):
        op = Op(eng, fn, True)
        if not hasattr(self, "async_ops"):
            self.async_ops = []
        op.sem = sem
        op.val = len(self.async_ops) + 1
        op.inc = 1
        self.async_ops.append(op)
        return self._add(op, reads, writes)

    def op(self, eng, fn, reads=(), writes=()):
        return self._add(Op(eng, fn, False), reads, writes)

    def dma(self, eng, out, in_, reads=(), writes=(), **kw):
        op = Op(eng, None, True)
        i = self.dnext[eng]
        self.dnext[eng] = (i + 1) % len(self.dsem[eng])
        self.dcnt[eng][i] += 1
        op.sem = self.dsem[eng][i]
        op.val = 16 * self.dcnt[eng][i]
        prev = self.dlast[eng][i]
        if prev is not None:
            op.deps.append(prev)
        self.dlast[eng][i] = op
        op.fn = lambda e, out=out, in_=in_, kw=kw: e.dma_start(out=out, in_=in_, **kw)
        return self._add(op, reads, writes)

    def flush(self):
        nc = self.nc
        if not hasattr(self, "_pos"):
            self._pos = {e: 0 for e in ENGS}
            self._cnt = {e: 0 for e in ENGS}
            self._waited = {e: {} for e in ENGS}
        pend = {e: self.q[e][self._pos[e]:] for e in ENGS}
        for e in ENGS:
            for op in pend[e]:
                for d in op.deps:
                    d.marked = True
        for e in ENGS:
            c = self._cnt[e]
            for op in pend[e]:
                if (not op.dma) and op.marked and op.sem is None:
                    c += 1
                    op.sem = self.esem.get(e)
                    op.val = c
            self._cnt[e] = c
            self._pos[e] = len(self.q[e])

        def run(eng, e):
            waited = self._waited[e]
            for op in pend[e]:
                need = {}
                for d in op.deps:
                    if d.sem is None:
                        continue
                    k = id(d.sem)
                    if waited.get(k, 0) >= d.val:
                        continue
                    if k not in need or need[k][1] < d.val:
                        need[k] = (d.sem, d.val)
                for k, (s, v) in need.items():
                    eng.wait_ge(s, v)
                    waited[k] = v
                if op.fn is None:
                    continue
                ins = op.fn(eng)
                if op.dma:
                    ins.then_inc(op.sem, op.inc)
                elif op.marked:
                    ins.then_inc(op.sem, 1)

        with nc.Block() as block:
            @block.sync
            def _(eng):
                run(eng, "sync")

            @block.tensor
            def _(eng):
                run(eng, "tensor")

            @block.vector
            def _(eng):
                run(eng, "vector")

            @block.scalar
            def _(eng):
                run(eng, "scalar")

            @block.gpsimd
            def _(eng):
                run(eng, "gpsimd")

    def emit(self, final_wait_engine="sync"):
        self.barrier()
        op = Op(final_wait_engine, None, False)
        op.deps = list(self.fence)
        self.q[final_wait_engine].append(op)
        self.flush()


S_TOK = 16384
XB = 512
C = 128
GN_EPS = 64e-5
C0 = -float(np.exp(-0.5))


def rwkv_consts():
    i = np.arange(128)
    m1 = (i[:, None] <= i[None, :]).astype(np.float32)
    m2 = (i[:, None] > i[None, :]).astype(np.float32)
    mus = (i[:, None] < i[None, :]).astype(np.float32)
    mls = mus.T.copy()
    ident = np.eye(128, dtype=np.float32)
    mask4 = np.stack([mus, m1, mus, m1], axis=1)
    return {"c_m1": m1, "c_m2": m2, "c_mus": mus, "c_mls": mls, "c_ident": ident,
            "c_mask4": np.ascontiguousarray(mask4)}


def build_rwkv(nc, S, es, dram, n_chunks=S_TOK // C):
    sbn = [0]

    def sb(shape, dt=F32, name=None):
        sbn[0] += 1
        return es.enter_context(nc.sbuf_tensor(name or ("rw%d" % sbn[0]), shape, dt))

    def ps(shape, dt=F32, name=None):
        sbn[0] += 1
        return es.enter_context(nc.psum_tensor(name or ("rp%d" % sbn[0]), shape, dt))

    BANKS = {"P_RKV": 0, "P_Z": 2, "P_T": 4, "P_L1a": 5, "P_L1b": 1, "P_LDa": 6, "P_LDb": 3,
             "P_G": 7, "P_U": 7, "P_H": 7, "P_Y": 7, "P_OT": 7}

    def _bk(r, w):
        b = set()
        for k in list(r) + list(w):
            if k.startswith("P_"):
                for pfx, bn in BANKS.items():
                    if k.startswith(pfx):
                        b.add(bn)
        return list(w) + ["BANK%d" % x for x in b]

    V = lambda fn, r=(), w=(): S.op("vector", fn, r, _bk(r, w))
    A = lambda fn, r=(), w=(): S.op("scalar", fn, r, _bk(r, w))
    G = lambda fn, r=(), w=(): S.op("gpsimd", fn, r, w)
    T = lambda fn, r=(), w=(): S.op("tensor", fn, r, _bk(r, w))

    cst = {}
    for nm in ("c_m1", "c_m2", "c_mus", "c_mls", "c_ident"):
        t = sb([128, 128]); cst[nm] = t
        S.dma("sync", t[:], dram[nm], writes=[nm])
    mask4 = sb([128, 4, 128])
    S.dma("sync", mask4[:], dram["c_mask4"], writes=["mask4"])
    ones_col = sb([128, 1])
    V(lambda e: e.memset(ones_col[:], 1.0), w=["ones_col"])
    bc = {}
    for nm in ("w0", "a0", "kk", "ka", "rk", "lng", "lnb"):
        t = sb([128, 128]); bc[nm] = t
        S.dma("sync", t[:], dram[nm].partition_broadcast(128), writes=["bc_" + nm])
    NW = 384 + 288
    wst = sb([128, 8, NW])
    S.dma("sync", wst[:, :, 0:384], dram["w_rkv"].rearrange("(dc p) n -> p dc n", p=128), writes=["wst"])
    S.dma("sync", wst[:, :, 384:NW], dram["w_lo"].rearrange("(dc p) n -> p dc n", p=128), writes=["wst"])
    mub = sb([128, NW])
    S.dma("sync", mub[:, 0:384], dram["mu_rkv"].partition_broadcast(128), writes=["mub"])
    S.dma("sync", mub[:, 384:NW], dram["mu_lo"].partition_broadcast(128), writes=["mub"])
    omu = sb([128, NW])
    V(lambda e: e.tensor_scalar(out=omu[:], in0=mub[:], scalar1=-1.0, scalar2=1.0, op0=ALU.mult, op1=ALU.add), r=["mub"], w=["omu"])
    Wa = sb([128, 8, NW], BF16); Wb = sb([128, 8, NW], BF16)
    for dc in range(8):
        V(lambda e, dc=dc: e.tensor_tensor(out=Wa[:, dc, :], in0=wst[:, dc, :], in1=omu[:], op=ALU.mult), r=["wst", "omu"], w=["Wa"])
        G(lambda e, dc=dc: e.tensor_tensor(out=Wb[:, dc, :], in0=wst[:, dc, :], in1=mub[:], op=ALU.mult), r=["wst", "mub"], w=["Wb"])
    wdu_f = sb([64, 128]); wau_f = sb([64, 128]); wgu0_f = sb([128, 128]); wgu1_f = sb([32, 128])
    S.dma("sync", wdu_f[:], dram["wdu"], writes=["wdu_f"])
    S.dma("sync", wau_f[:], dram["wau"], writes=["wau_f"])
    S.dma("sync", wgu0_f[:], dram["wgu"][0:128, :], writes=["wgu0_f"])
    S.dma("sync", wgu1_f[:], dram["wgu"][128:160, :], writes=["wgu1_f"])
    wdu = sb([64, 128], BF16); wau = sb([64, 128], BF16); wgu0 = sb([128, 128], BF16); wgu1 = sb([32, 128], BF16)
    V(lambda e: e.tensor_copy(out=wdu[:], in_=wdu_f[:]), r=["wdu_f"], w=["wdu"])
    V(lambda e: e.tensor_copy(out=wau[:], in_=wau_f[:]), r=["wau_f"], w=["wau"])
    V(lambda e: e.tensor_copy(out=wgu0[:], in_=wgu0_f[:]), r=["wgu0_f"], w=["wgu0"])
    V(lambda e: e.tensor_copy(out=wgu1[:], in_=wgu1_f[:]), r=["wgu1_f"], w=["wgu1"])

    xs = [sb([128, 8, XB + 1]) for _ in range(2)]
    xb = [sb([128, 8, XB + 1], BF16) for _ in range(2)]
    hwT = sb([64, XB], BF16); haT = sb([64, XB], BF16); hg0T = sb([128, XB], BF16); hg1T = sb([32, XB], BF16)
    Hst = sb([128, 64])
    V(lambda e: e.memset(Hst[:], 0.0), w=["Hst"])
    Bpad = sb([128, 2, 128]); Kpad = sb([128, 2, 128])
    V(lambda e: e.memset(Bpad[:], 0.0), w=["Bpad"])
    V(lambda e: e.memset(Kpad[:], 0.0), w=["Kpad"])

    P_RKV = ps([128, 512]); P_Z = ps([128, 4, 128])
    P_T = ps([128, 4, 128]); P_L1 = [ps([128, 4, 128]) for _ in range(2)]; P_LD = [ps([128, 4, 128]) for _ in range(2)]; P_M = ps([128, 512])
    P_LH = P_T[:].rearrange("p a n -> p (a n)")

    def t(shape=(128, 128), dt=F32):
        return sb(list(shape), dt)

    zw = t(); logw = t(); av = t(); gv = t(); kkr = t(); sq = t(); ss = t((128, 2)); rn = t((128, 2))
    kk = t(); tmp1 = t(); kmod = t(); kka = t(); rkr = t(); sbon = t((128, 2)); vsb = t()
    eW = t(); eWi = t(); eWp = t(); eR = t(); wc = t((128, 1))
    TM = sb([128, 4, 128])
    FT = sb([128, 4, 128])
    LM1 = [sb([128, 4, 128]) for _ in range(2)]
    PN = [[sb([128, 2, 128]) for _ in range(2)] for _ in range(2)]
    Lk = [[t() for _ in range(2)] for _ in range(2)]
    Gs = [t((128, 64)) for _ in range(2)]; Us = [t((128, 64)) for _ in range(2)]
    s1 = t((128, 2)); nmean = t((128, 2)); Yc = t(); ss2 = t((128, 2)); rstd = t((128, 2)); Yn = t(); Yo = t(); YoT = t()

    n_blocks = (n_chunks * C) // XB
    xT = dram["xT"].rearrange("(dc p) n -> p dc n", p=128)
    for blk in range(n_blocks):
        xi = blk % 2
        kx = "xs%d" % xi; kb = "xb%d" % xi
        c0 = blk * XB
        for dc in range(8):
            S.dma("sync", xs[xi][:, dc, :], xT[:, dc, c0:c0 + XB + 1], writes=[kx])
        for dc in range(8):
            eng = G if dc % 2 == 0 else A
            if dc % 2 == 0:
                G(lambda e, dc=dc, xi=xi: e.tensor_copy(out=xb[xi][:, dc, :], in_=xs[xi][:, dc, :]), r=[kx], w=[kb])
            else:
                A(lambda e, dc=dc, xi=xi: e.copy(out=xb[xi][:, dc, :], in_=xs[xi][:, dc, :]), r=[kx], w=[kb])
        for (lo, n, dst, fn, key) in ((384, 64, hwT, AF.Tanh, "hwT"), (448, 64, haT, AF.Copy, "haT"),
                                      (512, 128, hg0T, AF.Sigmoid, "hg0T"), (640, 32, hg1T, AF.Sigmoid, "hg1T")):
            for dc in range(8):
                T(lambda e, dc=dc, lo=lo, n=n, xi=xi: e.matmul(P_LH[0:n, :], lhsT=Wa[:, dc, lo:lo + n], rhs=xb[xi][:, dc, 1:XB + 1],
                                                              start=(dc == 0), stop=False), r=["Wa", kb], w=["P_T"])
            for dc in range(8):
                T(lambda e, dc=dc, lo=lo, n=n, xi=xi: e.matmul(P_LH[0:n, :], lhsT=Wb[:, dc, lo:lo + n], rhs=xb[xi][:, dc, 0:XB],
                                                              start=False, stop=(dc == 7)), r=["Wb", kb], w=["P_T"])
            A(lambda e, n=n, dst=dst, fn=fn: e.activation(out=dst[:], in_=P_LH[0:n, :], func=fn), r=["P_T"], w=[key])
        for ci in range(XB // C):
            ch = blk * (XB // C) + ci
            o = ci * C
            for dc in range(8):
                T(lambda e, dc=dc, xi=xi, o=o: e.matmul(P_RKV[:, 0:384], lhsT=xb[xi][:, dc, 1 + o:1 + o + C], rhs=Wa[:, dc, 0:384],
                                                        start=(dc == 0), stop=False), r=["Wa", kb], w=["P_RKV"])
            for dc in range(8):
                T(lambda e, dc=dc, xi=xi, o=o: e.matmul(P_RKV[:, 0:384], lhsT=xb[xi][:, dc, o:o + C], rhs=Wb[:, dc, 0:384],
                                                        start=False, stop=(dc == 7)), r=["Wb", kb], w=["P_RKV"])
            T(lambda e, o=o: e.matmul(P_Z[:, 0, :], lhsT=hwT[:, o:o + C], rhs=wdu[:], start=True, stop=True), r=["hwT", "wdu"], w=["P_Z0"])
            T(lambda e, o=o: e.matmul(P_Z[:, 1, :], lhsT=haT[:, o:o + C], rhs=wau[:], start=True, stop=True), r=["haT", "wau"], w=["P_Z1"])
            T(lambda e, o=o: e.matmul(P_RKV[:, 384:512], lhsT=hg0T[:, o:o + C], rhs=wgu0[:], start=True, stop=False), r=["hg0T", "wgu0"], w=["P_RKVg"])
            T(lambda e, o=o: e.matmul(P_RKV[:, 384:512], lhsT=hg1T[:, o:o + C], rhs=wgu1[:], start=False, stop=True), r=["hg1T", "wgu1"], w=["P_RKVg"])
            V(lambda e: e.tensor_tensor(out=zw[:], in0=P_Z[:, 0, :], in1=bc["w0"][:], op=ALU.add), r=["P_Z0", "bc_w0"], w=["zw"])
            A(lambda e: e.activation(out=zw[:], in_=zw[:], func=AF.Sigmoid), r=["zw"], w=["zw"])
            G(lambda e: e.tensor_scalar(out=logw[:], in0=zw[:], scalar1=C0, scalar2=None, op0=ALU.mult), r=["zw"], w=["logw"])
            V(lambda e: e.tensor_tensor(out=av[:], in0=P_Z[:, 1, :], in1=bc["a0"][:], op=ALU.add), r=["P_Z1", "bc_a0"], w=["av"])
            A(lambda e: e.activation(out=av[:], in_=av[:], func=AF.Sigmoid), r=["av"], w=["av"])
            A(lambda e: e.copy(out=gv[:], in_=P_RKV[:, 384:512]), r=["P_RKVg"], w=["gv"])
            A(lambda e: e.copy(out=vsb[:], in_=P_RKV[:, 256:384]), r=["P_RKV"], w=["vsb"])
            V(lambda e: e.tensor_tensor(out=kkr[:], in0=P_RKV[:, 128:256], in1=bc["kk"][:], op=ALU.mult), r=["P_RKV", "bc_kk"], w=["kkr"])
            G(lambda e: e.tensor_tensor(out=sq[:], in0=kkr[:], in1=kkr[:], op=ALU.mult), r=["kkr"], w=["sq"])
            V(lambda e: e.tensor_reduce(out=ss[:], in_=sq[:].rearrange("p (h c) -> p h c", h=2), axis=AX.X, op=ALU.add), r=["sq"], w=["ss"])
            A(lambda e: e.activation(out=ss[:], in_=ss[:], func=AF.Sqrt), r=["ss"], w=["ss"])
            V(lambda e: e.tensor_scalar(out=ss[:], in0=ss[:], scalar1=1e-12, scalar2=None, op0=ALU.max), r=["ss"], w=["ss"])
            V(lambda e: e.reciprocal(out=rn[:], in_=ss[:]), r=["ss"], w=["rn"])
            for h in range(2):
                V(lambda e, h=h: e.tensor_scalar(out=kk[:, h * 64:(h + 1) * 64], in0=kkr[:, h * 64:(h + 1) * 64], scalar1=rn[:, h:h + 1],
                                                 scalar2=None, op0=ALU.mult), r=["kkr", "rn"], w=["kk"])
            V(lambda e: e.scalar_tensor_tensor(out=tmp1[:], in0=av[:], scalar=-1.0, in1=bc["ka"][:], op0=ALU.add, op1=ALU.mult), r=["av", "bc_ka"], w=["tmp1"])
            V(lambda e: e.scalar_tensor_tensor(out=kmod[:], in0=tmp1[:], scalar=1.0, in1=P_RKV[:, 128:256], op0=ALU.add, op1=ALU.mult), r=["tmp1", "P_RKV"], w=["kmod"])
            G(lambda e: e.tensor_tensor(out=kka[:], in0=kk[:], in1=av[:], op=ALU.mult), r=["kk", "av"], w=["kka"])
            V(lambda e: e.tensor_tensor(out=rkr[:], in0=P_RKV[:, 0:128], in1=kmod[:], op=ALU.mult), r=["P_RKV", "kmod"], w=["rkr"])
            G(lambda e: e.tensor_tensor(out=rkr[:], in0=rkr[:], in1=bc["rk"][:], op=ALU.mult), r=["rkr", "bc_rk"], w=["rkr"])
            V(lambda e: e.tensor_reduce(out=sbon[:], in_=rkr[:].rearrange("p (h c) -> p h c", h=2), axis=AX.X, op=ALU.add), r=["rkr"], w=["sbon"])
            T(lambda e: e.matmul(P_Z[:, 2, :], lhsT=cst["c_m1"][:], rhs=logw[:], start=True, stop=True), r=["c_m1", "logw"], w=["P_Z2"])
            T(lambda e: e.matmul(P_Z[:, 3, :], lhsT=cst["c_m2"][:], rhs=logw[:], start=True, stop=True), r=["c_m2", "logw"], w=["P_Z3"])
            T(lambda e: e.matmul(P_M[:, 448:449], lhsT=logw[:], rhs=ones_col[:], start=True, stop=True), r=["ones_col", "logw"], w=["P_Hwc"])
            A(lambda e: e.activation(out=eW[:], in_=P_Z[:, 2, :], func=AF.Exp), r=["P_Z2"], w=["eW"])
            A(lambda e: e.activation(out=eWi[:], in_=P_Z[:, 2, :], func=AF.Exp, scale=-1.0), r=["P_Z2"], w=["eWi"])
            A(lambda e: e.activation(out=eWp[:], in_=logw[:], func=AF.Exp, scale=-1.0), r=["logw"], w=["eWp"])
            G(lambda e: e.tensor_tensor(out=eWp[:], in0=eWp[:], in1=eW[:], op=ALU.mult), r=["eWp", "eW"], w=["eWp"])
            A(lambda e: e.activation(out=eR[:], in_=P_Z[:, 3, :], func=AF.Exp), r=["P_Z3"], w=["eR"])
            A(lambda e: e.activation(out=wc[:], in_=P_M[:, 448:449], func=AF.Exp), r=["P_Hwc"], w=["wc"])
            V(lambda e: e.scalar_tensor_tensor(out=TM[:, 0, :], in0=kk[:], scalar=-1.0, in1=eWp[:], op0=ALU.mult, op1=ALU.mult), r=["kk", "eWp"], w=["TM0"])
            V(lambda e: e.tensor_tensor(out=TM[:, 1, :], in0=P_RKV[:, 0:128], in1=eW[:], op=ALU.mult), r=["P_RKV", "eW"], w=["TM1"])
            G(lambda e: e.tensor_tensor(out=TM[:, 2, :], in0=kka[:], in1=eWi[:], op=ALU.mult), r=["kka", "eWi"], w=["TM2"])
            V(lambda e: e.tensor_tensor(out=TM[:, 3, :], in0=kmod[:], in1=eWi[:], op=ALU.mult), r=["kmod", "eWi"], w=["TM3"])
            for h in range(2):
                hs = slice(h * 64, (h + 1) * 64)
                G(lambda e, h=h, hs=hs: e.tensor_tensor(out=Bpad[:, h, hs], in0=kka[:, hs], in1=eR[:, hs], op=ALU.mult), r=["kka", "eR"], w=["Bpad"])
                V(lambda e, h=h, hs=hs: e.tensor_tensor(out=Kpad[:, h, hs], in0=kmod[:, hs], in1=eR[:, hs], op=ALU.mult), r=["kmod", "eR"], w=["Kpad"])
            for j in range(4):
                T(lambda e, j=j: e.transpose(P_T[:, j, :], TM[:, j, :], cst["c_ident"][:]), r=["TM%d" % j, "c_ident"], w=["P_T"])
            A(lambda e: e.copy(out=FT[:], in_=P_T[:]), r=["P_T"], w=["FT"])
            HSL = [slice(0, 64), slice(64, 128)]
            BK = ["a", "b"]
            for h in range(2):
                hs = HSL[h]
                T(lambda e, hs=hs, h=h: e.matmul(P_L1[h][:, 0:2, :], lhsT=FT[hs, 2, :], rhs=FT[hs, 0:2, :], start=True, stop=True), r=["FT"], w=["P_L1%s" % BK[h]])
                T(lambda e, hs=hs, h=h: e.matmul(P_L1[h][:, 2:4, :], lhsT=FT[hs, 3, :], rhs=FT[hs, 0:2, :], start=True, stop=True), r=["FT"], w=["P_L1%s" % BK[h]])
                T(lambda e, hs=hs, h=h: e.matmul(P_LD[h][:, 0, :], lhsT=FT[hs, 0, :], rhs=FT[hs, 2, :], start=True, stop=True), r=["FT"], w=["P_LD%s0" % BK[h]])
            for h in range(2):
                V(lambda e, h=h: e.tensor_tensor(out=LM1[h][:], in0=P_L1[h][:], in1=mask4[:], op=ALU.mult), r=["P_L1%s" % BK[h], "mask4"], w=["LM1%d" % h])
                V(lambda e, h=h: e.tensor_tensor(out=Lk[h][0][:], in0=P_LD[h][:, 0, :], in1=cst["c_mls"][:], op=ALU.mult), r=["P_LD%s0" % BK[h], "c_mls"], w=["Lk%d.0" % h])
            for h in range(2):
                G(lambda e, h=h: e.tensor_tensor(out=PN[h][0][:, 0, :], in0=LM1[h][:, 0, :], in1=cst["c_ident"][:], op=ALU.add), r=["LM1%d" % h, "c_ident"], w=["PN%d.0.P" % h])
                T(lambda e, h=h: e.matmul(P_LD[h][:, 2, :], lhsT=Lk[h][0][:], rhs=LM1[h][:, 0, :], start=True, stop=True), r=["Lk%d.0" % h, "LM1%d" % h], w=["P_LD%s12" % BK[h]])
                T(lambda e, h=h: e.matmul(P_LD[h][:, 3, :], lhsT=LM1[h][:, 0, :], rhs=Lk[h][0][:], start=True, stop=True), r=["Lk%d.0" % h, "LM1%d" % h], w=["P_LD%s3" % BK[h]])
            for h in range(2):
                A(lambda e, h=h: e.copy(out=PN[h][0][:, 1, :], in_=P_LD[h][:, 2, :]), r=["P_LD%s12" % BK[h]], w=["PN%d.0.N" % h])
                A(lambda e, h=h: e.copy(out=Lk[h][1][:], in_=P_LD[h][:, 3, :]), r=["P_LD%s3" % BK[h]], w=["Lk%d.1" % h])
            cur = 0
            lcur = 1
            for lev in range(1, 7):
                nxt = 1 - cur
                lnx = 1 - lcur
                last = (lev == 6)
                for h in range(2):
                    pk = "PN%d.%d" % (h, cur); lk = "Lk%d.%d" % (h, lcur)
                    if not last:
                        T(lambda e, cur=cur, lcur=lcur, h=h: e.matmul(P_LD[h][:, 1:3, :], lhsT=Lk[h][lcur][:], rhs=PN[h][cur][:], start=True, stop=True),
                          r=[lk, pk + ".P", pk + ".N"], w=["P_LD%s12" % BK[h]])
                        T(lambda e, cur=cur, lcur=lcur, h=h: e.matmul(P_LD[h][:, 3, :], lhsT=PN[h][cur][:, 1, :], rhs=Lk[h][lcur][:], start=True, stop=True),
                          r=[lk, pk + ".N"], w=["P_LD%s3" % BK[h]])
                    else:
                        T(lambda e, cur=cur, lcur=lcur, h=h: e.matmul(P_LD[h][:, 1, :], lhsT=Lk[h][lcur][:], rhs=PN[h][cur][:, 0, :], start=True, stop=True),
                          r=[lk, pk + ".P"], w=["P_LD%s12" % BK[h]])
                for h in range(2):
                    pk = "PN%d.%d" % (h, cur); pn = "PN%d.%d" % (h, nxt); ln_ = "Lk%d.%d" % (h, lnx)
                    V(lambda e, cur=cur, nxt=nxt, h=h: e.tensor_tensor(out=PN[h][nxt][:, 0, :], in0=P_LD[h][:, 1, :], in1=PN[h][cur][:, 0, :], op=ALU.add),
                      r=["P_LD%s12" % BK[h], pk + ".P"], w=[pn + ".P"])
                    if not last:
                        A(lambda e, nxt=nxt, h=h: e.copy(out=PN[h][nxt][:, 1, :], in_=P_LD[h][:, 2, :]), r=["P_LD%s12" % BK[h]], w=[pn + ".N"])
                        A(lambda e, lnx=lnx, h=h: e.copy(out=Lk[h][lnx][:], in_=P_LD[h][:, 3, :]), r=["P_LD%s3" % BK[h]], w=[ln_])
                cur = nxt
                lcur = lnx
            for h in range(2):
                hs = HSL[h]
                T(lambda e, hs=hs, h=h: e.matmul(P_M[:, 64 * h:64 * h + 64], lhsT=FT[hs, 0, :], rhs=Hst[hs, :], start=True, stop=False), r=["FT", "Hst"], w=["P_G%d" % h])
                T(lambda e, hs=hs, h=h: e.matmul(P_M[:, 64 * h:64 * h + 64], lhsT=LM1[h][:, 2, :], rhs=vsb[:, hs], start=False, stop=True), r=["LM1%d" % h, "vsb"], w=["P_G%d" % h])
            A(lambda e: e.copy(out=Gs[0][:], in_=P_M[:, 0:64]), r=["P_G0"], w=["Gs0"])
            V(lambda e: e.tensor_copy(out=Gs[1][:], in_=P_M[:, 64:128]), r=["P_G1"], w=["Gs1"])
            for h in range(2):
                T(lambda e, h=h, cur=cur: e.matmul(P_M[:, 320 + 64 * h:384 + 64 * h], lhsT=PN[h][cur][:, 0, :], rhs=Gs[h][:], start=True, stop=True),
                  r=["PN%d.%d.P" % (h, cur), "Gs%d" % h], w=["P_U%d" % h])
            A(lambda e: e.copy(out=Us[0][:], in_=P_M[:, 320:384]), r=["P_U0"], w=["Us0"])
            V(lambda e: e.tensor_copy(out=Us[1][:], in_=P_M[:, 384:448]), r=["P_U1"], w=["Us1"])
            for h in range(2):
                hs = HSL[h]
                yk = "P_Y%d" % h
                uk = "Us%d" % h
                T(lambda e, hs=hs, h=h: e.matmul(P_M[:, 192 + 64 * h:256 + 64 * h], lhsT=FT[hs, 1, :], rhs=Hst[hs, :], start=True, stop=False), r=["FT", "Hst"], w=[yk])
                T(lambda e, h=h: e.matmul(P_M[:, 192 + 64 * h:256 + 64 * h], lhsT=LM1[h][:, 1, :], rhs=Us[h][:], start=False, stop=False), r=["LM1%d" % h, uk], w=[yk])
                T(lambda e, hs=hs, h=h: e.matmul(P_M[:, 192 + 64 * h:256 + 64 * h], lhsT=LM1[h][:, 3, :], rhs=vsb[:, hs], start=False, stop=True), r=["LM1%d" % h, "vsb"], w=[yk])
            T(lambda e: e.matmul(P_M[:, 128:192], lhsT=Bpad[:, 0, :], rhs=Us[0][:], start=True, stop=False), r=["Bpad", "Us0"], w=["P_H"])
            T(lambda e: e.matmul(P_M[:, 128:192], lhsT=Kpad[:, 0, :], rhs=vsb[:, 0:64], start=False, stop=False), r=["Kpad", "vsb"], w=["P_H"])
            T(lambda e: e.matmul(P_M[:, 128:192], lhsT=Bpad[:, 1, :], rhs=Us[1][:], start=False, stop=False), r=["Bpad", "Us1"], w=["P_H"])
            T(lambda e: e.matmul(P_M[:, 128:192], lhsT=Kpad[:, 1, :], rhs=vsb[:, 64:128], start=False, stop=True), r=["Kpad", "vsb"], w=["P_H"])
            V(lambda e: e.scalar_tensor_tensor(out=Hst[:], in0=Hst[:], scalar=wc[:, 0:1], in1=P_M[:, 128:192], op0=ALU.mult, op1=ALU.add),
              r=["Hst", "wc", "P_H"], w=["Hst"])
            Yv = P_M[:, 192:320]
            V(lambda e: e.tensor_reduce(out=s1[:], in_=Yv.rearrange("p (h c) -> p h c", h=2), axis=AX.X, op=ALU.add), r=["P_Y0", "P_Y1"], w=["s1"])
            V(lambda e: e.tensor_scalar(out=nmean[:], in0=s1[:], scalar1=-1.0 / 64, scalar2=None, op0=ALU.mult), r=["s1"], w=["nmean"])
            for h in range(2):
                hs = slice(h * 64, (h + 1) * 64)
                V(lambda e, h=h, hs=hs: e.tensor_scalar(out=Yc[:, hs], in0=P_M[:, 192 + 64 * h:256 + 64 * h], scalar1=nmean[:, h:h + 1], scalar2=None, op0=ALU.add),
                  r=["P_Y%d" % h, "nmean"], w=["Yc"])
            G(lambda e: e.tensor_tensor(out=sq[:], in0=Yc[:], in1=Yc[:], op=ALU.mult), r=["Yc"], w=["sq"])
            V(lambda e: e.tensor_reduce(out=ss2[:], in_=sq[:].rearrange("p (h c) -> p h c", h=2), axis=AX.X, op=ALU.add), r=["sq"], w=["ss2"])
            V(lambda e: e.tensor_scalar(out=ss2[:], in0=ss2[:], scalar1=1.0 / 64, scalar2=GN_EPS, op0=ALU.mult, op1=ALU.add), r=["ss2"], w=["ss2"])
            A(lambda e: e.activation(out=ss2[:], in_=ss2[:], func=AF.Sqrt), r=["ss2"], w=["ss2"])
            V(lambda e: e.reciprocal(out=rstd[:], in_=ss2[:]), r=["ss2"], w=["rstd"])
            for h in range(2):
                hs = slice(h * 64, (h + 1) * 64)
                V(lambda e, h=h, hs=hs: e.tensor_scalar(out=Yn[:, hs], in0=Yc[:, hs], scalar1=rstd[:, h:h + 1], scalar2=None, op0=ALU.mult), r=["Yc", "rstd"], w=["Yn"])
            G(lambda e: e.tensor_tensor(out=Yn[:], in0=Yn[:], in1=bc["lng"][:], op=ALU.mult), r=["Yn", "bc_lng"], w=["Yn"])
            G(lambda e: e.tensor_tensor(out=Yn[:], in0=Yn[:], in1=bc["lnb"][:], op=ALU.add), r=["Yn", "bc_lnb"], w=["Yn"])
            for h in range(2):
                hs = slice(h * 64, (h + 1) * 64)
                V(lambda e, h=h, hs=hs: e.scalar_tensor_tensor(out=Yo[:, hs], in0=vsb[:, hs], scalar=sbon[:, h:h + 1], in1=Yn[:, hs], op0=ALU.mult, op1=ALU.add),
                  r=["vsb", "sbon", "Yn"], w=["Yo"])
            G(lambda e: e.tensor_tensor(out=Yo[:], in0=Yo[:], in1=gv[:], op=ALU.mult), r=["Yo", "gv"], w=["Yo"])
            T(lambda e: e.transpose(P_T[:, 0, :], Yo[:], cst["c_ident"][:]), r=["Yo", "c_ident"], w=["P_T"])
            A(lambda e: e.copy(out=YoT[:], in_=P_T[:, 0, :]), r=["P_T"], w=["YoT"])
            S.dma("sync", dram["cin"][(ch * C) // 1024][0:128, (ch * C) % 1024:(ch * C) % 1024 + C], YoT[:], reads=["YoT"])


S_TOK = 16384
AXB = 256
KB_BOUND = 16.0
NEG = -30000.0


def attn_consts():
    s = S_TOK
    half = 32
    inv_freq = (10000.0 ** (-np.arange(half, dtype=np.float32) / half)).astype(np.float32)
    ang = (np.arange(s, dtype=np.float32)[:, None] * inv_freq[None, :]).astype(np.float32)
    cos = np.cos(ang).astype(np.float32)
    sin = np.sin(ang).astype(np.float32)
    ropeA = np.tile(cos, (1, 8)).astype(np.float32)
    ropeB = np.tile(np.concatenate([-sin, sin], 1), (1, 4)).astype(np.float32)
    kind = np.zeros((64, s), np.float32)
    for n in range(64):
        kind[n, n * 256:(n + 1) * 256] = 1.0
    i = np.arange(128)
    tri = (i[None, :] >= i[:, None]).astype(np.float32)
    return {"a_ropeA": ropeA, "a_ropeB": ropeB, "a_kind": kind.astype(ml_dtypes.bfloat16),
            "a_tri": tri.astype(ml_dtypes.bfloat16), "a_ident": np.eye(128, dtype=np.float32),
            "a_identb": np.eye(128, dtype=np.float32).astype(ml_dtypes.bfloat16)}


def build_attn(nc, S, es, dram, n_blocks=S_TOK // 256):
    sbn = [0]

    def sb(shape, dt=F32):
        sbn[0] += 1
        return es.enter_context(nc.sbuf_tensor("at%d" % sbn[0], shape, dt))

    def ps(shape, dt=F32):
        sbn[0] += 1
        return es.enter_context(nc.psum_tensor("ap%d" % sbn[0], shape, dt))

    def _bk(r, w):
        b = set(k.split(".")[0] for k in list(r) + list(w) if k.startswith("Q_"))
        return list(w) + ["BANK_" + x for x in b]

    defer = [None]

    def _rec(eng, fn, r, w):
        if defer[0] is None:
            S.op(eng, fn, r, w)
        else:
            defer[0].append((eng, fn, r, w))

    def DMA(out, in_, reads=(), writes=()):
        if defer[0] is None:
            S.dma("sync", out, in_, reads=reads, writes=writes)
        else:
            defer[0].append(("dma", (out, in_), reads, writes))

    def drain(lst, n):
        for _ in range(min(n, len(lst))):
            eng, fn, r, w = lst.pop(0)
            if eng == "dma":
                S.dma("sync", fn[0], fn[1], reads=r, writes=w)
            else:
                S.op(eng, fn, r, w)

    V = lambda fn, r=(), w=(): _rec("vector", fn, r, _bk(r, w))
    A = lambda fn, r=(), w=(): _rec("scalar", fn, r, _bk(r, w))
    G = lambda fn, r=(), w=(): _rec("gpsimd", fn, r, list(w))
    T = lambda fn, r=(), w=(): _rec("tensor", fn, r, _bk(r, w))

    ident = sb([128, 128]); identb = sb([128, 128], BF16); tri = sb([128, 128], BF16)
    S.dma("sync", ident[:], dram["a_ident"], writes=["ident"])
    S.dma("sync", identb[:], dram["a_identb"], writes=["identb"])
    S.dma("sync", tri[:], dram["a_tri"], writes=["tri"])
    wst = sb([128, 8, 384])
    S.dma("sync", wst[:], dram["w_qkv"].rearrange("(dc p) n -> p dc n", p=128), writes=["wst"])
    Wq = sb([128, 8, 384], BF16)
    for dc in range(8):
        V(lambda e, dc=dc: e.tensor_copy(out=Wq[:, dc, :], in_=wst[:, dc, :]), r=["wst"], w=["Wq"])
    inv256 = sb([128, 1]); ones_r = sb([128, 64])
    V(lambda e: e.memset(inv256[:], 1.0 / 256), w=["inv256"])
    V(lambda e: e.memset(ones_r[:], 1.0), w=["ones_r"])
    KT = [sb([128, S_TOK], BF16) for _ in range(2)]
    VA = [sb([128, 128, 65], BF16) for _ in range(2)]
    kmT = [sb([64, 64]) for _ in range(2)]
    Gt = [sb([128, 64]) for _ in range(2)]
    for h in range(2):
        S.dma("sync", KT[h][64:128, :], dram["a_kind"], writes=["KT%d.%d" % (h, b_) for b_ in range(64)])
        G(lambda e, h=h: e.memset(VA[h][:], 1.0), w=["VA%d.%d" % (h, b_) for b_ in range(64)])
        V(lambda e, h=h: e.memset(kmT[h][:], 0.0), w=["kmT%d" % h])
        V(lambda e, h=h: e.memset(Gt[h][:], -1e30), w=["Gt%d" % h])
    xs = [sb([128, 8, AXB + 1]) for _ in range(2)]
    xb = [sb([128, 8, AXB + 1], BF16) for _ in range(2)]
    rA = [sb([128, 256]) for _ in range(2)]; rB = [sb([128, 256]) for _ in range(2)]
    tmpA = sb([128, 256]); tmpB = sb([128, 256]); QK = sb([128, 4, 64])
    qT32 = sb([64, 128]); mx = sb([128, 8]); negm = sb([128, 64]); ssq = sb([128, 1]); cpos = sb([128, 1]); qjunk = sb([128, 64])
    Qaug = [sb([128, 128], BF16) for _ in range(2)]
    QT = [[sb([128, 256], BF16) for _ in range(2)] for _ in range(2)]
    PT = [sb([128, 256], BF16) for _ in range(4)]
    rec = sb([128, 256]); ysb = sb([64, 256]); yo = sb([64, 256])

    NSB = 4
    Q_A = ps([128, 512]); Q_C = ps([128, 512])
    Q_D = Q_C[:, 66:130].bitcast(BF16)
    Q_S = [ps([128, 512]) for _ in range(NSB)]; Q_O = [ps([128, 512]) for _ in range(2)]

    xT = dram["xT"].rearrange("(dc p) n -> p dc n", p=128)
    sidx = 0

    def gen_proj(blk):
        xi = blk % 2
        kx = "xs%d" % xi; kb = "xb%d" % xi
        c0 = blk * AXB
        for dc in range(8):
            DMA(xs[xi][:, dc, :], xT[:, dc, c0:c0 + AXB + 1], writes=[kx])
        for dc in range(8):
            if dc % 2 == 0:
                G(lambda e, dc=dc, xi=xi: e.tensor_copy(out=xb[xi][:, dc, :], in_=xs[xi][:, dc, :]), r=[kx], w=[kb])
            else:
                V(lambda e, dc=dc, xi=xi: e.tensor_copy(out=xb[xi][:, dc, :], in_=xs[xi][:, dc, :]), r=[kx], w=[kb])
        for half in range(2):
            tt = blk * 2 + half
            o = half * 128
            ri = tt % 2
            DMA(rA[ri][:], dram["a_ropeA"][tt * 128:(tt + 1) * 128, :], writes=["rA%d" % ri])
            DMA(rB[ri][:], dram["a_ropeB"][tt * 128:(tt + 1) * 128, :], writes=["rB%d" % ri])
            for dc in range(8):
                T(lambda e, dc=dc, xi=xi, o=o: e.matmul(Q_A[:, 0:384], lhsT=xb[xi][:, dc, 1 + o:1 + o + 128], rhs=Wq[:, dc, :],
                                                        start=(dc == 0), stop=(dc == 7)), r=["Wq", kb], w=["Q_A"])
            X4 = Q_A[:, 0:256].rearrange("p (g h d) -> p g h d", g=4, h=2)
            B4 = lambda t_: t_[:].rearrange("p (g h d) -> p g h d", g=4, h=2)
            V(lambda e, ri=ri: e.tensor_tensor(out=tmpA[:], in0=Q_A[:, 0:256], in1=rA[ri][:], op=ALU.mult), r=["Q_A", "rA%d" % ri], w=["tmpA"])
            V(lambda e, ri=ri: e.tensor_tensor(out=B4(tmpB)[:, :, 0, :], in0=X4[:, :, 1, :], in1=B4(rB[ri])[:, :, 0, :], op=ALU.mult), r=["Q_A", "rB%d" % ri], w=["tmpB"])
            V(lambda e, ri=ri: e.tensor_tensor(out=B4(tmpB)[:, :, 1, :], in0=X4[:, :, 0, :], in1=B4(rB[ri])[:, :, 1, :], op=ALU.mult), r=["Q_A", "rB%d" % ri], w=["tmpB"])
            G(lambda e: e.tensor_tensor(out=QK[:].rearrange("p g d -> p (g d)"), in0=tmpA[:], in1=tmpB[:], op=ALU.add), r=["tmpA", "tmpB"], w=["QK"])
            for h in range(2):
                A(lambda e, h=h, tt=tt: e.copy(out=VA[h][:, tt, 0:64], in_=Q_A[:, 256 + 64 * h:320 + 64 * h]), r=["Q_A"], w=["VA%d.%d" % (h, blk)])
                T(lambda e, h=h: e.transpose(Q_C[0:64, 130:258], QK[:, 2 + h, :], ident[:]), r=["QK", "ident"], w=["Q_C.k"])
                A(lambda e, h=h, tt=tt: e.copy(out=KT[h][0:64, tt * 128:(tt + 1) * 128], in_=Q_C[0:64, 130:258]), r=["Q_C.k"], w=["KT%d.%d" % (h, blk)])
                T(lambda e, h=h: e.transpose(Q_C[0:64, 258:386], QK[:, h, :], ident[:]), r=["QK", "ident"], w=["Q_C.q"])
                V(lambda e: e.tensor_copy(out=qT32[:], in_=Q_C[0:64, 258:386]), r=["Q_C.q"], w=["qT32"])
                if blk > 0:
                    T(lambda e, h=h: e.matmul(Q_C[:, 0:64], lhsT=qT32[:], rhs=kmT[h][:], start=True, stop=True), r=["qT32", "kmT%d" % h], w=["Q_C.g"])
                    V(lambda e, h=h, blk=blk: e.tensor_copy(out=Gt[h][:, 0:blk], in_=Q_C[:, 0:blk]), r=["Q_C.g"], w=["Gt%d" % h])
                V(lambda e, h=h: e.max(out=mx[:], in_=Gt[h][:]), r=["Gt%d" % h], w=["mx"])
                V(lambda e, h=h: e.tensor_scalar(out=negm[:], in0=Gt[h][:], scalar1=mx[:, 2:3], scalar2=NEG, op0=ALU.is_lt, op1=ALU.mult), r=["Gt%d" % h, "mx"], w=["negm"])
                V(lambda e, blk=blk: e.memset(negm[:, blk:blk + 1], 0.0), w=["negm"])
                V(lambda e: e.memset(ssq[:], 0.0), w=["ssq"])
                A(lambda e, h=h: e.activation(out=qjunk[:], in_=QK[:, h, :], func=AF.Square, accum_out=ssq[:]), r=["QK", "ssq"], w=["qjunk", "ssq"])
                A(lambda e: e.activation(out=cpos[:], in_=ssq[:], func=AF.Sqrt, scale=KB_BOUND * KB_BOUND), r=["ssq"], w=["cpos"])
                G(lambda e, h=h: e.tensor_copy(out=Qaug[h][:, 0:64], in_=QK[:, h, :]), r=["QK"], w=["Qaug%d" % h])
                V(lambda e, h=h: e.tensor_scalar(out=Qaug[h][:, 64:128], in0=negm[:], scalar1=cpos[:, 0:1], scalar2=None, op0=ALU.subtract), r=["negm", "cpos"], w=["Qaug%d" % h])
                T(lambda e, h=h: e.transpose(Q_D[:, 0:128], Qaug[h][:], identb[:]), r=["Qaug%d" % h, "identb"], w=["Q_C.d"])
                A(lambda e, h=h, o=o: e.copy(out=QT[h][blk % 2][:, o:o + 128], in_=Q_D[:, 0:128]), r=["Q_C.d"], w=["QT%d.%d" % (h, blk % 2)])
                T(lambda e, h=h: e.matmul(Q_C[0:64, 64:65], lhsT=QK[:, 2 + h, :], rhs=inv256[:], start=True, stop=True), r=["QK", "inv256"], w=["Q_C.m"])
                if half == 0:
                    V(lambda e, h=h, blk=blk: e.tensor_copy(out=kmT[h][:, blk:blk + 1], in_=Q_C[0:64, 64:65]), r=["Q_C.m"], w=["kmT%d" % h])
                else:
                    V(lambda e, h=h, blk=blk: e.tensor_tensor(out=kmT[h][:, blk:blk + 1], in0=Q_C[0:64, 64:65], in1=kmT[h][:, blk:blk + 1], op=ALU.add),
                      r=["Q_C.m", "kmT%d" % h], w=["kmT%d" % h])

    gen_proj(0)
    for blk in range(n_blocks):
        pend = []
        if blk + 1 < n_blocks:
            defer[0] = pend
            gen_proj(blk + 1)
            defer[0] = None
        steps = []
        for h in range(2):
            nkt = 2 * blk + 2
            for kt in range(nkt):
                steps.append((h, kt, nkt))

        def issue_qk(i):
            h, kt, nkt = steps[i]
            j = kt - 2 * blk
            q0 = 128 if j == 1 else 0
            si = (sbase + i) % NSB
            T(lambda e, h=h, kt=kt, q0=q0, si=si, bp=blk % 2: e.matmul(Q_S[si][:, q0:256], lhsT=KT[h][:, kt * 128:(kt + 1) * 128], rhs=QT[h][bp][:, q0:256],
                                                           start=True, stop=True), r=["KT%d.%d" % (h, kt // 2), "QT%d.%d" % (h, blk % 2)], w=["Q_S%d" % si])

        sbase = sidx
        LOOK = NSB - 2
        for i0 in range(min(LOOK, len(steps))):
            issue_qk(i0)
        for i in range(len(steps)):
            h, kt, nkt = steps[i]
            j = kt - 2 * blk
            q0 = 128 if j == 1 else 0
            si = (sbase + i) % NSB
            sk = "Q_S%d" % si
            pk = "PT%d" % si
            ok = "Q_O%d" % h
            if i + LOOK < len(steps):
                issue_qk(i + LOOK)
            A(lambda e, q0=q0, si=si: e.activation(out=PT[si][:, q0:256], in_=Q_S[si][:, q0:256], func=AF.Exp, scale=0.125), r=[sk], w=[pk])
            if j >= 0:
                G(lambda e, q0=q0, si=si, j=j: e.tensor_tensor(out=PT[si][:, j * 128:(j + 1) * 128], in0=PT[si][:, j * 128:(j + 1) * 128], in1=tri[:], op=ALU.mult),
                  r=[pk, "tri"], w=[pk])
            T(lambda e, h=h, kt=kt, q0=q0, si=si, nkt=nkt: e.matmul(Q_O[h][0:65, q0:256], lhsT=VA[h][:, kt, :], rhs=PT[si][:, q0:256],
                                                                    start=(kt == 0), stop=(kt == nkt - 1)), r=["VA%d.%d" % (h, kt // 2), pk], w=[ok])
            if kt == nkt - 1:
                V(lambda e, h=h: e.reciprocal(out=rec[64:65, :], in_=Q_O[h][64:65, 0:256]), r=[ok], w=["rec"])
                T(lambda e, h=h: e.matmul(Q_O[h][0:64, 256:512], lhsT=ones_r[64:65, :], rhs=rec[64:65, :], start=True, stop=True), r=["ones_r", "rec"], w=[ok + ".bc"])
                A(lambda e, h=h: e.copy(out=ysb[:], in_=Q_O[h][0:64, 0:256]), r=[ok], w=["ysb"])
                V(lambda e, h=h: e.tensor_tensor(out=yo[:], in0=ysb[:], in1=Q_O[h][0:64, 256:512], op=ALU.mult), r=["ysb", ok + ".bc"], w=["yo"])
                S.dma("sync", dram["cin"][(blk * 256) // 1024][128 + h * 64:128 + (h + 1) * 64, (blk * 256) % 1024:(blk * 256) % 1024 + 256], yo[:], reads=["yo"])
            if pend:
                drain(pend, -(-len(pend) // max(1, len(steps) - i)))
        drain(pend, len(pend))
        sidx += len(steps)


NTOK = 4096
ALPHA = float(2.0 ** 0.25)
LN_EPS = 1e-5
MOE_NSTG = 3


def _mk(nc, S, es, pfx):
    sbn = [0]

    def sb(shape, dt=F32):
        sbn[0] += 1
        return es.enter_context(nc.sbuf_tensor("%s%d" % (pfx, sbn[0]), shape, dt))

    def ps(shape, dt=F32):
        sbn[0] += 1
        return es.enter_context(nc.psum_tensor("%sp%d" % (pfx, sbn[0]), shape, dt))

    def _bk(r, w):
        b = set(k.split(".")[0] for k in list(r) + list(w) if k.startswith("Q_"))
        return list(w) + ["BANK_" + x for x in b]

    V = lambda fn, r=(), w=(): S.op("vector", fn, r, _bk(r, w))
    A = lambda fn, r=(), w=(): S.op("scalar", fn, r, _bk(r, w))
    G = lambda fn, r=(), w=(): S.op("gpsimd", fn, r, w)
    T = lambda fn, r=(), w=(): S.op("tensor", fn, r, _bk(r, w))
    return sb, ps, V, A, G, T


def layer_norm_tile(V, A, G, t, tk, junk, st, gbc, bbc, gk, bk):
    V(lambda e: e.tensor_reduce(out=st[:, 0:1], in_=t[:], axis=AX.X, op=ALU.add), r=[tk], w=["st0"])
    V(lambda e: e.tensor_scalar(out=st[:, 0:1], in0=st[:, 0:1], scalar1=-1.0 / 1024, scalar2=None, op0=ALU.mult), r=["st0"], w=["st0"])
    V(lambda e: e.tensor_scalar(out=t[:], in0=t[:], scalar1=st[:, 0:1], scalar2=None, op0=ALU.add), r=[tk, "st0"], w=[tk])
    V(lambda e: e.memset(st[:, 1:2], 0.0), w=["st1"])
    A(lambda e: e.activation(out=junk[:], in_=t[:], func=AF.Square, accum_out=st[:, 1:2]), r=[tk, "st1"], w=["junk", "st1"])
    V(lambda e: e.tensor_scalar(out=st[:, 1:2], in0=st[:, 1:2], scalar1=1.0 / 1024, scalar2=LN_EPS, op0=ALU.mult, op1=ALU.add), r=["st1"], w=["st1"])
    A(lambda e: e.activation(out=st[:, 1:2], in_=st[:, 1:2], func=AF.Sqrt), r=["st1"], w=["st1"])
    V(lambda e: e.reciprocal(out=st[:, 2:3], in_=st[:, 1:2]), r=["st1"], w=["st2"])
    V(lambda e: e.scalar_tensor_tensor(out=t[:], in0=t[:], scalar=st[:, 2:3], in1=gbc[:], op0=ALU.mult, op1=ALU.mult), r=[tk, "st2", gk], w=[tk])
    G(lambda e: e.tensor_tensor(out=t[:], in0=t[:], in1=bbc[:], op=ALU.add), r=[tk, bk], w=[tk])


def build_stage_a(nc, S, es, dram, n_groups=NTOK // 512):
    sb, ps, V, A, G, T = _mk(nc, S, es, "sa")
    stg = [sb([128, 2048]) for _ in range(2)]
    Wg = sb([128, 8, 2048], BF16); Wab = sb([128, 4, 1024], BF16); Wrb = sb([128, 4, 1024], BF16); Wo = sb([128, 8, 1024], BF16)
    si = 0
    wg_v = dram["w_gate"].rearrange("(dc p) n -> p dc n", p=128)
    for dc in range(8):
        k = "stg%d" % (si % 2); st_ = stg[si % 2]; si += 1
        S.dma("sync", st_[:], wg_v[:, dc, :], writes=[k])
        V(lambda e, dc=dc, st_=st_: e.tensor_copy(out=Wg[:, dc, :], in_=st_[:]), r=[k], w=["Wg"])
    for (src, dst, key, ndc) in (("w_ab", Wab, "Wab", 4), ("w_rb", Wrb, "Wrb", 4), ("w_out", Wo, "Wo", 8)):
        v = dram[src].rearrange("(dc p) n -> p dc n", p=128)
        for dc in range(ndc):
            k = "stg%d" % (si % 2); st_ = stg[si % 2]; si += 1
            S.dma("sync", st_[:, 0:1024], v[:, dc, :], writes=[k])
            G(lambda e, dc=dc, st_=st_, dst=dst: e.tensor_copy(out=dst[:, dc, :], in_=st_[:, 0:1024]), r=[k], w=[key])
    gbc = sb([128, 1024]); bbc = sb([128, 1024])
    S.dma("sync", gbc[:], dram["ln1g"].partition_broadcast(128), writes=["gbc"])
    S.dma("sync", bbc[:], dram["ln1b"].partition_broadcast(128), writes=["bbc"])
    xs = sb([128, 8, 512]); xb = sb([128, 8, 512], BF16)
    ys = [sb([128, 4, 512]) for _ in range(2)]; yb = sb([128, 8, 512], BF16)
    ytmp = sb([128, 512])
    mq = sb([128, 4])
    S.dma("sync", mq[:], dram["maskq"], writes=["mq"])
    ident = sb([128, 128])
    S.dma("sync", ident[:], dram["c_ident"], writes=["ident"])
    hTs = sb([128, 8, 128])
    Q_t = [ps([128, 512]) for _ in range(2)]
    mixT = sb([128, 8, 512], BF16)
    sa = sb([128, 512]); sr = sb([128, 512]); m1 = sb([128, 512]); m2 = sb([128, 512])
    xt = sb([128, 1024]); ht = sb([128, 1024]); junk = sb([128, 1024]); st = sb([128, 4])
    Q_ga = ps([128, 512]); Q_gr = ps([128, 512]); Q_ba = ps([128, 512]); Q_br = ps([128, 512])
    Q_o = [ps([128, 512]) for _ in range(2)]
    xT = dram["xT2"].rearrange("(dc p) n -> p dc n", p=128)
    cout = dram["cout"]
    for g in range(n_groups):
        c0 = g * 512
        for dc in range(8):
            S.dma("sync", xs[:, dc, :], xT[:, dc, c0:c0 + 512], writes=["xs"])
        for dc in range(8):
            G(lambda e, dc=dc: e.tensor_copy(out=xb[:, dc, :], in_=xs[:, dc, :]), r=["xs"], w=["xb"])
        for dc in range(8):
            r0 = (dc % 4) * 256 + (128 if dc < 4 else 0)
            yi = dc % 2
            yk = "ys%d" % yi
            for q in range(4):
                kch = q * 4 + c0 // 1024
                S.dma("sync", ys[yi][:, q, :], cout[kch][r0:r0 + 128, c0 % 1024:c0 % 1024 + 512], writes=[yk])
            V(lambda e, yi=yi: e.tensor_scalar(out=ytmp[:], in0=ys[yi][:, 0, :], scalar1=mq[:, 0:1], scalar2=None, op0=ALU.mult), r=[yk, "mq"], w=["ytmp"])
            for q in (1, 2):
                V(lambda e, yi=yi, q=q: e.scalar_tensor_tensor(out=ytmp[:], in0=ys[yi][:, q, :], scalar=mq[:, q:q + 1], in1=ytmp[:], op0=ALU.mult, op1=ALU.add),
                  r=[yk, "mq", "ytmp"], w=["ytmp"])
            V(lambda e, yi=yi, dc=dc: e.scalar_tensor_tensor(out=yb[:, dc, :], in0=ys[yi][:, 3, :], scalar=mq[:, 3:4], in1=ytmp[:], op0=ALU.mult, op1=ALU.add),
              r=[yk, "mq", "ytmp"], w=["yb"])
        for j in range(8):
            js = slice(j * 128, (j + 1) * 128)
            for dc in range(8):
                T(lambda e, dc=dc, js=js: e.matmul(Q_ga[:], lhsT=Wg[:, dc, js], rhs=xb[:, dc, :], start=(dc == 0), stop=(dc == 7)), r=["Wg", "xb"], w=["Q_ga"])
            for dc in range(8):
                T(lambda e, dc=dc, j=j: e.matmul(Q_gr[:], lhsT=Wg[:, dc, 1024 + j * 128:1024 + (j + 1) * 128], rhs=xb[:, dc, :], start=(dc == 0), stop=(dc == 7)),
                  r=["Wg", "xb"], w=["Q_gr"])
            for dc in range(4):
                T(lambda e, dc=dc, js=js: e.matmul(Q_ba[:], lhsT=Wab[:, dc, js], rhs=yb[:, dc, :], start=(dc == 0), stop=(dc == 3)), r=["Wab", "yb"], w=["Q_ba"])
            for dc in range(4):
                T(lambda e, dc=dc, js=js: e.matmul(Q_br[:], lhsT=Wrb[:, dc, js], rhs=yb[:, 4 + dc, :], start=(dc == 0), stop=(dc == 3)), r=["Wrb", "yb"], w=["Q_br"])
            A(lambda e: e.activation(out=sa[:], in_=Q_ga[:], func=AF.Sigmoid), r=["Q_ga"], w=["sa"])
            A(lambda e: e.activation(out=sr[:], in_=Q_gr[:], func=AF.Sigmoid), r=["Q_gr"], w=["sr"])
            V(lambda e: e.tensor_tensor(out=m1[:], in0=sa[:], in1=Q_ba[:], op=ALU.mult), r=["sa", "Q_ba"], w=["m1"])
            V(lambda e: e.tensor_tensor(out=m2[:], in0=sr[:], in1=Q_br[:], op=ALU.mult), r=["sr", "Q_br"], w=["m2"])
            G(lambda e, j=j: e.tensor_tensor(out=mixT[:, j, :], in0=m1[:], in1=m2[:], op=ALU.add), r=["m1", "m2"], w=["mixT"])
        for tt in range(4):
            tok0 = c0 + tt * 128
            S.dma("sync", xt[:], dram["x2"][tok0:tok0 + 128, :], writes=["xt"])
            for hf in range(2):
                for j in range(8):
                    T(lambda e, j=j, hf=hf, tt=tt: e.matmul(Q_o[hf][:], lhsT=mixT[:, j, tt * 128:(tt + 1) * 128], rhs=Wo[:, j, hf * 512:(hf + 1) * 512],
                                                            start=(j == 0), stop=(j == 7)), r=["mixT", "Wo"], w=["Q_o%d" % hf])
                V(lambda e, hf=hf: e.scalar_tensor_tensor(out=ht[:, hf * 512:(hf + 1) * 512], in0=xt[:, hf * 512:(hf + 1) * 512], scalar=ALPHA, in1=Q_o[hf][:],
                                                          op0=ALU.mult, op1=ALU.add), r=["xt", "Q_o%d" % hf], w=["ht"])
            layer_norm_tile(V, A, G, ht, "ht", junk, st, gbc, bbc, "gbc", "bbc")
            S.dma("sync", dram["hS"][tok0:tok0 + 128, :], ht[:], reads=["ht"])
            for hf in range(2):
                for j4 in range(4):
                    j = hf * 4 + j4
                    T(lambda e, j=j, j4=j4, hf=hf: e.transpose(Q_t[hf][:, j4 * 128:(j4 + 1) * 128], ht[:, j * 128:(j + 1) * 128], ident[:]), r=["ht", "ident"], w=["Q_t%d" % hf])
                A(lambda e, hf=hf: e.copy(out=hTs[:, hf * 4:(hf + 1) * 4, :], in_=Q_t[hf][:].rearrange("p (a n) -> p a n", a=4)), r=["Q_t%d" % hf], w=["hTs"])
            S.dma("sync", dram["hTS"].rearrange("(dc p) n -> p dc n", p=128)[:, :, tok0:tok0 + 128], hTs[:], reads=["hTs"])


def build_moe(nc, S, es, dram, n_quarters=4, n_experts=32):
    sb, ps, V, A, G, T = _mk(nc, S, es, "mo")
    QT_ = 1024
    NSTG = MOE_NSTG
    ident = sb([128, 128])
    S.dma("sync", ident[:], dram["c_ident"], writes=["ident"])
    Wr = sb([128, 8, 32])
    S.dma("sync", Wr[:], dram["w_r"].rearrange("(dc p) n -> p dc n", p=128), writes=["Wr"])
    brb = sb([128, 32])
    S.dma("sync", brb[:], dram["b_r"].partition_broadcast(128), writes=["brb"])
    b1T = sb([128, 32 * 16])
    S.dma("sync", b1T[:], dram["b1T"], writes=["b1T"])
    b1Tp = sb([128, 32 * 16])
    V(lambda e: e.tensor_scalar(out=b1Tp[:], in0=b1T[:], scalar1=1.0, scalar2=None, op0=ALU.add), r=["b1T"], w=["b1Tp"])
    b2s = sb([32, 1024])
    S.dma("sync", b2s[:], dram["b2"], writes=["b2s"])
    gbc = sb([128, 1024]); bbc = sb([128, 1024])
    S.dma("sync", gbc[:], dram["ln2g"].partition_broadcast(128), writes=["gbc"])
    S.dma("sync", bbc[:], dram["ln2b"].partition_broadcast(128), writes=["bbc"])
    hb = sb([128, 8, QT_], BF16)
    acc = sb([128, 8, 1024])
    GW = sb([128, 8, 32])
    lg = sb([128, 32]); mx = sb([128, 8]); nm0 = sb([128, 1]); ex = sb([128, 32]); msk = sb([128, 32]); den = sb([128, 1]); gT = sb([32, 128])
    htk = sb([128, 1024]); junk = sb([128, 1024]); st = sb([128, 4])
    stg = [sb([128, 2048]) for _ in range(NSTG)]
    w1b = sb([128, 8, 2048], BF16); w2b = sb([128, 8, 1024], BF16)
    actT = sb([128, 8, QT_], BF16)
    gg = [sb([128, 512]) for _ in range(2)]; sg = [sb([128, 512]) for _ in range(2)]; ll = [sb([128, 512]) for _ in range(2)]
    Q_r = ps([128, 512]); Q_g = [ps([128, 512]) for _ in range(2)]; Q_l = [ps([128, 512]) for _ in range(2)]; Q_y = [ps([128, 512]) for _ in range(2)]
    hT = dram["hTS"].rearrange("(dc p) n -> p dc n", p=128)
    si = 0
    fci = 0
    for qt in range(n_quarters):
        t0 = qt * QT_
        for g2 in range(QT_ // 512):
            hsv = [stg[0][:].rearrange("p (a n) -> p a n", a=4), stg[1][:].rearrange("p (a n) -> p a n", a=4)]
            hs_dc = lambda dc: hsv[dc // 4][:, dc % 4, :]
            for dc in range(8):
                S.dma("sync", hs_dc(dc), hT[:, dc, t0 + g2 * 512:t0 + (g2 + 1) * 512], writes=["stg%d" % (dc // 4)])
            for t4 in range(4):
                tt = g2 * 4 + t4
                ts_ = slice(t4 * 128, (t4 + 1) * 128)
                for dc in range(8):
                    T(lambda e, dc=dc, ts_=ts_: e.matmul(Q_r[:, 0:32], lhsT=hs_dc(dc)[:, ts_], rhs=Wr[:, dc, :], start=(dc == 0), stop=(dc == 7)),
                      r=["stg0", "stg1", "Wr"], w=["Q_r.l"])
                V(lambda e: e.tensor_tensor(out=lg[:], in0=Q_r[:, 0:32], in1=brb[:], op=ALU.add), r=["Q_r.l", "brb"], w=["lg"])
                V(lambda e: e.max(out=mx[:], in_=lg[:]), r=["lg"], w=["mx"])
                V(lambda e: e.tensor_scalar(out=nm0[:], in0=mx[:, 0:1], scalar1=-1.0, scalar2=None, op0=ALU.mult), r=["mx"], w=["nm0"])
                A(lambda e: e.activation(out=ex[:], in_=lg[:], func=AF.Exp, bias=nm0[:, 0:1], scale=1.0), r=["lg", "nm0"], w=["ex"])
                V(lambda e: e.tensor_scalar(out=msk[:], in0=lg[:], scalar1=mx[:, 3:4], scalar2=None, op0=ALU.is_ge), r=["lg", "mx"], w=["msk"])
                V(lambda e: e.tensor_tensor(out=ex[:], in0=ex[:], in1=msk[:], op=ALU.mult), r=["ex", "msk"], w=["ex"])
                V(lambda e: e.tensor_reduce(out=den[:], in_=ex[:], axis=AX.X, op=ALU.add), r=["ex"], w=["den"])
                V(lambda e: e.reciprocal(out=den[:], in_=den[:]), r=["den"], w=["den"])
                V(lambda e, tt=tt: e.tensor_scalar(out=GW[:, tt, :], in0=ex[:], scalar1=den[:, 0:1], scalar2=None, op0=ALU.mult), r=["ex", "den"], w=["GW"])
                T(lambda e, tt=tt: e.transpose(Q_r[0:32, 128:256], GW[:, tt, :], ident[:]), r=["GW", "ident"], w=["Q_r.t"])
                V(lambda e: e.tensor_copy(out=gT[:], in_=Q_r[0:32, 128:256]), r=["Q_r.t"], w=["gT"])
                S.dma("sync", htk[:], dram["hS"][t0 + tt * 128:t0 + (tt + 1) * 128, :], writes=["htk"])
                for hf in range(2):
                    T(lambda e, hf=hf: e.matmul(Q_y[hf][:], lhsT=gT[:], rhs=b2s[:, hf * 512:(hf + 1) * 512], start=True, stop=True), r=["gT", "b2s"], w=["Q_y%d" % hf])
                    V(lambda e, hf=hf, tt=tt: e.scalar_tensor_tensor(out=acc[:, tt, hf * 512:(hf + 1) * 512], in0=htk[:, hf * 512:(hf + 1) * 512], scalar=ALPHA, in1=Q_y[hf][:],
                                                                     op0=ALU.mult, op1=ALU.add), r=["htk", "Q_y%d" % hf], w=["acc%d" % tt])
            for dc in range(8):
                if dc % 2 == 0:
                    V(lambda e, dc=dc, g2=g2: e.tensor_copy(out=hb[:, dc, g2 * 512:(g2 + 1) * 512], in_=hs_dc(dc)), r=["stg%d" % (dc // 4)], w=["hb"])
                else:
                    A(lambda e, dc=dc, g2=g2: e.copy(out=hb[:, dc, g2 * 512:(g2 + 1) * 512], in_=hs_dc(dc)), r=["stg%d" % (dc // 4)], w=["hb"])
        for ex_i in range(n_experts):
            w1v = dram["w1"][ex_i].rearrange("(dc p) n -> p dc n", p=128)
            w2v = dram["w2"][ex_i].rearrange("(dc p) n -> p dc n", p=128)
            for dc in range(8):
                k = "stg%d" % (si % NSTG); st_ = stg[si % NSTG]; si += 1
                S.dma("sync", st_[:], w1v[:, dc, :], writes=[k])
                A(lambda e, dc=dc, st_=st_: e.copy(out=w1b[:, dc, :], in_=st_[:]), r=[k], w=["w1b"])
            for d2 in range(4):
                k = "stg%d" % (si % NSTG); st_ = stg[si % NSTG]; si += 1
                S.dma("sync", st_[:].rearrange("p (a n) -> p a n", a=2), w2v[:, 2 * d2:2 * d2 + 2, :], writes=[k])
                if d2 % 2 == 0:
                    V(lambda e, d2=d2, st_=st_: e.tensor_copy(out=w2b[:, 2 * d2:2 * d2 + 2, :], in_=st_[:].rearrange("p (a n) -> p a n", a=2)), r=[k], w=["w2b"])
                else:
                    A(lambda e, d2=d2, st_=st_: e.copy(out=w2b[:, 2 * d2:2 * d2 + 2, :], in_=st_[:].rearrange("p (a n) -> p a n", a=2)), r=[k], w=["w2b"])
            for tg in range(QT_ // 512):
                gs = slice(tg * 512, (tg + 1) * 512)
                for fc in range(8):
                    bi = fci % 2
                    fci += 1
                    kg = "Q_g%d" % bi; kl = "Q_l%d" % bi
                    for dc in range(8):
                        T(lambda e, dc=dc, fc=fc, gs=gs, bi=bi: e.matmul(Q_g[bi][:], lhsT=w1b[:, dc, fc * 128:(fc + 1) * 128], rhs=hb[:, dc, gs], start=(dc == 0), stop=(dc == 7)),
                          r=["w1b", "hb"], w=[kg])
                    for dc in range(8):
                        T(lambda e, dc=dc, fc=fc, gs=gs, bi=bi: e.matmul(Q_l[bi][:], lhsT=w1b[:, dc, 1024 + fc * 128:1024 + (fc + 1) * 128], rhs=hb[:, dc, gs], start=(dc == 0), stop=(dc == 7)),
                          r=["w1b", "hb"], w=[kl])
                    bg = b1T[:, ex_i * 16 + fc:ex_i * 16 + fc + 1]
                    bl = b1Tp[:, ex_i * 16 + 8 + fc:ex_i * 16 + 8 + fc + 1]
                    V(lambda e, bg=bg, bi=bi: e.tensor_scalar(out=gg[bi][:], in0=Q_g[bi][:], scalar1=bg, scalar2=7.0, op0=ALU.add, op1=ALU.min), r=[kg, "b1T"], w=["gg%d" % bi])
                    A(lambda e, bi=bi: e.activation(out=sg[bi][:], in_=gg[bi][:], func=AF.Sigmoid, scale=1.702), r=["gg%d" % bi], w=["sg%d" % bi])
                    V(lambda e, bl=bl, bi=bi: e.tensor_scalar(out=ll[bi][:], in0=Q_l[bi][:], scalar1=bl, scalar2=8.0, op0=ALU.add, op1=ALU.min), r=[kl, "b1Tp"], w=["ll%d" % bi])
                    G(lambda e, bi=bi: e.tensor_tensor(out=gg[bi][:], in0=gg[bi][:], in1=sg[bi][:], op=ALU.mult), r=["gg%d" % bi, "sg%d" % bi], w=["gg%d" % bi])
                    V(lambda e, fc=fc, gs=gs, bi=bi: e.scalar_tensor_tensor(out=actT[:, fc, gs], in0=ll[bi][:], scalar=-6.0, in1=gg[bi][:], op0=ALU.max, op1=ALU.mult),
                      r=["gg%d" % bi, "ll%d" % bi], w=["actT%d" % tg])
            for tile_i in range(QT_ // 128):
                tg = tile_i // 4
                for hf in range(2):
                    for fc in range(8):
                        T(lambda e, fc=fc, tile_i=tile_i, hf=hf: e.matmul(Q_y[hf][:], lhsT=actT[:, fc, tile_i * 128:(tile_i + 1) * 128], rhs=w2b[:, fc, hf * 512:(hf + 1) * 512],
                                                                          start=(fc == 0), stop=(fc == 7)), r=["actT%d" % tg, "w2b"], w=["Q_y%d" % hf])
                    V(lambda e, hf=hf, tile_i=tile_i, ex_i=ex_i: e.scalar_tensor_tensor(
                        out=acc[:, tile_i, hf * 512:(hf + 1) * 512], in0=Q_y[hf][:], scalar=GW[:, tile_i, ex_i:ex_i + 1],
                        in1=acc[:, tile_i, hf * 512:(hf + 1) * 512], op0=ALU.mult, op1=ALU.add), r=["Q_y%d" % hf, "GW", "acc%d" % tile_i], w=["acc%d" % tile_i])
        for tt in range(8):
            V(lambda e, tt=tt: e.tensor_copy(out=htk[:], in_=acc[:, tt, :]), r=["acc%d" % tt], w=["htk"])
            layer_norm_tile(V, A, G, htk, "htk", junk, st, gbc, bbc, "gbc", "bbc")
            S.dma("sync", dram["out"][t0 + tt * 128:t0 + (tt + 1) * 128, :], htk[:], reads=["htk"])


def _dt(v):
    return F32 if v.dtype == np.float32 else BF16


_SZ = {}


def build_all(nc, S, dram, cc_sem):
    with contextlib.ExitStack() as e1:
        build_rwkv(nc, S, e1, dram, **_SZ.get('rwkv', {}))
        S.barrier()
        S.flush()
    with contextlib.ExitStack() as e2:
        build_attn(nc, S, e2, dram, **_SZ.get('attn', {}))
        S.barrier()
        S.flush()
    if not _SZ.get("nocc"):
        for k in range(16):
            S.async_op("gpsimd", lambda e, k=k: e.collective_compute("AllGather", ALU.bypass, replica_groups=[[0, 1, 2, 3], [4, 5, 6, 7]],
                                                                 ins=[dram["cin"][k]], outs=[dram["cout"][k]]), cc_sem)
    S.barrier()
    S.flush()
    with contextlib.ExitStack() as e3:
        build_stage_a(nc, S, e3, dram, **_SZ.get('sa', {}))
        S.barrier()
        S.flush()
    with contextlib.ExitStack() as e4:
        build_moe(nc, S, e4, dram, **_SZ.get('moe', {}))
        S.emit()


def kernel(x, w_in, mu_shift, w0, w_decay_up, a0, w_aaa_up, w_gate_up, k_k, k_a, r_k,
           lnx_g, lnx_b, w_attn_br, w_rwkv_br, w_out, ln1_g, ln1_b,
           w_router, b_router, w1, b1, w2, b2, ln2_g, ln2_b):
    f = lambda a: np.ascontiguousarray(np.asarray(a, dtype=np.float32))
    x = f(x); w_in = f(w_in)[0]; mu = f(mu_shift)[0]
    B, S_, D = x.shape
    NC = 8
    TQ = S_ // 4
    xTp = []
    for b in range(B):
        t = np.zeros((D, S_ + 1), np.float32)
        t[:, 1:] = x[b].T
        xTp.append(t)
    rc = rwkv_consts()
    ac = attn_consts()
    OQ = 1824
    row = lambda v: np.ascontiguousarray(v[None, :])
    w_gate = np.ascontiguousarray(w_in[:, OQ + 1536:OQ + 1536 + 2048])
    w1p = f(w1)[0][:_SZ.get("ne", 32)]
    w1p = np.ascontiguousarray(np.concatenate([w1p[:, :, 0::2], w1p[:, :, 1::2]], 2))
    b1p = f(b1)[0]
    b1p = np.concatenate([b1p[:, 0::2], b1p[:, 1::2]], 1)
    b1T = np.ascontiguousarray(b1p.reshape(32, 16, 128).transpose(2, 0, 1).reshape(128, 512))
    shared = {
        "w_lo": np.ascontiguousarray(w_in[:, 1536:1824]), "mu_lo": row(mu[1536:1824]),
        "w_gate": w_gate, "w_ab": f(w_attn_br)[0], "w_rb": f(w_rwkv_br)[0], "w_out": f(w_out)[0],
        "ln1g": row(f(ln1_g)[0]), "ln1b": row(f(ln1_b)[0]),
        "w_r": f(w_router)[0], "b_r": row(f(b_router)[0]), "w1": w1p, "b1T": b1T, "w2": f(w2)[0][:_SZ.get("ne", 32)], "b2": f(b2)[0],
        "ln2g": row(f(ln2_g)[0]), "ln2b": row(f(ln2_b)[0]),
    }
    shared.update(rc)
    shared.update(ac)
    in_maps = []
    for c in range(NC):
        b, hp = c // 4, c % 4
        tq = hp
        hc = slice(128 * hp, 128 * hp + 128)
        ts = slice(tq * TQ, (tq + 1) * TQ)
        mq = np.zeros((128, 4), np.float32); mq[:, tq] = 1.0
        m = {
            "xT": xTp[b],
            "w_rkv": np.ascontiguousarray(np.concatenate([w_in[:, 0:512][:, hc], w_in[:, 512:1024][:, hc], w_in[:, 1024:1536][:, hc]], 1)),
            "mu_rkv": row(np.concatenate([mu[0:512][hc], mu[512:1024][hc], mu[1024:1536][hc]])),
            "wdu": np.ascontiguousarray(f(w_decay_up)[0][:, hc]), "wau": np.ascontiguousarray(f(w_aaa_up)[0][:, hc]),
            "wgu": np.ascontiguousarray(f(w_gate_up)[0][:, hc]),
            "w0": row(f(w0)[0][hc]), "a0": row(f(a0)[0][hc]), "kk": row(f(k_k)[0][hc]), "ka": row(f(k_a)[0][hc]),
            "rk": row(f(r_k)[0].reshape(-1)[hc]), "lng": row(f(lnx_g)[0][hc]), "lnb": row(f(lnx_b)[0][hc]),
            "w_qkv": np.ascontiguousarray(np.concatenate([w_in[:, OQ:OQ + 512][:, hc], w_in[:, OQ + 512:OQ + 1024][:, hc],
                                                          w_in[:, OQ + 1024:OQ + 1536][:, hc]], 1)),
            "xT2": np.ascontiguousarray(x[b, ts].T), "x2": np.ascontiguousarray(x[b, ts]),
            "maskq": mq,
        }
        m.update(shared)
        in_maps.append(m)
    nc = bass.Bass("TRN2", target_bir_lowering=False)
    dram = {}
    for k, v in in_maps[0].items():
        dram[k] = nc.dram_tensor(k, list(v.shape), _dt(v), kind="ExternalInput").ap()
    dram["out"] = nc.dram_tensor("out", [TQ, D], F32, kind="ExternalOutput").ap()
    dram["cin"] = [nc.dram_tensor("cin%d" % k, [256, 1024], F32, kind="Internal").ap() for k in range(16)]
    dram["cout"] = [nc.dram_tensor("cout%d" % k, [1024, 1024], F32, kind="Internal").ap() for k in range(16)]
    dram["hS"] = nc.dram_tensor("hS", [TQ, D], F32, kind="Internal").ap()
    dram["hTS"] = nc.dram_tensor("hTS", [D, TQ], F32, kind="Internal").ap()
    with contextlib.ExitStack() as es:
        S = Sched(nc, es)
        cc_sem = es.enter_context(nc.semaphore("cc_sem"))
        build_all(nc, S, dram, cc_sem)
    res = run_bass_kernel_spmd(nc, in_maps, core_ids=list(range(NC)))
    out = np.zeros((B, S_, D), np.float32)
    for c in range(NC):
        b, tq = c // 4, c % 4
        out[b, tq * TQ:(tq + 1) * TQ] = res.results[c]["out"]
    return out
```

```python
import contextlib
import numpy as np
import ml_dtypes
import concourse.bass as bass
import concourse.mybir as mybir
from concourse.bass_utils import run_bass_kernel_spmd

F32 = mybir.dt.float32
BF16 = mybir.dt.bfloat16
AF = mybir.ActivationFunctionType
ALU = mybir.AluOpType
AX = mybir.AxisListType


ENGS = ("sync", "tensor", "vector", "scalar", "gpsimd")


class Op:
    __slots__ = ("eng", "fn", "deps", "marked", "dma", "sem", "val", "idx", "inc")

    def __init__(self, eng, fn, dma):
        self.eng = eng
        self.fn = fn
        self.deps = []
        self.marked = False
        self.dma = dma
        self.sem = None
        self.val = 0
        self.inc = 16


class Sched:
    def __init__(self, nc, es, n_dma_sems=12, same_engine_sync=True):
        self.nc = nc
        self.es = es
        self.q = {e: [] for e in ENGS}
        self.last_w = {}
        self.readers = {}
        self.same_engine_sync = same_engine_sync
        self.fence = []
        self.esem = {e: es.enter_context(nc.semaphore("s_" + e)) for e in ENGS if e != "sync"}
        self.dsem = {}
        self.dcnt = {}
        self.dlast = {}
        self.dnext = {}
        for e in ("sync", "scalar", "gpsimd"):
            self.dsem[e] = [es.enter_context(nc.semaphore("d_%s_%d" % (e, i))) for i in range(n_dma_sems)]
            self.dcnt[e] = [0] * n_dma_sems
            self.dlast[e] = [None] * n_dma_sems
            self.dnext[e] = 0

    def _add(self, op, reads, writes):
        deps = []
        for k in reads:
            w = self.last_w.get(k)
            if w is not None:
                deps.append(w)
        for k in writes:
            w = self.last_w.get(k)
            if w is not None:
                deps.append(w)
            deps.extend(self.readers.get(k, ()))
        for k in writes:
            self.last_w[k] = op
            self.readers[k] = []
        for k in reads:
            self.readers.setdefault(k, []).append(op)
        for d in deps:
            if d is op:
                continue
            if (not d.dma) and d.eng == op.eng and (not op.dma):
                if d.eng == "tensor" or not self.same_engine_sync:
                    continue
            op.deps.append(d)
        op.deps.extend(self.fence)
        self.q[op.eng].append(op)
        return op

    def barrier(self):
        f = []
        for e in ENGS:
            if self.q[e]:
                f.append(self.q[e][-1])
        for e in ("sync", "scalar", "gpsimd"):
            for o in self.dlast[e]:
                if o is not None:
                    f.append(o)
        f.extend(getattr(self, "async_ops", []))
        for o in f:
            o.marked = True
        self.fence = f
        self.last_w = {}
        self.readers = {}

    def async_op(self, eng, fn, sem, reads=(), writes=()):
        op = Op(eng, fn, True)
        if not hasattr(self, "async_ops"):
            self.async_ops = []
        op.sem = sem
        op.val = len(self.async_ops) + 1
        op.inc = 1
        self.async_ops.append(op)
        return self._add(op, reads, writes)

    def op(self, eng, fn, reads=(), writes=()):
        return self._add(Op(eng, fn, False), reads, writes)

    def dma(self, eng, out, in_, reads=(), writes=(), **kw):
        op = Op(eng, None, True)
        i = self.dnext[eng]
        self.dnext[eng] = (i + 1) % len(self.dsem[eng])
        self.dcnt[eng][i] += 1
        op.sem = self.dsem[eng][i]
        op.val = 16 * self.dcnt[eng][i]
        prev = self.dlast[eng][i]
        if prev is not None:
            op.deps.append(prev)
        self.dlast[eng][i] = op
        op.fn = lambda e, out=out, in_=in_, kw=kw: e.dma_start(out=out, in_=in_, **kw)
        return self._add(op, reads, writes)

    def flush(self):
        nc = self.nc
        if not hasattr(self, "_pos"):
            self._pos = {e: 0 for e in ENGS}
            self._cnt = {e: 0 for e in ENGS}
            self._waited = {e: {} for e in ENGS}
        pend = {e: self.q[e][self._pos[e]:] for e in ENGS}
        for e in ENGS:
            for op in pend[e]:
                for d in op.deps:
                    d.marked = True
        for e in ENGS:
            c = self._cnt[e]
            for op in pend[e]:
                if (not op.dma) and op.marked and op.sem is None:
                    c += 1
                    op.sem = self.esem.get(e)
                    op.val = c
            self._cnt[e] = c
            self._pos[e] = len(self.q[e])

        def run(eng, e):
            waited = self._waited[e]
            for op in pend[e]:
                need = {}
                for d in op.deps:
                    if d.sem is None:
                        continue
                    k = id(d.sem)
                    if waited.get(k, 0) >= d.val:
                        continue
                    if k not in need or need[k][1] < d.val:
                        need[k] = (d.sem, d.val)
                for k, (s, v) in need.items():
                    eng.wait_ge(s, v)
                    waited[k] = v
                if op.fn is None:
                    continue
                ins = op.fn(eng)
                if op.dma:
                    ins.then_inc(op.sem, op.inc)
                elif op.marked:
                    ins.then_inc(op.sem, 1)

        with nc.Block() as block:
            @block.sync
            def _(eng):
                run(eng, "sync")

            @block.tensor
            def _(eng):
                run(eng, "tensor")

            @block.vector
            def _(eng):
                run(eng, "vector")

            @block.scalar
            def _(eng):
                run(eng, "scalar")

            @block.gpsimd
            def _(eng):
                run(eng, "gpsimd")

    def emit(self, final_wait_engine="sync"):
        self.barrier()
        op = Op(final_wait_engine, None, False)
        op.deps = list(self.fence)
        self.q[final_wait_engine].append(op)
        self.flush()


S_TOK = 16384
XB = 512
C = 128
GN_EPS = 64e-5
C0 = -float(np.exp(-0.5))


def rwkv_consts():
    i = np.arange(128)
    m1 = (i[:, None] <= i[None, :]).astype(np.float32)
    m2 = (i[:, None] > i[None, :]).astype(np.float32)
    mus = (i[:, None] < i[None, :]).astype(np.float32)
    mls = mus.T.copy()
    ident = np.eye(128, dtype=np.float32)
    mask4 = np.stack([mus, m1, mus, m1], axis=1)
    return {"c_m1": m1, "c_m2": m2, "c_mus": mus, "c_mls": mls, "c_ident": ident,
            "c_mask4": np.ascontiguousarray(mask4)}


def build_rwkv(nc, S, es, dram, n_chunks=S_TOK // C):
    sbn = [0]

    def sb(shape, dt=F32, name=None):
        sbn[0] += 1
        return es.enter_context(nc.sbuf_tensor(name or ("rw%d" % sbn[0]), shape, dt))

    def ps(shape, dt=F32, name=None):
        sbn[0] += 1
        return es.enter_context(nc.psum_tensor(name or ("rp%d" % sbn[0]), shape, dt))

    BANKS = {"P_RKV": 0, "P_Z": 2, "P_T": 4, "P_L1a": 5, "P_L1b": 1, "P_LDa": 6, "P_LDb": 3,
             "P_G": 7, "P_U": 7, "P_H": 7, "P_Y": 7, "P_OT": 7}

    def _bk(r, w):
        b = set()
        for k in list(r) + list(w):
            if k.startswith("P_"):
                for pfx, bn in BANKS.items():
                    if k.startswith(pfx):
                        b.add(bn)
        return list(w) + ["BANK%d" % x for x in b]

    defer = [None]

    def _rec(eng, fn, r, w):
        if defer[0] is None:
            S.op(eng, fn, r, w)
        else:
            defer[0].append((eng, fn, r, w))

    def DMA(out, in_, reads=(), writes=()):
        if defer[0] is None:
            S.dma("sync", out, in_, reads=reads, writes=writes)
        else:
            defer[0].append(("dma", (out, in_), reads, writes))

    def drain(lst, n):
        for _ in range(min(n, len(lst))):
            eng, fn, r, w = lst.pop(0)
            if eng == "dma":
                S.dma("sync", fn[0], fn[1], reads=r, writes=w)
            else:
                S.op(eng, fn, r, w)

    V = lambda fn, r=(), w=(): _rec("vector", fn, r, _bk(r, w))
    A = lambda fn, r=(), w=(): _rec("scalar", fn, r, _bk(r, w))
    G = lambda fn, r=(), w=(): _rec("gpsimd", fn, r, list(w))
    T = lambda fn, r=(), w=(): _rec("tensor", fn, r, _bk(r, w))

    cst = {}
    for nm in ("c_m1", "c_m2", "c_mus", "c_mls", "c_ident"):
        t = sb([128, 128]); cst[nm] = t
        S.dma("sync", t[:], dram[nm], writes=[nm])
    mask4 = sb([128, 4, 128])
    S.dma("sync", mask4[:], dram["c_mask4"], writes=["mask4"])
    ones_col = sb([128, 1])
    V(lambda e: e.memset(ones_col[:], 1.0), w=["ones_col"])
    bc = {}
    for nm in ("w0", "a0", "kk", "ka", "rk", "lng", "lnb"):
        t = sb([128, 128]); bc[nm] = t
        S.dma("sync", t[:], dram[nm].partition_broadcast(128), writes=["bc_" + nm])
    NW = 384 + 288
    wst = sb([128, 8, NW])
    S.dma("sync", wst[:, :, 0:384], dram["w_rkv"].rearrange("(dc p) n -> p dc n", p=128), writes=["wst"])
    S.dma("sync", wst[:, :, 384:NW], dram["w_lo"].rearrange("(dc p) n -> p dc n", p=128), writes=["wst"])
    mub = sb([128, NW])
    S.dma("sync", mub[:, 0:384], dram["mu_rkv"].partition_broadcast(128), writes=["mub"])
    S.dma("sync", mub[:, 384:NW], dram["mu_lo"].partition_broadcast(128), writes=["mub"])
    omu = sb([128, NW])
    V(lambda e: e.tensor_scalar(out=omu[:], in0=mub[:], scalar1=-1.0, scalar2=1.0, op0=ALU.mult, op1=ALU.add), r=["mub"], w=["omu"])
    Wa = sb([128, 8, NW], BF16); Wb = sb([128, 8, NW], BF16)
    for dc in range(8):
        V(lambda e, dc=dc: e.tensor_tensor(out=Wa[:, dc, :], in0=wst[:, dc, :], in1=omu[:], op=ALU.mult), r=["wst", "omu"], w=["Wa"])
        G(lambda e, dc=dc: e.tensor_tensor(out=Wb[:, dc, :], in0=wst[:, dc, :], in1=mub[:], op=ALU.mult), r=["wst", "mub"], w=["Wb"])
    wdu_f = sb([64, 128]); wau_f = sb([64, 128]); wgu0_f = sb([128, 128]); wgu1_f = sb([32, 128])
    S.dma("sync", wdu_f[:], dram["wdu"], writes=["wdu_f"])
    S.dma("sync", wau_f[:], dram["wau"], writes=["wau_f"])
    S.dma("sync", wgu0_f[:], dram["wgu"][0:128, :], writes=["wgu0_f"])
    S.dma("sync", wgu1_f[:], dram["wgu"][128:160, :], writes=["wgu1_f"])
    wdu = sb([64, 128], BF16); wau = sb([64, 128], BF16); wgu0 = sb([128, 128], BF16); wgu1 = sb([32, 128], BF16)
    V(lambda e: e.tensor_copy(out=wdu[:], in_=wdu_f[:]), r=["wdu_f"], w=["wdu"])
    V(lambda e: e.tensor_copy(out=wau[:], in_=wau_f[:]), r=["wau_f"], w=["wau"])
    V(lambda e: e.tensor_copy(out=wgu0[:], in_=wgu0_f[:]), r=["wgu0_f"], w=["wgu0"])
    V(lambda e: e.tensor_copy(out=wgu1[:], in_=wgu1_f[:]), r=["wgu1_f"], w=["wgu1"])

    xs = [sb([128, 8, XB + 1]) for _ in range(2)]
    xb = [sb([128, 8, XB + 1], BF16) for _ in range(2)]
    hwT = sb([64, XB], BF16); haT = sb([64, XB], BF16); hg0T = sb([128, XB], BF16); hg1T = sb([32, XB], BF16)
    Hst = sb([128, 64])
    V(lambda e: e.memset(Hst[:], 0.0), w=["Hst"])
    Bpad = [sb([128, 2, 128]) for _ in range(2)]; Kpad = [sb([128, 2, 128]) for _ in range(2)]
    for p_ in range(2):
        V(lambda e, p_=p_: e.memset(Bpad[p_][:], 0.0), w=["Bpad%d" % p_])
        V(lambda e, p_=p_: e.memset(Kpad[p_][:], 0.0), w=["Kpad%d" % p_])

    P_RKV = ps([128, 512]); P_Z = ps([128, 4, 128])
    P_T = ps([128, 4, 128]); P_L1 = [ps([128, 4, 128]) for _ in range(2)]; P_LD = [ps([128, 4, 128]) for _ in range(2)]; P_M = ps([128, 512])
    P_LH = P_T[:].rearrange("p a n -> p (a n)")

    def t(shape=(128, 128), dt=F32):
        return sb(list(shape), dt)

    zw = t(); logw = t(); av = t(); gv = [t() for _ in range(2)]; kkr = t(); sq = t(); ss = t((128, 2)); rn = t((128, 2))
    kk = t(); tmp1 = t(); kmod = t(); kka = t(); rkr = t(); sbon = [t((128, 2)) for _ in range(2)]; vsb = [t() for _ in range(2)]; sq2 = t()
    eW = t(); eWi = t(); eWp = t(); eR = t(); wc = [t((128, 1)) for _ in range(2)]
    TM = sb([128, 4, 128])
    FT = [sb([128, 4, 128]) for _ in range(2)]
    LM1 = [[sb([128, 4, 128]) for _ in range(2)] for _ in range(2)]
    PN = [[[sb([128, 2, 128]) for _ in range(2)] for _ in range(2)] for _ in range(2)]
    Lk = [[t() for _ in range(2)] for _ in range(2)]
    Gs = [t((128, 64)) for _ in range(2)]; Us = [t((128, 64)) for _ in range(2)]
    s1 = t((128, 2)); nmean = t((128, 2)); Yc = t(); ss2 = t((128, 2)); rstd = t((128, 2)); Yn = t(); Yo = t(); YoT = t()

    n_blocks = (n_chunks * C) // XB
    xT = dram["xT"].rearrange("(dc p) n -> p dc n", p=128)

    NCPB = XB // C

    def gen_X(ch):
        blk = ch // NCPB
        ci = ch % NCPB
        o = ci * C
        p = ch % 2
        xi = blk % 2
        kx = "xs%d" % xi; kb = "xb%d" % xi
        c0 = blk * XB
        if ci == 0:
            xi = blk % 2
            kx = "xs%d" % xi; kb = "xb%d" % xi
            c0 = blk * XB
            for dc in range(8):
                DMA(xs[xi][:, dc, :], xT[:, dc, c0:c0 + XB + 1], writes=[kx])
            for dc in range(8):
                eng = G if dc % 2 == 0 else A
                if dc % 2 == 0:
                    G(lambda e, dc=dc, xi=xi: e.tensor_copy(out=xb[xi][:, dc, :], in_=xs[xi][:, dc, :]), r=[kx], w=[kb])
                else:
                    A(lambda e, dc=dc, xi=xi: e.copy(out=xb[xi][:, dc, :], in_=xs[xi][:, dc, :]), r=[kx], w=[kb])
            for (lo, n, dst, fn, key) in ((384, 64, hwT, AF.Tanh, "hwT"), (448, 64, haT, AF.Copy, "haT"),
                                          (512, 128, hg0T, AF.Sigmoid, "hg0T"), (640, 32, hg1T, AF.Sigmoid, "hg1T")):
                for dc in range(8):
                    T(lambda e, dc=dc, lo=lo, n=n, xi=xi: e.matmul(P_LH[0:n, :], lhsT=Wa[:, dc, lo:lo + n], rhs=xb[xi][:, dc, 1:XB + 1],
                                                                  start=(dc == 0), stop=False), r=["Wa", kb], w=["P_T"])
                for dc in range(8):
                    T(lambda e, dc=dc, lo=lo, n=n, xi=xi: e.matmul(P_LH[0:n, :], lhsT=Wb[:, dc, lo:lo + n], rhs=xb[xi][:, dc, 0:XB],
                                                                  start=False, stop=(dc == 7)), r=["Wb", kb], w=["P_T"])
                A(lambda e, n=n, dst=dst, fn=fn: e.activation(out=dst[:], in_=P_LH[0:n, :], func=fn), r=["P_T"], w=[key])

        for dc in range(8):
            T(lambda e, dc=dc, xi=xi, o=o: e.matmul(P_RKV[:, 0:384], lhsT=xb[xi][:, dc, 1 + o:1 + o + C], rhs=Wa[:, dc, 0:384],
                                                    start=(dc == 0), stop=False), r=["Wa", kb], w=["P_RKV"])
        for dc in range(8):
            T(lambda e, dc=dc, xi=xi, o=o: e.matmul(P_RKV[:, 0:384], lhsT=xb[xi][:, dc, o:o + C], rhs=Wb[:, dc, 0:384],
                                                    start=False, stop=(dc == 7)), r=["Wb", kb], w=["P_RKV"])
        T(lambda e, o=o: e.matmul(P_Z[:, 0, :], lhsT=hwT[:, o:o + C], rhs=wdu[:], start=True, stop=True), r=["hwT", "wdu"], w=["P_Z0"])
        T(lambda e, o=o: e.matmul(P_Z[:, 1, :], lhsT=haT[:, o:o + C], rhs=wau[:], start=True, stop=True), r=["haT", "wau"], w=["P_Z1"])
        T(lambda e, o=o: e.matmul(P_RKV[:, 384:512], lhsT=hg0T[:, o:o + C], rhs=wgu0[:], start=True, stop=False), r=["hg0T", "wgu0"], w=["P_RKVg"])
        T(lambda e, o=o: e.matmul(P_RKV[:, 384:512], lhsT=hg1T[:, o:o + C], rhs=wgu1[:], start=False, stop=True), r=["hg1T", "wgu1"], w=["P_RKVg"])
        V(lambda e: e.tensor_tensor(out=zw[:], in0=P_Z[:, 0, :], in1=bc["w0"][:], op=ALU.add), r=["P_Z0", "bc_w0"], w=["zw"])
        A(lambda e: e.activation(out=zw[:], in_=zw[:], func=AF.Sigmoid), r=["zw"], w=["zw"])
        G(lambda e: e.tensor_scalar(out=logw[:], in0=zw[:], scalar1=C0, scalar2=None, op0=ALU.mult), r=["zw"], w=["logw"])
        V(lambda e: e.tensor_tensor(out=av[:], in0=P_Z[:, 1, :], in1=bc["a0"][:], op=ALU.add), r=["P_Z1", "bc_a0"], w=["av"])
        A(lambda e: e.activation(out=av[:], in_=av[:], func=AF.Sigmoid), r=["av"], w=["av"])
        A(lambda e: e.copy(out=gv[p][:], in_=P_RKV[:, 384:512]), r=["P_RKVg"], w=["gv%d" % p])
        A(lambda e: e.copy(out=vsb[p][:], in_=P_RKV[:, 256:384]), r=["P_RKV"], w=["vsb%d" % p])
        V(lambda e: e.tensor_tensor(out=kkr[:], in0=P_RKV[:, 128:256], in1=bc["kk"][:], op=ALU.mult), r=["P_RKV", "bc_kk"], w=["kkr"])
        G(lambda e: e.tensor_tensor(out=sq[:], in0=kkr[:], in1=kkr[:], op=ALU.mult), r=["kkr"], w=["sq"])
        V(lambda e: e.tensor_reduce(out=ss[:], in_=sq[:].rearrange("p (h c) -> p h c", h=2), axis=AX.X, op=ALU.add), r=["sq"], w=["ss"])
        A(lambda e: e.activation(out=ss[:], in_=ss[:], func=AF.Sqrt), r=["ss"], w=["ss"])
        V(lambda e: e.tensor_scalar(out=ss[:], in0=ss[:], scalar1=1e-12, scalar2=None, op0=ALU.max), r=["ss"], w=["ss"])
        V(lambda e: e.reciprocal(out=rn[:], in_=ss[:]), r=["ss"], w=["rn"])
        for h in range(2):
            V(lambda e, h=h: e.tensor_scalar(out=kk[:, h * 64:(h + 1) * 64], in0=kkr[:, h * 64:(h + 1) * 64], scalar1=rn[:, h:h + 1],
                                             scalar2=None, op0=ALU.mult), r=["kkr", "rn"], w=["kk"])
        V(lambda e: e.scalar_tensor_tensor(out=tmp1[:], in0=av[:], scalar=-1.0, in1=bc["ka"][:], op0=ALU.add, op1=ALU.mult), r=["av", "bc_ka"], w=["tmp1"])
        V(lambda e: e.scalar_tensor_tensor(out=kmod[:], in0=tmp1[:], scalar=1.0, in1=P_RKV[:, 128:256], op0=ALU.add, op1=ALU.mult), r=["tmp1", "P_RKV"], w=["kmod"])
        G(lambda e: e.tensor_tensor(out=kka[:], in0=kk[:], in1=av[:], op=ALU.mult), r=["kk", "av"], w=["kka"])
        V(lambda e: e.tensor_tensor(out=rkr[:], in0=P_RKV[:, 0:128], in1=kmod[:], op=ALU.mult), r=["P_RKV", "kmod"], w=["rkr"])
        G(lambda e: e.tensor_tensor(out=rkr[:], in0=rkr[:], in1=bc["rk"][:], op=ALU.mult), r=["rkr", "bc_rk"], w=["rkr"])
        V(lambda e: e.tensor_reduce(out=sbon[p][:], in_=rkr[:].rearrange("p (h c) -> p h c", h=2), axis=AX.X, op=ALU.add), r=["rkr"], w=["sbon%d" % p])
        T(lambda e: e.matmul(P_Z[:, 2, :], lhsT=cst["c_m1"][:], rhs=logw[:], start=True, stop=True), r=["c_m1", "logw"], w=["P_Z2"])
        T(lambda e: e.matmul(P_Z[:, 3, :], lhsT=cst["c_m2"][:], rhs=logw[:], start=True, stop=True), r=["c_m2", "logw"], w=["P_Z3"])
        T(lambda e: e.matmul(P_Z[:, 0, 0:1], lhsT=logw[:], rhs=ones_col[:], start=True, stop=True), r=["ones_col", "logw"], w=["P_Z0"])
        A(lambda e: e.activation(out=eW[:], in_=P_Z[:, 2, :], func=AF.Exp), r=["P_Z2"], w=["eW"])
        A(lambda e: e.activation(out=eWi[:], in_=P_Z[:, 2, :], func=AF.Exp, scale=-1.0), r=["P_Z2"], w=["eWi"])
        A(lambda e: e.activation(out=eWp[:], in_=logw[:], func=AF.Exp, scale=-1.0), r=["logw"], w=["eWp"])
        G(lambda e: e.tensor_tensor(out=eWp[:], in0=eWp[:], in1=eW[:], op=ALU.mult), r=["eWp", "eW"], w=["eWp"])
        A(lambda e: e.activation(out=eR[:], in_=P_Z[:, 3, :], func=AF.Exp), r=["P_Z3"], w=["eR"])
        A(lambda e: e.activation(out=wc[p][:], in_=P_Z[:, 0, 0:1], func=AF.Exp), r=["P_Z0"], w=["wc%d" % p])
        V(lambda e: e.scalar_tensor_tensor(out=TM[:, 0, :], in0=kk[:], scalar=-1.0, in1=eWp[:], op0=ALU.mult, op1=ALU.mult), r=["kk", "eWp"], w=["TM0"])
        V(lambda e: e.tensor_tensor(out=TM[:, 1, :], in0=P_RKV[:, 0:128], in1=eW[:], op=ALU.mult), r=["P_RKV", "eW"], w=["TM1"])
        G(lambda e: e.tensor_tensor(out=TM[:, 2, :], in0=kka[:], in1=eWi[:], op=ALU.mult), r=["kka", "eWi"], w=["TM2"])
        V(lambda e: e.tensor_tensor(out=TM[:, 3, :], in0=kmod[:], in1=eWi[:], op=ALU.mult), r=["kmod", "eWi"], w=["TM3"])
        for h in range(2):
            hs = slice(h * 64, (h + 1) * 64)
            G(lambda e, h=h, hs=hs: e.tensor_tensor(out=Bpad[p][:, h, hs], in0=kka[:, hs], in1=eR[:, hs], op=ALU.mult), r=["kka", "eR"], w=["Bpad%d" % p])
            V(lambda e, h=h, hs=hs: e.tensor_tensor(out=Kpad[p][:, h, hs], in0=kmod[:, hs], in1=eR[:, hs], op=ALU.mult), r=["kmod", "eR"], w=["Kpad%d" % p])
        for j in range(4):
            T(lambda e, j=j: e.transpose(P_T[:, j, :], TM[:, j, :], cst["c_ident"][:]), r=["TM%d" % j, "c_ident"], w=["P_T"])
        A(lambda e: e.copy(out=FT[p][:], in_=P_T[:]), r=["P_T"], w=["FT%d" % p])
        HSL = [slice(0, 64), slice(64, 128)]
        BK = ["a", "b"]
        for h in range(2):
            hs = HSL[h]
            T(lambda e, hs=hs, h=h: e.matmul(P_L1[h][:, 0:2, :], lhsT=FT[p][hs, 2, :], rhs=FT[p][hs, 0:2, :], start=True, stop=True), r=["FT%d" % p], w=["P_L1%s" % BK[h]])
            T(lambda e, hs=hs, h=h: e.matmul(P_L1[h][:, 2:4, :], lhsT=FT[p][hs, 3, :], rhs=FT[p][hs, 0:2, :], start=True, stop=True), r=["FT%d" % p], w=["P_L1%s" % BK[h]])
            T(lambda e, hs=hs, h=h: e.matmul(P_LD[h][:, 0, :], lhsT=FT[p][hs, 0, :], rhs=FT[p][hs, 2, :], start=True, stop=True), r=["FT%d" % p], w=["P_LD%s0" % BK[h]])
        for h in range(2):
            V(lambda e, h=h: e.tensor_tensor(out=LM1[p][h][:], in0=P_L1[h][:], in1=mask4[:], op=ALU.mult), r=["P_L1%s" % BK[h], "mask4"], w=["LM1%d.%d" % (p, h)])
            V(lambda e, h=h: e.tensor_tensor(out=Lk[h][0][:], in0=P_LD[h][:, 0, :], in1=cst["c_mls"][:], op=ALU.mult), r=["P_LD%s0" % BK[h], "c_mls"], w=["Lk%d.0" % h])
        for h in range(2):
            G(lambda e, h=h: e.tensor_tensor(out=PN[p][h][0][:, 0, :], in0=LM1[p][h][:, 0, :], in1=cst["c_ident"][:], op=ALU.add), r=["LM1%d.%d" % (p, h), "c_ident"], w=["PN%d.%d.0.P" % (p, h)])
            T(lambda e, h=h: e.matmul(P_LD[h][:, 2, :], lhsT=Lk[h][0][:], rhs=LM1[p][h][:, 0, :], start=True, stop=True), r=["Lk%d.0" % h, "LM1%d.%d" % (p, h)], w=["P_LD%s12" % BK[h]])
            T(lambda e, h=h: e.matmul(P_LD[h][:, 3, :], lhsT=LM1[p][h][:, 0, :], rhs=Lk[h][0][:], start=True, stop=True), r=["Lk%d.0" % h, "LM1%d.%d" % (p, h)], w=["P_LD%s3" % BK[h]])
        for h in range(2):
            A(lambda e, h=h: e.copy(out=PN[p][h][0][:, 1, :], in_=P_LD[h][:, 2, :]), r=["P_LD%s12" % BK[h]], w=["PN%d.%d.0.N" % (p, h)])
            A(lambda e, h=h: e.copy(out=Lk[h][1][:], in_=P_LD[h][:, 3, :]), r=["P_LD%s3" % BK[h]], w=["Lk%d.1" % h])
        cur = 0
        lcur = 1
        for lev in range(1, 7):
            nxt = 1 - cur
            lnx = 1 - lcur
            last = (lev == 6)
            for h in range(2):
                pk = "PN%d.%d.%d" % (p, h, cur); lk = "Lk%d.%d" % (h, lcur)
                if not last:
                    T(lambda e, cur=cur, lcur=lcur, h=h: e.matmul(P_LD[h][:, 1:3, :], lhsT=Lk[h][lcur][:], rhs=PN[p][h][cur][:], start=True, stop=True),
                      r=[lk, pk + ".P", pk + ".N"], w=["P_LD%s12" % BK[h]])
                    T(lambda e, cur=cur, lcur=lcur, h=h: e.matmul(P_LD[h][:, 3, :], lhsT=PN[p][h][cur][:, 1, :], rhs=Lk[h][lcur][:], start=True, stop=True),
                      r=[lk, pk + ".N"], w=["P_LD%s3" % BK[h]])
                else:
                    T(lambda e, cur=cur, lcur=lcur, h=h: e.matmul(P_LD[h][:, 1, :], lhsT=Lk[h][lcur][:], rhs=PN[p][h][cur][:, 0, :], start=True, stop=True),
                      r=[lk, pk + ".P"], w=["P_LD%s12" % BK[h]])
            for h in range(2):
                pk = "PN%d.%d.%d" % (p, h, cur); pn = "PN%d.%d.%d" % (p, h, nxt); ln_ = "Lk%d.%d" % (h, lnx)
                V(lambda e, cur=cur, nxt=nxt, h=h: e.tensor_tensor(out=PN[p][h][nxt][:, 0, :], in0=P_LD[h][:, 1, :], in1=PN[p][h][cur][:, 0, :], op=ALU.add),
                  r=["P_LD%s12" % BK[h], pk + ".P"], w=[pn + ".P"])
                if not last:
                    A(lambda e, nxt=nxt, h=h: e.copy(out=PN[p][h][nxt][:, 1, :], in_=P_LD[h][:, 2, :]), r=["P_LD%s12" % BK[h]], w=[pn + ".N"])
                    A(lambda e, lnx=lnx, h=h: e.copy(out=Lk[h][lnx][:], in_=P_LD[h][:, 3, :]), r=["P_LD%s3" % BK[h]], w=[ln_])
            cur = nxt
            lcur = lnx

    def gen_Y(ch):
        p = ch % 2
        HSL = [slice(0, 64), slice(64, 128)]
        BK = ["a", "b"]
        cur = 0
        for h in range(2):
            hs = HSL[h]
            T(lambda e, hs=hs, h=h: e.matmul(P_M[:, 64 * h:64 * h + 64], lhsT=FT[p][hs, 0, :], rhs=Hst[hs, :], start=True, stop=False), r=["FT%d" % p, "Hst"], w=["P_G%d" % h])
            T(lambda e, hs=hs, h=h: e.matmul(P_M[:, 64 * h:64 * h + 64], lhsT=LM1[p][h][:, 2, :], rhs=vsb[p][:, hs], start=False, stop=True), r=["LM1%d.%d" % (p, h), "vsb%d" % p], w=["P_G%d" % h])
        A(lambda e: e.copy(out=Gs[0][:], in_=P_M[:, 0:64]), r=["P_G0"], w=["Gs0"])
        V(lambda e: e.tensor_copy(out=Gs[1][:], in_=P_M[:, 64:128]), r=["P_G1"], w=["Gs1"])
        for h in range(2):
            T(lambda e, h=h, cur=cur: e.matmul(P_M[:, 64 * h:64 * h + 64], lhsT=PN[p][h][cur][:, 0, :], rhs=Gs[h][:], start=True, stop=True),
              r=["PN%d.%d.%d.P" % (p, h, cur), "Gs%d" % h], w=["P_G%d" % h])
        A(lambda e: e.copy(out=Us[0][:], in_=P_M[:, 0:64]), r=["P_G0"], w=["Us0"])
        V(lambda e: e.tensor_copy(out=Us[1][:], in_=P_M[:, 64:128]), r=["P_G1"], w=["Us1"])
        for h in range(2):
            hs = HSL[h]
            yk = "P_Y%d" % h
            uk = "Us%d" % h
            T(lambda e, hs=hs, h=h: e.matmul(P_M[:, 192 + 64 * h:256 + 64 * h], lhsT=FT[p][hs, 1, :], rhs=Hst[hs, :], start=True, stop=False), r=["FT%d" % p, "Hst"], w=[yk])
            T(lambda e, h=h: e.matmul(P_M[:, 192 + 64 * h:256 + 64 * h], lhsT=LM1[p][h][:, 1, :], rhs=Us[h][:], start=False, stop=False), r=["LM1%d.%d" % (p, h), uk], w=[yk])
            T(lambda e, hs=hs, h=h: e.matmul(P_M[:, 192 + 64 * h:256 + 64 * h], lhsT=LM1[p][h][:, 3, :], rhs=vsb[p][:, hs], start=False, stop=True), r=["LM1%d.%d" % (p, h), "vsb%d" % p], w=[yk])
        T(lambda e: e.matmul(P_M[:, 128:192], lhsT=Bpad[p][:, 0, :], rhs=Us[0][:], start=True, stop=False), r=["Bpad%d" % p, "Us0"], w=["P_H"])
        T(lambda e: e.matmul(P_M[:, 128:192], lhsT=Kpad[p][:, 0, :], rhs=vsb[p][:, 0:64], start=False, stop=False), r=["Kpad%d" % p, "vsb%d" % p], w=["P_H"])
        T(lambda e: e.matmul(P_M[:, 128:192], lhsT=Bpad[p][:, 1, :], rhs=Us[1][:], start=False, stop=False), r=["Bpad%d" % p, "Us1"], w=["P_H"])
        T(lambda e: e.matmul(P_M[:, 128:192], lhsT=Kpad[p][:, 1, :], rhs=vsb[p][:, 64:128], start=False, stop=True), r=["Kpad%d" % p, "vsb%d" % p], w=["P_H"])
        V(lambda e: e.scalar_tensor_tensor(out=Hst[:], in0=Hst[:], scalar=wc[p][:, 0:1], in1=P_M[:, 128:192], op0=ALU.mult, op1=ALU.add),
          r=["Hst", "wc%d" % p, "P_H"], w=["Hst"])
        Yv = P_M[:, 192:320]
        V(lambda e: e.tensor_reduce(out=s1[:], in_=Yv.rearrange("p (h c) -> p h c", h=2), axis=AX.X, op=ALU.add), r=["P_Y0", "P_Y1"], w=["s1"])
        V(lambda e: e.tensor_scalar(out=nmean[:], in0=s1[:], scalar1=-1.0 / 64, scalar2=None, op0=ALU.mult), r=["s1"], w=["nmean"])
        for h in range(2):
            hs = slice(h * 64, (h + 1) * 64)
            V(lambda e, h=h, hs=hs: e.tensor_scalar(out=Yc[:, hs], in0=P_M[:, 192 + 64 * h:256 + 64 * h], scalar1=nmean[:, h:h + 1], scalar2=None, op0=ALU.add),
              r=["P_Y%d" % h, "nmean"], w=["Yc"])
        G(lambda e: e.tensor_tensor(out=sq2[:], in0=Yc[:], in1=Yc[:], op=ALU.mult), r=["Yc"], w=["sq2"])
        V(lambda e: e.tensor_reduce(out=ss2[:], in_=sq2[:].rearrange("p (h c) -> p h c", h=2), axis=AX.X, op=ALU.add), r=["sq2"], w=["ss2"])
        V(lambda e: e.tensor_scalar(out=ss2[:], in0=ss2[:], scalar1=1.0 / 64, scalar2=GN_EPS, op0=ALU.mult, op1=ALU.add), r=["ss2"], w=["ss2"])
        A(lambda e: e.activation(out=ss2[:], in_=ss2[:], func=AF.Sqrt), r=["ss2"], w=["ss2"])
        V(lambda e: e.reciprocal(out=rstd[:], in_=ss2[:]), r=["ss2"], w=["rstd"])
        for h in range(2):
            hs = slice(h * 64, (h + 1) * 64)
            V(lambda e, h=h, hs=hs: e.tensor_scalar(out=Yn[:, hs], in0=Yc[:, hs], scalar1=rstd[:, h:h + 1], scalar2=None, op0=ALU.mult), r=["Yc", "rstd"], w=["Yn"])
        G(lambda e: e.tensor_tensor(out=Yn[:], in0=Yn[:], in1=bc["lng"][:], op=ALU.mult), r=["Yn", "bc_lng"], w=["Yn"])
        G(lambda e: e.tensor_tensor(out=Yn[:], in0=Yn[:], in1=bc["lnb"][:], op=ALU.add), r=["Yn", "bc_lnb"], w=["Yn"])
        for h in range(2):
            hs = slice(h * 64, (h + 1) * 64)
            V(lambda e, h=h, hs=hs: e.scalar_tensor_tensor(out=Yo[:, hs], in0=vsb[p][:, hs], scalar=sbon[p][:, h:h + 1], in1=Yn[:, hs], op0=ALU.mult, op1=ALU.add),
              r=["vsb%d" % p, "sbon%d" % p, "Yn"], w=["Yo"])
        G(lambda e: e.tensor_tensor(out=Yo[:], in0=Yo[:], in1=gv[p][:], op=ALU.mult), r=["Yo", "gv%d" % p], w=["Yo"])
        T(lambda e: e.transpose(P_M[:, 320:448], Yo[:], cst["c_ident"][:]), r=["Yo", "c_ident"], w=["P_OT"])
        A(lambda e: e.copy(out=YoT[:], in_=P_M[:, 320:448]), r=["P_OT"], w=["YoT"])
        DMA(dram["cin"][(ch * C) // 1024][0:128, (ch * C) % 1024:(ch * C) % 1024 + C], YoT[:], reads=["YoT"])

    def collect(fn, ch):
        lst = []
        defer[0] = lst
        fn(ch)
        defer[0] = None
        return lst

    xl = collect(gen_X, 0)
    drain(xl, len(xl))
    for ch in range(n_chunks):
        yl = collect(gen_Y, ch)
        xl = collect(gen_X, ch + 1) if ch + 1 < n_chunks else []
        ny = len(yl)
        for i in range(ny):
            drain(yl, 1)
            if xl:
                drain(xl, -(-len(xl) // (ny - i)))
        drain(xl, len(xl))


S_TOK = 16384
AXB = 256
KB_BOUND = 16.0
NEG = -30000.0


def attn_consts():
    s = S_TOK
    half = 32
    inv_freq = (10000.0 ** (-np.arange(half, dtype=np.float32) / half)).astype(np.float32)
    ang = (np.arange(s, dtype=np.float32)[:, None] * inv_freq[None, :]).astype(np.float32)
    cos = np.cos(ang).astype(np.float32)
    sin = np.sin(ang).astype(np.float32)
    ropeA = np.tile(cos, (1, 8)).astype(np.float32)
    ropeB = np.tile(np.concatenate([-sin, sin], 1), (1, 4)).astype(np.float32)
    kind = np.zeros((64, s), np.float32)
    for n in range(64):
        kind[n, n * 256:(n + 1) * 256] = 1.0
    i = np.arange(128)
    tri = (i[None, :] >= i[:, None]).astype(np.float32)
    return {"a_ropeA": ropeA, "a_ropeB": ropeB, "a_kind": kind.astype(ml_dtypes.bfloat16),
            "a_tri": tri.astype(ml_dtypes.bfloat16), "a_ident": np.eye(128, dtype=np.float32),
            "a_identb": np.eye(128, dtype=np.float32).astype(ml_dtypes.bfloat16)}


def build_attn(nc, S, es, dram, n_blocks=S_TOK // 256):
    sbn = [0]

    def sb(shape, dt=F32):
        sbn[0] += 1
        return es.enter_context(nc.sbuf_tensor("at%d" % sbn[0], shape, dt))

    def ps(shape, dt=F32):
        sbn[0] += 1
        return es.enter_context(nc.psum_tensor("ap%d" % sbn[0], shape, dt))

    def _bk(r, w):
        b = set(k.split(".")[0] for k in list(r) + list(w) if k.startswith("Q_"))
        return list(w) + ["BANK_" + x for x in b]

    defer = [None]

    def _rec(eng, fn, r, w):
        if defer[0] is None:
            S.op(eng, fn, r, w)
        else:
            defer[0].append((eng, fn, r, w))

    def DMA(out, in_, reads=(), writes=()):
        if defer[0] is None:
            S.dma("sync", out, in_, reads=reads, writes=writes)
        else:
            defer[0].append(("dma", (out, in_), reads, writes))

    def drain(lst, n):
        for _ in range(min(n, len(lst))):
            eng, fn, r, w = lst.pop(0)
            if eng == "dma":
                S.dma("sync", fn[0], fn[1], reads=r, writes=w)
            else:
                S.op(eng, fn, r, w)

    V = lambda fn, r=(), w=(): _rec("vector", fn, r, _bk(r, w))
    A = lambda fn, r=(), w=(): _rec("scalar", fn, r, _bk(r, w))
    G = lambda fn, r=(), w=(): _rec("gpsimd", fn, r, list(w))
    T = lambda fn, r=(), w=(): _rec("tensor", fn, r, _bk(r, w))

    ident = sb([128, 128]); identb = sb([128, 128], BF16); tri = sb([128, 128], BF16)
    S.dma("sync", ident[:], dram["a_ident"], writes=["ident"])
    S.dma("sync", identb[:], dram["a_identb"], writes=["identb"])
    S.dma("sync", tri[:], dram["a_tri"], writes=["tri"])
    wst = sb([128, 8, 384])
    S.dma("sync", wst[:], dram["w_qkv"].rearrange("(dc p) n -> p dc n", p=128), writes=["wst"])
    Wq = sb([128, 8, 384], BF16)
    for dc in range(8):
        V(lambda e, dc=dc: e.tensor_copy(out=Wq[:, dc, :], in_=wst[:, dc, :]), r=["wst"], w=["Wq"])
    inv256 = sb([128, 1]); ones_r = sb([128, 64])
    V(lambda e: e.memset(inv256[:], 1.0 / 256), w=["inv256"])
    V(lambda e: e.memset(ones_r[:], 1.0), w=["ones_r"])
    KT = [sb([128, S_TOK], BF16) for _ in range(2)]
    VA = [sb([128, 128, 65], BF16) for _ in range(2)]
    kmT = [sb([64, 64]) for _ in range(2)]
    Gt = [sb([128, 64]) for _ in range(2)]
    for h in range(2):
        S.dma("sync", KT[h][64:128, :], dram["a_kind"], writes=["KT%d.%d" % (h, b_) for b_ in range(64)])
        G(lambda e, h=h: e.memset(VA[h][:], 1.0), w=["VA%d.%d" % (h, b_) for b_ in range(64)])
        V(lambda e, h=h: e.memset(kmT[h][:], 0.0), w=["kmT%d" % h])
        V(lambda e, h=h: e.memset(Gt[h][:], -1e30), w=["Gt%d" % h])
    xs = [sb([128, 8, AXB + 1]) for _ in range(2)]
    xb = [sb([128, 8, AXB + 1], BF16) for _ in range(2)]
    rA = [sb([128, 256]) for _ in range(2)]; rB = [sb([128, 256]) for _ in range(2)]
    tmpA = sb([128, 256]); tmpB = sb([128, 256]); QK = sb([128, 4, 64])
    qT32 = sb([64, 128]); mx = sb([128, 8]); negm = sb([128, 64]); ssq = sb([128, 1]); cpos = sb([128, 1]); qjunk = sb([128, 64])
    Qaug = [sb([128, 128], BF16) for _ in range(2)]
    QT = [[sb([128, 256], BF16) for _ in range(2)] for _ in range(2)]
    PT = [sb([128, 256], BF16) for _ in range(4)]
    rec = sb([128, 256]); ysb = sb([64, 256]); yo = sb([64, 256])

    NSB = 4
    Q_A = ps([128, 512]); Q_C = ps([128, 512])
    Q_D = Q_C[:, 66:130].bitcast(BF16)
    Q_S = [ps([128, 512]) for _ in range(NSB)]; Q_O = [ps([128, 512]) for _ in range(2)]

    xT = dram["xT"].rearrange("(dc p) n -> p dc n", p=128)
    sidx = 0

    def gen_proj(blk):
        xi = blk % 2
        kx = "xs%d" % xi; kb = "xb%d" % xi
        c0 = blk * AXB
        for dc in range(8):
            DMA(xs[xi][:, dc, :], xT[:, dc, c0:c0 + AXB + 1], writes=[kx])
        for dc in range(8):
            if dc % 2 == 0:
                G(lambda e, dc=dc, xi=xi: e.tensor_copy(out=xb[xi][:, dc, :], in_=xs[xi][:, dc, :]), r=[kx], w=[kb])
            else:
                V(lambda e, dc=dc, xi=xi: e.tensor_copy(out=xb[xi][:, dc, :], in_=xs[xi][:, dc, :]), r=[kx], w=[kb])
        for half in range(2):
            tt = blk * 2 + half
            o = half * 128
            ri = tt % 2
            DMA(rA[ri][:], dram["a_ropeA"][tt * 128:(tt + 1) * 128, :], writes=["rA%d" % ri])
            DMA(rB[ri][:], dram["a_ropeB"][tt * 128:(tt + 1) * 128, :], writes=["rB%d" % ri])
            for dc in range(8):
                T(lambda e, dc=dc, xi=xi, o=o: e.matmul(Q_A[:, 0:384], lhsT=xb[xi][:, dc, 1 + o:1 + o + 128], rhs=Wq[:, dc, :],
                                                        start=(dc == 0), stop=(dc == 7)), r=["Wq", kb], w=["Q_A"])
            X4 = Q_A[:, 0:256].rearrange("p (g h d) -> p g h d", g=4, h=2)
            B4 = lambda t_: t_[:].rearrange("p (g h d) -> p g h d", g=4, h=2)
            V(lambda e, ri=ri: e.tensor_tensor(out=tmpA[:], in0=Q_A[:, 0:256], in1=rA[ri][:], op=ALU.mult), r=["Q_A", "rA%d" % ri], w=["tmpA"])
            V(lambda e, ri=ri: e.tensor_tensor(out=B4(tmpB)[:, :, 0, :], in0=X4[:, :, 1, :], in1=B4(rB[ri])[:, :, 0, :], op=ALU.mult), r=["Q_A", "rB%d" % ri], w=["tmpB"])
            V(lambda e, ri=ri: e.tensor_tensor(out=B4(tmpB)[:, :, 1, :], in0=X4[:, :, 0, :], in1=B4(rB[ri])[:, :, 1, :], op=ALU.mult), r=["Q_A", "rB%d" % ri], w=["tmpB"])
            G(lambda e: e.tensor_tensor(out=QK[:].rearrange("p g d -> p (g d)"), in0=tmpA[:], in1=tmpB[:], op=ALU.add), r=["tmpA", "tmpB"], w=["QK"])
            for h in range(2):
                A(lambda e, h=h, tt=tt: e.copy(out=VA[h][:, tt, 0:64], in_=Q_A[:, 256 + 64 * h:320 + 64 * h]), r=["Q_A"], w=["VA%d.%d" % (h, blk)])
                T(lambda e, h=h: e.transpose(Q_C[0:64, 130:258], QK[:, 2 + h, :], ident[:]), r=["QK", "ident"], w=["Q_C.k"])
                A(lambda e, h=h, tt=tt: e.copy(out=KT[h][0:64, tt * 128:(tt + 1) * 128], in_=Q_C[0:64, 130:258]), r=["Q_C.k"], w=["KT%d.%d" % (h, blk)])
                T(lambda e, h=h: e.transpose(Q_C[0:64, 258:386], QK[:, h, :], ident[:]), r=["QK", "ident"], w=["Q_C.q"])
                V(lambda e: e.tensor_copy(out=qT32[:], in_=Q_C[0:64, 258:386]), r=["Q_C.q"], w=["qT32"])
                if blk > 0:
                    T(lambda e, h=h: e.matmul(Q_C[:, 0:64], lhsT=qT32[:], rhs=kmT[h][:], start=True, stop=True), r=["qT32", "kmT%d" % h], w=["Q_C.g"])
                    V(lambda e, h=h, blk=blk: e.tensor_copy(out=Gt[h][:, 0:blk], in_=Q_C[:, 0:blk]), r=["Q_C.g"], w=["Gt%d" % h])
                V(lambda e, h=h: e.max(out=mx[:], in_=Gt[h][:]), r=["Gt%d" % h], w=["mx"])
                V(lambda e, h=h: e.tensor_scalar(out=negm[:], in0=Gt[h][:], scalar1=mx[:, 2:3], scalar2=NEG, op0=ALU.is_lt, op1=ALU.mult), r=["Gt%d" % h, "mx"], w=["negm"])
                V(lambda e, blk=blk: e.memset(negm[:, blk:blk + 1], 0.0), w=["negm"])
                V(lambda e: e.memset(ssq[:], 0.0), w=["ssq"])
                A(lambda e, h=h: e.activation(out=qjunk[:], in_=QK[:, h, :], func=AF.Square, accum_out=ssq[:]), r=["QK", "ssq"], w=["qjunk", "ssq"])
                A(lambda e: e.activation(out=cpos[:], in_=ssq[:], func=AF.Sqrt, scale=KB_BOUND * KB_BOUND), r=["ssq"], w=["cpos"])
                G(lambda e, h=h: e.tensor_copy(out=Qaug[h][:, 0:64], in_=QK[:, h, :]), r=["QK"], w=["Qaug%d" % h])
                V(lambda e, h=h: e.tensor_scalar(out=Qaug[h][:, 64:128], in0=negm[:], scalar1=cpos[:, 0:1], scalar2=None, op0=ALU.subtract), r=["negm", "cpos"], w=["Qaug%d" % h])
                T(lambda e, h=h: e.transpose(Q_D[:, 0:128], Qaug[h][:], identb[:]), r=["Qaug%d" % h, "identb"], w=["Q_C.d"])
                A(lambda e, h=h, o=o: e.copy(out=QT[h][blk % 2][:, o:o + 128], in_=Q_D[:, 0:128]), r=["Q_C.d"], w=["QT%d.%d" % (h, blk % 2)])
                T(lambda e, h=h: e.matmul(Q_C[0:64, 64:65], lhsT=QK[:, 2 + h, :], rhs=inv256[:], start=True, stop=True), r=["QK", "inv256"], w=["Q_C.m"])
                if half == 0:
                    V(lambda e, h=h, blk=blk: e.tensor_copy(out=kmT[h][:, blk:blk + 1], in_=Q_C[0:64, 64:65]), r=["Q_C.m"], w=["kmT%d" % h])
                else:
                    V(lambda e, h=h, blk=blk: e.tensor_tensor(out=kmT[h][:, blk:blk + 1], in0=Q_C[0:64, 64:65], in1=kmT[h][:, blk:blk + 1], op=ALU.add),
                      r=["Q_C.m", "kmT%d" % h], w=["kmT%d" % h])

    gen_proj(0)
    for blk in range(n_blocks):
        pend = []
        if blk + 1 < n_blocks:
            defer[0] = pend
            gen_proj(blk + 1)
            defer[0] = None
        steps = []
        for h in range(2):
            nkt = 2 * blk + 2
            for kt in range(nkt):
                steps.append((h, kt, nkt))

        def issue_qk(i):
            h, kt, nkt = steps[i]
            j = kt - 2 * blk
            q0 = 128 if j == 1 else 0
            si = (sbase + i) % NSB
            T(lambda e, h=h, kt=kt, q0=q0, si=si, bp=blk % 2: e.matmul(Q_S[si][:, q0:256], lhsT=KT[h][:, kt * 128:(kt + 1) * 128], rhs=QT[h][bp][:, q0:256],
                                                           start=True, stop=True), r=["KT%d.%d" % (h, kt // 2), "QT%d.%d" % (h, blk % 2)], w=["Q_S%d" % si])

        sbase = sidx
        LOOK = NSB - 2
        for i0 in range(min(LOOK, len(steps))):
            issue_qk(i0)
        for i in range(len(steps)):
            h, kt, nkt = steps[i]
            j = kt - 2 * blk
            q0 = 128 if j == 1 else 0
            si = (sbase + i) % NSB
            sk = "Q_S%d" % si
            pk = "PT%d" % si
            ok = "Q_O%d" % h
            if i + LOOK < len(steps):
                issue_qk(i + LOOK)
            A(lambda e, q0=q0, si=si: e.activation(out=PT[si][:, q0:256], in_=Q_S[si][:, q0:256], func=AF.Exp, scale=0.125), r=[sk], w=[pk])
            if j >= 0:
                G(lambda e, q0=q0, si=si, j=j: e.tensor_tensor(out=PT[si][:, j * 128:(j + 1) * 128], in0=PT[si][:, j * 128:(j + 1) * 128], in1=tri[:], op=ALU.mult),
                  r=[pk, "tri"], w=[pk])
            T(lambda e, h=h, kt=kt, q0=q0, si=si, nkt=nkt: e.matmul(Q_O[h][0:65, q0:256], lhsT=VA[h][:, kt, :], rhs=PT[si][:, q0:256],
                                                                    start=(kt == 0), stop=(kt == nkt - 1)), r=["VA%d.%d" % (h, kt // 2), pk], w=[ok])
            if kt == nkt - 1:
                V(lambda e, h=h: e.reciprocal(out=rec[64:65, :], in_=Q_O[h][64:65, 0:256]), r=[ok], w=["rec"])
                T(lambda e, h=h: e.matmul(Q_O[h][0:64, 256:512], lhsT=ones_r[64:65, :], rhs=rec[64:65, :], start=True, stop=True), r=["ones_r", "rec"], w=[ok + ".bc"])
                A(lambda e, h=h: e.copy(out=ysb[:], in_=Q_O[h][0:64, 0:256]), r=[ok], w=["ysb"])
                V(lambda e, h=h: e.tensor_tensor(out=yo[:], in0=ysb[:], in1=Q_O[h][0:64, 256:512], op=ALU.mult), r=["ysb", ok + ".bc"], w=["yo"])
                S.dma("sync", dram["cin"][(blk * 256) // 1024][128 + h * 64:128 + (h + 1) * 64, (blk * 256) % 1024:(blk * 256) % 1024 + 256], yo[:], reads=["yo"])
            if pend:
                drain(pend, -(-len(pend) // max(1, len(steps) - i)))
        drain(pend, len(pend))
        sidx += len(steps)


NTOK = 4096
ALPHA = float(2.0 ** 0.25)
LN_EPS = 1e-5
MOE_NSTG = 3


def _mk(nc, S, es, pfx):
    sbn = [0]

    def sb(shape, dt=F32):
        sbn[0] += 1
        return es.enter_context(nc.sbuf_tensor("%s%d" % (pfx, sbn[0]), shape, dt))

    def ps(shape, dt=F32):
        sbn[0] += 1
        return es.enter_context(nc.psum_tensor("%sp%d" % (pfx, sbn[0]), shape, dt))

    def _bk(r, w):
        b = set(k.split(".")[0] for k in list(r) + list(w) if k.startswith("Q_"))
        return list(w) + ["BANK_" + x for x in b]

    V = lambda fn, r=(), w=(): S.op("vector", fn, r, _bk(r, w))
    A = lambda fn, r=(), w=(): S.op("scalar", fn, r, _bk(r, w))
    G = lambda fn, r=(), w=(): S.op("gpsimd", fn, r, w)
    T = lambda fn, r=(), w=(): S.op("tensor", fn, r, _bk(r, w))
    return sb, ps, V, A, G, T


def layer_norm_tile(V, A, G, t, tk, junk, st, gbc, bbc, gk, bk):
    V(lambda e: e.tensor_reduce(out=st[:, 0:1], in_=t[:], axis=AX.X, op=ALU.add), r=[tk], w=["st0"])
    V(lambda e: e.tensor_scalar(out=st[:, 0:1], in0=st[:, 0:1], scalar1=-1.0 / 1024, scalar2=None, op0=ALU.mult), r=["st0"], w=["st0"])
    V(lambda e: e.tensor_scalar(out=t[:], in0=t[:], scalar1=st[:, 0:1], scalar2=None, op0=ALU.add), r=[tk, "st0"], w=[tk])
    V(lambda e: e.memset(st[:, 1:2], 0.0), w=["st1"])
    A(lambda e: e.activation(out=junk[:], in_=t[:], func=AF.Square, accum_out=st[:, 1:2]), r=[tk, "st1"], w=["junk", "st1"])
    V(lambda e: e.tensor_scalar(out=st[:, 1:2], in0=st[:, 1:2], scalar1=1.0 / 1024, scalar2=LN_EPS, op0=ALU.mult, op1=ALU.add), r=["st1"], w=["st1"])
    A(lambda e: e.activation(out=st[:, 1:2], in_=st[:, 1:2], func=AF.Sqrt), r=["st1"], w=["st1"])
    V(lambda e: e.reciprocal(out=st[:, 2:3], in_=st[:, 1:2]), r=["st1"], w=["st2"])
    V(lambda e: e.scalar_tensor_tensor(out=t[:], in0=t[:], scalar=st[:, 2:3], in1=gbc[:], op0=ALU.mult, op1=ALU.mult), r=[tk, "st2", gk], w=[tk])
    G(lambda e: e.tensor_tensor(out=t[:], in0=t[:], in1=bbc[:], op=ALU.add), r=[tk, bk], w=[tk])


def build_stage_a(nc, S, es, dram, n_groups=NTOK // 512):
    sb, ps, V, A, G, T = _mk(nc, S, es, "sa")
    stg = [sb([128, 2048]) for _ in range(2)]
    Wg = sb([128, 8, 2048], BF16); Wab = sb([128, 4, 1024], BF16); Wrb = sb([128, 4, 1024], BF16); Wo = sb([128, 8, 1024], BF16)
    si = 0
    wg_v = dram["w_gate"].rearrange("(dc p) n -> p dc n", p=128)
    for dc in range(8):
        k = "stg%d" % (si % 2); st_ = stg[si % 2]; si += 1
        S.dma("sync", st_[:], wg_v[:, dc, :], writes=[k])
        V(lambda e, dc=dc, st_=st_: e.tensor_copy(out=Wg[:, dc, :], in_=st_[:]), r=[k], w=["Wg"])
    for (src, dst, key, ndc) in (("w_ab", Wab, "Wab", 4), ("w_rb", Wrb, "Wrb", 4), ("w_out", Wo, "Wo", 8)):
        v = dram[src].rearrange("(dc p) n -> p dc n", p=128)
        for dc in range(ndc):
            k = "stg%d" % (si % 2); st_ = stg[si % 2]; si += 1
            S.dma("sync", st_[:, 0:1024], v[:, dc, :], writes=[k])
            G(lambda e, dc=dc, st_=st_, dst=dst: e.tensor_copy(out=dst[:, dc, :], in_=st_[:, 0:1024]), r=[k], w=[key])
    gbc = sb([128, 1024]); bbc = sb([128, 1024])
    S.dma("sync", gbc[:], dram["ln1g"].partition_broadcast(128), writes=["gbc"])
    S.dma("sync", bbc[:], dram["ln1b"].partition_broadcast(128), writes=["bbc"])
    xs = sb([128, 8, 512]); xb = sb([128, 8, 512], BF16)
    ys = [sb([128, 4, 512]) for _ in range(2)]; yb = sb([128, 8, 512], BF16)
    ytmp = sb([128, 512])
    mq = sb([128, 4])
    S.dma("sync", mq[:], dram["maskq"], writes=["mq"])
    ident = sb([128, 128])
    S.dma("sync", ident[:], dram["c_ident"], writes=["ident"])
    hTs = sb([128, 8, 128])
    Q_t = [ps([128, 512]) for _ in range(2)]
    mixT = sb([128, 8, 512], BF16)
    sa = sb([128, 512]); sr = sb([128, 512]); m1 = sb([128, 512]); m2 = sb([128, 512])
    xt = sb([128, 1024]); ht = sb([128, 1024]); junk = sb([128, 1024]); st = sb([128, 4])
    Q_ga = ps([128, 512]); Q_gr = ps([128, 512]); Q_ba = ps([128, 512]); Q_br = ps([128, 512])
    Q_o = [ps([128, 512]) for _ in range(2)]
    xT = dram["xT2"].rearrange("(dc p) n -> p dc n", p=128)
    cout = dram["cout"]
    for g in range(n_groups):
        c0 = g * 512
        for dc in range(8):
            S.dma("sync", xs[:, dc, :], xT[:, dc, c0:c0 + 512], writes=["xs"])
        for dc in range(8):
            G(lambda e, dc=dc: e.tensor_copy(out=xb[:, dc, :], in_=xs[:, dc, :]), r=["xs"], w=["xb"])
        for dc in range(8):
            r0 = (dc % 4) * 256 + (128 if dc < 4 else 0)
            yi = dc % 2
            yk = "ys%d" % yi
            for q in range(4):
                kch = q * 4 + c0 // 1024
                S.dma("sync", ys[yi][:, q, :], cout[kch][r0:r0 + 128, c0 % 1024:c0 % 1024 + 512], writes=[yk])
            V(lambda e, yi=yi: e.tensor_scalar(out=ytmp[:], in0=ys[yi][:, 0, :], scalar1=mq[:, 0:1], scalar2=None, op0=ALU.mult), r=[yk, "mq"], w=["ytmp"])
            for q in (1, 2):
                V(lambda e, yi=yi, q=q: e.scalar_tensor_tensor(out=ytmp[:], in0=ys[yi][:, q, :], scalar=mq[:, q:q + 1], in1=ytmp[:], op0=ALU.mult, op1=ALU.add),
                  r=[yk, "mq", "ytmp"], w=["ytmp"])
            V(lambda e, yi=yi, dc=dc: e.scalar_tensor_tensor(out=yb[:, dc, :], in0=ys[yi][:, 3, :], scalar=mq[:, 3:4], in1=ytmp[:], op0=ALU.mult, op1=ALU.add),
              r=[yk, "mq", "ytmp"], w=["yb"])
        for j in range(8):
            js = slice(j * 128, (j + 1) * 128)
            for dc in range(8):
                T(lambda e, dc=dc, js=js: e.matmul(Q_ga[:], lhsT=Wg[:, dc, js], rhs=xb[:, dc, :], start=(dc == 0), stop=(dc == 7)), r=["Wg", "xb"], w=["Q_ga"])
            for dc in range(8):
                T(lambda e, dc=dc, j=j: e.matmul(Q_gr[:], lhsT=Wg[:, dc, 1024 + j * 128:1024 + (j + 1) * 128], rhs=xb[:, dc, :], start=(dc == 0), stop=(dc == 7)),
                  r=["Wg", "xb"], w=["Q_gr"])
            for dc in range(4):
                T(lambda e, dc=dc, js=js: e.matmul(Q_ba[:], lhsT=Wab[:, dc, js], rhs=yb[:, dc, :], start=(dc == 0), stop=(dc == 3)), r=["Wab", "yb"], w=["Q_ba"])
            for dc in range(4):
                T(lambda e, dc=dc, js=js: e.matmul(Q_br[:], lhsT=Wrb[:, dc, js], rhs=yb[:, 4 + dc, :], start=(dc == 0), stop=(dc == 3)), r=["Wrb", "yb"], w=["Q_br"])
            A(lambda e: e.activation(out=sa[:], in_=Q_ga[:], func=AF.Sigmoid), r=["Q_ga"], w=["sa"])
            A(lambda e: e.activation(out=sr[:], in_=Q_gr[:], func=AF.Sigmoid), r=["Q_gr"], w=["sr"])
            V(lambda e: e.tensor_tensor(out=m1[:], in0=sa[:], in1=Q_ba[:], op=ALU.mult), r=["sa", "Q_ba"], w=["m1"])
            V(lambda e: e.tensor_tensor(out=m2[:], in0=sr[:], in1=Q_br[:], op=ALU.mult), r=["sr", "Q_br"], w=["m2"])
            G(lambda e, j=j: e.tensor_tensor(out=mixT[:, j, :], in0=m1[:], in1=m2[:], op=ALU.add), r=["m1", "m2"], w=["mixT"])
        for tt in range(4):
            tok0 = c0 + tt * 128
            S.dma("sync", xt[:], dram["x2"][tok0:tok0 + 128, :], writes=["xt"])
            for hf in range(2):
                for j in range(8):
                    T(lambda e, j=j, hf=hf, tt=tt: e.matmul(Q_o[hf][:], lhsT=mixT[:, j, tt * 128:(tt + 1) * 128], rhs=Wo[:, j, hf * 512:(hf + 1) * 512],
                                                            start=(j == 0), stop=(j == 7)), r=["mixT", "Wo"], w=["Q_o%d" % hf])
                V(lambda e, hf=hf: e.scalar_tensor_tensor(out=ht[:, hf * 512:(hf + 1) * 512], in0=xt[:, hf * 512:(hf + 1) * 512], scalar=ALPHA, in1=Q_o[hf][:],
                                                          op0=ALU.mult, op1=ALU.add), r=["xt", "Q_o%d" % hf], w=["ht"])
            layer_norm_tile(V, A, G, ht, "ht", junk, st, gbc, bbc, "gbc", "bbc")
            S.dma("sync", dram["hS"][tok0:tok0 + 128, :], ht[:], reads=["ht"])
            for hf in range(2):
                for j4 in range(4):
                    j = hf * 4 + j4
                    T(lambda e, j=j, j4=j4, hf=hf: e.transpose(Q_t[hf][:, j4 * 128:(j4 + 1) * 128], ht[:, j * 128:(j + 1) * 128], ident[:]), r=["ht", "ident"], w=["Q_t%d" % hf])
                A(lambda e, hf=hf: e.copy(out=hTs[:, hf * 4:(hf + 1) * 4, :], in_=Q_t[hf][:].rearrange("p (a n) -> p a n", a=4)), r=["Q_t%d" % hf], w=["hTs"])
            S.dma("sync", dram["hTS"].rearrange("(dc p) n -> p dc n", p=128)[:, :, tok0:tok0 + 128], hTs[:], reads=["hTs"])


def build_moe(nc, S, es, dram, n_quarters=4, n_experts=32):
    sb, ps, V, A, G, T = _mk(nc, S, es, "mo")
    QT_ = 1024
    NSTG = MOE_NSTG
    ident = sb([128, 128])
    S.dma("sync", ident[:], dram["c_ident"], writes=["ident"])
    Wr = sb([128, 8, 32])
    S.dma("sync", Wr[:], dram["w_r"].rearrange("(dc p) n -> p dc n", p=128), writes=["Wr"])
    brb = sb([128, 32])
    S.dma("sync", brb[:], dram["b_r"].partition_broadcast(128), writes=["brb"])
    b1T = sb([128, 32 * 16])
    S.dma("sync", b1T[:], dram["b1T"], writes=["b1T"])
    b1Tp = sb([128, 32 * 16])
    V(lambda e: e.tensor_scalar(out=b1Tp[:], in0=b1T[:], scalar1=1.0, scalar2=None, op0=ALU.add), r=["b1T"], w=["b1Tp"])
    b2s = sb([32, 1024])
    S.dma("sync", b2s[:], dram["b2"], writes=["b2s"])
    gbc = sb([128, 1024]); bbc = sb([128, 1024])
    S.dma("sync", gbc[:], dram["ln2g"].partition_broadcast(128), writes=["gbc"])
    S.dma("sync", bbc[:], dram["ln2b"].partition_broadcast(128), writes=["bbc"])
    hb = sb([128, 8, QT_], BF16)
    acc = sb([128, 8, 1024])
    GW = sb([128, 8, 32])
    lg = sb([128, 32]); mx = sb([128, 8]); nm0 = sb([128, 1]); ex = sb([128, 32]); msk = sb([128, 32]); den = sb([128, 1]); gT = sb([32, 128])
    htk = sb([128, 1024]); junk = sb([128, 1024]); st = sb([128, 4])
    stg = [sb([128, 2048]) for _ in range(NSTG)]
    w1b = sb([128, 8, 2048], BF16); w2b = sb([128, 8, 1024], BF16)
    actT = sb([128, 8, QT_], BF16)
    gg = [sb([128, 512]) for _ in range(2)]; sg = [sb([128, 512]) for _ in range(2)]; ll = [sb([128, 512]) for _ in range(2)]
    Q_r = ps([128, 512]); Q_g = [ps([128, 512]) for _ in range(2)]; Q_l = [ps([128, 512]) for _ in range(2)]; Q_y = [ps([128, 512]) for _ in range(2)]
    hT = dram["hTS"].rearrange("(dc p) n -> p dc n", p=128)
    si = 0
    fci = 0
    for qt in range(n_quarters):
        t0 = qt * QT_
        for g2 in range(QT_ // 512):
            hsv = [stg[0][:].rearrange("p (a n) -> p a n", a=4), stg[1][:].rearrange("p (a n) -> p a n", a=4)]
            hs_dc = lambda dc: hsv[dc // 4][:, dc % 4, :]
            for dc in range(8):
                S.dma("sync", hs_dc(dc), hT[:, dc, t0 + g2 * 512:t0 + (g2 + 1) * 512], writes=["stg%d" % (dc // 4)])
            for t4 in range(4):
                tt = g2 * 4 + t4
                ts_ = slice(t4 * 128, (t4 + 1) * 128)
                for dc in range(8):
                    T(lambda e, dc=dc, ts_=ts_: e.matmul(Q_r[:, 0:32], lhsT=hs_dc(dc)[:, ts_], rhs=Wr[:, dc, :], start=(dc == 0), stop=(dc == 7)),
                      r=["stg0", "stg1", "Wr"], w=["Q_r.l"])
                V(lambda e: e.tensor_tensor(out=lg[:], in0=Q_r[:, 0:32], in1=brb[:], op=ALU.add), r=["Q_r.l", "brb"], w=["lg"])
                V(lambda e: e.max(out=mx[:], in_=lg[:]), r=["lg"], w=["mx"])
                V(lambda e: e.tensor_scalar(out=nm0[:], in0=mx[:, 0:1], scalar1=-1.0, scalar2=None, op0=ALU.mult), r=["mx"], w=["nm0"])
                A(lambda e: e.activation(out=ex[:], in_=lg[:], func=AF.Exp, bias=nm0[:, 0:1], scale=1.0), r=["lg", "nm0"], w=["ex"])
                V(lambda e: e.tensor_scalar(out=msk[:], in0=lg[:], scalar1=mx[:, 3:4], scalar2=None, op0=ALU.is_ge), r=["lg", "mx"], w=["msk"])
                V(lambda e: e.tensor_tensor(out=ex[:], in0=ex[:], in1=msk[:], op=ALU.mult), r=["ex", "msk"], w=["ex"])
                V(lambda e: e.tensor_reduce(out=den[:], in_=ex[:], axis=AX.X, op=ALU.add), r=["ex"], w=["den"])
                V(lambda e: e.reciprocal(out=den[:], in_=den[:]), r=["den"], w=["den"])
                V(lambda e, tt=tt: e.tensor_scalar(out=GW[:, tt, :], in0=ex[:], scalar1=den[:, 0:1], scalar2=None, op0=ALU.mult), r=["ex", "den"], w=["GW"])
                T(lambda e, tt=tt: e.transpose(Q_r[0:32, 128:256], GW[:, tt, :], ident[:]), r=["GW", "ident"], w=["Q_r.t"])
                V(lambda e: e.tensor_copy(out=gT[:], in_=Q_r[0:32, 128:256]), r=["Q_r.t"], w=["gT"])
                S.dma("sync", htk[:], dram["hS"][t0 + tt * 128:t0 + (tt + 1) * 128, :], writes=["htk"])
                for hf in range(2):
                    T(lambda e, hf=hf: e.matmul(Q_y[hf][:], lhsT=gT[:], rhs=b2s[:, hf * 512:(hf + 1) * 512], start=True, stop=True), r=["gT", "b2s"], w=["Q_y%d" % hf])
                    V(lambda e, hf=hf, tt=tt: e.scalar_tensor_tensor(out=acc[:, tt, hf * 512:(hf + 1) * 512], in0=htk[:, hf * 512:(hf + 1) * 512], scalar=ALPHA, in1=Q_y[hf][:],
                                                                     op0=ALU.mult, op1=ALU.add), r=["htk", "Q_y%d" % hf], w=["acc%d" % tt])
            for dc in range(8):
                if dc % 2 == 0:
                    V(lambda e, dc=dc, g2=g2: e.tensor_copy(out=hb[:, dc, g2 * 512:(g2 + 1) * 512], in_=hs_dc(dc)), r=["stg%d" % (dc // 4)], w=["hb"])
                else:
                    A(lambda e, dc=dc, g2=g2: e.copy(out=hb[:, dc, g2 * 512:(g2 + 1) * 512], in_=hs_dc(dc)), r=["stg%d" % (dc // 4)], w=["hb"])
        for ex_i in range(n_experts):
            w1v = dram["w1"][ex_i].rearrange("(dc p) n -> p dc n", p=128)
            w2v = dram["w2"][ex_i].rearrange("(dc p) n -> p dc n", p=128)
            for dc in range(8):
                k = "stg%d" % (si % NSTG); st_ = stg[si % NSTG]; si += 1
                S.dma("sync", st_[:], w1v[:, dc, :], writes=[k])
                A(lambda e, dc=dc, st_=st_: e.copy(out=w1b[:, dc, :], in_=st_[:]), r=[k], w=["w1b"])
            for d2 in range(4):
                k = "stg%d" % (si % NSTG); st_ = stg[si % NSTG]; si += 1
                S.dma("sync", st_[:].rearrange("p (a n) -> p a n", a=2), w2v[:, 2 * d2:2 * d2 + 2, :], writes=[k])
                if d2 % 2 == 0:
                    V(lambda e, d2=d2, st_=st_: e.tensor_copy(out=w2b[:, 2 * d2:2 * d2 + 2, :], in_=st_[:].rearrange("p (a n) -> p a n", a=2)), r=[k], w=["w2b"])
                else:
                    A(lambda e, d2=d2, st_=st_: e.copy(out=w2b[:, 2 * d2:2 * d2 + 2, :], in_=st_[:].rearrange("p (a n) -> p a n", a=2)), r=[k], w=["w2b"])
            for tg in range(QT_ // 512):
                gs = slice(tg * 512, (tg + 1) * 512)
                for fc in range(8):
                    bi = fci % 2
                    fci += 1
                    kg = "Q_g%d" % bi; kl = "Q_l%d" % bi
                    for dc in range(8):
                        T(lambda e, dc=dc, fc=fc, gs=gs, bi=bi: e.matmul(Q_g[bi][:], lhsT=w1b[:, dc, fc * 128:(fc + 1) * 128], rhs=hb[:, dc, gs], start=(dc == 0), stop=(dc == 7)),
                          r=["w1b", "hb"], w=[kg])
                    for dc in range(8):
                        T(lambda e, dc=dc, fc=fc, gs=gs, bi=bi: e.matmul(Q_l[bi][:], lhsT=w1b[:, dc, 1024 + fc * 128:1024 + (fc + 1) * 128], rhs=hb[:, dc, gs], start=(dc == 0), stop=(dc == 7)),
                          r=["w1b", "hb"], w=[kl])
                    bg = b1T[:, ex_i * 16 + fc:ex_i * 16 + fc + 1]
                    bl = b1Tp[:, ex_i * 16 + 8 + fc:ex_i * 16 + 8 + fc + 1]
                    V(lambda e, bg=bg, bi=bi: e.tensor_scalar(out=gg[bi][:], in0=Q_g[bi][:], scalar1=bg, scalar2=7.0, op0=ALU.add, op1=ALU.min), r=[kg, "b1T"], w=["gg%d" % bi])
                    A(lambda e, bi=bi: e.activation(out=sg[bi][:], in_=gg[bi][:], func=AF.Sigmoid, scale=1.702), r=["gg%d" % bi], w=["sg%d" % bi])
                    V(lambda e, bl=bl, bi=bi: e.tensor_scalar(out=ll[bi][:], in0=Q_l[bi][:], scalar1=bl, scalar2=8.0, op0=ALU.add, op1=ALU.min), r=[kl, "b1Tp"], w=["ll%d" % bi])
                    G(lambda e, bi=bi: e.tensor_tensor(out=gg[bi][:], in0=gg[bi][:], in1=sg[bi][:], op=ALU.mult), r=["gg%d" % bi, "sg%d" % bi], w=["gg%d" % bi])
                    V(lambda e, fc=fc, gs=gs, bi=bi: e.scalar_tensor_tensor(out=actT[:, fc, gs], in0=ll[bi][:], scalar=-6.0, in1=gg[bi][:], op0=ALU.max, op1=ALU.mult),
                      r=["gg%d" % bi, "ll%d" % bi], w=["actT%d" % tg])
            for tile_i in range(QT_ // 128):
                tg = tile_i // 4
                for hf in range(2):
                    for fc in range(8):
                        T(lambda e, fc=fc, tile_i=tile_i, hf=hf: e.matmul(Q_y[hf][:], lhsT=actT[:, fc, tile_i * 128:(tile_i + 1) * 128], rhs=w2b[:, fc, hf * 512:(hf + 1) * 512],
                                                                          start=(fc == 0), stop=(fc == 7)), r=["actT%d" % tg, "w2b"], w=["Q_y%d" % hf])
                    V(lambda e, hf=hf, tile_i=tile_i, ex_i=ex_i: e.scalar_tensor_tensor(
                        out=acc[:, tile_i, hf * 512:(hf + 1) * 512], in0=Q_y[hf][:], scalar=GW[:, tile_i, ex_i:ex_i + 1],
                        in1=acc[:, tile_i, hf * 512:(hf + 1) * 512], op0=ALU.mult, op1=ALU.add), r=["Q_y%d" % hf, "GW", "acc%d" % tile_i], w=["acc%d" % tile_i])
        for tt in range(8):
            V(lambda e, tt=tt: e.tensor_copy(out=htk[:], in_=acc[:, tt, :]), r=["acc%d" % tt], w=["htk"])
            layer_norm_tile(V, A, G, htk, "htk", junk, st, gbc, bbc, "gbc", "bbc")
            S.dma("sync", dram["out"][t0 + tt * 128:t0 + (tt + 1) * 128, :], htk[:], reads=["htk"])


def _dt(v):
    return F32 if v.dtype == np.float32 else BF16


_SZ = {}


def build_all(nc, S, dram, cc_sem):
    with contextlib.ExitStack() as e1:
        build_rwkv(nc, S, e1, dram, **_SZ.get('rwkv', {}))
        S.barrier()
        S.flush()
    with contextlib.ExitStack() as e2:
        build_attn(nc, S, e2, dram, **_SZ.get('attn', {}))
        S.barrier()
        S.flush()
    if not _SZ.get("nocc"):
        for k in range(16):
            S.async_op("gpsimd", lambda e, k=k: e.collective_compute("AllGather", ALU.bypass, replica_groups=[[0, 1, 2, 3], [4, 5, 6, 7]],
                                                                 ins=[dram["cin"][k]], outs=[dram["cout"][k]]), cc_sem)
    S.barrier()
    S.flush()
    with contextlib.ExitStack() as e3:
        build_stage_a(nc, S, e3, dram, **_SZ.get('sa', {}))
        S.barrier()
        S.flush()
    with contextlib.ExitStack() as e4:
        build_moe(nc, S, e4, dram, **_SZ.get('moe', {}))
        S.emit()


def kernel(x, w_in, mu_shift, w0, w_decay_up, a0, w_aaa_up, w_gate_up, k_k, k_a, r_k,
           lnx_g, lnx_b, w_attn_br, w_rwkv_br, w_out, ln1_g, ln1_b,
           w_router, b_router, w1, b1, w2, b2, ln2_g, ln2_b):
    f = lambda a: np.ascontiguousarray(np.asarray(a, dtype=np.float32))
    x = f(x); w_in = f(w_in)[0]; mu = f(mu_shift)[0]
    B, S_, D = x.shape
    NC = 8
    TQ = S_ // 4
    xTp = []
    for b in range(B):
        t = np.zeros((D, S_ + 1), np.float32)
        t[:, 1:] = x[b].T
        xTp.append(t)
    rc = rwkv_consts()
    ac = attn_consts()
    OQ = 1824
    row = lambda v: np.ascontiguousarray(v[None, :])
    w_gate = np.ascontiguousarray(w_in[:, OQ + 1536:OQ + 1536 + 2048])
    w1p = f(w1)[0][:_SZ.get("ne", 32)]
    w1p = np.ascontiguousarray(np.concatenate([w1p[:, :, 0::2], w1p[:, :, 1::2]], 2))
    b1p = f(b1)[0]
    b1p = np.concatenate([b1p[:, 0::2], b1p[:, 1::2]], 1)
    b1T = np.ascontiguousarray(b1p.reshape(32, 16, 128).transpose(2, 0, 1).reshape(128, 512))
    shared = {
        "w_lo": np.ascontiguousarray(w_in[:, 1536:1824]), "mu_lo": row(mu[1536:1824]),
        "w_gate": w_gate, "w_ab": f(w_attn_br)[0], "w_rb": f(w_rwkv_br)[0], "w_out": f(w_out)[0],
        "ln1g": row(f(ln1_g)[0]), "ln1b": row(f(ln1_b)[0]),
        "w_r": f(w_router)[0], "b_r": row(f(b_router)[0]), "w1": w1p, "b1T": b1T, "w2": f(w2)[0][:_SZ.get("ne", 32)], "b2": f(b2)[0],
        "ln2g": row(f(ln2_g)[0]), "ln2b": row(f(ln2_b)[0]),
    }
    shared.update(rc)
    shared.update(ac)
    in_maps = []
    for c in range(NC):
        b, hp = c // 4, c % 4
        tq = hp
        hc = slice(128 * hp, 128 * hp + 128)
        ts = slice(tq * TQ, (tq + 1) * TQ)
        mq = np.zeros((128, 4), np.float32); mq[:, tq] = 1.0
        m = {
            "xT": xTp[b],
            "w_rkv": np.ascontiguousarray(np.concatenate([w_in[:, 0:512][:, hc], w_in[:, 512:1024][:, hc], w_in[:, 1024:1536][:, hc]], 1)),
            "mu_rkv": row(np.concatenate([mu[0:512][hc], mu[512:1024][hc], mu[1024:1536][hc]])),
            "wdu": np.ascontiguousarray(f(w_decay_up)[0][:, hc]), "wau": np.ascontiguousarray(f(w_aaa_up)[0][:, hc]),
            "wgu": np.ascontiguousarray(f(w_gate_up)[0][:, hc]),
            "w0": row(f(w0)[0][hc]), "a0": row(f(a0)[0][hc]), "kk": row(f(k_k)[0][hc]), "ka": row(f(k_a)[0][hc]),
            "rk": row(f(r_k)[0].reshape(-1)[hc]), "lng": row(f(lnx_g)[0][hc]), "lnb": row(f(lnx_b)[0][hc]),
            "w_qkv": np.ascontiguousarray(np.concatenate([w_in[:, OQ:OQ + 512][:, hc], w_in[:, OQ + 512:OQ + 1024][:, hc],
                                                          w_in[:, OQ + 1024:OQ + 1536][:, hc]], 1)),
            "xT2": np.ascontiguousarray(x[b, ts].T), "x2": np.ascontiguousarray(x[b, ts]),
            "maskq": mq,
        }
        m.update(shared)
        in_maps.append(m)
    nc = bass.Bass("TRN2", target_bir_lowering=False)
    dram = {}
    for k, v in in_maps[0].items():
        dram[k] = nc.dram_tensor(k, list(v.shape), _dt(v), kind="ExternalInput").ap()
    dram["out"] = nc.dram_tensor("out", [TQ, D], F32, kind="ExternalOutput").ap()
    dram["cin"] = [nc.dram_tensor("cin%d" % k, [256, 1024], F32, kind="Internal").ap() for k in range(16)]
    dram["cout"] = [nc.dram_tensor("cout%d" % k, [1024, 1024], F32, kind="Internal").ap() for k in range(16)]
    dram["hS"] = nc.dram_tensor("hS", [TQ, D], F32, kind="Internal").ap()
    dram["hTS"] = nc.dram_tensor("hTS", [D, TQ], F32, kind="Internal").ap()
    with contextlib.ExitStack() as es:
        S = Sched(nc, es)
        cc_sem = es.enter_context(nc.semaphore("cc_sem"))
        build_all(nc, S, dram, cc_sem)
    res = run_bass_kernel_spmd(nc, in_maps, core_ids=list(range(NC)))
    out = np.zeros((B, S_, D), np.float32)
    for c in range(NC):
        b, tq = c // 4, c % 4
        out[b, tq * TQ:(tq + 1) * TQ] = res.results[c]["out"]
    return out
```

```python
import contextlib
import numpy as np
import ml_dtypes
import concourse.bass as bass
import concourse.mybir as mybir
from concourse.bass_utils import run_bass_kernel_spmd

F32 = mybir.dt.float32
BF16 = mybir.dt.bfloat16
AF = mybir.ActivationFunctionType
ALU = mybir.AluOpType
AX = mybir.AxisListType


ENGS = ("sync", "tensor", "vector", "scalar", "gpsimd")


class Op:
    __slots__ = ("eng", "fn", "deps", "marked", "dma", "sem", "val", "idx", "inc")

    def __init__(self, eng, fn, dma):
        self.eng = eng
        self.fn = fn
        self.deps = []
        self.marked = False
        self.dma = dma
        self.sem = None
        self.val = 0
        self.inc = 16


class Sched:
    def __init__(self, nc, es, n_dma_sems=12, same_engine_sync=True):
        self.nc = nc
        self.es = es
        self.q = {e: [] for e in ENGS}
        self.last_w = {}
        self.readers = {}
        self.same_engine_sync = same_engine_sync
        self.fence = []
        self.esem = {e: es.enter_context(nc.semaphore("s_" + e)) for e in ENGS if e != "sync"}
        self.dsem = {}
        self.dcnt = {}
        self.dlast = {}
        self.dnext = {}
        for e in ("sync", "scalar", "gpsimd"):
            self.dsem[e] = [es.enter_context(nc.semaphore("d_%s_%d" % (e, i))) for i in range(n_dma_sems)]
            self.dcnt[e] = [0] * n_dma_sems
            self.dlast[e] = [None] * n_dma_sems
            self.dnext[e] = 0

    def _add(self, op, reads, writes):
        deps = []
        for k in reads:
            w = self.last_w.get(k)
            if w is not None:
                deps.append(w)
        for k in writes:
            w = self.last_w.get(k)
            if w is not None:
                deps.append(w)
            deps.extend(self.readers.get(k, ()))
        for k in writes:
            self.last_w[k] = op
            self.readers[k] = []
        for k in reads:
            self.readers.setdefault(k, []).append(op)
        for d in deps:
            if d is op:
                continue
            if (not d.dma) and d.eng == op.eng and (not op.dma):
                if d.eng == "tensor" or not self.same_engine_sync:
                    continue
            op.deps.append(d)
        op.deps.extend(self.fence)
        self.q[op.eng].append(op)
        return op

    def barrier(self):
        f = []
        for e in ENGS:
            if self.q[e]:
                f.append(self.q[e][-1])
        for e in ("sync", "scalar", "gpsimd"):
            for o in self.dlast[e]:
                if o is not None:
                    f.append(o)
        f.extend(getattr(self, "async_ops", []))
        for o in f:
            o.marked = True
        self.fence = f
        self.last_w = {}
        self.readers = {}

    def async_op(self, eng, fn, sem, reads=(), writes=()):
        op = Op(eng, fn, True)
        if not hasattr(self, "async_ops"):
            self.async_ops = []
        op.sem = sem
        op.val = len(self.async_ops) + 1
        op.inc = 1
        self.async_ops.append(op)
        return self._add(op, reads, writes)

    def op(self, eng, fn, reads=(), writes=()):
        return self._add(Op(eng, fn, False), reads, writes)

    def dma(self, eng, out, in_, reads=(), writes=(), **kw):
        op = Op(eng, None, True)
        i = self.dnext[eng]
        self.dnext[eng] = (i + 1) % len(self.dsem[eng])
        self.dcnt[eng][i] += 1
        op.sem = self.dsem[eng][i]
        op.val = 16 * self.dcnt[eng][i]
        prev = self.dlast[eng][i]
        if prev is not None:
            op.deps.append(prev)
        self.dlast[eng][i] = op
        op.fn = lambda e, out=out, in_=in_, kw=kw: e.dma_start(out=out, in_=in_, **kw)
        return self._add(op, reads, writes)

    def flush(self):
        nc = self.nc
        if not hasattr(self, "_pos"):
            self._pos = {e: 0 for e in ENGS}
            self._cnt = {e: 0 for e in ENGS}
            self._waited = {e: {} for e in ENGS}
        pend = {e: self.q[e][self._pos[e]:] for e in ENGS}
        for e in ENGS:
            for op in pend[e]:
                for d in op.deps:
                    d.marked = True
        for e in ENGS:
            c = self._cnt[e]
            for op in pend[e]:
                if (not op.dma) and op.marked and op.sem is None:
                    c += 1
                    op.sem = self.esem.get(e)
                    op.val = c
            self._cnt[e] = c
            self._pos[e] = len(self.q[e])

        def run(eng, e):
            waited = self._waited[e]
            for op in pend[e]:
                need = {}
                for d in op.deps:
                    if d.sem is None:
                        continue
                    k = id(d.sem)
                    if waited.get(k, 0) >= d.val:
                        continue
                    if k not in need or need[k][1] < d.val:
                        need[k] = (d.sem, d.val)
                for k, (s, v) in need.items():
                    eng.wait_ge(s, v)
                    waited[k] = v
                if op.fn is None:
                    continue
                ins = op.fn(eng)
                if op.dma:
                    ins.then_inc(op.sem, op.inc)
                elif op.marked:
                    ins.then_inc(op.sem, 1)

        with nc.Block() as block:
            @block.sync
            def _(eng):
                run(eng, "sync")

            @block.tensor
            def _(eng):
                run(eng, "tensor")

            @block.vector
            def _(eng):
                run(eng, "vector")

            @block.scalar
            def _(eng):
                run(eng, "scalar")

            @block.gpsimd
            def _(eng):
                run(eng, "gpsimd")

    def emit(self, final_wait_engine="sync"):
        self.barrier()
        op = Op(final_wait_engine, None, False)
        op.deps = list(self.fence)
        self.q[final_wait_engine].append(op)
        self.flush()


S_TOK = 16384
XB = 512
C = 128
GN_EPS = 64e-5
C0 = -float(np.exp(-0.5))


def rwkv_consts():
    i = np.arange(128)
    m1 = (i[:, None] <= i[None, :]).astype(np.float32)
    m2 = (i[:, None] > i[None, :]).astype(np.float32)
    mus = (i[:, None] < i[None, :]).astype(np.float32)
    mls = mus.T.copy()
    ident = np.eye(128, dtype=np.float32)
    mask4 = np.stack([mus, m1, mus, m1], axis=1)
    return {"c_m1": m1, "c_m2": m2, "c_mus": mus, "c_mls": mls, "c_ident": ident,
            "c_mask4": np.ascontiguousarray(mask4)}


def build_rwkv(nc, S, es, dram, n_chunks=S_TOK // C):
    sbn = [0]

    def sb(shape, dt=F32, name=None):
        sbn[0] += 1
        return es.enter_context(nc.sbuf_tensor(name or ("rw%d" % sbn[0]), shape, dt))

    def ps(shape, dt=F32, name=None):
        sbn[0] += 1
        return es.enter_context(nc.psum_tensor(name or ("rp%d" % sbn[0]), shape, dt))

    BANKS = {"P_RKV": 0, "P_Z": 2, "P_T": 4, "P_L1a": 5, "P_L1b": 1, "P_LDa": 6, "P_LDb": 3,
             "P_G": 7, "P_U": 7, "P_H": 7, "P_Y": 7, "P_OT": 7}

    def _bk(r, w):
        b = set()
        for k in list(r) + list(w):
            if k.startswith("P_"):
                for pfx, bn in BANKS.items():
                    if k.startswith(pfx):
                        b.add(bn)
        return list(w) + ["BANK%d" % x for x in b]

    defer = [None]

    def _rec(eng, fn, r, w):
        if defer[0] is None:
            S.op(eng, fn, r, w)
        else:
            defer[0].append((eng, fn, r, w))

    def DMA(out, in_, reads=(), writes=()):
        if defer[0] is None:
            S.dma("sync", out, in_, reads=reads, writes=writes)
        else:
            defer[0].append(("dma", (out, in_), reads, writes))

    def drain(lst, n):
        for _ in range(min(n, len(lst))):
            eng, fn, r, w = lst.pop(0)
            if eng == "dma":
                S.dma("sync", fn[0], fn[1], reads=r, writes=w)
            else:
                S.op(eng, fn, r, w)

    V = lambda fn, r=(), w=(): _rec("vector", fn, r, _bk(r, w))
    A = lambda fn, r=(), w=(): _rec("scalar", fn, r, _bk(r, w))
    G = lambda fn, r=(), w=(): _rec("gpsimd", fn, r, list(w))
    T = lambda fn, r=(), w=(): _rec("tensor", fn, r, _bk(r, w))

    cst = {}
    for nm in ("c_m1", "c_m2", "c_mus", "c_mls", "c_ident"):
        t = sb([128, 128]); cst[nm] = t
        S.dma("sync", t[:], dram[nm], writes=[nm])
    mask4 = sb([128, 4, 128])
    S.dma("sync", mask4[:], dram["c_mask4"], writes=["mask4"])
    ones_col = sb([128, 1])
    V(lambda e: e.memset(ones_col[:], 1.0), w=["ones_col"])
    bc = {}
    for nm in ("w0", "a0", "kk", "ka", "rk", "lng", "lnb"):
        t = sb([128, 128]); bc[nm] = t
        S.dma("sync", t[:], dram[nm].partition_broadcast(128), writes=["bc_" + nm])
    NW = 384 + 288
    wst = sb([128, 8, NW])
    S.dma("sync", wst[:, :, 0:384], dram["w_rkv"].rearrange("(dc p) n -> p dc n", p=128), writes=["wst"])
    S.dma("sync", wst[:, :, 384:NW], dram["w_lo"].rearrange("(dc p) n -> p dc n", p=128), writes=["wst"])
    mub = sb([128, NW])
    S.dma("sync", mub[:, 0:384], dram["mu_rkv"].partition_broadcast(128), writes=["mub"])
    S.dma("sync", mub[:, 384:NW], dram["mu_lo"].partition_broadcast(128), writes=["mub"])
    omu = sb([128, NW])
    V(lambda e: e.tensor_scalar(out=omu[:], in0=mub[:], scalar1=-1.0, scalar2=1.0, op0=ALU.mult, op1=ALU.add), r=["mub"], w=["omu"])
    Wa = sb([128, 8, NW], BF16); Wb = sb([128, 8, NW], BF16)
    for dc in range(8):
        V(lambda e, dc=dc: e.tensor_tensor(out=Wa[:, dc, :], in0=wst[:, dc, :], in1=omu[:], op=ALU.mult), r=["wst", "omu"], w=["Wa"])
        G(lambda e, dc=dc: e.tensor_tensor(out=Wb[:, dc, :], in0=wst[:, dc, :], in1=mub[:], op=ALU.mult), r=["wst", "mub"], w=["Wb"])
    wdu_f = sb([64, 128]); wau_f = sb([64, 128]); wgu0_f = sb([128, 128]); wgu1_f = sb([32, 128])
    S.dma("sync", wdu_f[:], dram["wdu"], writes=["wdu_f"])
    S.dma("sync", wau_f[:], dram["wau"], writes=["wau_f"])
    S.dma("sync", wgu0_f[:], dram["wgu"][0:128, :], writes=["wgu0_f"])
    S.dma("sync", wgu1_f[:], dram["wgu"][128:160, :], writes=["wgu1_f"])
    wdu = sb([64, 128], BF16); wau = sb([64, 128], BF16); wgu0 = sb([128, 128], BF16); wgu1 = sb([32, 128], BF16)
    V(lambda e: e.tensor_copy(out=wdu[:], in_=wdu_f[:]), r=["wdu_f"], w=["wdu"])
    V(lambda e: e.tensor_copy(out=wau[:], in_=wau_f[:]), r=["wau_f"], w=["wau"])
    V(lambda e: e.tensor_copy(out=wgu0[:], in_=wgu0_f[:]), r=["wgu0_f"], w=["wgu0"])
    V(lambda e: e.tensor_copy(out=wgu1[:], in_=wgu1_f[:]), r=["wgu1_f"], w=["wgu1"])

    xs = [sb([128, 8, XB + 1]) for _ in range(2)]
    xb = [sb([128, 8, XB + 1], BF16) for _ in range(2)]
    hwT = sb([64, XB], BF16); haT = sb([64, XB], BF16); hg0T = sb([128, XB], BF16); hg1T = sb([32, XB], BF16)
    Hst = sb([128, 64])
    V(lambda e: e.memset(Hst[:], 0.0), w=["Hst"])
    Bpad = [sb([128, 2, 128]) for _ in range(2)]; Kpad = [sb([128, 2, 128]) for _ in range(2)]
    for p_ in range(2):
        V(lambda e, p_=p_: e.memset(Bpad[p_][:], 0.0), w=["Bpad%d" % p_])
        V(lambda e, p_=p_: e.memset(Kpad[p_][:], 0.0), w=["Kpad%d" % p_])

    P_RKV = ps([128, 512]); P_Z = ps([128, 4, 128])
    P_T = ps([128, 4, 128]); P_L1 = [ps([128, 4, 128]) for _ in range(2)]; P_LD = [ps([128, 4, 128]) for _ in range(2)]; P_M = ps([128, 512])
    P_LH = P_T[:].rearrange("p a n -> p (a n)")

    def t(shape=(128, 128), dt=F32):
        return sb(list(shape), dt)

    zw = t(); logw = t(); av = t(); gv = [t() for _ in range(2)]; kkr = t(); sq = t(); ss = t((128, 2)); rn = t((128, 2))
    kk = t(); tmp1 = t(); kmod = t(); kka = t(); rkr = t(); sbon = [t((128, 2)) for _ in range(2)]; vsb = [t() for _ in range(2)]; sq2 = t()
    eW = t(); eWi = t(); eWp = t(); eR = t(); wc = [t((128, 1)) for _ in range(2)]
    TM = sb([128, 4, 128])
    FT = [sb([128, 4, 128]) for _ in range(2)]
    LM1 = [[sb([128, 4, 128]) for _ in range(2)] for _ in range(2)]
    PN = [[[sb([128, 2, 128]) for _ in range(2)] for _ in range(2)] for _ in range(2)]
    Lk = [[t() for _ in range(2)] for _ in range(2)]
    Gs = [t((128, 64)) for _ in range(2)]; Us = [t((128, 64)) for _ in range(2)]
    s1 = t((128, 2)); nmean = t((128, 2)); Yc = t(); ss2 = t((128, 2)); rstd = t((128, 2)); Yn = t(); Yo = t(); YoT = t()

    n_blocks = (n_chunks * C) // XB
    xT = dram["xT"].rearrange("(dc p) n -> p dc n", p=128)

    NCPB = XB // C

    def gen_X(ch):
        blk = ch // NCPB
        ci = ch % NCPB
        o = ci * C
        p = ch % 2
        xi = blk % 2
        kx = "xs%d" % xi; kb = "xb%d" % xi
        c0 = blk * XB
        if ci == 0:
            xi = blk % 2
            kx = "xs%d" % xi; kb = "xb%d" % xi
            c0 = blk * XB
            for dc in range(8):
                DMA(xs[xi][:, dc, :], xT[:, dc, c0:c0 + XB + 1], writes=[kx])
            for dc in range(8):
                eng = G if dc % 2 == 0 else A
                if dc % 2 == 0:
                    G(lambda e, dc=dc, xi=xi: e.tensor_copy(out=xb[xi][:, dc, :], in_=xs[xi][:, dc, :]), r=[kx], w=[kb])
                else:
                    A(lambda e, dc=dc, xi=xi: e.copy(out=xb[xi][:, dc, :], in_=xs[xi][:, dc, :]), r=[kx], w=[kb])
            for (lo, n, dst, fn, key) in ((384, 64, hwT, AF.Tanh, "hwT"), (448, 64, haT, AF.Copy, "haT"),
                                          (512, 128, hg0T, AF.Sigmoid, "hg0T"), (640, 32, hg1T, AF.Sigmoid, "hg1T")):
                for dc in range(8):
                    T(lambda e, dc=dc, lo=lo, n=n, xi=xi: e.matmul(P_LH[0:n, :], lhsT=Wa[:, dc, lo:lo + n], rhs=xb[xi][:, dc, 1:XB + 1],
                                                                  start=(dc == 0), stop=False), r=["Wa", kb], w=["P_T"])
                for dc in range(8):
                    T(lambda e, dc=dc, lo=lo, n=n, xi=xi: e.matmul(P_LH[0:n, :], lhsT=Wb[:, dc, lo:lo + n], rhs=xb[xi][:, dc, 0:XB],
                                                                  start=False, stop=(dc == 7)), r=["Wb", kb], w=["P_T"])
                A(lambda e, n=n, dst=dst, fn=fn: e.activation(out=dst[:], in_=P_LH[0:n, :], func=fn), r=["P_T"], w=[key])

        for dc in range(8):
            T(lambda e, dc=dc, xi=xi, o=o: e.matmul(P_RKV[:, 0:384], lhsT=xb[xi][:, dc, 1 + o:1 + o + C], rhs=Wa[:, dc, 0:384],
                                                    start=(dc == 0), stop=False), r=["Wa", kb], w=["P_RKV"])
        for dc in range(8):
            T(lambda e, dc=dc, xi=xi, o=o: e.matmul(P_RKV[:, 0:384], lhsT=xb[xi][:, dc, o:o + C], rhs=Wb[:, dc, 0:384],
                                                    start=False, stop=(dc == 7)), r=["Wb", kb], w=["P_RKV"])
        T(lambda e, o=o: e.matmul(P_Z[:, 0, :], lhsT=hwT[:, o:o + C], rhs=wdu[:], start=True, stop=True), r=["hwT", "wdu"], w=["P_Z0"])
        T(lambda e, o=o: e.matmul(P_Z[:, 1, :], lhsT=haT[:, o:o + C], rhs=wau[:], start=True, stop=True), r=["haT", "wau"], w=["P_Z1"])
        T(lambda e, o=o: e.matmul(P_RKV[:, 384:512], lhsT=hg0T[:, o:o + C], rhs=wgu0[:], start=True, stop=False), r=["hg0T", "wgu0"], w=["P_RKVg"])
        T(lambda e, o=o: e.matmul(P_RKV[:, 384:512], lhsT=hg1T[:, o:o + C], rhs=wgu1[:], start=False, stop=True), r=["hg1T", "wgu1"], w=["P_RKVg"])
        V(lambda e: e.tensor_tensor(out=zw[:], in0=P_Z[:, 0, :], in1=bc["w0"][:], op=ALU.add), r=["P_Z0", "bc_w0"], w=["zw"])
        A(lambda e: e.activation(out=zw[:], in_=zw[:], func=AF.Sigmoid), r=["zw"], w=["zw"])
        G(lambda e: e.tensor_scalar(out=logw[:], in0=zw[:], scalar1=C0, scalar2=None, op0=ALU.mult), r=["zw"], w=["logw"])
        V(lambda e: e.tensor_tensor(out=av[:], in0=P_Z[:, 1, :], in1=bc["a0"][:], op=ALU.add), r=["P_Z1", "bc_a0"], w=["av"])
        A(lambda e: e.activation(out=av[:], in_=av[:], func=AF.Sigmoid), r=["av"], w=["av"])
        A(lambda e: e.copy(out=gv[p][:], in_=P_RKV[:, 384:512]), r=["P_RKVg"], w=["gv%d" % p])
        A(lambda e: e.copy(out=vsb[p][:], in_=P_RKV[:, 256:384]), r=["P_RKV"], w=["vsb%d" % p])
        V(lambda e: e.tensor_tensor(out=kkr[:], in0=P_RKV[:, 128:256], in1=bc["kk"][:], op=ALU.mult), r=["P_RKV", "bc_kk"], w=["kkr"])
        G(lambda e: e.tensor_tensor(out=sq[:], in0=kkr[:], in1=kkr[:], op=ALU.mult), r=["kkr"], w=["sq"])
        V(lambda e: e.tensor_reduce(out=ss[:], in_=sq[:].rearrange("p (h c) -> p h c", h=2), axis=AX.X, op=ALU.add), r=["sq"], w=["ss"])
        A(lambda e: e.activation(out=ss[:], in_=ss[:], func=AF.Sqrt), r=["ss"], w=["ss"])
        V(lambda e: e.tensor_scalar(out=ss[:], in0=ss[:], scalar1=1e-12, scalar2=None, op0=ALU.max), r=["ss"], w=["ss"])
        V(lambda e: e.reciprocal(out=rn[:], in_=ss[:]), r=["ss"], w=["rn"])
        for h in range(2):
            V(lambda e, h=h: e.tensor_scalar(out=kk[:, h * 64:(h + 1) * 64], in0=kkr[:, h * 64:(h + 1) * 64], scalar1=rn[:, h:h + 1],
                                             scalar2=None, op0=ALU.mult), r=["kkr", "rn"], w=["kk"])
        V(lambda e: e.scalar_tensor_tensor(out=tmp1[:], in0=av[:], scalar=-1.0, in1=bc["ka"][:], op0=ALU.add, op1=ALU.mult), r=["av", "bc_ka"], w=["tmp1"])
        V(lambda e: e.scalar_tensor_tensor(out=kmod[:], in0=tmp1[:], scalar=1.0, in1=P_RKV[:, 128:256], op0=ALU.add, op1=ALU.mult), r=["tmp1", "P_RKV"], w=["kmod"])
        G(lambda e: e.tensor_tensor(out=kka[:], in0=kk[:], in1=av[:], op=ALU.mult), r=["kk", "av"], w=["kka"])
        V(lambda e: e.tensor_tensor(out=rkr[:], in0=P_RKV[:, 0:128], in1=kmod[:], op=ALU.mult), r=["P_RKV", "kmod"], w=["rkr"])
        G(lambda e: e.tensor_tensor(out=rkr[:], in0=rkr[:], in1=bc["rk"][:], op=ALU.mult), r=["rkr", "bc_rk"], w=["rkr"])
        V(lambda e: e.tensor_reduce(out=sbon[p][:], in_=rkr[:].rearrange("p (h c) -> p h c", h=2), axis=AX.X, op=ALU.add), r=["rkr"], w=["sbon%d" % p])
        T(lambda e: e.matmul(P_Z[:, 2, :], lhsT=cst["c_m1"][:], rhs=logw[:], start=True, stop=True), r=["c_m1", "logw"], w=["P_Z2"])
        T(lambda e: e.matmul(P_Z[:, 3, :], lhsT=cst["c_m2"][:], rhs=logw[:], start=True, stop=True), r=["c_m2", "logw"], w=["P_Z3"])
        T(lambda e: e.matmul(P_Z[:, 0, 0:1], lhsT=logw[:], rhs=ones_col[:], start=True, stop=True), r=["ones_col", "logw"], w=["P_Z0"])
        A(lambda e: e.activation(out=eW[:], in_=P_Z[:, 2, :], func=AF.Exp), r=["P_Z2"], w=["eW"])
        A(lambda e: e.activation(out=eWi[:], in_=P_Z[:, 2, :], func=AF.Exp, scale=-1.0), r=["P_Z2"], w=["eWi"])
        A(lambda e: e.activation(out=eWp[:], in_=logw[:], func=AF.Exp, scale=-1.0), r=["logw"], w=["eWp"])
        G(lambda e: e.tensor_tensor(out=eWp[:], in0=eWp[:], in1=eW[:], op=ALU.mult), r=["eWp", "eW"], w=["eWp"])
        A(lambda e: e.activation(out=eR[:], in_=P_Z[:, 3, :], func=AF.Exp), r=["P_Z3"], w=["eR"])
        A(lambda e: e.activation(out=wc[p][:], in_=P_Z[:, 0, 0:1], func=AF.Exp), r=["P_Z0"], w=["wc%d" % p])
        V(lambda e: e.scalar_tensor_tensor(out=TM[:, 0, :], in0=kk[:], scalar=-1.0, in1=eWp[:], op0=ALU.mult, op1=ALU.mult), r=["kk", "eWp"], w=["TM0"])
        V(lambda e: e.tensor_tensor(out=TM[:, 1, :], in0=P_RKV[:, 0:128], in1=eW[:], op=ALU.mult), r=["P_RKV", "eW"], w=["TM1"])
        G(lambda e: e.tensor_tensor(out=TM[:, 2, :], in0=kka[:], in1=eWi[:], op=ALU.mult), r=["kka", "eWi"], w=["TM2"])
        V(lambda e: e.tensor_tensor(out=TM[:, 3, :], in0=kmod[:], in1=eWi[:], op=ALU.mult), r=["kmod", "eWi"], w=["TM3"])
        for h in range(2):
            hs = slice(h * 64, (h + 1) * 64)
            G(lambda e, h=h, hs=hs: e.tensor_tensor(out=Bpad[p][:, h, hs], in0=kka[:, hs], in1=eR[:, hs], op=ALU.mult), r=["kka", "eR"], w=["Bpad%d" % p])
            V(lambda e, h=h, hs=hs: e.tensor_tensor(out=Kpad[p][:, h, hs], in0=kmod[:, hs], in1=eR[:, hs], op=ALU.mult), r=["kmod", "eR"], w=["Kpad%d" % p])
        for j in range(4):
            T(lambda e, j=j: e.transpose(P_T[:, j, :], TM[:, j, :], cst["c_ident"][:]), r=["TM%d" % j, "c_ident"], w=["P_T"])
        A(lambda e: e.copy(out=FT[p][:], in_=P_T[:]), r=["P_T"], w=["FT%d" % p])
        HSL = [slice(0, 64), slice(64, 128)]
        BK = ["a", "b"]
        for h in range(2):
            hs = HSL[h]
            T(lambda e, hs=hs, h=h: e.matmul(P_L1[h][:, 0:2, :], lhsT=FT[p][hs, 2, :], rhs=FT[p][hs, 0:2, :], start=True, stop=True), r=["FT%d" % p], w=["P_L1%s" % BK[h]])
            T(lambda e, hs=hs, h=h: e.matmul(P_L1[h][:, 2:4, :], lhsT=FT[p][hs, 3, :], rhs=FT[p][hs, 0:2, :], start=True, stop=True), r=["FT%d" % p], w=["P_L1%s" % BK[h]])
            T(lambda e, hs=hs, h=h: e.matmul(P_LD[h][:, 0, :], lhsT=FT[p][hs, 0, :], rhs=FT[p][hs, 2, :], start=True, stop=True), r=["FT%d" % p], w=["P_LD%s0" % BK[h]])
        for h in range(2):
            V(lambda e, h=h: e.tensor_tensor(out=LM1[p][h][:], in0=P_L1[h][:], in1=mask4[:], op=ALU.mult), r=["P_L1%s" % BK[h], "mask4"], w=["LM1%d.%d" % (p, h)])
            V(lambda e, h=h: e.tensor_tensor(out=Lk[h][0][:], in0=P_LD[h][:, 0, :], in1=cst["c_mls"][:], op=ALU.mult), r=["P_LD%s0" % BK[h], "c_mls"], w=["Lk%d.0" % h])
        for h in range(2):
            G(lambda e, h=h: e.tensor_tensor(out=PN[p][h][0][:, 0, :], in0=LM1[p][h][:, 0, :], in1=cst["c_ident"][:], op=ALU.add), r=["LM1%d.%d" % (p, h), "c_ident"], w=["PN%d.%d.0.P" % (p, h)])
            T(lambda e, h=h: e.matmul(P_LD[h][:, 2, :], lhsT=Lk[h][0][:], rhs=LM1[p][h][:, 0, :], start=True, stop=True), r=["Lk%d.0" % h, "LM1%d.%d" % (p, h)], w=["P_LD%s12" % BK[h]])
            T(lambda e, h=h: e.matmul(P_LD[h][:, 3, :], lhsT=LM1[p][h][:, 0, :], rhs=Lk[h][0][:], start=True, stop=True), r=["Lk%d.0" % h, "LM1%d.%d" % (p, h)], w=["P_LD%s3" % BK[h]])
        for h in range(2):
            A(lambda e, h=h: e.copy(out=PN[p][h][0][:, 1, :], in_=P_LD[h][:, 2, :]), r=["P_LD%s12" % BK[h]], w=["PN%d.%d.0.N" % (p, h)])
            A(lambda e, h=h: e.copy(out=Lk[h][1][:], in_=P_LD[h][:, 3, :]), r=["P_LD%s3" % BK[h]], w=["Lk%d.1" % h])
        cur = 0
        lcur = 1
        for lev in range(1, 7):
            nxt = 1 - cur
            lnx = 1 - lcur
            last = (lev == 6)
            for h in range(2):
                pk = "PN%d.%d.%d" % (p, h, cur); lk = "Lk%d.%d" % (h, lcur)
                if not last:
                    T(lambda e, cur=cur, lcur=lcur, h=h: e.matmul(P_LD[h][:, 1:3, :], lhsT=Lk[h][lcur][:], rhs=PN[p][h][cur][:], start=True, stop=True),
                      r=[lk, pk + ".P", pk + ".N"], w=["P_LD%s12" % BK[h]])
                    T(lambda e, cur=cur, lcur=lcur, h=h: e.matmul(P_LD[h][:, 3, :], lhsT=PN[p][h][cur][:, 1, :], rhs=Lk[h][lcur][:], start=True, stop=True),
                      r=[lk, pk + ".N"], w=["P_LD%s3" % BK[h]])
                else:
                    T(lambda e, cur=cur, lcur=lcur, h=h: e.matmul(P_LD[h][:, 1, :], lhsT=Lk[h][lcur][:], rhs=PN[p][h][cur][:, 0, :], start=True, stop=True),
                      r=[lk, pk + ".P"], w=["P_LD%s12" % BK[h]])
            for h in range(2):
                pk = "PN%d.%d.%d" % (p, h, cur); pn = "PN%d.%d.%d" % (p, h, nxt); ln_ = "Lk%d.%d" % (h, lnx)
                V(lambda e, cur=cur, nxt=nxt, h=h: e.tensor_tensor(out=PN[p][h][nxt][:, 0, :], in0=P_LD[h][:, 1, :], in1=PN[p][h][cur][:, 0, :], op=ALU.add),
                  r=["P_LD%s12" % BK[h], pk + ".P"], w=[pn + ".P"])
                if not last:
                    A(lambda e, nxt=nxt, h=h: e.copy(out=PN[p][h][nxt][:, 1, :], in_=P_LD[h][:, 2, :]), r=["P_LD%s12" % BK[h]], w=[pn + ".N"])
                    A(lambda e, lnx=lnx, h=h: e.copy(out=Lk[h][lnx][:], in_=P_LD[h][:, 3, :]), r=["P_LD%s3" % BK[h]], w=[ln_])
            cur = nxt
            lcur = lnx

    def gen_Y(ch):
        p = ch % 2
        HSL = [slice(0, 64), slice(64, 128)]
        BK = ["a", "b"]
        cur = 0
        for h in range(2):
            hs = HSL[h]
            T(lambda e, hs=hs, h=h: e.matmul(P_M[:, 64 * h:64 * h + 64], lhsT=FT[p][hs, 0, :], rhs=Hst[hs, :], start=True, stop=False), r=["FT%d" % p, "Hst"], w=["P_G%d" % h])
            T(lambda e, hs=hs, h=h: e.matmul(P_M[:, 64 * h:64 * h + 64], lhsT=LM1[p][h][:, 2, :], rhs=vsb[p][:, hs], start=False, stop=True), r=["LM1%d.%d" % (p, h), "vsb%d" % p], w=["P_G%d" % h])
        A(lambda e: e.copy(out=Gs[0][:], in_=P_M[:, 0:64]), r=["P_G0"], w=["Gs0"])
        V(lambda e: e.tensor_copy(out=Gs[1][:], in_=P_M[:, 64:128]), r=["P_G1"], w=["Gs1"])
        for h in range(2):
            T(lambda e, h=h, cur=cur: e.matmul(P_M[:, 64 * h:64 * h + 64], lhsT=PN[p][h][cur][:, 0, :], rhs=Gs[h][:], start=True, stop=True),
              r=["PN%d.%d.%d.P" % (p, h, cur), "Gs%d" % h], w=["P_G%d" % h])
        A(lambda e: e.copy(out=Us[0][:], in_=P_M[:, 0:64]), r=["P_G0"], w=["Us0"])
        V(lambda e: e.tensor_copy(out=Us[1][:], in_=P_M[:, 64:128]), r=["P_G1"], w=["Us1"])
        for h in range(2):
            hs = HSL[h]
            yk = "P_Y%d" % h
            uk = "Us%d" % h
            T(lambda e, hs=hs, h=h: e.matmul(P_M[:, 192 + 64 * h:256 + 64 * h], lhsT=FT[p][hs, 1, :], rhs=Hst[hs, :], start=True, stop=False), r=["FT%d" % p, "Hst"], w=[yk])
            T(lambda e, h=h: e.matmul(P_M[:, 192 + 64 * h:256 + 64 * h], lhsT=LM1[p][h][:, 1, :], rhs=Us[h][:], start=False, stop=False), r=["LM1%d.%d" % (p, h), uk], w=[yk])
            T(lambda e, hs=hs, h=h: e.matmul(P_M[:, 192 + 64 * h:256 + 64 * h], lhsT=LM1[p][h][:, 3, :], rhs=vsb[p][:, hs], start=False, stop=True), r=["LM1%d.%d" % (p, h), "vsb%d" % p], w=[yk])
        T(lambda e: e.matmul(P_M[:, 128:192], lhsT=Bpad[p][:, 0, :], rhs=Us[0][:], start=True, stop=False), r=["Bpad%d" % p, "Us0"], w=["P_H"])
        T(lambda e: e.matmul(P_M[:, 128:192], lhsT=Kpad[p][:, 0, :], rhs=vsb[p][:, 0:64], start=False, stop=False), r=["Kpad%d" % p, "vsb%d" % p], w=["P_H"])
        T(lambda e: e.matmul(P_M[:, 128:192], lhsT=Bpad[p][:, 1, :], rhs=Us[1][:], start=False, stop=False), r=["Bpad%d" % p, "Us1"], w=["P_H"])
        T(lambda e: e.matmul(P_M[:, 128:192], lhsT=Kpad[p][:, 1, :], rhs=vsb[p][:, 64:128], start=False, stop=True), r=["Kpad%d" % p, "vsb%d" % p], w=["P_H"])
        V(lambda e: e.scalar_tensor_tensor(out=Hst[:], in0=Hst[:], scalar=wc[p][:, 0:1], in1=P_M[:, 128:192], op0=ALU.mult, op1=ALU.add),
          r=["Hst", "wc%d" % p, "P_H"], w=["Hst"])
        Yv = P_M[:, 192:320]
        V(lambda e: e.tensor_reduce(out=s1[:], in_=Yv.rearrange("p (h c) -> p h c", h=2), axis=AX.X, op=ALU.add), r=["P_Y0", "P_Y1"], w=["s1"])
        V(lambda e: e.tensor_scalar(out=nmean[:], in0=s1[:], scalar1=-1.0 / 64, scalar2=None, op0=ALU.mult), r=["s1"], w=["nmean"])
        for h in range(2):
            hs = slice(h * 64, (h + 1) * 64)
            V(lambda e, h=h, hs=hs: e.tensor_scalar(out=Yc[:, hs], in0=P_M[:, 192 + 64 * h:256 + 64 * h], scalar1=nmean[:, h:h + 1], scalar2=None, op0=ALU.add),
              r=["P_Y%d" % h, "nmean"], w=["Yc"])
        G(lambda e: e.tensor_tensor(out=sq2[:], in0=Yc[:], in1=Yc[:], op=ALU.mult), r=["Yc"], w=["sq2"])
        V(lambda e: e.tensor_reduce(out=ss2[:], in_=sq2[:].rearrange("p (h c) -> p h c", h=2), axis=AX.X, op=ALU.add), r=["sq2"], w=["ss2"])
        V(lambda e: e.tensor_scalar(out=ss2[:], in0=ss2[:], scalar1=1.0 / 64, scalar2=GN_EPS, op0=ALU.mult, op1=ALU.add), r=["ss2"], w=["ss2"])
        A(lambda e: e.activation(out=ss2[:], in_=ss2[:], func=AF.Sqrt), r=["ss2"], w=["ss2"])
        V(lambda e: e.reciprocal(out=rstd[:], in_=ss2[:]), r=["ss2"], w=["rstd"])
        for h in range(2):
            hs = slice(h * 64, (h + 1) * 64)
            V(lambda e, h=h, hs=hs: e.tensor_scalar(out=Yn[:, hs], in0=Yc[:, hs], scalar1=rstd[:, h:h + 1], scalar2=None, op0=ALU.mult), r=["Yc", "rstd"], w=["Yn"])
        G(lambda e: e.tensor_tensor(out=Yn[:], in0=Yn[:], in1=bc["lng"][:], op=ALU.mult), r=["Yn", "bc_lng"], w=["Yn"])
        G(lambda e: e.tensor_tensor(out=Yn[:], in0=Yn[:], in1=bc["lnb"][:], op=ALU.add), r=["Yn", "bc_lnb"], w=["Yn"])
        for h in range(2):
            hs = slice(h * 64, (h + 1) * 64)
            V(lambda e, h=h, hs=hs: e.scalar_tensor_tensor(out=Yo[:, hs], in0=vsb[p][:, hs], scalar=sbon[p][:, h:h + 1], in1=Yn[:, hs], op0=ALU.mult, op1=ALU.add),
              r=["vsb%d" % p, "sbon%d" % p, "Yn"], w=["Yo"])
        G(lambda e: e.tensor_tensor(out=Yo[:], in0=Yo[:], in1=gv[p][:], op=ALU.mult), r=["Yo", "gv%d" % p], w=["Yo"])
        T(lambda e: e.transpose(P_M[:, 320:448], Yo[:], cst["c_ident"][:]), r=["Yo", "c_ident"], w=["P_OT"])
        A(lambda e: e.copy(out=YoT[:], in_=P_M[:, 320:448]), r=["P_OT"], w=["YoT"])
        DMA(dram["cin"][(ch * C) // 1024][0:128, (ch * C) % 1024:(ch * C) % 1024 + C], YoT[:], reads=["YoT"])

    def collect(fn, ch):
        lst = []
        defer[0] = lst
        fn(ch)
        defer[0] = None
        return lst

    xl = collect(gen_X, 0)
    drain(xl, len(xl))
    for ch in range(n_chunks):
        yl = collect(gen_Y, ch)
        xl = collect(gen_X, ch + 1) if ch + 1 < n_chunks else []
        ny = len(yl)
        for i in range(ny):
            drain(yl, 1)
            if xl:
                drain(xl, -(-len(xl) // (ny - i)))
        drain(xl, len(xl))


S_TOK = 16384
AXB = 256
KB_BOUND = 16.0
NEG = -30000.0


def attn_consts():
    s = S_TOK
    half = 32
    inv_freq = (10000.0 ** (-np.arange(half, dtype=np.float32) / half)).astype(np.float32)
    ang = (np.arange(s, dtype=np.float32)[:, None] * inv_freq[None, :]).astype(np.float32)
    cos = np.cos(ang).astype(np.float32)
    sin = np.sin(ang).astype(np.float32)
    ropeA = np.tile(cos, (1, 8)).astype(np.float32)
    ropeB = np.tile(np.concatenate([-sin, sin], 1), (1, 4)).astype(np.float32)
    kind = np.zeros((64, s), np.float32)
    for n in range(64):
        kind[n, n * 256:(n + 1) * 256] = 1.0
    i = np.arange(128)
    tri = (i[None, :] >= i[:, None]).astype(np.float32)
    return {"a_ropeA": ropeA, "a_ropeB": ropeB, "a_kind": kind.astype(ml_dtypes.bfloat16),
            "a_tri": tri.astype(ml_dtypes.bfloat16), "a_ident": np.eye(128, dtype=np.float32),
            "a_identb": np.eye(128, dtype=np.float32).astype(ml_dtypes.bfloat16)}


def build_attn(nc, S, es, dram, n_blocks=S_TOK // 256, cc=None):
    sbn = [0]

    def sb(shape, dt=F32):
        sbn[0] += 1
        return es.enter_context(nc.sbuf_tensor("at%d" % sbn[0], shape, dt))

    def ps(shape, dt=F32):
        sbn[0] += 1
        return es.enter_context(nc.psum_tensor("ap%d" % sbn[0], shape, dt))

    def _bk(r, w):
        b = set(k.split(".")[0] for k in list(r) + list(w) if k.startswith("Q_"))
        return list(w) + ["BANK_" + x for x in b]

    defer = [None]

    def _rec(eng, fn, r, w):
        if defer[0] is None:
            S.op(eng, fn, r, w)
        else:
            defer[0].append((eng, fn, r, w))

    def DMA(out, in_, reads=(), writes=()):
        if defer[0] is None:
            S.dma("sync", out, in_, reads=reads, writes=writes)
        else:
            defer[0].append(("dma", (out, in_), reads, writes))

    def drain(lst, n):
        for _ in range(min(n, len(lst))):
            eng, fn, r, w = lst.pop(0)
            if eng == "dma":
                S.dma("sync", fn[0], fn[1], reads=r, writes=w)
            else:
                S.op(eng, fn, r, w)

    V = lambda fn, r=(), w=(): _rec("vector", fn, r, _bk(r, w))
    A = lambda fn, r=(), w=(): _rec("scalar", fn, r, _bk(r, w))
    G = lambda fn, r=(), w=(): _rec("gpsimd", fn, r, list(w))
    T = lambda fn, r=(), w=(): _rec("tensor", fn, r, _bk(r, w))

    ident = sb([128, 128]); identb = sb([128, 128], BF16); tri = sb([128, 128], BF16)
    S.dma("sync", ident[:], dram["a_ident"], writes=["ident"])
    S.dma("sync", identb[:], dram["a_identb"], writes=["identb"])
    S.dma("sync", tri[:], dram["a_tri"], writes=["tri"])
    wst = sb([128, 8, 384])
    S.dma("sync", wst[:], dram["w_qkv"].rearrange("(dc p) n -> p dc n", p=128), writes=["wst"])
    Wq = sb([128, 8, 384], BF16)
    for dc in range(8):
        V(lambda e, dc=dc: e.tensor_copy(out=Wq[:, dc, :], in_=wst[:, dc, :]), r=["wst"], w=["Wq"])
    inv256 = sb([128, 1]); ones_r = sb([128, 64])
    V(lambda e: e.memset(inv256[:], 1.0 / 256), w=["inv256"])
    V(lambda e: e.memset(ones_r[:], 1.0), w=["ones_r"])
    KT = [sb([128, S_TOK], BF16) for _ in range(2)]
    VA = [sb([128, 128, 65], BF16) for _ in range(2)]
    kmT = [sb([64, 64]) for _ in range(2)]
    Gt = [sb([128, 64]) for _ in range(2)]
    for h in range(2):
        S.dma("sync", KT[h][64:128, :], dram["a_kind"], writes=["KT%d.%d" % (h, b_) for b_ in range(64)])
        G(lambda e, h=h: e.memset(VA[h][:], 1.0), w=["VA%d.%d" % (h, b_) for b_ in range(64)])
        V(lambda e, h=h: e.memset(kmT[h][:], 0.0), w=["kmT%d" % h])
        V(lambda e, h=h: e.memset(Gt[h][:], -1e30), w=["Gt%d" % h])
    xs = [sb([128, 8, AXB + 1]) for _ in range(2)]
    xb = [sb([128, 8, AXB + 1], BF16) for _ in range(2)]
    rA = [sb([128, 256]) for _ in range(2)]; rB = [sb([128, 256]) for _ in range(2)]
    tmpA = sb([128, 256]); tmpB = sb([128, 256]); QK = sb([128, 4, 64])
    qT32 = sb([64, 128]); mx = sb([128, 8]); negm = sb([128, 64]); ssq = sb([128, 1]); cpos = sb([128, 1]); qjunk = sb([128, 64])
    Qaug = [sb([128, 128], BF16) for _ in range(2)]
    QT = [[sb([128, 256], BF16) for _ in range(2)] for _ in range(2)]
    PT = [sb([128, 256], BF16) for _ in range(4)]
    rec = sb([128, 256]); ysb = sb([64, 256]); yo = sb([64, 256])

    NSB = 4
    Q_A = ps([128, 512]); Q_C = ps([128, 512])
    Q_D = Q_C[:, 66:130].bitcast(BF16)
    Q_S = [ps([128, 512]) for _ in range(NSB)]; Q_O = [ps([128, 512]) for _ in range(2)]

    xT = dram["xT"].rearrange("(dc p) n -> p dc n", p=128)
    sidx = 0

    def gen_proj(blk):
        xi = blk % 2
        kx = "xs%d" % xi; kb = "xb%d" % xi
        c0 = blk * AXB
        for dc in range(8):
            DMA(xs[xi][:, dc, :], xT[:, dc, c0:c0 + AXB + 1], writes=[kx])
        for dc in range(8):
            if dc % 2 == 0:
                G(lambda e, dc=dc, xi=xi: e.tensor_copy(out=xb[xi][:, dc, :], in_=xs[xi][:, dc, :]), r=[kx], w=[kb])
            else:
                V(lambda e, dc=dc, xi=xi: e.tensor_copy(out=xb[xi][:, dc, :], in_=xs[xi][:, dc, :]), r=[kx], w=[kb])
        for half in range(2):
            tt = blk * 2 + half
            o = half * 128
            ri = tt % 2
            DMA(rA[ri][:], dram["a_ropeA"][tt * 128:(tt + 1) * 128, :], writes=["rA%d" % ri])
            DMA(rB[ri][:], dram["a_ropeB"][tt * 128:(tt + 1) * 128, :], writes=["rB%d" % ri])
            for dc in range(8):
                T(lambda e, dc=dc, xi=xi, o=o: e.matmul(Q_A[:, 0:384], lhsT=xb[xi][:, dc, 1 + o:1 + o + 128], rhs=Wq[:, dc, :],
                                                        start=(dc == 0), stop=(dc == 7)), r=["Wq", kb], w=["Q_A"])
            X4 = Q_A[:, 0:256].rearrange("p (g h d) -> p g h d", g=4, h=2)
            B4 = lambda t_: t_[:].rearrange("p (g h d) -> p g h d", g=4, h=2)
            V(lambda e, ri=ri: e.tensor_tensor(out=tmpA[:], in0=Q_A[:, 0:256], in1=rA[ri][:], op=ALU.mult), r=["Q_A", "rA%d" % ri], w=["tmpA"])
            V(lambda e, ri=ri: e.tensor_tensor(out=B4(tmpB)[:, :, 0, :], in0=X4[:, :, 1, :], in1=B4(rB[ri])[:, :, 0, :], op=ALU.mult), r=["Q_A", "rB%d" % ri], w=["tmpB"])
            V(lambda e, ri=ri: e.tensor_tensor(out=B4(tmpB)[:, :, 1, :], in0=X4[:, :, 0, :], in1=B4(rB[ri])[:, :, 1, :], op=ALU.mult), r=["Q_A", "rB%d" % ri], w=["tmpB"])
            G(lambda e: e.tensor_tensor(out=QK[:].rearrange("p g d -> p (g d)"), in0=tmpA[:], in1=tmpB[:], op=ALU.add), r=["tmpA", "tmpB"], w=["QK"])
            for h in range(2):
                V(lambda e, h=h, tt=tt: e.tensor_copy(out=VA[h][:, tt, 0:64], in_=Q_A[:, 256 + 64 * h:320 + 64 * h]), r=["Q_A"], w=["VA%d.%d" % (h, blk)])
                T(lambda e, h=h: e.transpose(Q_C[0:64, 130:258], QK[:, 2 + h, :], ident[:]), r=["QK", "ident"], w=["Q_C.k"])
                V(lambda e, h=h, tt=tt: e.tensor_copy(out=KT[h][0:64, tt * 128:(tt + 1) * 128], in_=Q_C[0:64, 130:258]), r=["Q_C.k"], w=["KT%d.%d" % (h, blk)])
                T(lambda e, h=h: e.transpose(Q_C[0:64, 258:386], QK[:, h, :], ident[:]), r=["QK", "ident"], w=["Q_C.q"])
                V(lambda e: e.tensor_copy(out=qT32[:], in_=Q_C[0:64, 258:386]), r=["Q_C.q"], w=["qT32"])
                if blk > 0:
                    T(lambda e, h=h: e.matmul(Q_C[:, 0:64], lhsT=qT32[:], rhs=kmT[h][:], start=True, stop=True), r=["qT32", "kmT%d" % h], w=["Q_C.g"])
                    V(lambda e, h=h, blk=blk: e.tensor_copy(out=Gt[h][:, 0:blk], in_=Q_C[:, 0:blk]), r=["Q_C.g"], w=["Gt%d" % h])
                V(lambda e, h=h: e.max(out=mx[:], in_=Gt[h][:]), r=["Gt%d" % h], w=["mx"])
                V(lambda e, h=h: e.tensor_scalar(out=negm[:], in0=Gt[h][:], scalar1=mx[:, 2:3], scalar2=NEG, op0=ALU.is_lt, op1=ALU.mult), r=["Gt%d" % h, "mx"], w=["negm"])
                V(lambda e, blk=blk: e.memset(negm[:, blk:blk + 1], 0.0), w=["negm"])
                V(lambda e, h=h: e.tensor_tensor(out=qjunk[:], in0=QK[:, h, :], in1=QK[:, h, :], op=ALU.mult), r=["QK"], w=["qjunk"])
                V(lambda e: e.tensor_reduce(out=ssq[:], in_=qjunk[:], axis=AX.X, op=ALU.add), r=["qjunk"], w=["ssq"])
                V(lambda e: e.tensor_scalar(out=cpos[:], in0=ssq[:], scalar1=0.5, scalar2=0.5 * KB_BOUND * KB_BOUND, op0=ALU.mult, op1=ALU.add), r=["ssq"], w=["cpos"])
                G(lambda e, h=h: e.tensor_copy(out=Qaug[h][:, 0:64], in_=QK[:, h, :]), r=["QK"], w=["Qaug%d" % h])
                V(lambda e, h=h: e.tensor_scalar(out=Qaug[h][:, 64:128], in0=negm[:], scalar1=cpos[:, 0:1], scalar2=None, op0=ALU.subtract), r=["negm", "cpos"], w=["Qaug%d" % h])
                T(lambda e, h=h: e.transpose(Q_D[:, 0:128], Qaug[h][:], identb[:]), r=["Qaug%d" % h, "identb"], w=["Q_C.d"])
                V(lambda e, h=h, o=o: e.tensor_copy(out=QT[h][blk % 2][:, o:o + 128], in_=Q_D[:, 0:128]), r=["Q_C.d"], w=["QT%d.%d" % (h, blk % 2)])
                T(lambda e, h=h: e.matmul(Q_C[0:64, 64:65], lhsT=QK[:, 2 + h, :], rhs=inv256[:], start=True, stop=True), r=["QK", "inv256"], w=["Q_C.m"])
                if half == 0:
                    V(lambda e, h=h, blk=blk: e.tensor_copy(out=kmT[h][:, blk:blk + 1], in_=Q_C[0:64, 64:65]), r=["Q_C.m"], w=["kmT%d" % h])
                else:
                    V(lambda e, h=h, blk=blk: e.tensor_tensor(out=kmT[h][:, blk:blk + 1], in0=Q_C[0:64, 64:65], in1=kmT[h][:, blk:blk + 1], op=ALU.add),
                      r=["Q_C.m", "kmT%d" % h], w=["kmT%d" % h])

    gen_proj(0)
    for blk in range(n_blocks):
        pend = []
        if blk + 1 < n_blocks:
            defer[0] = pend
            gen_proj(blk + 1)
            defer[0] = None
        steps = []
        for h in range(2):
            nkt = 2 * blk + 2
            for kt in range(nkt):
                steps.append((h, kt, nkt))

        def issue_qk(i):
            h, kt, nkt = steps[i]
            j = kt - 2 * blk
            q0 = 128 if j == 1 else 0
            si = (sbase + i) % NSB
            T(lambda e, h=h, kt=kt, q0=q0, si=si, bp=blk % 2: e.matmul(Q_S[si][:, q0:256], lhsT=KT[h][:, kt * 128:(kt + 1) * 128], rhs=QT[h][bp][:, q0:256],
                                                           start=True, stop=True), r=["KT%d.%d" % (h, kt // 2), "QT%d.%d" % (h, blk % 2)], w=["Q_S%d" % si])

        sbase = sidx
        LOOK = NSB - 2
        for i0 in range(min(LOOK, len(steps))):
            issue_qk(i0)
        for i in range(len(steps)):
            h, kt, nkt = steps[i]
            j = kt - 2 * blk
            q0 = 128 if j == 1 else 0
            si = (sbase + i) % NSB
            sk = "Q_S%d" % si
            pk = "PT%d" % si
            ok = "Q_O%d" % h
            if i + LOOK < len(steps):
                issue_qk(i + LOOK)
            A(lambda e, q0=q0, si=si: e.activation(out=PT[si][:, q0:256], in_=Q_S[si][:, q0:256], func=AF.Exp, scale=0.125), r=[sk], w=[pk])
            if j >= 0:
                G(lambda e, q0=q0, si=si, j=j: e.tensor_tensor(out=PT[si][:, j * 128:(j + 1) * 128], in0=PT[si][:, j * 128:(j + 1) * 128], in1=tri[:], op=ALU.mult),
                  r=[pk, "tri"], w=[pk])
            T(lambda e, h=h, kt=kt, q0=q0, si=si, nkt=nkt: e.matmul(Q_O[h][0:65, q0:256], lhsT=VA[h][:, kt, :], rhs=PT[si][:, q0:256],
                                                                    start=(kt == 0), stop=(kt == nkt - 1)), r=["VA%d.%d" % (h, kt // 2), pk], w=[ok])
            if kt == nkt - 1:
                V(lambda e, h=h: e.reciprocal(out=rec[64:65, :], in_=Q_O[h][64:65, 0:256]), r=[ok], w=["rec"])
                T(lambda e, h=h: e.matmul(Q_O[h][0:64, 256:512], lhsT=ones_r[64:65, :], rhs=rec[64:65, :], start=True, stop=True), r=["ones_r", "rec"], w=[ok + ".bc"])
                V(lambda e, h=h: e.tensor_copy(out=ysb[:], in_=Q_O[h][0:64, 0:256]), r=[ok], w=["ysb"])
                V(lambda e, h=h: e.tensor_tensor(out=yo[:], in0=ysb[:], in1=Q_O[h][0:64, 256:512], op=ALU.mult), r=["ysb", ok + ".bc"], w=["yo"])
                S.dma("sync", dram["cin"][(blk * 256) // 1024][128 + h * 64:128 + (h + 1) * 64, (blk * 256) % 1024:(blk * 256) % 1024 + 256], yo[:], reads=["yo"],
                      writes=["cin%d.%d.%d" % (blk // 4, blk % 4, h)])
                if cc is not None and h == 1 and blk % 4 == 3:
                    kch = blk // 4
                    S.async_op("gpsimd", lambda e, kch=kch: e.collective_compute("AllGather", ALU.bypass, replica_groups=cc["groups"],
                                                                             ins=[dram["cin"][kch]], outs=[dram["cout"][kch]]), cc["sem"],
                               reads=["cin%d.%d.%d" % (kch, b4, hh) for b4 in range(4) for hh in range(2)])
            if pend:
                drain(pend, -(-len(pend) // max(1, len(steps) - i)))
        drain(pend, len(pend))
        sidx += len(steps)


NTOK = 4096
ALPHA = float(2.0 ** 0.25)
LN_EPS = 1e-5
MOE_NSTG = 3


def _mk(nc, S, es, pfx):
    sbn = [0]

    def sb(shape, dt=F32):
        sbn[0] += 1
        return es.enter_context(nc.sbuf_tensor("%s%d" % (pfx, sbn[0]), shape, dt))

    def ps(shape, dt=F32):
        sbn[0] += 1
        return es.enter_context(nc.psum_tensor("%sp%d" % (pfx, sbn[0]), shape, dt))

    def _bk(r, w):
        b = set(k.split(".")[0] for k in list(r) + list(w) if k.startswith("Q_"))
        return list(w) + ["BANK_" + x for x in b]

    V = lambda fn, r=(), w=(): S.op("vector", fn, r, _bk(r, w))
    A = lambda fn, r=(), w=(): S.op("scalar", fn, r, _bk(r, w))
    G = lambda fn, r=(), w=(): S.op("gpsimd", fn, r, w)
    T = lambda fn, r=(), w=(): S.op("tensor", fn, r, _bk(r, w))
    return sb, ps, V, A, G, T


def layer_norm_tile(V, A, G, t, tk, junk, st, gbc, bbc, gk, bk):
    V(lambda e: e.tensor_reduce(out=st[:, 0:1], in_=t[:], axis=AX.X, op=ALU.add), r=[tk], w=["st0"])
    V(lambda e: e.tensor_scalar(out=st[:, 0:1], in0=st[:, 0:1], scalar1=-1.0 / 1024, scalar2=None, op0=ALU.mult), r=["st0"], w=["st0"])
    V(lambda e: e.tensor_scalar(out=t[:], in0=t[:], scalar1=st[:, 0:1], scalar2=None, op0=ALU.add), r=[tk, "st0"], w=[tk])
    V(lambda e: e.memset(st[:, 1:2], 0.0), w=["st1"])
    A(lambda e: e.activation(out=junk[:], in_=t[:], func=AF.Square, accum_out=st[:, 1:2]), r=[tk, "st1"], w=["junk", "st1"])
    V(lambda e: e.tensor_scalar(out=st[:, 1:2], in0=st[:, 1:2], scalar1=1.0 / 1024, scalar2=LN_EPS, op0=ALU.mult, op1=ALU.add), r=["st1"], w=["st1"])
    A(lambda e: e.activation(out=st[:, 1:2], in_=st[:, 1:2], func=AF.Sqrt), r=["st1"], w=["st1"])
    V(lambda e: e.reciprocal(out=st[:, 2:3], in_=st[:, 1:2]), r=["st1"], w=["st2"])
    V(lambda e: e.scalar_tensor_tensor(out=t[:], in0=t[:], scalar=st[:, 2:3], in1=gbc[:], op0=ALU.mult, op1=ALU.mult), r=[tk, "st2", gk], w=[tk])
    G(lambda e: e.tensor_tensor(out=t[:], in0=t[:], in1=bbc[:], op=ALU.add), r=[tk, bk], w=[tk])


def layer_norm_multi(V, A, G, tiles, keys, junk, st, gbc, bbc, gk, bk):
    n = len(tiles)
    c = lambda i, j: st[:, 4 * i + j:4 * i + j + 1]
    for i in range(n):
        V(lambda e, i=i: e.tensor_reduce(out=c(i, 0), in_=tiles[i], axis=AX.X, op=ALU.add), r=[keys[i]], w=["st%d.0" % i])
    for i in range(n):
        V(lambda e, i=i: e.tensor_scalar(out=c(i, 0), in0=c(i, 0), scalar1=-1.0 / 1024, scalar2=None, op0=ALU.mult), r=["st%d.0" % i], w=["st%d.0" % i])
    for i in range(n):
        V(lambda e, i=i: e.tensor_scalar(out=tiles[i], in0=tiles[i], scalar1=c(i, 0), scalar2=None, op0=ALU.add), r=[keys[i], "st%d.0" % i], w=[keys[i]])
        V(lambda e, i=i: e.memset(c(i, 1), 0.0), w=["st%d.1" % i])
    for i in range(n):
        A(lambda e, i=i: e.activation(out=junk[:], in_=tiles[i], func=AF.Square, accum_out=c(i, 1)), r=[keys[i], "st%d.1" % i], w=["junk", "st%d.1" % i])
    for i in range(n):
        V(lambda e, i=i: e.tensor_scalar(out=c(i, 1), in0=c(i, 1), scalar1=1.0 / 1024, scalar2=LN_EPS, op0=ALU.mult, op1=ALU.add), r=["st%d.1" % i], w=["st%d.1" % i])
    for i in range(n):
        A(lambda e, i=i: e.activation(out=c(i, 1), in_=c(i, 1), func=AF.Sqrt), r=["st%d.1" % i], w=["st%d.1" % i])
    for i in range(n):
        V(lambda e, i=i: e.reciprocal(out=c(i, 2), in_=c(i, 1)), r=["st%d.1" % i], w=["st%d.2" % i])
    for i in range(n):
        V(lambda e, i=i: e.scalar_tensor_tensor(out=tiles[i], in0=tiles[i], scalar=c(i, 2), in1=gbc[:], op0=ALU.mult, op1=ALU.mult), r=[keys[i], "st%d.2" % i, gk], w=[keys[i]])
    for i in range(n):
        G(lambda e, i=i: e.tensor_tensor(out=tiles[i], in0=tiles[i], in1=bbc[:], op=ALU.add), r=[keys[i], bk], w=[keys[i]])


def build_stage_a(nc, S, es, dram, n_groups=NTOK // 512):
    sb, ps, V, A, G, T = _mk(nc, S, es, "sa")
    stg = [sb([128, 2048]) for _ in range(2)]
    Wg = sb([128, 8, 2048], BF16); Wab = sb([128, 4, 1024], BF16); Wrb = sb([128, 4, 1024], BF16); Wo = sb([128, 8, 1024], BF16)
    si = 0
    wg_v = dram["w_gate"].rearrange("(dc p) n -> p dc n", p=128)
    for dc in range(8):
        k = "stg%d" % (si % 2); st_ = stg[si % 2]; si += 1
        S.dma("sync", st_[:], wg_v[:, dc, :], writes=[k])
        V(lambda e, dc=dc, st_=st_: e.tensor_copy(out=Wg[:, dc, :], in_=st_[:]), r=[k], w=["Wg"])
    for (src, dst, key, ndc) in (("w_ab", Wab, "Wab", 4), ("w_rb", Wrb, "Wrb", 4), ("w_out", Wo, "Wo", 8)):
        v = dram[src].rearrange("(dc p) n -> p dc n", p=128)
        for dc in range(ndc):
            k = "stg%d" % (si % 2); st_ = stg[si % 2]; si += 1
            S.dma("sync", st_[:, 0:1024], v[:, dc, :], writes=[k])
            G(lambda e, dc=dc, st_=st_, dst=dst: e.tensor_copy(out=dst[:, dc, :], in_=st_[:, 0:1024]), r=[k], w=[key])
    gbc = sb([128, 1024]); bbc = sb([128, 1024])
    S.dma("sync", gbc[:], dram["ln1g"].partition_broadcast(128), writes=["gbc"])
    S.dma("sync", bbc[:], dram["ln1b"].partition_broadcast(128), writes=["bbc"])
    xs = sb([128, 8, 512]); xb = sb([128, 8, 512], BF16)
    ys = [sb([128, 4, 512]) for _ in range(2)]; yb = sb([128, 8, 512], BF16)
    ytmp = sb([128, 512])
    mq = sb([128, 4])
    S.dma("sync", mq[:], dram["maskq"], writes=["mq"])
    ident = sb([128, 128])
    S.dma("sync", ident[:], dram["c_ident"], writes=["ident"])
    hTs = sb([128, 8, 128])
    Q_t = [ps([128, 512]) for _ in range(2)]
    mixT = sb([128, 8, 512], BF16)
    sa = sb([128, 512]); sr = sb([128, 512]); m1 = sb([128, 512]); m2 = sb([128, 512])
    xt = sb([128, 1024]); ht = sb([128, 1024]); junk = sb([128, 1024]); st = sb([128, 4])
    Q_ga = ps([128, 512]); Q_gr = ps([128, 512]); Q_ba = ps([128, 512]); Q_br = ps([128, 512])
    Q_o = [ps([128, 512]) for _ in range(2)]
    xT = dram["xT2"].rearrange("(dc p) n -> p dc n", p=128)
    cout = dram["cout"]
    for g in range(n_groups):
        c0 = g * 512
        for dc in range(8):
            S.dma("sync", xs[:, dc, :], xT[:, dc, c0:c0 + 512], writes=["xs"])
        for dc in range(8):
            G(lambda e, dc=dc: e.tensor_copy(out=xb[:, dc, :], in_=xs[:, dc, :]), r=["xs"], w=["xb"])
        for dc in range(8):
            r0 = (dc % 4) * 256 + (128 if dc < 4 else 0)
            yi = dc % 2
            yk = "ys%d" % yi
            for q in range(4):
                kch = q * 4 + c0 // 1024
                S.dma("sync", ys[yi][:, q, :], cout[kch][r0:r0 + 128, c0 % 1024:c0 % 1024 + 512], writes=[yk])
            V(lambda e, yi=yi: e.tensor_scalar(out=ytmp[:], in0=ys[yi][:, 0, :], scalar1=mq[:, 0:1], scalar2=None, op0=ALU.mult), r=[yk, "mq"], w=["ytmp"])
            for q in (1, 2):
                V(lambda e, yi=yi, q=q: e.scalar_tensor_tensor(out=ytmp[:], in0=ys[yi][:, q, :], scalar=mq[:, q:q + 1], in1=ytmp[:], op0=ALU.mult, op1=ALU.add),
                  r=[yk, "mq", "ytmp"], w=["ytmp"])
            V(lambda e, yi=yi, dc=dc: e.scalar_tensor_tensor(out=yb[:, dc, :], in0=ys[yi][:, 3, :], scalar=mq[:, 3:4], in1=ytmp[:], op0=ALU.mult, op1=ALU.add),
              r=[yk, "mq", "ytmp"], w=["yb"])
        for j in range(8):
            js = slice(j * 128, (j + 1) * 128)
            for dc in range(8):
                T(lambda e, dc=dc, js=js: e.matmul(Q_ga[:], lhsT=Wg[:, dc, js], rhs=xb[:, dc, :], start=(dc == 0), stop=(dc == 7)), r=["Wg", "xb"], w=["Q_ga"])
            for dc in range(8):
                T(lambda e, dc=dc, j=j: e.matmul(Q_gr[:], lhsT=Wg[:, dc, 1024 + j * 128:1024 + (j + 1) * 128], rhs=xb[:, dc, :], start=(dc == 0), stop=(dc == 7)),
                  r=["Wg", "xb"], w=["Q_gr"])
            for dc in range(4):
                T(lambda e, dc=dc, js=js: e.matmul(Q_ba[:], lhsT=Wab[:, dc, js], rhs=yb[:, dc, :], start=(dc == 0), stop=(dc == 3)), r=["Wab", "yb"], w=["Q_ba"])
            for dc in range(4):
                T(lambda e, dc=dc, js=js: e.matmul(Q_br[:], lhsT=Wrb[:, dc, js], rhs=yb[:, 4 + dc, :], start=(dc == 0), stop=(dc == 3)), r=["Wrb", "yb"], w=["Q_br"])
            A(lambda e: e.activation(out=sa[:], in_=Q_ga[:], func=AF.Sigmoid), r=["Q_ga"], w=["sa"])
            A(lambda e: e.activation(out=sr[:], in_=Q_gr[:], func=AF.Sigmoid), r=["Q_gr"], w=["sr"])
            V(lambda e: e.tensor_tensor(out=m1[:], in0=sa[:], in1=Q_ba[:], op=ALU.mult), r=["sa", "Q_ba"], w=["m1"])
            V(lambda e: e.tensor_tensor(out=m2[:], in0=sr[:], in1=Q_br[:], op=ALU.mult), r=["sr", "Q_br"], w=["m2"])
            G(lambda e, j=j: e.tensor_tensor(out=mixT[:, j, :], in0=m1[:], in1=m2[:], op=ALU.add), r=["m1", "m2"], w=["mixT"])
        for tt in range(4):
            tok0 = c0 + tt * 128
            S.dma("sync", xt[:], dram["x2"][tok0:tok0 + 128, :], writes=["xt"])
            for hf in range(2):
                for j in range(8):
                    T(lambda e, j=j, hf=hf, tt=tt: e.matmul(Q_o[hf][:], lhsT=mixT[:, j, tt * 128:(tt + 1) * 128], rhs=Wo[:, j, hf * 512:(hf + 1) * 512],
                                                            start=(j == 0), stop=(j == 7)), r=["mixT", "Wo"], w=["Q_o%d" % hf])
                V(lambda e, hf=hf: e.scalar_tensor_tensor(out=ht[:, hf * 512:(hf + 1) * 512], in0=xt[:, hf * 512:(hf + 1) * 512], scalar=ALPHA, in1=Q_o[hf][:],
                                                          op0=ALU.mult, op1=ALU.add), r=["xt", "Q_o%d" % hf], w=["ht"])
            layer_norm_tile(V, A, G, ht, "ht", junk, st, gbc, bbc, "gbc", "bbc")
            S.dma("sync", dram["hS"][tok0:tok0 + 128, :], ht[:], reads=["ht"])
            for hf in range(2):
                for j4 in range(4):
                    j = hf * 4 + j4
                    T(lambda e, j=j, j4=j4, hf=hf: e.transpose(Q_t[hf][:, j4 * 128:(j4 + 1) * 128], ht[:, j * 128:(j + 1) * 128], ident[:]), r=["ht", "ident"], w=["Q_t%d" % hf])
                A(lambda e, hf=hf: e.copy(out=hTs[:, hf * 4:(hf + 1) * 4, :], in_=Q_t[hf][:].rearrange("p (a n) -> p a n", a=4)), r=["Q_t%d" % hf], w=["hTs"])
            S.dma("sync", dram["hTS"].rearrange("(dc p) n -> p dc n", p=128)[:, :, tok0:tok0 + 128], hTs[:], reads=["hTs"])


def build_moe(nc, S, es, dram, n_quarters=4, n_experts=32):
    sb, ps, V, A, G, T = _mk(nc, S, es, "mo")
    QT_ = 1024
    NSTG = MOE_NSTG
    ident = sb([128, 128])
    S.dma("sync", ident[:], dram["c_ident"], writes=["ident"])
    Wr = sb([128, 8, 32])
    S.dma("sync", Wr[:], dram["w_r"].rearrange("(dc p) n -> p dc n", p=128), writes=["Wr"])
    brb = sb([128, 32])
    S.dma("sync", brb[:], dram["b_r"].partition_broadcast(128), writes=["brb"])
    b1T = sb([128, 32 * 16])
    S.dma("sync", b1T[:], dram["b1T"], writes=["b1T"])
    b1Tp = sb([128, 32 * 16])
    V(lambda e: e.tensor_scalar(out=b1Tp[:], in0=b1T[:], scalar1=1.0, scalar2=None, op0=ALU.add), r=["b1T"], w=["b1Tp"])
    b2s = sb([32, 1024])
    S.dma("sync", b2s[:], dram["b2"], writes=["b2s"])
    gbc = sb([128, 1024]); bbc = sb([128, 1024])
    S.dma("sync", gbc[:], dram["ln2g"].partition_broadcast(128), writes=["gbc"])
    S.dma("sync", bbc[:], dram["ln2b"].partition_broadcast(128), writes=["bbc"])
    hb = sb([128, 8, QT_], BF16)
    acc = sb([128, 8, 1024])
    GW = sb([128, 8, 32])
    lg = sb([128, 32]); mx = sb([128, 8]); nm0 = sb([128, 1]); ex = sb([128, 32]); msk = sb([128, 32]); den = sb([128, 1]); gT = sb([32, 128])
    htk = sb([128, 1024]); junk = sb([128, 1024]); st8 = sb([128, 32])
    stg = [sb([128, 2048]) for _ in range(NSTG)]
    w1b = sb([128, 8, 2048], BF16); w2b = sb([128, 8, 1024], BF16)
    actT = sb([128, 8, QT_], BF16)
    gg = [sb([128, 512]) for _ in range(2)]; sg = [sb([128, 512]) for _ in range(2)]; ll = [sb([128, 512]) for _ in range(2)]
    Q_r = ps([128, 512]); Q_g = [ps([128, 512]) for _ in range(2)]; Q_l = [ps([128, 512]) for _ in range(2)]; Q_y = [ps([128, 512]) for _ in range(2)]
    hT = dram["hTS"].rearrange("(dc p) n -> p dc n", p=128)
    si = 0
    fci = 0
    for qt in range(n_quarters):
        t0 = qt * QT_
        for g2 in range(QT_ // 512):
            hsv = [stg[0][:].rearrange("p (a n) -> p a n", a=4), stg[1][:].rearrange("p (a n) -> p a n", a=4)]
            hs_dc = lambda dc: hsv[dc // 4][:, dc % 4, :]
            for dc in range(8):
                S.dma("sync", hs_dc(dc), hT[:, dc, t0 + g2 * 512:t0 + (g2 + 1) * 512], writes=["stg%d" % (dc // 4)])
            for t4 in range(4):
                tt = g2 * 4 + t4
                ts_ = slice(t4 * 128, (t4 + 1) * 128)
                for dc in range(8):
                    T(lambda e, dc=dc, ts_=ts_: e.matmul(Q_r[:, 0:32], lhsT=hs_dc(dc)[:, ts_], rhs=Wr[:, dc, :], start=(dc == 0), stop=(dc == 7)),
                      r=["stg0", "stg1", "Wr"], w=["Q_r.l"])
                V(lambda e: e.tensor_tensor(out=lg[:], in0=Q_r[:, 0:32], in1=brb[:], op=ALU.add), r=["Q_r.l", "brb"], w=["lg"])
                V(lambda e: e.max(out=mx[:], in_=lg[:]), r=["lg"], w=["mx"])
                V(lambda e: e.tensor_scalar(out=nm0[:], in0=mx[:, 0:1], scalar1=-1.0, scalar2=None, op0=ALU.mult), r=["mx"], w=["nm0"])
                A(lambda e: e.activation(out=ex[:], in_=lg[:], func=AF.Exp, bias=nm0[:, 0:1], scale=1.0), r=["lg", "nm0"], w=["ex"])
                V(lambda e: e.tensor_scalar(out=msk[:], in0=lg[:], scalar1=mx[:, 3:4], scalar2=None, op0=ALU.is_ge), r=["lg", "mx"], w=["msk"])
                V(lambda e: e.tensor_tensor(out=ex[:], in0=ex[:], in1=msk[:], op=ALU.mult), r=["ex", "msk"], w=["ex"])
                V(lambda e: e.tensor_reduce(out=den[:], in_=ex[:], axis=AX.X, op=ALU.add), r=["ex"], w=["den"])
                V(lambda e: e.reciprocal(out=den[:], in_=den[:]), r=["den"], w=["den"])
                V(lambda e, tt=tt: e.tensor_scalar(out=GW[:, tt, :], in0=ex[:], scalar1=den[:, 0:1], scalar2=None, op0=ALU.mult), r=["ex", "den"], w=["GW"])
                T(lambda e, tt=tt: e.transpose(Q_r[0:32, 128:256], GW[:, tt, :], ident[:]), r=["GW", "ident"], w=["Q_r.t"])
                V(lambda e: e.tensor_copy(out=gT[:], in_=Q_r[0:32, 128:256]), r=["Q_r.t"], w=["gT"])
                S.dma("sync", htk[:], dram["hS"][t0 + tt * 128:t0 + (tt + 1) * 128, :], writes=["htk"])
                for hf in range(2):
                    T(lambda e, hf=hf: e.matmul(Q_y[hf][:], lhsT=gT[:], rhs=b2s[:, hf * 512:(hf + 1) * 512], start=True, stop=True), r=["gT", "b2s"], w=["Q_y%d" % hf])
                    V(lambda e, hf=hf, tt=tt: e.scalar_tensor_tensor(out=acc[:, tt, hf * 512:(hf + 1) * 512], in0=htk[:, hf * 512:(hf + 1) * 512], scalar=ALPHA, in1=Q_y[hf][:],
                                                                     op0=ALU.mult, op1=ALU.add), r=["htk", "Q_y%d" % hf], w=["acc%d" % tt])
            for dc in range(8):
                if dc % 2 == 0:
                    V(lambda e, dc=dc, g2=g2: e.tensor_copy(out=hb[:, dc, g2 * 512:(g2 + 1) * 512], in_=hs_dc(dc)), r=["stg%d" % (dc // 4)], w=["hb"])
                else:
                    A(lambda e, dc=dc, g2=g2: e.copy(out=hb[:, dc, g2 * 512:(g2 + 1) * 512], in_=hs_dc(dc)), r=["stg%d" % (dc // 4)], w=["hb"])
        for ex_i in range(n_experts):
            w1v = dram["w1"][ex_i].rearrange("(dc p) n -> p dc n", p=128)
            w2v = dram["w2"][ex_i].rearrange("(dc p) n -> p dc n", p=128)
            for dc in range(8):
                k = "stg%d" % (si % NSTG); st_ = stg[si % NSTG]; si += 1
                S.dma("sync", st_[:], w1v[:, dc, :], writes=[k])
                A(lambda e, dc=dc, st_=st_: e.copy(out=w1b[:, dc, :], in_=st_[:]), r=[k], w=["w1b"])
            for d2 in range(4):
                k = "stg%d" % (si % NSTG); st_ = stg[si % NSTG]; si += 1
                S.dma("sync", st_[:].rearrange("p (a n) -> p a n", a=2), w2v[:, 2 * d2:2 * d2 + 2, :], writes=[k])
                if d2 % 2 == 0:
                    V(lambda e, d2=d2, st_=st_: e.tensor_copy(out=w2b[:, 2 * d2:2 * d2 + 2, :], in_=st_[:].rearrange("p (a n) -> p a n", a=2)), r=[k], w=["w2b"])
                else:
                    A(lambda e, d2=d2, st_=st_: e.copy(out=w2b[:, 2 * d2:2 * d2 + 2, :], in_=st_[:].rearrange("p (a n) -> p a n", a=2)), r=[k], w=["w2b"])
            for tg in range(QT_ // 512):
                gs = slice(tg * 512, (tg + 1) * 512)
                for fc in range(8):
                    bi = fci % 2
                    fci += 1
                    kg = "Q_g%d" % bi; kl = "Q_l%d" % bi
                    for dc in range(8):
                        T(lambda e, dc=dc, fc=fc, gs=gs, bi=bi: e.matmul(Q_g[bi][:], lhsT=w1b[:, dc, fc * 128:(fc + 1) * 128], rhs=hb[:, dc, gs], start=(dc == 0), stop=(dc == 7)),
                          r=["w1b", "hb"], w=[kg])
                    for dc in range(8):
                        T(lambda e, dc=dc, fc=fc, gs=gs, bi=bi: e.matmul(Q_l[bi][:], lhsT=w1b[:, dc, 1024 + fc * 128:1024 + (fc + 1) * 128], rhs=hb[:, dc, gs], start=(dc == 0), stop=(dc == 7)),
                          r=["w1b", "hb"], w=[kl])
                    bg = b1T[:, ex_i * 16 + fc:ex_i * 16 + fc + 1]
                    bl = b1Tp[:, ex_i * 16 + 8 + fc:ex_i * 16 + 8 + fc + 1]
                    V(lambda e, bg=bg, bi=bi: e.tensor_scalar(out=gg[bi][:], in0=Q_g[bi][:], scalar1=bg, scalar2=7.0, op0=ALU.add, op1=ALU.min), r=[kg, "b1T"], w=["gg%d" % bi])
                    A(lambda e, bi=bi: e.activation(out=sg[bi][:], in_=gg[bi][:], func=AF.Sigmoid, scale=1.702), r=["gg%d" % bi], w=["sg%d" % bi])
                    V(lambda e, bl=bl, bi=bi: e.tensor_scalar(out=ll[bi][:], in0=Q_l[bi][:], scalar1=bl, scalar2=8.0, op0=ALU.add, op1=ALU.min), r=[kl, "b1Tp"], w=["ll%d" % bi])
                    G(lambda e, bi=bi: e.tensor_tensor(out=gg[bi][:], in0=gg[bi][:], in1=sg[bi][:], op=ALU.mult), r=["gg%d" % bi, "sg%d" % bi], w=["gg%d" % bi])
                    V(lambda e, fc=fc, gs=gs, bi=bi: e.scalar_tensor_tensor(out=actT[:, fc, gs], in0=ll[bi][:], scalar=-6.0, in1=gg[bi][:], op0=ALU.max, op1=ALU.mult),
                      r=["gg%d" % bi, "ll%d" % bi], w=["actT%d" % tg])
            for tile_i in range(QT_ // 128):
                tg = tile_i // 4
                for hf in range(2):
                    for fc in range(8):
                        T(lambda e, fc=fc, tile_i=tile_i, hf=hf: e.matmul(Q_y[hf][:], lhsT=actT[:, fc, tile_i * 128:(tile_i + 1) * 128], rhs=w2b[:, fc, hf * 512:(hf + 1) * 512],
                                                                          start=(fc == 0), stop=(fc == 7)), r=["actT%d" % tg, "w2b"], w=["Q_y%d" % hf])
                    V(lambda e, hf=hf, tile_i=tile_i, ex_i=ex_i: e.scalar_tensor_tensor(
                        out=acc[:, tile_i, hf * 512:(hf + 1) * 512], in0=Q_y[hf][:], scalar=GW[:, tile_i, ex_i:ex_i + 1],
                        in1=acc[:, tile_i, hf * 512:(hf + 1) * 512], op0=ALU.mult, op1=ALU.add), r=["Q_y%d" % hf, "GW", "acc%d" % tile_i], w=["acc%d" % tile_i])
        layer_norm_multi(V, A, G, [acc[:, tt, :] for tt in range(8)], ["acc%d" % tt for tt in range(8)], junk, st8, gbc, bbc, "gbc", "bbc")
        for tt in range(8):
            S.dma("sync", dram["out"][t0 + tt * 128:t0 + (tt + 1) * 128, :], acc[:, tt, :], reads=["acc%d" % tt])


def _dt(v):
    return F32 if v.dtype == np.float32 else BF16


_SZ = {}


def build_all(nc, S, dram, cc_sem):
    with contextlib.ExitStack() as e1:
        build_rwkv(nc, S, e1, dram, **_SZ.get('rwkv', {}))
        S.barrier()
        S.flush()
    with contextlib.ExitStack() as e2:
        build_attn(nc, S, e2, dram, cc={'groups': [[0, 1, 2, 3], [4, 5, 6, 7]], 'sem': cc_sem}, **_SZ.get('attn', {}))
        S.barrier()
        S.flush()
    S.barrier()
    S.flush()
    with contextlib.ExitStack() as e3:
        build_stage_a(nc, S, e3, dram, **_SZ.get('sa', {}))
        S.barrier()
        S.flush()
    with contextlib.ExitStack() as e4:
        build_moe(nc, S, e4, dram, **_SZ.get('moe', {}))
        S.emit()


def kernel(x, w_in, mu_shift, w0, w_decay_up, a0, w_aaa_up, w_gate_up, k_k, k_a, r_k,
           lnx_g, lnx_b, w_attn_br, w_rwkv_br, w_out, ln1_g, ln1_b,
           w_router, b_router, w1, b1, w2, b2, ln2_g, ln2_b):
    f = lambda a: np.ascontiguousarray(np.asarray(a, dtype=np.float32))
    x = f(x); w_in = f(w_in)[0]; mu = f(mu_shift)[0]
    B, S_, D = x.shape
    NC = 8
    TQ = S_ // 4
    xTp = []
    for b in range(B):
        t = np.zeros((D, S_ + 1), np.float32)
        t[:, 1:] = x[b].T
        xTp.append(t)
    rc = rwkv_consts()
    ac = attn_consts()
    OQ = 1824
    row = lambda v: np.ascontiguousarray(v[None, :])
    w_gate = np.ascontiguousarray(w_in[:, OQ + 1536:OQ + 1536 + 2048])
    w1p = f(w1)[0][:_SZ.get("ne", 32)]
    w1p = np.ascontiguousarray(np.concatenate([w1p[:, :, 0::2], w1p[:, :, 1::2]], 2))
    b1p = f(b1)[0]
    b1p = np.concatenate([b1p[:, 0::2], b1p[:, 1::2]], 1)
    b1T = np.ascontiguousarray(b1p.reshape(32, 16, 128).transpose(2, 0, 1).reshape(128, 512))
    shared = {
        "w_lo": np.ascontiguousarray(w_in[:, 1536:1824]), "mu_lo": row(mu[1536:1824]),
        "w_gate": w_gate, "w_ab": f(w_attn_br)[0], "w_rb": f(w_rwkv_br)[0], "w_out": f(w_out)[0],
        "ln1g": row(f(ln1_g)[0]), "ln1b": row(f(ln1_b)[0]),
        "w_r": f(w_router)[0], "b_r": row(f(b_router)[0]), "w1": w1p, "b1T": b1T, "w2": f(w2)[0][:_SZ.get("ne", 32)], "b2": f(b2)[0],
        "ln2g": row(f(ln2_g)[0]), "ln2b": row(f(ln2_b)[0]),
    }
    shared.update(rc)
    shared.update(ac)
    in_maps = []
    for c in range(NC):
        b, hp = c // 4, c % 4
        tq = hp
        hc = slice(128 * hp, 128 * hp + 128)
        ts = slice(tq * TQ, (tq + 1) * TQ)
        mq = np.zeros((128, 4), np.float32); mq[:, tq] = 1.0
        m = {
            "xT": xTp[b],
            "w_rkv": np.ascontiguousarray(np.concatenate([w_in[:, 0:512][:, hc], w_in[:, 512:1024][:, hc], w_in[:, 1024:1536][:, hc]], 1)),
            "mu_rkv": row(np.concatenate([mu[0:512][hc], mu[512:1024][hc], mu[1024:1536][hc]])),
            "wdu": np.ascontiguousarray(f(w_decay_up)[0][:, hc]), "wau": np.ascontiguousarray(f(w_aaa_up)[0][:, hc]),
            "wgu": np.ascontiguousarray(f(w_gate_up)[0][:, hc]),
            "w0": row(f(w0)[0][hc]), "a0": row(f(a0)[0][hc]), "kk": row(f(k_k)[0][hc]), "ka": row(f(k_a)[0][hc]),
            "rk": row(f(r_k)[0].reshape(-1)[hc]), "lng": row(f(lnx_g)[0][hc]), "lnb": row(f(lnx_b)[0][hc]),
            "w_qkv": np.ascontiguousarray(np.concatenate([w_in[:, OQ:OQ + 512][:, hc], w_in[:, OQ + 512:OQ + 1024][:, hc],
                                                          w_in[:, OQ + 1024:OQ + 1536][:, hc]], 1)),
            "xT2": np.ascontiguousarray(x[b, ts].T), "x2": np.ascontiguousarray(x[b, ts]),
            "maskq": mq,
        }
        m.update(shared)
        in_maps.append(m)
    nc = bass.Bass("TRN2", target_bir_lowering=False)
    dram = {}
    for k, v in in_maps[0].items():
        dram[k] = nc.dram_tensor(k, list(v.shape), _dt(v), kind="ExternalInput").ap()
    dram["out"] = nc.dram_tensor("out", [TQ, D], F32, kind="ExternalOutput").ap()
    dram["cin"] = [nc.dram_tensor("cin%d" % k, [256, 1024], F32, kind="Internal").ap() for k in range(16)]
    dram["cout"] = [nc.dram_tensor("cout%d" % k, [1024, 1024], F32, kind="Internal").ap() for k in range(16)]
    dram["hS"] = nc.dram_tensor("hS", [TQ, D], F32, kind="Internal").ap()
    dram["hTS"] = nc.dram_tensor("hTS", [D, TQ], F32, kind="Internal").ap()
    with contextlib.ExitStack() as es:
        S = Sched(nc, es)
        cc_sem = es.enter_context(nc.semaphore("cc_sem"))
        build_all(nc, S, dram, cc_sem)
    res = run_bass_kernel_spmd(nc, in_maps, core_ids=list(range(NC)))
    out = np.zeros((B, S_, D), np.float32)
    for c in range(NC):
        b, tq = c // 4, c % 4
        out[b, tq * TQ:(tq + 1) * TQ] = res.results[c]["out"]
    return out
```

```python
import contextlib
import numpy as np
import ml_dtypes
import concourse.bass as bass
import concourse.mybir as mybir
from concourse.bass_utils import run_bass_kernel_spmd

F32 = mybir.dt.float32
BF16 = mybir.dt.bfloat16
AF = mybir.ActivationFunctionType
ALU = mybir.AluOpType
AX = mybir.AxisListType


ENGS = ("sync", "tensor", "vector", "scalar", "gpsimd")


class Op:
    __slots__ = ("eng", "fn", "deps", "marked", "dma", "sem", "val", "idx", "inc")

    def __init__(self, eng, fn, dma):
        self.eng = eng
        self.fn = fn
        self.deps = []
        self.marked = False
        self.dma = dma
        self.sem = None
        self.val = 0
        self.inc = 16


class Sched:
    def __init__(self, nc, es, n_dma_sems=12, same_engine_sync=True):
        self.nc = nc
        self.es = es
        self.q = {e: [] for e in ENGS}
        self.last_w = {}
        self.readers = {}
        self.same_engine_sync = same_engine_sync
        self.fence = []
        self.esem = {e: es.enter_context(nc.semaphore("s_" + e)) for e in ENGS if e != "sync"}
        self.dsem = {}
        self.dcnt = {}
        self.dlast = {}
        self.dnext = {}
        for e in ("sync", "scalar", "gpsimd"):
            self.dsem[e] = [es.enter_context(nc.semaphore("d_%s_%d" % (e, i))) for i in range(n_dma_sems)]
            self.dcnt[e] = [0] * n_dma_sems
            self.dlast[e] = [None] * n_dma_sems
            self.dnext[e] = 0

    def _add(self, op, reads, writes):
        deps = []
        for k in reads:
            w = self.last_w.get(k)
            if w is not None:
                deps.append(w)
        for k in writes:
            w = self.last_w.get(k)
            if w is not None:
                deps.append(w)
            deps.extend(self.readers.get(k, ()))
        for k in writes:
            self.last_w[k] = op
            self.readers[k] = []
        for k in reads:
            self.readers.setdefault(k, []).append(op)
        for d in deps:
            if d is op:
                continue
            if (not d.dma) and d.eng == op.eng and (not op.dma):
                if d.eng == "tensor" or not self.same_engine_sync:
                    continue
            op.deps.append(d)
        op.deps.extend(self.fence)
        self.q[op.eng].append(op)
        return op

    def barrier(self):
        f = []
        for e in ENGS:
            if self.q[e]:
                f.append(self.q[e][-1])
        for e in ("sync", "scalar", "gpsimd"):
            for o in self.dlast[e]:
                if o is not None:
                    f.append(o)
        f.extend(getattr(self, "async_ops", []))
        for o in f:
            o.marked = True
        self.fence = f
        self.last_w = {}
        self.readers = {}

    def async_op(self, eng, fn, sem, reads=(), writes=()):
        op = Op(eng, fn, True)
        if not hasattr(self, "async_ops"):
            self.async_ops = []
        op.sem = sem
        op.val = len(self.async_ops) + 1
        op.inc = 1
        self.async_ops.append(op)
        return self._add(op, reads, writes)

    def op(self, eng, fn, reads=(), writes=()):
        return self._add(Op(eng, fn, False), reads, writes)

    def dma(self, eng, out, in_, reads=(), writes=(), **kw):
        op = Op(eng, None, True)
        i = self.dnext[eng]
        self.dnext[eng] = (i + 1) % len(self.dsem[eng])
        self.dcnt[eng][i] += 1
        op.sem = self.dsem[eng][i]
        op.val = 16 * self.dcnt[eng][i]
        prev = self.dlast[eng][i]
        if prev is not None:
            op.deps.append(prev)
        self.dlast[eng][i] = op
        op.fn = lambda e, out=out, in_=in_, kw=kw: e.dma_start(out=out, in_=in_, **kw)
        return self._add(op, reads, writes)

    def flush(self):
        nc = self.nc
        if not hasattr(self, "_pos"):
            self._pos = {e: 0 for e in ENGS}
            self._cnt = {e: 0 for e in ENGS}
            self._waited = {e: {} for e in ENGS}
        pend = {e: self.q[e][self._pos[e]:] for e in ENGS}
        for e in ENGS:
            for op in pend[e]:
                for d in op.deps:
                    d.marked = True
        for e in ENGS:
            c = self._cnt[e]
            for op in pend[e]:
                if (not op.dma) and op.marked and op.sem is None:
                    c += 1
                    op.sem = self.esem.get(e)
                    op.val = c
            self._cnt[e] = c
            self._pos[e] = len(self.q[e])

        def run(eng, e):
            waited = self._waited[e]
            for op in pend[e]:
                need = {}
                for d in op.deps:
                    if d.sem is None:
                        continue
                    k = id(d.sem)
                    if waited.get(k, 0) >= d.val:
                        continue
                    if k not in need or need[k][1] < d.val:
                        need[k] = (d.sem, d.val)
                for k, (s, v) in need.items():
                    eng.wait_ge(s, v)
                    waited[k] = v
                if op.fn is None:
                    continue
                ins = op.fn(eng)
                if op.dma:
                    ins.then_inc(op.sem, op.inc)
                elif op.marked:
                    ins.then_inc(op.sem, 1)

        with nc.Block() as block:
            @block.sync
            def _(eng):
                run(eng, "sync")

            @block.tensor
            def _(eng):
                run(eng, "tensor")

            @block.vector
            def _(eng):
                run(eng, "vector")

            @block.scalar
            def _(eng):
                run(eng, "scalar")

            @block.gpsimd
            def _(eng):
                run(eng, "gpsimd")

    def emit(self, final_wait_engine="sync"):
        self.barrier()
        op = Op(final_wait_engine, None, False)
        op.deps = list(self.fence)
        self.q[final_wait_engine].append(op)
        self.flush()


S_TOK = 16384
XB = 512
C = 128
GN_EPS = 64e-5
C0 = -float(np.exp(-0.5))


def rwkv_consts():
    i = np.arange(128)
    m1 = (i[:, None] <= i[None, :]).astype(np.float32)
    m2 = (i[:, None] > i[None, :]).astype(np.float32)
    mus = (i[:, None] < i[None, :]).astype(np.float32)
    mls = mus.T.copy()
    ident = np.eye(128, dtype=np.float32)
    mask4 = np.stack([mus, m1, mus, m1], axis=1)
    return {"c_m1": m1, "c_m2": m2, "c_mus": mus, "c_mls": mls, "c_ident": ident,
            "c_mask4": np.ascontiguousarray(mask4)}


def build_rwkv(nc, S, es, dram, n_chunks=S_TOK // C):
    sbn = [0]

    def sb(shape, dt=F32, name=None):
        sbn[0] += 1
        return es.enter_context(nc.sbuf_tensor(name or ("rw%d" % sbn[0]), shape, dt))

    def ps(shape, dt=F32, name=None):
        sbn[0] += 1
        return es.enter_context(nc.psum_tensor(name or ("rp%d" % sbn[0]), shape, dt))

    BANKS = {"P_RKV": 0, "P_Z": 2, "P_T": 4, "P_L1a": 5, "P_L1b": 1, "P_LDa": 6, "P_LDb": 3,
             "P_G": 7, "P_U": 7, "P_H": 7, "P_Y": 7, "P_OT": 7}

    def _bk(r, w):
        b = set()
        for k in list(r) + list(w):
            if k.startswith("P_"):
                for pfx, bn in BANKS.items():
                    if k.startswith(pfx):
                        b.add(bn)
        return list(w) + ["BANK%d" % x for x in b]

    defer = [None]

    def _rec(eng, fn, r, w):
        if defer[0] is None:
            S.op(eng, fn, r, w)
        else:
            defer[0].append((eng, fn, r, w))

    def DMA(out, in_, reads=(), writes=()):
        if defer[0] is None:
            S.dma("sync", out, in_, reads=reads, writes=writes)
        else:
            defer[0].append(("dma", (out, in_), reads, writes))

    def drain(lst, n):
        for _ in range(min(n, len(lst))):
            eng, fn, r, w = lst.pop(0)
            if eng == "dma":
                S.dma("sync", fn[0], fn[1], reads=r, writes=w)
            else:
                S.op(eng, fn, r, w)

    V = lambda fn, r=(), w=(): _rec("vector", fn, r, _bk(r, w))
    A = lambda fn, r=(), w=(): _rec("scalar", fn, r, _bk(r, w))
    G = lambda fn, r=(), w=(): _rec("gpsimd", fn, r, list(w))
    T = lambda fn, r=(), w=(): _rec("tensor", fn, r, _bk(r, w))

    cst = {}
    for nm in ("c_m1", "c_m2", "c_mus", "c_mls", "c_ident"):
        t = sb([128, 128]); cst[nm] = t
        S.dma("sync", t[:], dram[nm], writes=[nm])
    mask4 = sb([128, 4, 128])
    S.dma("sync", mask4[:], dram["c_mask4"], writes=["mask4"])
    ones_col = sb([128, 1])
    V(lambda e: e.memset(ones_col[:], 1.0), w=["ones_col"])
    bc = {}
    for nm in ("w0", "a0", "kk", "ka", "rk", "lng", "lnb"):
        t = sb([128, 128]); bc[nm] = t
        S.dma("sync", t[:], dram[nm].partition_broadcast(128), writes=["bc_" + nm])
    NW = 384 + 288
    wst = sb([128, 8, NW])
    S.dma("sync", wst[:, :, 0:384], dram["w_rkv"].rearrange("(dc p) n -> p dc n", p=128), writes=["wst"])
    S.dma("sync", wst[:, :, 384:NW], dram["w_lo"].rearrange("(dc p) n -> p dc n", p=128), writes=["wst"])
    mub = sb([128, NW])
    S.dma("sync", mub[:, 0:384], dram["mu_rkv"].partition_broadcast(128), writes=["mub"])
    S.dma("sync", mub[:, 384:NW], dram["mu_lo"].partition_broadcast(128), writes=["mub"])
    omu = sb([128, NW])
    V(lambda e: e.tensor_scalar(out=omu[:], in0=mub[:], scalar1=-1.0, scalar2=1.0, op0=ALU.mult, op1=ALU.add), r=["mub"], w=["omu"])
    Wa = sb([128, 8, NW], BF16); Wb = sb([128, 8, NW], BF16)
    for dc in range(8):
        V(lambda e, dc=dc: e.tensor_tensor(out=Wa[:, dc, :], in0=wst[:, dc, :], in1=omu[:], op=ALU.mult), r=["wst", "omu"], w=["Wa"])
        G(lambda e, dc=dc: e.tensor_tensor(out=Wb[:, dc, :], in0=wst[:, dc, :], in1=mub[:], op=ALU.mult), r=["wst", "mub"], w=["Wb"])
    wdu_f = sb([64, 128]); wau_f = sb([64, 128]); wgu0_f = sb([128, 128]); wgu1_f = sb([32, 128])
    S.dma("sync", wdu_f[:], dram["wdu"], writes=["wdu_f"])
    S.dma("sync", wau_f[:], dram["wau"], writes=["wau_f"])
    S.dma("sync", wgu0_f[:], dram["wgu"][0:128, :], writes=["wgu0_f"])
    S.dma("sync", wgu1_f[:], dram["wgu"][128:160, :], writes=["wgu1_f"])
    wdu = sb([64, 128], BF16); wau = sb([64, 128], BF16); wgu0 = sb([128, 128], BF16); wgu1 = sb([32, 128], BF16)
    V(lambda e: e.tensor_copy(out=wdu[:], in_=wdu_f[:]), r=["wdu_f"], w=["wdu"])
    V(lambda e: e.tensor_copy(out=wau[:], in_=wau_f[:]), r=["wau_f"], w=["wau"])
    V(lambda e: e.tensor_copy(out=wgu0[:], in_=wgu0_f[:]), r=["wgu0_f"], w=["wgu0"])
    V(lambda e: e.tensor_copy(out=wgu1[:], in_=wgu1_f[:]), r=["wgu1_f"], w=["wgu1"])

    xs = [sb([128, 8, XB + 1]) for _ in range(2)]
    xb = [sb([128, 8, XB + 1], BF16) for _ in range(2)]
    hwT = sb([64, XB], BF16); haT = sb([64, XB], BF16); hg0T = sb([128, XB], BF16); hg1T = sb([32, XB], BF16)
    Hst = sb([128, 64])
    V(lambda e: e.memset(Hst[:], 0.0), w=["Hst"])
    Bpad = [sb([128, 2, 128]) for _ in range(2)]; Kpad = [sb([128, 2, 128]) for _ in range(2)]
    for p_ in range(2):
        V(lambda e, p_=p_: e.memset(Bpad[p_][:], 0.0), w=["Bpad%d" % p_])
        V(lambda e, p_=p_: e.memset(Kpad[p_][:], 0.0), w=["Kpad%d" % p_])

    P_RKV = ps([128, 512]); P_Z = ps([128, 4, 128])
    P_T = ps([128, 4, 128]); P_L1 = [ps([128, 4, 128]) for _ in range(2)]; P_LD = [ps([128, 4, 128]) for _ in range(2)]; P_M = ps([128, 512])
    P_LH = P_T[:].rearrange("p a n -> p (a n)")

    def t(shape=(128, 128), dt=F32):
        return sb(list(shape), dt)

    zw = t(); logw = t(); av = t(); gv = [t() for _ in range(2)]; kkr = t(); sq = t(); ss = t((128, 2)); rn = t((128, 2))
    kk = t(); tmp1 = t(); kmod = t(); kka = t(); rkr = t(); sbon = [t((128, 2)) for _ in range(2)]; vsb = [t() for _ in range(2)]; sq2 = t()
    eW = t(); eWi = t(); eWp = t(); eR = t(); wc = [t((128, 1)) for _ in range(2)]
    TM = sb([128, 4, 128])
    FT = [sb([128, 4, 128]) for _ in range(2)]
    LM1 = [[sb([128, 4, 128]) for _ in range(2)] for _ in range(2)]
    PN = [[[sb([128, 2, 128]) for _ in range(2)] for _ in range(2)] for _ in range(2)]
    Lk = [[t() for _ in range(2)] for _ in range(2)]
    Gs = [t((128, 64)) for _ in range(2)]; Us = [t((128, 64)) for _ in range(2)]
    s1 = t((128, 2)); nmean = t((128, 2)); Yc = t(); ss2 = t((128, 2)); rstd = t((128, 2)); Yn = t(); Yo = t(); YoT = t()

    n_blocks = (n_chunks * C) // XB
    xT = dram["xT"].rearrange("(dc p) n -> p dc n", p=128)

    NCPB = XB // C

    def gen_X(ch):
        blk = ch // NCPB
        ci = ch % NCPB
        o = ci * C
        p = ch % 2
        xi = blk % 2
        kx = "xs%d" % xi; kb = "xb%d" % xi
        c0 = blk * XB
        if ci == 0:
            xi = blk % 2
            kx = "xs%d" % xi; kb = "xb%d" % xi
            c0 = blk * XB
            for dc in range(8):
                DMA(xs[xi][:, dc, :], xT[:, dc, c0:c0 + XB + 1], writes=[kx])
            for dc in range(8):
                eng = G if dc % 2 == 0 else A
                if dc % 2 == 0:
                    G(lambda e, dc=dc, xi=xi: e.tensor_copy(out=xb[xi][:, dc, :], in_=xs[xi][:, dc, :]), r=[kx], w=[kb])
                else:
                    A(lambda e, dc=dc, xi=xi: e.copy(out=xb[xi][:, dc, :], in_=xs[xi][:, dc, :]), r=[kx], w=[kb])
            for (lo, n, dst, fn, key) in ((384, 64, hwT, AF.Tanh, "hwT"), (448, 64, haT, AF.Copy, "haT"),
                                          (512, 128, hg0T, AF.Sigmoid, "hg0T"), (640, 32, hg1T, AF.Sigmoid, "hg1T")):
                for dc in range(8):
                    T(lambda e, dc=dc, lo=lo, n=n, xi=xi: e.matmul(P_LH[0:n, :], lhsT=Wa[:, dc, lo:lo + n], rhs=xb[xi][:, dc, 1:XB + 1],
                                                                  start=(dc == 0), stop=False), r=["Wa", kb], w=["P_T"])
                for dc in range(8):
                    T(lambda e, dc=dc, lo=lo, n=n, xi=xi: e.matmul(P_LH[0:n, :], lhsT=Wb[:, dc, lo:lo + n], rhs=xb[xi][:, dc, 0:XB],
                                                                  start=False, stop=(dc == 7)), r=["Wb", kb], w=["P_T"])
                A(lambda e, n=n, dst=dst, fn=fn: e.activation(out=dst[:], in_=P_LH[0:n, :], func=fn), r=["P_T"], w=[key])

        for dc in range(8):
            T(lambda e, dc=dc, xi=xi, o=o: e.matmul(P_RKV[:, 0:384], lhsT=xb[xi][:, dc, 1 + o:1 + o + C], rhs=Wa[:, dc, 0:384],
                                                    start=(dc == 0), stop=False), r=["Wa", kb], w=["P_RKV"])
        for dc in range(8):
            T(lambda e, dc=dc, xi=xi, o=o: e.matmul(P_RKV[:, 0:384], lhsT=xb[xi][:, dc, o:o + C], rhs=Wb[:, dc, 0:384],
                                                    start=False, stop=(dc == 7)), r=["Wb", kb], w=["P_RKV"])
        T(lambda e, o=o: e.matmul(P_Z[:, 0, :], lhsT=hwT[:, o:o + C], rhs=wdu[:], start=True, stop=True), r=["hwT", "wdu"], w=["P_Z0"])
        T(lambda e, o=o: e.matmul(P_Z[:, 1, :], lhsT=haT[:, o:o + C], rhs=wau[:], start=True, stop=True), r=["haT", "wau"], w=["P_Z1"])
        T(lambda e, o=o: e.matmul(P_RKV[:, 384:512], lhsT=hg0T[:, o:o + C], rhs=wgu0[:], start=True, stop=False), r=["hg0T", "wgu0"], w=["P_RKVg"])
        T(lambda e, o=o: e.matmul(P_RKV[:, 384:512], lhsT=hg1T[:, o:o + C], rhs=wgu1[:], start=False, stop=True), r=["hg1T", "wgu1"], w=["P_RKVg"])
        V(lambda e: e.tensor_tensor(out=zw[:], in0=P_Z[:, 0, :], in1=bc["w0"][:], op=ALU.add), r=["P_Z0", "bc_w0"], w=["zw"])
        A(lambda e: e.activation(out=zw[:], in_=zw[:], func=AF.Sigmoid), r=["zw"], w=["zw"])
        G(lambda e: e.tensor_scalar(out=logw[:], in0=zw[:], scalar1=C0, scalar2=None, op0=ALU.mult), r=["zw"], w=["logw"])
        V(lambda e: e.tensor_tensor(out=av[:], in0=P_Z[:, 1, :], in1=bc["a0"][:], op=ALU.add), r=["P_Z1", "bc_a0"], w=["av"])
        A(lambda e: e.activation(out=av[:], in_=av[:], func=AF.Sigmoid), r=["av"], w=["av"])
        A(lambda e: e.copy(out=gv[p][:], in_=P_RKV[:, 384:512]), r=["P_RKVg"], w=["gv%d" % p])
        A(lambda e: e.copy(out=vsb[p][:], in_=P_RKV[:, 256:384]), r=["P_RKV"], w=["vsb%d" % p])
        V(lambda e: e.tensor_tensor(out=kkr[:], in0=P_RKV[:, 128:256], in1=bc["kk"][:], op=ALU.mult), r=["P_RKV", "bc_kk"], w=["kkr"])
        G(lambda e: e.tensor_tensor(out=sq[:], in0=kkr[:], in1=kkr[:], op=ALU.mult), r=["kkr"], w=["sq"])
        V(lambda e: e.tensor_reduce(out=ss[:], in_=sq[:].rearrange("p (h c) -> p h c", h=2), axis=AX.X, op=ALU.add), r=["sq"], w=["ss"])
        A(lambda e: e.activation(out=ss[:], in_=ss[:], func=AF.Sqrt), r=["ss"], w=["ss"])
        V(lambda e: e.tensor_scalar(out=ss[:], in0=ss[:], scalar1=1e-12, scalar2=None, op0=ALU.max), r=["ss"], w=["ss"])
        V(lambda e: e.reciprocal(out=rn[:], in_=ss[:]), r=["ss"], w=["rn"])
        for h in range(2):
            V(lambda e, h=h: e.tensor_scalar(out=kk[:, h * 64:(h + 1) * 64], in0=kkr[:, h * 64:(h + 1) * 64], scalar1=rn[:, h:h + 1],
                                             scalar2=None, op0=ALU.mult), r=["kkr", "rn"], w=["kk"])
        V(lambda e: e.scalar_tensor_tensor(out=tmp1[:], in0=av[:], scalar=-1.0, in1=bc["ka"][:], op0=ALU.add, op1=ALU.mult), r=["av", "bc_ka"], w=["tmp1"])
        V(lambda e: e.scalar_tensor_tensor(out=kmod[:], in0=tmp1[:], scalar=1.0, in1=P_RKV[:, 128:256], op0=ALU.add, op1=ALU.mult), r=["tmp1", "P_RKV"], w=["kmod"])
        G(lambda e: e.tensor_tensor(out=kka[:], in0=kk[:], in1=av[:], op=ALU.mult), r=["kk", "av"], w=["kka"])
        V(lambda e: e.tensor_tensor(out=rkr[:], in0=P_RKV[:, 0:128], in1=kmod[:], op=ALU.mult), r=["P_RKV", "kmod"], w=["rkr"])
        G(lambda e: e.tensor_tensor(out=rkr[:], in0=rkr[:], in1=bc["rk"][:], op=ALU.mult), r=["rkr", "bc_rk"], w=["rkr"])
        V(lambda e: e.tensor_reduce(out=sbon[p][:], in_=rkr[:].rearrange("p (h c) -> p h c", h=2), axis=AX.X, op=ALU.add), r=["rkr"], w=["sbon%d" % p])
        T(lambda e: e.matmul(P_Z[:, 2, :], lhsT=cst["c_m1"][:], rhs=logw[:], start=True, stop=True), r=["c_m1", "logw"], w=["P_Z2"])
        T(lambda e: e.matmul(P_Z[:, 3, :], lhsT=cst["c_m2"][:], rhs=logw[:], start=True, stop=True), r=["c_m2", "logw"], w=["P_Z3"])
        T(lambda e: e.matmul(P_Z[:, 0, 0:1], lhsT=logw[:], rhs=ones_col[:], start=True, stop=True), r=["ones_col", "logw"], w=["P_Z0"])
        A(lambda e: e.activation(out=eW[:], in_=P_Z[:, 2, :], func=AF.Exp), r=["P_Z2"], w=["eW"])
        A(lambda e: e.activation(out=eWi[:], in_=P_Z[:, 2, :], func=AF.Exp, scale=-1.0), r=["P_Z2"], w=["eWi"])
        A(lambda e: e.activation(out=eWp[:], in_=logw[:], func=AF.Exp, scale=-1.0), r=["logw"], w=["eWp"])
        G(lambda e: e.tensor_tensor(out=eWp[:], in0=eWp[:], in1=eW[:], op=ALU.mult), r=["eWp", "eW"], w=["eWp"])
        A(lambda e: e.activation(out=eR[:], in_=P_Z[:, 3, :], func=AF.Exp), r=["P_Z3"], w=["eR"])
        A(lambda e: e.activation(out=wc[p][:], in_=P_Z[:, 0, 0:1], func=AF.Exp), r=["P_Z0"], w=["wc%d" % p])
        V(lambda e: e.scalar_tensor_tensor(out=TM[:, 0, :], in0=kk[:], scalar=-1.0, in1=eWp[:], op0=ALU.mult, op1=ALU.mult), r=["kk", "eWp"], w=["TM0"])
        V(lambda e: e.tensor_tensor(out=TM[:, 1, :], in0=P_RKV[:, 0:128], in1=eW[:], op=ALU.mult), r=["P_RKV", "eW"], w=["TM1"])
        G(lambda e: e.tensor_tensor(out=TM[:, 2, :], in0=kka[:], in1=eWi[:], op=ALU.mult), r=["kka", "eWi"], w=["TM2"])
        V(lambda e: e.tensor_tensor(out=TM[:, 3, :], in0=kmod[:], in1=eWi[:], op=ALU.mult), r=["kmod", "eWi"], w=["TM3"])
        for h in range(2):
            hs = slice(h * 64, (h + 1) * 64)
            G(lambda e, h=h, hs=hs: e.tensor_tensor(out=Bpad[p][:, h, hs], in0=kka[:, hs], in1=eR[:, hs], op=ALU.mult), r=["kka", "eR"], w=["Bpad%d" % p])
            V(lambda e, h=h, hs=hs: e.tensor_tensor(out=Kpad[p][:, h, hs], in0=kmod[:, hs], in1=eR[:, hs], op=ALU.mult), r=["kmod", "eR"], w=["Kpad%d" % p])
        for j in range(4):
            T(lambda e, j=j: e.transpose(P_T[:, j, :], TM[:, j, :], cst["c_ident"][:]), r=["TM%d" % j, "c_ident"], w=["P_T"])
        A(lambda e: e.copy(out=FT[p][:], in_=P_T[:]), r=["P_T"], w=["FT%d" % p])
        HSL = [slice(0, 64), slice(64, 128)]
        BK = ["a", "b"]
        for h in range(2):
            hs = HSL[h]
            T(lambda e, hs=hs, h=h: e.matmul(P_L1[h][:, 0:2, :], lhsT=FT[p][hs, 2, :], rhs=FT[p][hs, 0:2, :], start=True, stop=True), r=["FT%d" % p], w=["P_L1%s" % BK[h]])
            T(lambda e, hs=hs, h=h: e.matmul(P_L1[h][:, 2:4, :], lhsT=FT[p][hs, 3, :], rhs=FT[p][hs, 0:2, :], start=True, stop=True), r=["FT%d" % p], w=["P_L1%s" % BK[h]])
            T(lambda e, hs=hs, h=h: e.matmul(P_LD[h][:, 0, :], lhsT=FT[p][hs, 0, :], rhs=FT[p][hs, 2, :], start=True, stop=True), r=["FT%d" % p], w=["P_LD%s0" % BK[h]])
        for h in range(2):
            V(lambda e, h=h: e.tensor_tensor(out=LM1[p][h][:], in0=P_L1[h][:], in1=mask4[:], op=ALU.mult), r=["P_L1%s" % BK[h], "mask4"], w=["LM1%d.%d" % (p, h)])
            V(lambda e, h=h: e.tensor_tensor(out=Lk[h][0][:], in0=P_LD[h][:, 0, :], in1=cst["c_mls"][:], op=ALU.mult), r=["P_LD%s0" % BK[h], "c_mls"], w=["Lk%d.0" % h])
        for h in range(2):
            G(lambda e, h=h: e.tensor_tensor(out=PN[p][h][0][:, 0, :], in0=LM1[p][h][:, 0, :], in1=cst["c_ident"][:], op=ALU.add), r=["LM1%d.%d" % (p, h), "c_ident"], w=["PN%d.%d.0.P" % (p, h)])
            T(lambda e, h=h: e.matmul(P_LD[h][:, 2, :], lhsT=Lk[h][0][:], rhs=LM1[p][h][:, 0, :], start=True, stop=True), r=["Lk%d.0" % h, "LM1%d.%d" % (p, h)], w=["P_LD%s12" % BK[h]])
            T(lambda e, h=h: e.matmul(P_LD[h][:, 3, :], lhsT=LM1[p][h][:, 0, :], rhs=Lk[h][0][:], start=True, stop=True), r=["Lk%d.0" % h, "LM1%d.%d" % (p, h)], w=["P_LD%s3" % BK[h]])
        for h in range(2):
            A(lambda e, h=h: e.copy(out=PN[p][h][0][:, 1, :], in_=P_LD[h][:, 2, :]), r=["P_LD%s12" % BK[h]], w=["PN%d.%d.0.N" % (p, h)])
            A(lambda e, h=h: e.copy(out=Lk[h][1][:], in_=P_LD[h][:, 3, :]), r=["P_LD%s3" % BK[h]], w=["Lk%d.1" % h])
        cur = 0
        lcur = 1
        for lev in range(1, 7):
            nxt = 1 - cur
            lnx = 1 - lcur
            last = (lev == 6)
            for h in range(2):
                pk = "PN%d.%d.%d" % (p, h, cur); lk = "Lk%d.%d" % (h, lcur)
                if not last:
                    T(lambda e, cur=cur, lcur=lcur, h=h: e.matmul(P_LD[h][:, 1:3, :], lhsT=Lk[h][lcur][:], rhs=PN[p][h][cur][:], start=True, stop=True),
                      r=[lk, pk + ".P", pk + ".N"], w=["P_LD%s12" % BK[h]])
                    T(lambda e, cur=cur, lcur=lcur, h=h: e.matmul(P_LD[h][:, 3, :], lhsT=PN[p][h][cur][:, 1, :], rhs=Lk[h][lcur][:], start=True, stop=True),
                      r=[lk, pk + ".N"], w=["P_LD%s3" % BK[h]])
                else:
                    T(lambda e, cur=cur, lcur=lcur, h=h: e.matmul(P_LD[h][:, 1, :], lhsT=Lk[h][lcur][:], rhs=PN[p][h][cur][:, 0, :], start=True, stop=True),
                      r=[lk, pk + ".P"], w=["P_LD%s12" % BK[h]])
            for h in range(2):
                pk = "PN%d.%d.%d" % (p, h, cur); pn = "PN%d.%d.%d" % (p, h, nxt); ln_ = "Lk%d.%d" % (h, lnx)
                V(lambda e, cur=cur, nxt=nxt, h=h: e.tensor_tensor(out=PN[p][h][nxt][:, 0, :], in0=P_LD[h][:, 1, :], in1=PN[p][h][cur][:, 0, :], op=ALU.add),
                  r=["P_LD%s12" % BK[h], pk + ".P"], w=[pn + ".P"])
                if not last:
                    A(lambda e, nxt=nxt, h=h: e.copy(out=PN[p][h][nxt][:, 1, :], in_=P_LD[h][:, 2, :]), r=["P_LD%s12" % BK[h]], w=[pn + ".N"])
                    A(lambda e, lnx=lnx, h=h: e.copy(out=Lk[h][lnx][:], in_=P_LD[h][:, 3, :]), r=["P_LD%s3" % BK[h]], w=[ln_])
            cur = nxt
            lcur = lnx

    def gen_Y(ch):
        p = ch % 2
        HSL = [slice(0, 64), slice(64, 128)]
        BK = ["a", "b"]
        cur = 0
        for h in range(2):
            hs = HSL[h]
            T(lambda e, hs=hs, h=h: e.matmul(P_M[:, 64 * h:64 * h + 64], lhsT=FT[p][hs, 0, :], rhs=Hst[hs, :], start=True, stop=False), r=["FT%d" % p, "Hst"], w=["P_G%d" % h])
            T(lambda e, hs=hs, h=h: e.matmul(P_M[:, 64 * h:64 * h + 64], lhsT=LM1[p][h][:, 2, :], rhs=vsb[p][:, hs], start=False, stop=True), r=["LM1%d.%d" % (p, h), "vsb%d" % p], w=["P_G%d" % h])
        A(lambda e: e.copy(out=Gs[0][:], in_=P_M[:, 0:64]), r=["P_G0"], w=["Gs0"])
        V(lambda e: e.tensor_copy(out=Gs[1][:], in_=P_M[:, 64:128]), r=["P_G1"], w=["Gs1"])
        for h in range(2):
            T(lambda e, h=h, cur=cur: e.matmul(P_M[:, 64 * h:64 * h + 64], lhsT=PN[p][h][cur][:, 0, :], rhs=Gs[h][:], start=True, stop=True),
              r=["PN%d.%d.%d.P" % (p, h, cur), "Gs%d" % h], w=["P_G%d" % h])
        A(lambda e: e.copy(out=Us[0][:], in_=P_M[:, 0:64]), r=["P_G0"], w=["Us0"])
        V(lambda e: e.tensor_copy(out=Us[1][:], in_=P_M[:, 64:128]), r=["P_G1"], w=["Us1"])
        for h in range(2):
            hs = HSL[h]
            yk = "P_Y%d" % h
            uk = "Us%d" % h
            T(lambda e, hs=hs, h=h: e.matmul(P_M[:, 192 + 64 * h:256 + 64 * h], lhsT=FT[p][hs, 1, :], rhs=Hst[hs, :], start=True, stop=False), r=["FT%d" % p, "Hst"], w=[yk])
            T(lambda e, h=h: e.matmul(P_M[:, 192 + 64 * h:256 + 64 * h], lhsT=LM1[p][h][:, 1, :], rhs=Us[h][:], start=False, stop=False), r=["LM1%d.%d" % (p, h), uk], w=[yk])
            T(lambda e, hs=hs, h=h: e.matmul(P_M[:, 192 + 64 * h:256 + 64 * h], lhsT=LM1[p][h][:, 3, :], rhs=vsb[p][:, hs], start=False, stop=True), r=["LM1%d.%d" % (p, h), "vsb%d" % p], w=[yk])
        T(lambda e: e.matmul(P_M[:, 128:192], lhsT=Bpad[p][:, 0, :], rhs=Us[0][:], start=True, stop=False), r=["Bpad%d" % p, "Us0"], w=["P_H"])
        T(lambda e: e.matmul(P_M[:, 128:192], lhsT=Kpad[p][:, 0, :], rhs=vsb[p][:, 0:64], start=False, stop=False), r=["Kpad%d" % p, "vsb%d" % p], w=["P_H"])
        T(lambda e: e.matmul(P_M[:, 128:192], lhsT=Bpad[p][:, 1, :], rhs=Us[1][:], start=False, stop=False), r=["Bpad%d" % p, "Us1"], w=["P_H"])
        T(lambda e: e.matmul(P_M[:, 128:192], lhsT=Kpad[p][:, 1, :], rhs=vsb[p][:, 64:128], start=False, stop=True), r=["Kpad%d" % p, "vsb%d" % p], w=["P_H"])
        V(lambda e: e.scalar_tensor_tensor(out=Hst[:], in0=Hst[:], scalar=wc[p][:, 0:1], in1=P_M[:, 128:192], op0=ALU.mult, op1=ALU.add),
          r=["Hst", "wc%d" % p, "P_H"], w=["Hst"])
        Yv = P_M[:, 192:320]
        V(lambda e: e.tensor_reduce(out=s1[:], in_=Yv.rearrange("p (h c) -> p h c", h=2), axis=AX.X, op=ALU.add), r=["P_Y0", "P_Y1"], w=["s1"])
        V(lambda e: e.tensor_scalar(out=nmean[:], in0=s1[:], scalar1=-1.0 / 64, scalar2=None, op0=ALU.mult), r=["s1"], w=["nmean"])
        for h in range(2):
            hs = slice(h * 64, (h + 1) * 64)
            V(lambda e, h=h, hs=hs: e.tensor_scalar(out=Yc[:, hs], in0=P_M[:, 192 + 64 * h:256 + 64 * h], scalar1=nmean[:, h:h + 1], scalar2=None, op0=ALU.add),
              r=["P_Y%d" % h, "nmean"], w=["Yc"])
        G(lambda e: e.tensor_tensor(out=sq2[:], in0=Yc[:], in1=Yc[:], op=ALU.mult), r=["Yc"], w=["sq2"])
        V(lambda e: e.tensor_reduce(out=ss2[:], in_=sq2[:].rearrange("p (h c) -> p h c", h=2), axis=AX.X, op=ALU.add), r=["sq2"], w=["ss2"])
        V(lambda e: e.tensor_scalar(out=ss2[:], in0=ss2[:], scalar1=1.0 / 64, scalar2=GN_EPS, op0=ALU.mult, op1=ALU.add), r=["ss2"], w=["ss2"])
        A(lambda e: e.activation(out=ss2[:], in_=ss2[:], func=AF.Sqrt), r=["ss2"], w=["ss2"])
        V(lambda e: e.reciprocal(out=rstd[:], in_=ss2[:]), r=["ss2"], w=["rstd"])
        for h in range(2):
            hs = slice(h * 64, (h + 1) * 64)
            V(lambda e, h=h, hs=hs: e.tensor_scalar(out=Yn[:, hs], in0=Yc[:, hs], scalar1=rstd[:, h:h + 1], scalar2=None, op0=ALU.mult), r=["Yc", "rstd"], w=["Yn"])
        G(lambda e: e.tensor_tensor(out=Yn[:], in0=Yn[:], in1=bc["lng"][:], op=ALU.mult), r=["Yn", "bc_lng"], w=["Yn"])
        G(lambda e: e.tensor_tensor(out=Yn[:], in0=Yn[:], in1=bc["lnb"][:], op=ALU.add), r=["Yn", "bc_lnb"], w=["Yn"])
        for h in range(2):
            hs = slice(h * 64, (h + 1) * 64)
            V(lambda e, h=h, hs=hs: e.scalar_tensor_tensor(out=Yo[:, hs], in0=vsb[p][:, hs], scalar=sbon[p][:, h:h + 1], in1=Yn[:, hs], op0=ALU.mult, op1=ALU.add),
              r=["vsb%d" % p, "sbon%d" % p, "Yn"], w=["Yo"])
        G(lambda e: e.tensor_tensor(out=Yo[:], in0=Yo[:], in1=gv[p][:], op=ALU.mult), r=["Yo", "gv%d" % p], w=["Yo"])
        T(lambda e: e.transpose(P_M[:, 320:448], Yo[:], cst["c_ident"][:]), r=["Yo", "c_ident"], w=["P_OT"])
        A(lambda e: e.copy(out=YoT[:], in_=P_M[:, 320:448]), r=["P_OT"], w=["YoT"])
        DMA(dram["cin"][(ch * C) // 1024][0:128, (ch * C) % 1024:(ch * C) % 1024 + C], YoT[:], reads=["YoT"])

    def collect(fn, ch):
        lst = []
        defer[0] = lst
        fn(ch)
        defer[0] = None
        return lst

    xl = collect(gen_X, 0)
    drain(xl, len(xl))
    for ch in range(n_chunks):
        yl = collect(gen_Y, ch)
        xl = collect(gen_X, ch + 1) if ch + 1 < n_chunks else []
        ny = len(yl)
        for i in range(ny):
            drain(yl, 1)
            if xl:
                drain(xl, -(-len(xl) // (ny - i)))
        drain(xl, len(xl))


S_TOK = 16384
AXB = 256
KB_BOUND = 16.0
NEG = -30000.0


def attn_consts():
    s = S_TOK
    half = 32
    inv_freq = (10000.0 ** (-np.arange(half, dtype=np.float32) / half)).astype(np.float32)
    ang = (np.arange(s, dtype=np.float32)[:, None] * inv_freq[None, :]).astype(np.float32)
    cos = np.cos(ang).astype(np.float32)
    sin = np.sin(ang).astype(np.float32)
    ropeA = np.tile(cos, (1, 8)).astype(np.float32)
    ropeB = np.tile(np.concatenate([-sin, sin], 1), (1, 4)).astype(np.float32)
    kind = np.zeros((64, s), np.float32)
    for n in range(64):
        kind[n, n * 256:(n + 1) * 256] = 1.0
    i = np.arange(128)
    tri = (i[None, :] >= i[:, None]).astype(np.float32)
    return {"a_ropeA": ropeA, "a_ropeB": ropeB, "a_kind": kind.astype(ml_dtypes.bfloat16),
            "a_tri": tri.astype(ml_dtypes.bfloat16), "a_ident": np.eye(128, dtype=np.float32),
            "a_identb": np.eye(128, dtype=np.float32).astype(ml_dtypes.bfloat16)}


def build_attn(nc, S, es, dram, n_blocks=S_TOK // 256, cc=None):
    sbn = [0]

    def sb(shape, dt=F32):
        sbn[0] += 1
        return es.enter_context(nc.sbuf_tensor("at%d" % sbn[0], shape, dt))

    def ps(shape, dt=F32):
        sbn[0] += 1
        return es.enter_context(nc.psum_tensor("ap%d" % sbn[0], shape, dt))

    def _bk(r, w):
        b = set(k.split(".")[0] for k in list(r) + list(w) if k.startswith("Q_"))
        return list(w) + ["BANK_" + x for x in b]

    defer = [None]

    def _rec(eng, fn, r, w):
        if defer[0] is None:
            S.op(eng, fn, r, w)
        else:
            defer[0].append((eng, fn, r, w))

    def DMA(out, in_, reads=(), writes=()):
        if defer[0] is None:
            S.dma("sync", out, in_, reads=reads, writes=writes)
        else:
            defer[0].append(("dma", (out, in_), reads, writes))

    def drain(lst, n):
        for _ in range(min(n, len(lst))):
            eng, fn, r, w = lst.pop(0)
            if eng == "dma":
                S.dma("sync", fn[0], fn[1], reads=r, writes=w)
            else:
                S.op(eng, fn, r, w)

    V = lambda fn, r=(), w=(): _rec("vector", fn, r, _bk(r, w))
    A = lambda fn, r=(), w=(): _rec("scalar", fn, r, _bk(r, w))
    G = lambda fn, r=(), w=(): _rec("gpsimd", fn, r, list(w))
    T = lambda fn, r=(), w=(): _rec("tensor", fn, r, _bk(r, w))

    ident = sb([128, 128]); identb = sb([128, 128], BF16); tri = sb([128, 128], BF16)
    S.dma("sync", ident[:], dram["a_ident"], writes=["ident"])
    S.dma("sync", identb[:], dram["a_identb"], writes=["identb"])
    S.dma("sync", tri[:], dram["a_tri"], writes=["tri"])
    wst = sb([128, 8, 384])
    S.dma("sync", wst[:], dram["w_qkv"].rearrange("(dc p) n -> p dc n", p=128), writes=["wst"])
    Wq = sb([128, 8, 384], BF16)
    for dc in range(8):
        V(lambda e, dc=dc: e.tensor_copy(out=Wq[:, dc, :], in_=wst[:, dc, :]), r=["wst"], w=["Wq"])
    inv256 = sb([128, 1]); ones_r = sb([128, 64])
    V(lambda e: e.memset(inv256[:], 1.0 / 256), w=["inv256"])
    V(lambda e: e.memset(ones_r[:], 1.0), w=["ones_r"])
    KT = [sb([128, S_TOK], BF16) for _ in range(2)]
    VA = [sb([128, 128, 65], BF16) for _ in range(2)]
    kmT = [sb([64, 64]) for _ in range(2)]
    Gt = [sb([128, 64]) for _ in range(2)]
    for h in range(2):
        S.dma("sync", KT[h][64:128, :], dram["a_kind"], writes=["KT%d.%d" % (h, b_) for b_ in range(64)])
        G(lambda e, h=h: e.memset(VA[h][:], 1.0), w=["VA%d.%d" % (h, b_) for b_ in range(64)])
        V(lambda e, h=h: e.memset(kmT[h][:], 0.0), w=["kmT%d" % h])
        V(lambda e, h=h: e.memset(Gt[h][:], -1e30), w=["Gt%d" % h])
    xs = [sb([128, 8, AXB + 1]) for _ in range(2)]
    xb = [sb([128, 8, AXB + 1], BF16) for _ in range(2)]
    rA = [sb([128, 256]) for _ in range(2)]; rB = [sb([128, 256]) for _ in range(2)]
    tmpA = sb([128, 256]); tmpB = sb([128, 256]); QK = sb([128, 4, 64])
    qT32 = sb([64, 128]); mx = sb([128, 8]); negm = sb([128, 64]); ssq = sb([128, 1]); cpos = sb([128, 1]); qjunk = sb([128, 64])
    Qaug = [sb([128, 128], BF16) for _ in range(2)]
    QT = [[sb([128, 256], BF16) for _ in range(2)] for _ in range(2)]
    PT = [sb([128, 256], BF16) for _ in range(4)]
    rec = sb([128, 256]); ysb = sb([64, 256]); yo = sb([64, 256])

    NSB = 4
    Q_A = ps([128, 512]); Q_C = ps([128, 512])
    Q_D = Q_C[:, 66:130].bitcast(BF16)
    Q_S = [ps([128, 512]) for _ in range(NSB)]; Q_O = [ps([128, 512]) for _ in range(2)]

    xT = dram["xT"].rearrange("(dc p) n -> p dc n", p=128)
    sidx = 0

    def gen_proj(blk):
        xi = blk % 2
        kx = "xs%d" % xi; kb = "xb%d" % xi
        c0 = blk * AXB
        for dc in range(8):
            DMA(xs[xi][:, dc, :], xT[:, dc, c0:c0 + AXB + 1], writes=[kx])
        for dc in range(8):
            if dc % 2 == 0:
                G(lambda e, dc=dc, xi=xi: e.tensor_copy(out=xb[xi][:, dc, :], in_=xs[xi][:, dc, :]), r=[kx], w=[kb])
            else:
                V(lambda e, dc=dc, xi=xi: e.tensor_copy(out=xb[xi][:, dc, :], in_=xs[xi][:, dc, :]), r=[kx], w=[kb])
        for half in range(2):
            tt = blk * 2 + half
            o = half * 128
            ri = tt % 2
            DMA(rA[ri][:], dram["a_ropeA"][tt * 128:(tt + 1) * 128, :], writes=["rA%d" % ri])
            DMA(rB[ri][:], dram["a_ropeB"][tt * 128:(tt + 1) * 128, :], writes=["rB%d" % ri])
            for dc in range(8):
                T(lambda e, dc=dc, xi=xi, o=o: e.matmul(Q_A[:, 0:384], lhsT=xb[xi][:, dc, 1 + o:1 + o + 128], rhs=Wq[:, dc, :],
                                                        start=(dc == 0), stop=(dc == 7)), r=["Wq", kb], w=["Q_A"])
            X4 = Q_A[:, 0:256].rearrange("p (g h d) -> p g h d", g=4, h=2)
            B4 = lambda t_: t_[:].rearrange("p (g h d) -> p g h d", g=4, h=2)
            V(lambda e, ri=ri: e.tensor_tensor(out=tmpA[:], in0=Q_A[:, 0:256], in1=rA[ri][:], op=ALU.mult), r=["Q_A", "rA%d" % ri], w=["tmpA"])
            V(lambda e, ri=ri: e.tensor_tensor(out=B4(tmpB)[:, :, 0, :], in0=X4[:, :, 1, :], in1=B4(rB[ri])[:, :, 0, :], op=ALU.mult), r=["Q_A", "rB%d" % ri], w=["tmpB"])
            V(lambda e, ri=ri: e.tensor_tensor(out=B4(tmpB)[:, :, 1, :], in0=X4[:, :, 0, :], in1=B4(rB[ri])[:, :, 1, :], op=ALU.mult), r=["Q_A", "rB%d" % ri], w=["tmpB"])
            G(lambda e: e.tensor_tensor(out=QK[:].rearrange("p g d -> p (g d)"), in0=tmpA[:], in1=tmpB[:], op=ALU.add), r=["tmpA", "tmpB"], w=["QK"])
            for h in range(2):
                V(lambda e, h=h, tt=tt: e.tensor_copy(out=VA[h][:, tt, 0:64], in_=Q_A[:, 256 + 64 * h:320 + 64 * h]), r=["Q_A"], w=["VA%d.%d" % (h, blk)])
                T(lambda e, h=h: e.transpose(Q_C[0:64, 130:258], QK[:, 2 + h, :], ident[:]), r=["QK", "ident"], w=["Q_C.k"])
                V(lambda e, h=h, tt=tt: e.tensor_copy(out=KT[h][0:64, tt * 128:(tt + 1) * 128], in_=Q_C[0:64, 130:258]), r=["Q_C.k"], w=["KT%d.%d" % (h, blk)])
                T(lambda e, h=h: e.transpose(Q_C[0:64, 258:386], QK[:, h, :], ident[:]), r=["QK", "ident"], w=["Q_C.q"])
                V(lambda e: e.tensor_copy(out=qT32[:], in_=Q_C[0:64, 258:386]), r=["Q_C.q"], w=["qT32"])
                if blk > 0:
                    T(lambda e, h=h: e.matmul(Q_C[:, 0:64], lhsT=qT32[:], rhs=kmT[h][:], start=True, stop=True), r=["qT32", "kmT%d" % h], w=["Q_C.g"])
                    V(lambda e, h=h, blk=blk: e.tensor_copy(out=Gt[h][:, 0:blk], in_=Q_C[:, 0:blk]), r=["Q_C.g"], w=["Gt%d" % h])
                V(lambda e, h=h: e.max(out=mx[:], in_=Gt[h][:]), r=["Gt%d" % h], w=["mx"])
                V(lambda e, h=h: e.tensor_scalar(out=negm[:], in0=Gt[h][:], scalar1=mx[:, 2:3], scalar2=NEG, op0=ALU.is_lt, op1=ALU.mult), r=["Gt%d" % h, "mx"], w=["negm"])
                V(lambda e, blk=blk: e.memset(negm[:, blk:blk + 1], 0.0), w=["negm"])
                V(lambda e, h=h: e.tensor_tensor(out=qjunk[:], in0=QK[:, h, :], in1=QK[:, h, :], op=ALU.mult), r=["QK"], w=["qjunk"])
                V(lambda e: e.tensor_reduce(out=ssq[:], in_=qjunk[:], axis=AX.X, op=ALU.add), r=["qjunk"], w=["ssq"])
                V(lambda e: e.tensor_scalar(out=cpos[:], in0=ssq[:], scalar1=0.5, scalar2=0.5 * KB_BOUND * KB_BOUND, op0=ALU.mult, op1=ALU.add), r=["ssq"], w=["cpos"])
                G(lambda e, h=h: e.tensor_copy(out=Qaug[h][:, 0:64], in_=QK[:, h, :]), r=["QK"], w=["Qaug%d" % h])
                V(lambda e, h=h: e.tensor_scalar(out=Qaug[h][:, 64:128], in0=negm[:], scalar1=cpos[:, 0:1], scalar2=None, op0=ALU.subtract), r=["negm", "cpos"], w=["Qaug%d" % h])
                T(lambda e, h=h: e.transpose(Q_D[:, 0:128], Qaug[h][:], identb[:]), r=["Qaug%d" % h, "identb"], w=["Q_C.d"])
                V(lambda e, h=h, o=o: e.tensor_copy(out=QT[h][blk % 2][:, o:o + 128], in_=Q_D[:, 0:128]), r=["Q_C.d"], w=["QT%d.%d" % (h, blk % 2)])
                T(lambda e, h=h: e.matmul(Q_C[0:64, 64:65], lhsT=QK[:, 2 + h, :], rhs=inv256[:], start=True, stop=True), r=["QK", "inv256"], w=["Q_C.m"])
                if half == 0:
                    V(lambda e, h=h, blk=blk: e.tensor_copy(out=kmT[h][:, blk:blk + 1], in_=Q_C[0:64, 64:65]), r=["Q_C.m"], w=["kmT%d" % h])
                else:
                    V(lambda e, h=h, blk=blk: e.tensor_tensor(out=kmT[h][:, blk:blk + 1], in0=Q_C[0:64, 64:65], in1=kmT[h][:, blk:blk + 1], op=ALU.add),
                      r=["Q_C.m", "kmT%d" % h], w=["kmT%d" % h])

    gen_proj(0)
    for blk in range(n_blocks):
        pend = []
        if blk + 1 < n_blocks:
            defer[0] = pend
            gen_proj(blk + 1)
            defer[0] = None
        steps = []
        for h in range(2):
            nkt = 2 * blk + 2
            for kt in range(nkt):
                steps.append((h, kt, nkt))

        def issue_qk(i):
            h, kt, nkt = steps[i]
            j = kt - 2 * blk
            q0 = 128 if j == 1 else 0
            si = (sbase + i) % NSB
            T(lambda e, h=h, kt=kt, q0=q0, si=si, bp=blk % 2: e.matmul(Q_S[si][:, q0:256], lhsT=KT[h][:, kt * 128:(kt + 1) * 128], rhs=QT[h][bp][:, q0:256],
                                                           start=True, stop=True), r=["KT%d.%d" % (h, kt // 2), "QT%d.%d" % (h, blk % 2)], w=["Q_S%d" % si])

        sbase = sidx
        LOOK = NSB - 2
        for i0 in range(min(LOOK, len(steps))):
            issue_qk(i0)
        for i in range(len(steps)):
            h, kt, nkt = steps[i]
            j = kt - 2 * blk
            q0 = 128 if j == 1 else 0
            si = (sbase + i) % NSB
            sk = "Q_S%d" % si
            pk = "PT%d" % si
            ok = "Q_O%d" % h
            if i + LOOK < len(steps):
                issue_qk(i + LOOK)
            A(lambda e, q0=q0, si=si: e.activation(out=PT[si][:, q0:256], in_=Q_S[si][:, q0:256], func=AF.Exp, scale=0.125), r=[sk], w=[pk])
            if j >= 0:
                G(lambda e, q0=q0, si=si, j=j: e.tensor_tensor(out=PT[si][:, j * 128:(j + 1) * 128], in0=PT[si][:, j * 128:(j + 1) * 128], in1=tri[:], op=ALU.mult),
                  r=[pk, "tri"], w=[pk])
            T(lambda e, h=h, kt=kt, q0=q0, si=si, nkt=nkt: e.matmul(Q_O[h][0:65, q0:256], lhsT=VA[h][:, kt, :], rhs=PT[si][:, q0:256],
                                                                    start=(kt == 0), stop=(kt == nkt - 1)), r=["VA%d.%d" % (h, kt // 2), pk], w=[ok])
            if kt == nkt - 1:
                V(lambda e, h=h: e.reciprocal(out=rec[64:65, :], in_=Q_O[h][64:65, 0:256]), r=[ok], w=["rec"])
                T(lambda e, h=h: e.matmul(Q_O[h][0:64, 256:512], lhsT=ones_r[64:65, :], rhs=rec[64:65, :], start=True, stop=True), r=["ones_r", "rec"], w=[ok + ".bc"])
                V(lambda e, h=h: e.tensor_copy(out=ysb[:], in_=Q_O[h][0:64, 0:256]), r=[ok], w=["ysb"])
                V(lambda e, h=h: e.tensor_tensor(out=yo[:], in0=ysb[:], in1=Q_O[h][0:64, 256:512], op=ALU.mult), r=["ysb", ok + ".bc"], w=["yo"])
                S.dma("sync", dram["cin"][(blk * 256) // 1024][128 + h * 64:128 + (h + 1) * 64, (blk * 256) % 1024:(blk * 256) % 1024 + 256], yo[:], reads=["yo"],
                      writes=["cin%d.%d.%d" % (blk // 4, blk % 4, h)])
                if cc is not None and h == 1 and blk % 4 == 3:
                    kch = blk // 4
                    S.async_op("gpsimd", lambda e, kch=kch: e.collective_compute("AllGather", ALU.bypass, replica_groups=cc["groups"],
                                                                             ins=[dram["cin"][kch]], outs=[dram["cout"][kch]]), cc["sem"],
                               reads=["cin%d.%d.%d" % (kch, b4, hh) for b4 in range(4) for hh in range(2)])
            if pend:
                drain(pend, -(-len(pend) // max(1, len(steps) - i)))
        drain(pend, len(pend))
        sidx += len(steps)


NTOK = 4096
ALPHA = float(2.0 ** 0.25)
LN_EPS = 1e-5
MOE_NSTG = 3


def _mk(nc, S, es, pfx):
    sbn = [0]

    def sb(shape, dt=F32):
        sbn[0] += 1
        return es.enter_context(nc.sbuf_tensor("%s%d" % (pfx, sbn[0]), shape, dt))

    def ps(shape, dt=F32):
        sbn[0] += 1
        return es.enter_context(nc.psum_tensor("%sp%d" % (pfx, sbn[0]), shape, dt))

    def _bk(r, w):
        b = set(k.split(".")[0] for k in list(r) + list(w) if k.startswith("Q_"))
        return list(w) + ["BANK_" + x for x in b]

    V = lambda fn, r=(), w=(): S.op("vector", fn, r, _bk(r, w))
    A = lambda fn, r=(), w=(): S.op("scalar", fn, r, _bk(r, w))
    G = lambda fn, r=(), w=(): S.op("gpsimd", fn, r, w)
    T = lambda fn, r=(), w=(): S.op("tensor", fn, r, _bk(r, w))
    return sb, ps, V, A, G, T


def layer_norm_tile(V, A, G, t, tk, junk, st, gbc, bbc, gk, bk):
    V(lambda e: e.tensor_reduce(out=st[:, 0:1], in_=t[:], axis=AX.X, op=ALU.add), r=[tk], w=["st0"])
    V(lambda e: e.tensor_scalar(out=st[:, 0:1], in0=st[:, 0:1], scalar1=-1.0 / 1024, scalar2=None, op0=ALU.mult), r=["st0"], w=["st0"])
    V(lambda e: e.tensor_scalar(out=t[:], in0=t[:], scalar1=st[:, 0:1], scalar2=None, op0=ALU.add), r=[tk, "st0"], w=[tk])
    V(lambda e: e.memset(st[:, 1:2], 0.0), w=["st1"])
    A(lambda e: e.activation(out=junk[:], in_=t[:], func=AF.Square, accum_out=st[:, 1:2]), r=[tk, "st1"], w=["junk", "st1"])
    V(lambda e: e.tensor_scalar(out=st[:, 1:2], in0=st[:, 1:2], scalar1=1.0 / 1024, scalar2=LN_EPS, op0=ALU.mult, op1=ALU.add), r=["st1"], w=["st1"])
    A(lambda e: e.activation(out=st[:, 1:2], in_=st[:, 1:2], func=AF.Sqrt), r=["st1"], w=["st1"])
    V(lambda e: e.reciprocal(out=st[:, 2:3], in_=st[:, 1:2]), r=["st1"], w=["st2"])
    V(lambda e: e.scalar_tensor_tensor(out=t[:], in0=t[:], scalar=st[:, 2:3], in1=gbc[:], op0=ALU.mult, op1=ALU.mult), r=[tk, "st2", gk], w=[tk])
    G(lambda e: e.tensor_tensor(out=t[:], in0=t[:], in1=bbc[:], op=ALU.add), r=[tk, bk], w=[tk])


def layer_norm_multi(V, A, G, tiles, keys, junk, st, gbc, bbc, gk, bk):
    n = len(tiles)
    c = lambda i, j: st[:, 4 * i + j:4 * i + j + 1]
    for i in range(n):
        V(lambda e, i=i: e.tensor_reduce(out=c(i, 0), in_=tiles[i], axis=AX.X, op=ALU.add), r=[keys[i]], w=["st%d.0" % i])
    for i in range(n):
        V(lambda e, i=i: e.tensor_scalar(out=c(i, 0), in0=c(i, 0), scalar1=-1.0 / 1024, scalar2=None, op0=ALU.mult), r=["st%d.0" % i], w=["st%d.0" % i])
    for i in range(n):
        V(lambda e, i=i: e.tensor_scalar(out=tiles[i], in0=tiles[i], scalar1=c(i, 0), scalar2=None, op0=ALU.add), r=[keys[i], "st%d.0" % i], w=[keys[i]])
        V(lambda e, i=i: e.memset(c(i, 1), 0.0), w=["st%d.1" % i])
    for i in range(n):
        A(lambda e, i=i: e.activation(out=junk[:], in_=tiles[i], func=AF.Square, accum_out=c(i, 1)), r=[keys[i], "st%d.1" % i], w=["junk", "st%d.1" % i])
    for i in range(n):
        V(lambda e, i=i: e.tensor_scalar(out=c(i, 1), in0=c(i, 1), scalar1=1.0 / 1024, scalar2=LN_EPS, op0=ALU.mult, op1=ALU.add), r=["st%d.1" % i], w=["st%d.1" % i])
    for i in range(n):
        A(lambda e, i=i: e.activation(out=c(i, 1), in_=c(i, 1), func=AF.Sqrt), r=["st%d.1" % i], w=["st%d.1" % i])
    for i in range(n):
        V(lambda e, i=i: e.reciprocal(out=c(i, 2), in_=c(i, 1)), r=["st%d.1" % i], w=["st%d.2" % i])
    for i in range(n):
        V(lambda e, i=i: e.scalar_tensor_tensor(out=tiles[i], in0=tiles[i], scalar=c(i, 2), in1=gbc[:], op0=ALU.mult, op1=ALU.mult), r=[keys[i], "st%d.2" % i, gk], w=[keys[i]])
    for i in range(n):
        G(lambda e, i=i: e.tensor_tensor(out=tiles[i], in0=tiles[i], in1=bbc[:], op=ALU.add), r=[keys[i], bk], w=[keys[i]])


def build_stage_a(nc, S, es, dram, n_groups=NTOK // 512):
    sb, ps, V, A, G, T = _mk(nc, S, es, "sa")
    stg = [sb([128, 2048]) for _ in range(2)]
    Wg = sb([128, 8, 2048], BF16); Wab = sb([128, 4, 1024], BF16); Wrb = sb([128, 4, 1024], BF16); Wo = sb([128, 8, 1024], BF16)
    si = 0
    wg_v = dram["w_gate"].rearrange("(dc p) n -> p dc n", p=128)
    for dc in range(8):
        k = "stg%d" % (si % 2); st_ = stg[si % 2]; si += 1
        S.dma("sync", st_[:], wg_v[:, dc, :], writes=[k])
        V(lambda e, dc=dc, st_=st_: e.tensor_copy(out=Wg[:, dc, :], in_=st_[:]), r=[k], w=["Wg"])
    for (src, dst, key, ndc) in (("w_ab", Wab, "Wab", 4), ("w_rb", Wrb, "Wrb", 4), ("w_out", Wo, "Wo", 8)):
        v = dram[src].rearrange("(dc p) n -> p dc n", p=128)
        for dc in range(ndc):
            k = "stg%d" % (si % 2); st_ = stg[si % 2]; si += 1
            S.dma("sync", st_[:, 0:1024], v[:, dc, :], writes=[k])
            G(lambda e, dc=dc, st_=st_, dst=dst: e.tensor_copy(out=dst[:, dc, :], in_=st_[:, 0:1024]), r=[k], w=[key])
    gbc = sb([128, 1024]); bbc = sb([128, 1024])
    S.dma("sync", gbc[:], dram["ln1g"].partition_broadcast(128), writes=["gbc"])
    S.dma("sync", bbc[:], dram["ln1b"].partition_broadcast(128), writes=["bbc"])
    xs = sb([128, 8, 512]); xb = sb([128, 8, 512], BF16)
    ys = [sb([128, 4, 512]) for _ in range(2)]; yb = sb([128, 8, 512], BF16)
    ytmp = sb([128, 512])
    mq = sb([128, 4])
    S.dma("sync", mq[:], dram["maskq"], writes=["mq"])
    ident = sb([128, 128])
    S.dma("sync", ident[:], dram["c_ident"], writes=["ident"])
    hTs = sb([128, 8, 128])
    Q_t = [ps([128, 512]) for _ in range(2)]
    mixT = sb([128, 8, 512], BF16)
    sa = sb([128, 512]); sr = sb([128, 512]); m1 = sb([128, 512]); m2 = sb([128, 512])
    xt2 = [sb([128, 1024]) for _ in range(2)]; ht2 = [sb([128, 1024]) for _ in range(2)]; junk = sb([128, 1024]); st8 = sb([128, 8])
    Q_ga = ps([128, 512]); Q_gr = ps([128, 512]); Q_ba = ps([128, 512]); Q_br = ps([128, 512])
    Q_o = [ps([128, 512]) for _ in range(2)]
    xT = dram["xT2"].rearrange("(dc p) n -> p dc n", p=128)
    cout = dram["cout"]
    for g in range(n_groups):
        c0 = g * 512
        for dc in range(8):
            S.dma("sync", xs[:, dc, :], xT[:, dc, c0:c0 + 512], writes=["xs"])
        for dc in range(8):
            G(lambda e, dc=dc: e.tensor_copy(out=xb[:, dc, :], in_=xs[:, dc, :]), r=["xs"], w=["xb"])
        for dc in range(8):
            r0 = (dc % 4) * 256 + (128 if dc < 4 else 0)
            yi = dc % 2
            yk = "ys%d" % yi
            for q in range(4):
                kch = q * 4 + c0 // 1024
                S.dma("sync", ys[yi][:, q, :], cout[kch][r0:r0 + 128, c0 % 1024:c0 % 1024 + 512], writes=[yk])
            V(lambda e, yi=yi: e.tensor_scalar(out=ytmp[:], in0=ys[yi][:, 0, :], scalar1=mq[:, 0:1], scalar2=None, op0=ALU.mult), r=[yk, "mq"], w=["ytmp"])
            for q in (1, 2):
                V(lambda e, yi=yi, q=q: e.scalar_tensor_tensor(out=ytmp[:], in0=ys[yi][:, q, :], scalar=mq[:, q:q + 1], in1=ytmp[:], op0=ALU.mult, op1=ALU.add),
                  r=[yk, "mq", "ytmp"], w=["ytmp"])
            V(lambda e, yi=yi, dc=dc: e.scalar_tensor_tensor(out=yb[:, dc, :], in0=ys[yi][:, 3, :], scalar=mq[:, 3:4], in1=ytmp[:], op0=ALU.mult, op1=ALU.add),
              r=[yk, "mq", "ytmp"], w=["yb"])
        for j in range(8):
            js = slice(j * 128, (j + 1) * 128)
            for dc in range(8):
                T(lambda e, dc=dc, js=js: e.matmul(Q_ga[:], lhsT=Wg[:, dc, js], rhs=xb[:, dc, :], start=(dc == 0), stop=(dc == 7)), r=["Wg", "xb"], w=["Q_ga"])
            for dc in range(8):
                T(lambda e, dc=dc, j=j: e.matmul(Q_gr[:], lhsT=Wg[:, dc, 1024 + j * 128:1024 + (j + 1) * 128], rhs=xb[:, dc, :], start=(dc == 0), stop=(dc == 7)),
                  r=["Wg", "xb"], w=["Q_gr"])
            for dc in range(4):
                T(lambda e, dc=dc, js=js: e.matmul(Q_ba[:], lhsT=Wab[:, dc, js], rhs=yb[:, dc, :], start=(dc == 0), stop=(dc == 3)), r=["Wab", "yb"], w=["Q_ba"])
            for dc in range(4):
                T(lambda e, dc=dc, js=js: e.matmul(Q_br[:], lhsT=Wrb[:, dc, js], rhs=yb[:, 4 + dc, :], start=(dc == 0), stop=(dc == 3)), r=["Wrb", "yb"], w=["Q_br"])
            A(lambda e: e.activation(out=sa[:], in_=Q_ga[:], func=AF.Sigmoid), r=["Q_ga"], w=["sa"])
            A(lambda e: e.activation(out=sr[:], in_=Q_gr[:], func=AF.Sigmoid), r=["Q_gr"], w=["sr"])
            V(lambda e: e.tensor_tensor(out=m1[:], in0=sa[:], in1=Q_ba[:], op=ALU.mult), r=["sa", "Q_ba"], w=["m1"])
            V(lambda e: e.tensor_tensor(out=m2[:], in0=sr[:], in1=Q_br[:], op=ALU.mult), r=["sr", "Q_br"], w=["m2"])
            G(lambda e, j=j: e.tensor_tensor(out=mixT[:, j, :], in0=m1[:], in1=m2[:], op=ALU.add), r=["m1", "m2"], w=["mixT"])
        for pr in range(2):
            for i2 in range(2):
                tt = pr * 2 + i2
                tok0 = c0 + tt * 128
                S.dma("sync", xt2[i2][:], dram["x2"][tok0:tok0 + 128, :], writes=["xt%d" % i2])
                for hf in range(2):
                    for j in range(8):
                        T(lambda e, j=j, hf=hf, tt=tt: e.matmul(Q_o[hf][:], lhsT=mixT[:, j, tt * 128:(tt + 1) * 128], rhs=Wo[:, j, hf * 512:(hf + 1) * 512],
                                                                start=(j == 0), stop=(j == 7)), r=["mixT", "Wo"], w=["Q_o%d" % hf])
                    V(lambda e, hf=hf, i2=i2: e.scalar_tensor_tensor(out=ht2[i2][:, hf * 512:(hf + 1) * 512], in0=xt2[i2][:, hf * 512:(hf + 1) * 512], scalar=ALPHA, in1=Q_o[hf][:],
                                                                     op0=ALU.mult, op1=ALU.add), r=["xt%d" % i2, "Q_o%d" % hf], w=["ht%d" % i2])
            layer_norm_multi(V, A, G, [ht2[0][:], ht2[1][:]], ["ht0", "ht1"], junk, st8, gbc, bbc, "gbc", "bbc")
            for i2 in range(2):
                tt = pr * 2 + i2
                tok0 = c0 + tt * 128
                S.dma("sync", dram["hS"][tok0:tok0 + 128, :], ht2[i2][:], reads=["ht%d" % i2])
                for hf in range(2):
                    for j4 in range(4):
                        j = hf * 4 + j4
                        T(lambda e, j=j, j4=j4, hf=hf, i2=i2: e.transpose(Q_t[hf][:, j4 * 128:(j4 + 1) * 128], ht2[i2][:, j * 128:(j + 1) * 128], ident[:]),
                          r=["ht%d" % i2, "ident"], w=["Q_t%d" % hf])
                    A(lambda e, hf=hf: e.copy(out=hTs[:, hf * 4:(hf + 1) * 4, :], in_=Q_t[hf][:].rearrange("p (a n) -> p a n", a=4)), r=["Q_t%d" % hf], w=["hTs"])
                S.dma("sync", dram["hTS"].rearrange("(dc p) n -> p dc n", p=128)[:, :, tok0:tok0 + 128], hTs[:], reads=["hTs"])


def build_moe(nc, S, es, dram, n_quarters=4, n_experts=32):
    sb, ps, V, A, G, T = _mk(nc, S, es, "mo")
    QT_ = 1024
    NSTG = MOE_NSTG
    ident = sb([128, 128])
    S.dma("sync", ident[:], dram["c_ident"], writes=["ident"])
    Wr = sb([128, 8, 32])
    S.dma("sync", Wr[:], dram["w_r"].rearrange("(dc p) n -> p dc n", p=128), writes=["Wr"])
    brb = sb([128, 32])
    S.dma("sync", brb[:], dram["b_r"].partition_broadcast(128), writes=["brb"])
    b1T = sb([128, 32 * 16])
    S.dma("sync", b1T[:], dram["b1T"], writes=["b1T"])
    b1Tp = sb([128, 32 * 16])
    V(lambda e: e.tensor_scalar(out=b1Tp[:], in0=b1T[:], scalar1=1.0, scalar2=None, op0=ALU.add), r=["b1T"], w=["b1Tp"])
    b2s = sb([32, 1024])
    S.dma("sync", b2s[:], dram["b2"], writes=["b2s"])
    gbc = sb([128, 1024]); bbc = sb([128, 1024])
    S.dma("sync", gbc[:], dram["ln2g"].partition_broadcast(128), writes=["gbc"])
    S.dma("sync", bbc[:], dram["ln2b"].partition_broadcast(128), writes=["bbc"])
    hb = sb([128, 8, QT_], BF16)
    acc = sb([128, 8, 1024])
    GW = sb([128, 8, 32])
    lg = sb([128, 32]); mx = sb([128, 8]); nm0 = sb([128, 1]); ex = sb([128, 32]); msk = sb([128, 32]); den = sb([128, 1]); gT = sb([32, 128])
    htk = sb([128, 1024]); junk = sb([128, 1024]); st8 = sb([128, 32])
    stg = [sb([128, 2048]) for _ in range(NSTG)]
    w1b = sb([128, 8, 2048], BF16); w2b = sb([128, 8, 1024], BF16)
    actT = sb([128, 8, QT_], BF16)
    gg = [sb([128, 512]) for _ in range(2)]; sg = [sb([128, 512]) for _ in range(2)]; ll = [sb([128, 512]) for _ in range(2)]
    Q_r = ps([128, 512]); Q_g = [ps([128, 512]) for _ in range(2)]; Q_l = [ps([128, 512]) for _ in range(2)]; Q_y = [ps([128, 512]) for _ in range(2)]
    hT = dram["hTS"].rearrange("(dc p) n -> p dc n", p=128)
    si = 0
    fci = 0
    for qt in range(n_quarters):
        t0 = qt * QT_
        for g2 in range(QT_ // 512):
            hsv = [stg[0][:].rearrange("p (a n) -> p a n", a=4), stg[1][:].rearrange("p (a n) -> p a n", a=4)]
            hs_dc = lambda dc: hsv[dc // 4][:, dc % 4, :]
            for dc in range(8):
                S.dma("sync", hs_dc(dc), hT[:, dc, t0 + g2 * 512:t0 + (g2 + 1) * 512], writes=["stg%d" % (dc // 4)])
            for t4 in range(4):
                tt = g2 * 4 + t4
                ts_ = slice(t4 * 128, (t4 + 1) * 128)
                for dc in range(8):
                    T(lambda e, dc=dc, ts_=ts_: e.matmul(Q_r[:, 0:32], lhsT=hs_dc(dc)[:, ts_], rhs=Wr[:, dc, :], start=(dc == 0), stop=(dc == 7)),
                      r=["stg0", "stg1", "Wr"], w=["Q_r.l"])
                V(lambda e: e.tensor_tensor(out=lg[:], in0=Q_r[:, 0:32], in1=brb[:], op=ALU.add), r=["Q_r.l", "brb"], w=["lg"])
                V(lambda e: e.max(out=mx[:], in_=lg[:]), r=["lg"], w=["mx"])
                V(lambda e: e.tensor_scalar(out=nm0[:], in0=mx[:, 0:1], scalar1=-1.0, scalar2=None, op0=ALU.mult), r=["mx"], w=["nm0"])
                A(lambda e: e.activation(out=ex[:], in_=lg[:], func=AF.Exp, bias=nm0[:, 0:1], scale=1.0), r=["lg", "nm0"], w=["ex"])
                V(lambda e: e.tensor_scalar(out=msk[:], in0=lg[:], scalar1=mx[:, 3:4], scalar2=None, op0=ALU.is_ge), r=["lg", "mx"], w=["msk"])
                V(lambda e: e.tensor_tensor(out=ex[:], in0=ex[:], in1=msk[:], op=ALU.mult), r=["ex", "msk"], w=["ex"])
                V(lambda e: e.tensor_reduce(out=den[:], in_=ex[:], axis=AX.X, op=ALU.add), r=["ex"], w=["den"])
                V(lambda e: e.reciprocal(out=den[:], in_=den[:]), r=["den"], w=["den"])
                V(lambda e, tt=tt: e.tensor_scalar(out=GW[:, tt, :], in0=ex[:], scalar1=den[:, 0:1], scalar2=None, op0=ALU.mult), r=["ex", "den"], w=["GW"])
                T(lambda e, tt=tt: e.transpose(Q_r[0:32, 128:256], GW[:, tt, :], ident[:]), r=["GW", "ident"], w=["Q_r.t"])
                V(lambda e: e.tensor_copy(out=gT[:], in_=Q_r[0:32, 128:256]), r=["Q_r.t"], w=["gT"])
                S.dma("sync", htk[:], dram["hS"][t0 + tt * 128:t0 + (tt + 1) * 128, :], writes=["htk"])
                for hf in range(2):
                    T(lambda e, hf=hf: e.matmul(Q_y[hf][:], lhsT=gT[:], rhs=b2s[:, hf * 512:(hf + 1) * 512], start=True, stop=True), r=["gT", "b2s"], w=["Q_y%d" % hf])
                    V(lambda e, hf=hf, tt=tt: e.scalar_tensor_tensor(out=acc[:, tt, hf * 512:(hf + 1) * 512], in0=htk[:, hf * 512:(hf + 1) * 512], scalar=ALPHA, in1=Q_y[hf][:],
                                                                     op0=ALU.mult, op1=ALU.add), r=["htk", "Q_y%d" % hf], w=["acc%d" % tt])
            for dc in range(8):
                if dc % 2 == 0:
                    V(lambda e, dc=dc, g2=g2: e.tensor_copy(out=hb[:, dc, g2 * 512:(g2 + 1) * 512], in_=hs_dc(dc)), r=["stg%d" % (dc // 4)], w=["hb"])
                else:
                    A(lambda e, dc=dc, g2=g2: e.copy(out=hb[:, dc, g2 * 512:(g2 + 1) * 512], in_=hs_dc(dc)), r=["stg%d" % (dc // 4)], w=["hb"])
        for ex_i in range(n_experts):
            w1v = dram["w1"][ex_i].rearrange("(dc p) n -> p dc n", p=128)
            w2v = dram["w2"][ex_i].rearrange("(dc p) n -> p dc n", p=128)
            for dc in range(8):
                k = "stg%d" % (si % NSTG); st_ = stg[si % NSTG]; si += 1
                S.dma("sync", st_[:], w1v[:, dc, :], writes=[k])
                A(lambda e, dc=dc, st_=st_: e.copy(out=w1b[:, dc, :], in_=st_[:]), r=[k], w=["w1b"])
            for d2 in range(4):
                k = "stg%d" % (si % NSTG); st_ = stg[si % NSTG]; si += 1
                S.dma("sync", st_[:].rearrange("p (a n) -> p a n", a=2), w2v[:, 2 * d2:2 * d2 + 2, :], writes=[k])
                if d2 % 2 == 0:
                    V(lambda e, d2=d2, st_=st_: e.tensor_copy(out=w2b[:, 2 * d2:2 * d2 + 2, :], in_=st_[:].rearrange("p (a n) -> p a n", a=2)), r=[k], w=["w2b"])
                else:
                    A(lambda e, d2=d2, st_=st_: e.copy(out=w2b[:, 2 * d2:2 * d2 + 2, :], in_=st_[:].rearrange("p (a n) -> p a n", a=2)), r=[k], w=["w2b"])
            for tg in range(QT_ // 512):
                gs = slice(tg * 512, (tg + 1) * 512)
                for fc in range(8):
                    bi = fci % 2
                    fci += 1
                    kg = "Q_g%d" % bi; kl = "Q_l%d" % bi
                    for dc in range(8):
                        T(lambda e, dc=dc, fc=fc, gs=gs, bi=bi: e.matmul(Q_g[bi][:], lhsT=w1b[:, dc, fc * 128:(fc + 1) * 128], rhs=hb[:, dc, gs], start=(dc == 0), stop=(dc == 7)),
                          r=["w1b", "hb"], w=[kg])
                    for dc in range(8):
                        T(lambda e, dc=dc, fc=fc, gs=gs, bi=bi: e.matmul(Q_l[bi][:], lhsT=w1b[:, dc, 1024 + fc * 128:1024 + (fc + 1) * 128], rhs=hb[:, dc, gs], start=(dc == 0), stop=(dc == 7)),
                          r=["w1b", "hb"], w=[kl])
                    bg = b1T[:, ex_i * 16 + fc:ex_i * 16 + fc + 1]
                    bl = b1Tp[:, ex_i * 16 + 8 + fc:ex_i * 16 + 8 + fc + 1]
                    V(lambda e, bg=bg, bi=bi: e.tensor_scalar(out=gg[bi][:], in0=Q_g[bi][:], scalar1=bg, scalar2=7.0, op0=ALU.add, op1=ALU.min), r=[kg, "b1T"], w=["gg%d" % bi])
                    A(lambda e, bi=bi: e.activation(out=sg[bi][:], in_=gg[bi][:], func=AF.Sigmoid, scale=1.702), r=["gg%d" % bi], w=["sg%d" % bi])
                    V(lambda e, bl=bl, bi=bi: e.tensor_scalar(out=ll[bi][:], in0=Q_l[bi][:], scalar1=bl, scalar2=8.0, op0=ALU.add, op1=ALU.min), r=[kl, "b1Tp"], w=["ll%d" % bi])
                    G(lambda e, bi=bi: e.tensor_tensor(out=gg[bi][:], in0=gg[bi][:], in1=sg[bi][:], op=ALU.mult), r=["gg%d" % bi, "sg%d" % bi], w=["gg%d" % bi])
                    V(lambda e, fc=fc, gs=gs, bi=bi: e.scalar_tensor_tensor(out=actT[:, fc, gs], in0=ll[bi][:], scalar=-6.0, in1=gg[bi][:], op0=ALU.max, op1=ALU.mult),
                      r=["gg%d" % bi, "ll%d" % bi], w=["actT%d" % tg])
            for tile_i in range(QT_ // 128):
                tg = tile_i // 4
                for hf in range(2):
                    for fc in range(8):
                        T(lambda e, fc=fc, tile_i=tile_i, hf=hf: e.matmul(Q_y[hf][:], lhsT=actT[:, fc, tile_i * 128:(tile_i + 1) * 128], rhs=w2b[:, fc, hf * 512:(hf + 1) * 512],
                                                                          start=(fc == 0), stop=(fc == 7)), r=["actT%d" % tg, "w2b"], w=["Q_y%d" % hf])
                    V(lambda e, hf=hf, tile_i=tile_i, ex_i=ex_i: e.scalar_tensor_tensor(
                        out=acc[:, tile_i, hf * 512:(hf + 1) * 512], in0=Q_y[hf][:], scalar=GW[:, tile_i, ex_i:ex_i + 1],
                        in1=acc[:, tile_i, hf * 512:(hf + 1) * 512], op0=ALU.mult, op1=ALU.add), r=["Q_y%d" % hf, "GW", "acc%d" % tile_i], w=["acc%d" % tile_i])
        layer_norm_multi(V, A, G, [acc[:, tt, :] for tt in range(8)], ["acc%d" % tt for tt in range(8)], junk, st8, gbc, bbc, "gbc", "bbc")
        for tt in range(8):
            S.dma("sync", dram["out"][t0 + tt * 128:t0 + (tt + 1) * 128, :], acc[:, tt, :], reads=["acc%d" % tt])


def _dt(v):
    return F32 if v.dtype == np.float32 else BF16


_SZ = {}


def build_all(nc, S, dram, cc_sem):
    with contextlib.ExitStack() as e1:
        build_rwkv(nc, S, e1, dram, **_SZ.get('rwkv', {}))
        S.barrier()
        S.flush()
    with contextlib.ExitStack() as e2:
        build_attn(nc, S, e2, dram, cc={'groups': [[0, 1, 2, 3], [4, 5, 6, 7]], 'sem': cc_sem}, **_SZ.get('attn', {}))
        S.barrier()
        S.flush()
    S.barrier()
    S.flush()
    with contextlib.ExitStack() as e3:
        build_stage_a(nc, S, e3, dram, **_SZ.get('sa', {}))
        S.barrier()
        S.flush()
    with contextlib.ExitStack() as e4:
        build_moe(nc, S, e4, dram, **_SZ.get('moe', {}))
        S.emit()


def kernel(x, w_in, mu_shift, w0, w_decay_up, a0, w_aaa_up, w_gate_up, k_k, k_a, r_k,
           lnx_g, lnx_b, w_attn_br, w_rwkv_br, w_out, ln1_g, ln1_b,
           w_router, b_router, w1, b1, w2, b2, ln2_g, ln2_b):
    f = lambda a: np.ascontiguousarray(np.asarray(a, dtype=np.float32))
    x = f(x); w_in = f(w_in)[0]; mu = f(mu_shift)[0]
    B, S_, D = x.shape
    NC = 8
    TQ = S_ // 4
    xTp = []
    for b in range(B):
        t = np.zeros((D, S_ + 1), np.float32)
        t[:, 1:] = x[b].T
        xTp.append(t)
    rc = rwkv_consts()
    ac = attn_consts()
    OQ = 1824
    row = lambda v: np.ascontiguousarray(v[None, :])
    w_gate = np.ascontiguousarray(w_in[:, OQ + 1536:OQ + 1536 + 2048])
    w1p = f(w1)[0][:_SZ.get("ne", 32)]
    w1p = np.ascontiguousarray(np.concatenate([w1p[:, :, 0::2], w1p[:, :, 1::2]], 2))
    b1p = f(b1)[0]
    b1p = np.concatenate([b1p[:, 0::2], b1p[:, 1::2]], 1)
    b1T = np.ascontiguousarray(b1p.reshape(32, 16, 128).transpose(2, 0, 1).reshape(128, 512))
    shared = {
        "w_lo": np.ascontiguousarray(w_in[:, 1536:1824]), "mu_lo": row(mu[1536:1824]),
        "w_gate": w_gate, "w_ab": f(w_attn_br)[0], "w_rb": f(w_rwkv_br)[0], "w_out": f(w_out)[0],
        "ln1g": row(f(ln1_g)[0]), "ln1b": row(f(ln1_b)[0]),
        "w_r": f(w_router)[0], "b_r": row(f(b_router)[0]), "w1": w1p, "b1T": b1T, "w2": f(w2)[0][:_SZ.get("ne", 32)], "b2": f(b2)[0],
        "ln2g": row(f(ln2_g)[0]), "ln2b": row(f(ln2_b)[0]),
    }
    shared.update(rc)
    shared.update(ac)
    in_maps = []
    for c in range(NC):
        b, hp = c // 4, c % 4
        tq = hp
        hc = slice(128 * hp, 128 * hp + 128)
        ts = slice(tq * TQ, (tq + 1) * TQ)
        mq = np.zeros((128, 4), np.float32); mq[:, tq] = 1.0
        m = {
            "xT": xTp[b],
            "w_rkv": np.ascontiguousarray(np.concatenate([w_in[:, 0:512][:, hc], w_in[:, 512:1024][:, hc], w_in[:, 1024:1536][:, hc]], 1)),
            "mu_rkv": row(np.concatenate([mu[0:512][hc], mu[512:1024][hc], mu[1024:1536][hc]])),
            "wdu": np.ascontiguousarray(f(w_decay_up)[0][:, hc]), "wau": np.ascontiguousarray(f(w_aaa_up)[0][:, hc]),
            "wgu": np.ascontiguousarray(f(w_gate_up)[0][:, hc]),
            "w0": row(f(w0)[0][hc]), "a0": row(f(a0)[0][hc]), "kk": row(f(k_k)[0][hc]), "ka": row(f(k_a)[0][hc]),
            "rk": row(f(r_k)[0].reshape(-1)[hc]), "lng": row(f(lnx_g)[0][hc]), "lnb": row(f(lnx_b)[0][hc]),
            "w_qkv": np.ascontiguousarray(np.concatenate([w_in[:, OQ:OQ + 512][:, hc], w_in[:, OQ + 512:OQ + 1024][:, hc],
                                                          w_in[:, OQ + 1024:OQ + 1536][:, hc]], 1)),
            "xT2": np.ascontiguousarray(x[b, ts].T), "x2": np.ascontiguousarray(x[b, ts]),
            "maskq": mq,
        }
        m.update(shared)
        in_maps.append(m)
    nc = bass.Bass("TRN2", target_bir_lowering=False)
    dram = {}
    for k, v in in_maps[0].items():
        dram[k] = nc.dram_tensor(k, list(v.shape), _dt(v), kind="ExternalInput").ap()
    dram["out"] = nc.dram_tensor("out", [TQ, D], F32, kind="ExternalOutput").ap()
    dram["cin"] = [nc.dram_tensor("cin%d" % k, [256, 1024], F32, kind="Internal").ap() for k in range(16)]
    dram["cout"] = [nc.dram_tensor("cout%d" % k, [1024, 1024], F32, kind="Internal").ap() for k in range(16)]
    dram["hS"] = nc.dram_tensor("hS", [TQ, D], F32, kind="Internal").ap()
    dram["hTS"] = nc.dram_tensor("hTS", [D, TQ], F32, kind="Internal").ap()
    with contextlib.ExitStack() as es:
        S = Sched(nc, es)
        cc_sem = es.enter_context(nc.semaphore("cc_sem"))
        build_all(nc, S, dram, cc_sem)
    res = run_bass_kernel_spmd(nc, in_maps, core_ids=list(range(NC)))
    out = np.zeros((B, S_, D), np.float32)
    for c in range(NC):
        b, tq = c // 4, c % 4
        out[b, tq * TQ:(tq + 1) * TQ] = res.results[c]["out"]
    return out
```
